# Optimizing a Trainium2 kernel written in Bass

```python
import math
import jax, jax.numpy as jnp
from jax import lax
import numpy as np


D_MODEL = 1024
BATCH = 4
SEQ = 4096
DEPTH = 2

N_MIXERS = 2
MLA_HEADS = 8
MLA_Q_LORA = 384
MLA_KV_LORA = 256
MLA_NOPE = 128
MLA_ROPE = 64
MLA_V = 128
MLA_THETA = 10000.0
MLA_IN = MLA_Q_LORA + MLA_KV_LORA + MLA_ROPE
DIFF_HEADS = 8
DIFF_HEAD_DIM = 64
DIFF_V = 2 * DIFF_HEAD_DIM
DIFF_ROT = DIFF_HEAD_DIM // 4
ROPE_THETA = 500000.0
DIFF_IN = 4 * DIFF_HEADS * DIFF_HEAD_DIM + DIFF_HEADS * DIFF_V
N_EXPERTS = 16
EXPERT_FF = 2048
EC_CAPACITY_FACTOR = 2
Q_BLOCK = 128
NORM_EPS = 1e-5
LATENT_EPS = 1e-6
ALPHA = (2 * DEPTH) ** 0.25
BETA = (8 * DEPTH) ** -0.25
N_MLA_LAYERS = (DEPTH + 1) // 2
N_DIFF_LAYERS = DEPTH // 2

kernel_name = 'hybrid_mla_diffattn_ec_moe_encoder'


def layer_norm(x, g, b):
    xf = x.astype(jnp.float32)
    xc = xf - xf.mean(-1, keepdims=True)
    var = (xc * xc).mean(-1, keepdims=True)
    return (xc * lax.rsqrt(var + NORM_EPS)).astype(x.dtype) * g + b


def rms_norm(x, g, eps):
    xf = x.astype(jnp.float32)
    return (xf * lax.rsqrt((xf * xf).mean(-1, keepdims=True) + eps)).astype(x.dtype) * g


def rope_cos_sin(positions, rot_dim, theta):
    inv = theta ** (-jnp.arange(0, rot_dim, 2, dtype=jnp.float32) / rot_dim)
    ang = positions.astype(jnp.float32)[..., None] * inv
    return jnp.cos(ang), jnp.sin(ang)


def apply_rope(x, cos, sin):
    shape = cos.shape[:2] + (1,) * (x.ndim - 3) + cos.shape[-1:]
    c = cos.reshape(shape).astype(x.dtype)
    s = sin.reshape(shape).astype(x.dtype)
    half = x.shape[-1] // 2
    x1, x2 = x[..., :half], x[..., half:]
    return jnp.concatenate([x1 * c - x2 * s, x2 * c + x1 * s], -1)


def mla_mixer(h, cos, sin, w_in, q_norm, kv_norm, w_uq, w_ukv, w_o):
    B, S, _ = h.shape
    H = MLA_HEADS
    cq, ckv, kr = jnp.split(h @ w_in, [MLA_Q_LORA, MLA_Q_LORA + MLA_KV_LORA], -1)
    q = (rms_norm(cq, q_norm, LATENT_EPS) @ w_uq).reshape(B, S, H, MLA_NOPE + MLA_ROPE)
    q = jnp.concatenate([q[..., :MLA_NOPE], apply_rope(q[..., MLA_NOPE:], cos, sin)], -1)
    kv = (rms_norm(ckv, kv_norm, LATENT_EPS) @ w_ukv).reshape(B, S, H, MLA_NOPE + MLA_V)
    k_pe = apply_rope(kr[:, :, None, :], cos, sin)
    k = jnp.concatenate([kv[..., :MLA_NOPE], jnp.broadcast_to(k_pe, (B, S, H, MLA_ROPE))], -1)
    v = kv[..., MLA_NOPE:]
    scale = (MLA_NOPE + MLA_ROPE) ** -0.5
    nb = S // Q_BLOCK
    qb = q.reshape(B, nb, Q_BLOCK, H, MLA_NOPE + MLA_ROPE).transpose(1, 0, 2, 3, 4)

    def block(qi):
        s = jnp.einsum('bqhd,bkhd->bhqk', qi, k).astype(jnp.float32) * scale
        p = jax.nn.softmax(s, -1).astype(v.dtype)
        return jnp.einsum('bhqk,bkhd->bqhd', p, v)

    o = lax.map(block, qb).transpose(1, 0, 2, 3, 4).reshape(B, S, H * MLA_V)
    return o @ w_o


def diff_mixer(h, cos, sin, w_in, lam_vec, subln, w_o, lambda_init):
    B, S, _ = h.shape
    H, d = DIFF_HEADS, DIFF_HEAD_DIM
    qd = H * 2 * d
    q, k, v = jnp.split(h @ w_in, [qd, 2 * qd], -1)
    q = q.reshape(B, S, H, 2, d)
    k = k.reshape(B, S, H, 2, d)
    v = v.reshape(B, S, H, DIFF_V)

    def partial_rope(t):
        return jnp.concatenate([apply_rope(t[..., :DIFF_ROT], cos, sin), t[..., DIFF_ROT:]], -1)

    q, k = partial_rope(q), partial_rope(k)
    lf = lam_vec.astype(jnp.float32)
    lam = jnp.exp(jnp.sum(lf[0] * lf[1])) - jnp.exp(jnp.sum(lf[2] * lf[3])) + lambda_init
    scale = d ** -0.5
    nb = S // Q_BLOCK
    qb = q.reshape(B, nb, Q_BLOCK, H, 2, d).transpose(1, 0, 2, 3, 4, 5)

    def block(qi):
        s = jnp.einsum('bqhcd,bkhcd->bhcqk', qi, k).astype(jnp.float32) * scale
        p = jax.nn.softmax(s, -1)
        a = (p[:, :, 0] - lam * p[:, :, 1]).astype(v.dtype)
        return jnp.einsum('bhqk,bkhe->bqhe', a, v)

    o = lax.map(block, qb).transpose(1, 0, 2, 3, 4)
    o = rms_norm(o, subln, NORM_EPS) * (1.0 - lambda_init)
    return o.reshape(B, S, H * DIFF_V) @ w_o


def ec_moe(h, router_w, w_in, w_out):
    B, S, D = h.shape
    cap = max(1, EC_CAPACITY_FACTOR * S // N_EXPERTS)
    aff = jax.nn.softmax((h @ router_w).astype(jnp.float32), -1)
    gate, idx = lax.top_k(aff.transpose(0, 2, 1), cap)
    xe = jax.vmap(lambda hb, ib: hb[ib])(h, idx)
    gu = jnp.einsum('becd,edf->becf', xe, w_in)
    g, u = jnp.split(gu, 2, -1)
    y = jnp.einsum('becf,efd->becd', jax.nn.silu(g) * u, w_out) * gate[..., None].astype(h.dtype)
    return jax.vmap(
        lambda yb, ib: jnp.zeros((S, D), yb.dtype).at[ib.reshape(-1)].add(yb.reshape(-1, D))
    )(y, idx)


def setup_inputs(seed: int = 0) -> dict:
    key = jax.random.key(seed)
    ks = jax.random.split(key, 24)
    f32 = jnp.float32
    D = D_MODEL

    def nrm(k, shape, scale):
        return jax.random.normal(k, shape, f32) * scale

    x = jax.random.normal(ks[0], (BATCH, SEQ, D), f32)
    c = jax.random.normal(ks[1], (BATCH, D), f32)
    positions = (jnp.arange(SEQ, dtype=jnp.int32)[None, :]
                 + jax.random.randint(ks[2], (BATCH, 1), 0, SEQ, dtype=jnp.int32))
    ada_w = nrm(ks[3], (DEPTH, D, 6 * D), 0.1 * D ** -0.5)
    ada_b = nrm(ks[4], (DEPTH, 6 * D), 0.01)
    ln1_g = 1.0 + nrm(ks[5], (DEPTH, D), 0.02)
    ln1_b = nrm(ks[6], (DEPTH, D), 0.01)
    ln2_g = 1.0 + nrm(ks[7], (DEPTH, D), 0.02)
    ln2_b = nrm(ks[8], (DEPTH, D), 0.01)
    mla_w_in = nrm(ks[9], (N_MLA_LAYERS, D, MLA_IN), D ** -0.5)
    mla_q_norm = 1.0 + nrm(ks[10], (N_MLA_LAYERS, MLA_Q_LORA), 0.02)
    mla_kv_norm = 1.0 + nrm(ks[11], (N_MLA_LAYERS, MLA_KV_LORA), 0.02)
    mla_w_uq = nrm(ks[12], (N_MLA_LAYERS, MLA_Q_LORA, MLA_HEADS * (MLA_NOPE + MLA_ROPE)), MLA_Q_LORA ** -0.5)
    mla_w_ukv = nrm(ks[13], (N_MLA_LAYERS, MLA_KV_LORA, MLA_HEADS * (MLA_NOPE + MLA_V)), MLA_KV_LORA ** -0.5)
    mla_w_o = nrm(ks[14], (N_MLA_LAYERS, MLA_HEADS * MLA_V, D), BETA * (MLA_HEADS * MLA_V) ** -0.5)
    diff_w_in = nrm(ks[15], (N_DIFF_LAYERS, D, DIFF_IN), D ** -0.5)
    diff_lambda = nrm(ks[16], (N_DIFF_LAYERS, 4, DIFF_HEAD_DIM), 0.1)
    diff_subln = 1.0 + nrm(ks[17], (N_DIFF_LAYERS, DIFF_V), 0.02)
    diff_w_o = nrm(ks[18], (N_DIFF_LAYERS, DIFF_HEADS * DIFF_V, D), BETA * (DIFF_HEADS * DIFF_V) ** -0.5)
    router_w = nrm(ks[19], (DEPTH, D, N_EXPERTS), D ** -0.5)
    moe_w_in = nrm(ks[20], (DEPTH, N_EXPERTS, D, 2 * EXPERT_FF), D ** -0.5)
    moe_w_out = nrm(ks[21], (DEPTH, N_EXPERTS, EXPERT_FF, D), BETA * EXPERT_FF ** -0.5)
    return {'x': x, 'c': c, 'positions': positions, 'ada_w': ada_w, 'ada_b': ada_b,
            'ln1_g': ln1_g, 'ln1_b': ln1_b, 'ln2_g': ln2_g, 'ln2_b': ln2_b,
            'mla_w_in': mla_w_in, 'mla_q_norm': mla_q_norm, 'mla_kv_norm': mla_kv_norm,
            'mla_w_uq': mla_w_uq, 'mla_w_ukv': mla_w_ukv, 'mla_w_o': mla_w_o,
            'diff_w_in': diff_w_in, 'diff_lambda': diff_lambda, 'diff_subln': diff_subln,
            'diff_w_o': diff_w_o, 'router_w': router_w, 'moe_w_in': moe_w_in, 'moe_w_out': moe_w_out}


def reference(x, c, positions, ada_w, ada_b, ln1_g, ln1_b, ln2_g, ln2_b,
              mla_w_in, mla_q_norm, mla_kv_norm, mla_w_uq, mla_w_ukv, mla_w_o,
              diff_w_in, diff_lambda, diff_subln, diff_w_o,
              router_w, moe_w_in, moe_w_out):
    cos_m, sin_m = rope_cos_sin(positions, MLA_ROPE, MLA_THETA)
    cos_d, sin_d = rope_cos_sin(positions, DIFF_ROT, ROPE_THETA)
    mods = jnp.einsum('bd,lde->lbe', jax.nn.silu(c), ada_w) + ada_b[:, None, :]
    for i in range(DEPTH):
        sh1, sc1, g1, sh2, sc2, g2 = jnp.split(mods[i][:, None, :], 6, -1)
        h = x * (1.0 + sc1) + sh1
        j = i // N_MIXERS
        if i % N_MIXERS == 0:
            t = mla_mixer(h, cos_m, sin_m, mla_w_in[j], mla_q_norm[j], mla_kv_norm[j],
                          mla_w_uq[j], mla_w_ukv[j], mla_w_o[j])
        else:
            lambda_init = 0.8 - 0.6 * math.exp(-0.3 * i)
            t = diff_mixer(h, cos_d, sin_d, diff_w_in[j], diff_lambda[j], diff_subln[j],
                           diff_w_o[j], lambda_init)
        x = layer_norm(ALPHA * x + (1.0 + g1) * t, ln1_g[i], ln1_b[i])
        h = x * (1.0 + sc2) + sh2
        f = ec_moe(h, router_w[i], moe_w_in[i], moe_w_out[i])
        x = layer_norm(ALPHA * x + (1.0 + g2) * f, ln2_g[i], ln2_b[i])
    return x
```

```python
import math
from contextlib import ExitStack

import numpy as np
import ml_dtypes
import concourse.bass as bass
import concourse.mybir as mybir
from concourse.bass_utils import run_bass_kernel_spmd

F32 = mybir.dt.float32
BF16 = mybir.dt.bfloat16
I32 = mybir.dt.int32
ALU = mybir.AluOpType
AF = mybir.ActivationFunctionType

S_LEN = 4096
D = 1024
NBLK = S_LEN // 128
NE = 16
CAP = 512
FF = 2048
ALPHA = 4 ** 0.25
LAMBDA_INIT1 = 0.8 - 0.6 * math.exp(-0.3)
PI = math.pi


class Sched:
    def __init__(self, nc, es):
        self.nc = nc
        self.es = es
        self.engs = {"pe": nc.tensor, "act": nc.scalar, "dve": nc.vector, "pool": nc.gpsimd, "sp": nc.sync}
        self.prog = {}
        for e in self.engs:
            self.prog[e] = [es.enter_context(nc.semaphore("prog_" + e)), 0]
        self.known = {e: {} for e in self.engs}
        self.lastw = {}
        self.readers = {}
        self.dsem = {}
        self.nsem = 0

    def _wait(self, e, tok):
        if tok is None:
            return
        sem, val = tok
        k = self.known[e]
        if k.get(sem.num, 0) >= val:
            return
        self.engs[e].wait_ge(sem, val)
        k[sem.num] = val

    def deps(self, e, reads, writes):
        for r in reads:
            self._wait(e, self.lastw.get(r))
        for w in writes:
            self._wait(e, self.lastw.get(w))
            for tok in list(self.readers.get(w, {}).values()):
                self._wait(e, tok)

    def commit(self, tok, reads, writes):
        for r in reads:
            self.readers.setdefault(r, {})[tok[0].num] = tok
        for w in writes:
            self.lastw[w] = tok
            self.readers[w] = {}

    def op(self, e, fn, reads=(), writes=(), cwrites=None):
        return self.ops(e, [fn], reads, writes, cwrites)

    def ops(self, e, fns, reads=(), writes=(), cwrites=None):
        self.deps(e, reads, writes)
        ins = None
        for fn in fns:
            ins = fn()
        p = self.prog[e]
        p[1] += 1
        ins.then_inc(p[0], 1)
        tok = (p[0], p[1])
        self.commit(tok, reads, writes if cwrites is None else cwrites)
        return tok

    def dma(self, q, key, fn, reads=(), writes=()):
        self.deps(q, reads, writes)
        ins = fn()
        if key not in self.dsem:
            self.dsem[key] = [self.es.enter_context(self.nc.semaphore("d%d" % self.nsem)), 0]
            self.nsem += 1
        s = self.dsem[key]
        s[1] += 16
        ins.then_inc(s[0], 16)
        tok = (s[0], s[1])
        self.commit(tok, reads, writes)
        return tok

    def _all(self):
        toks = [(p[0], p[1]) for p in self.prog.values() if p[1] > 0]
        toks += [(s[0], s[1]) for s in self.dsem.values() if s[1] > 0]
        return toks

    def barrier(self):
        toks = self._all()
        for e in self.engs:
            for t in toks:
                self._wait(e, t)
        self.lastw = {}
        self.readers = {}

    def finish(self, e="sp"):
        for t in self._all():
            self._wait(e, t)


def build(upto=99, dbg=None):
    nc = bass.Bass("TRN2", target_bir_lowering=False)

    def din(name, shape, dt=F32):
        return nc.dram_tensor(name, list(shape), dt, kind="ExternalInput").ap()

    def dscr(name, shape, dt):
        return nc.dram_tensor(name, list(shape), dt, kind="Internal").ap()

    x_in = din("x", [S_LEN, D])
    cT = din("cT", [128, 8])
    pos = din("pos", [1, S_LEN], I32)
    ada_w = din("ada_w", [2, D, 6 * D])
    ada_b = din("ada_b", [2, 6 * D])
    ln1_g = din("ln1_g", [2, D]); ln1_b = din("ln1_b", [2, D])
    ln2_g = din("ln2_g", [2, D]); ln2_b = din("ln2_b", [2, D])
    mla_w_in = din("mla_w_in", [D, 704])
    mla_q_norm = din("mla_q_norm", [384]); mla_kv_norm = din("mla_kv_norm", [256])
    mla_w_uq = din("mla_w_uq", [384, 1536])
    mla_w_ukv = din("mla_w_ukv", [256, 2048])
    mla_w_o = din("mla_w_o", [D, D])
    diff_w_in = din("diff_w_in", [D, 3072])
    diff_lambda = din("diff_lambda", [1, 256])
    diff_subln = din("diff_subln", [128, 1])
    diff_w_o = din("diff_w_o", [D, D])
    router_w = din("router_w", [2, D, NE])
    moe_w_in = din("moe_w_in", [2, NE, D, 2 * FF])
    moe_w_out = din("moe_w_out", [2, NE, FF, D])
    c_inv_m = din("c_inv_m", [64, 1])
    c_inv_d = din("c_inv_d", [128, 1])
    c_ident = din("c_ident", [128, 128])
    c_ltri = din("c_ltri", [128, 128])
    c_iota = din("c_iota", [128, CAP])
    c_rcp = din("c_rcp", [128, NBLK, 2])

    out = nc.dram_tensor("out", [S_LEN, D], F32, kind="ExternalOutput").ap()

    MODS = dscr("MODS", [2, 6 * D], F32)
    QT = dscr("QT", [8, 192, S_LEN], BF16)
    KT = dscr("KT", [8, 128, S_LEN], BF16)
    KPE = dscr("KPE", [64, S_LEN], BF16)
    VV = dscr("VV", [S_LEN, D], BF16)
    OT = dscr("OT", [D, S_LEN], BF16)
    XMID = dscr("XMID", [S_LEN, D], F32)
    H2 = dscr("H2", [S_LEN, D], BF16)
    FACC = dscr("FACC", [S_LEN, D], F32)
    X1 = dscr("X1", [S_LEN, D], F32)

    es = ExitStack()
    with es:
        S = Sched(nc, es)
        uid = [0]

        def sb(stack, shape, dt, name=None):
            uid[0] += 1
            return stack.enter_context(nc.sbuf_tensor("%s_%d" % (name or "t", uid[0]), list(shape), dt))

        banks = [es.enter_context(nc.psum_tensor("bank%d" % i, [128, 512], F32)) for i in range(8)]
        ident_f = sb(es, [128, 128], F32, "identf")
        ident_b = sb(es, [128, 128], BF16, "identb")
        ones_b = sb(es, [128, 128], BF16, "onesb")
        aff = sb(es, [128, NBLK, NE], F32, "aff")
        eps5 = sb(es, [128, 1], F32, "eps5")
        S.dma("sp", "c0", lambda: nc.sync.dma_start(out=ident_f[:], in_=c_ident), writes=["identf"])
        S.op("dve", lambda: nc.vector.tensor_copy(out=ident_b[:], in_=ident_f[:]), reads=["identf"], writes=["identb"])
        S.op("dve", lambda: nc.vector.memset(ones_b[:], 1.0), writes=["onesb"])
        S.op("dve", lambda: nc.vector.memset(eps5[:], 1e-5), writes=["eps5"])

        V = nc.vector
        A = nc.scalar
        PE = nc.tensor
        G = nc.gpsimd

        def bcast_load(stack, src_row_ap, name, plus_one=False, q="sp"):
            n = src_row_ap.shape[-1]
            t = sb(stack, [128, n], F32, name)
            key = "%s_%d" % (name, uid[0])
            S.dma(q, "bc_" + key, lambda: S.engs[q].dma_start(out=t[:], in_=src_row_ap.partition_broadcast(128)), writes=[key])
            if plus_one:
                S.op("pool", lambda: G.tensor_scalar(out=t[:], in0=t[:], scalar1=1.0, scalar2=None, op0=ALU.add), reads=[key], writes=[key])
            return t, key

        def phase_mods():
            with ExitStack() as ph:
                c_sb = sb(ph, [128, 8], F32, "c")
                c16 = sb(ph, [128, 8], BF16, "c16")
                adab = sb(ph, [1, 6 * D], F32, "adab")
                modrow = sb(ph, [1, 6 * D], F32, "modrow")
                wblk = [sb(ph, [128, 8, 512], BF16, "adaw") for _ in range(3)]
                S.dma("sp", "m_c", lambda: nc.sync.dma_start(out=c_sb[:], in_=cT), writes=["c"])
                S.op("act", lambda: A.activation(out=c16[:], in_=c_sb[:], func=AF.Silu), reads=["c"], writes=["c16"])
                it = 0
                for l in range(2):
                    S.dma("sp", "m_b", lambda: nc.sync.dma_start(out=adab[:], in_=ada_b[l:l + 1, :]), writes=["adab"])
                    wv = ada_w[l].rearrange("(k p) n -> p k n", p=128)
                    for j in range(12):
                        sl = it % 3
                        S.dma("pool", "m_w%d" % sl, lambda: G.dma_start(out=wblk[sl][:], in_=wv[:, :, j * 512:(j + 1) * 512]), writes=["adaw%d" % sl])
                        pb = banks[it % 2]
                        S.ops("pe", [(lambda k=k: PE.matmul(pb[0:1, :], lhsT=c16[:, k:k + 1], rhs=wblk[sl][:, k, :], start=(k == 0), stop=(k == 7))) for k in range(8)],
                              reads=["c16", "adaw%d" % sl], writes=["bank%d" % (it % 2)])
                        S.op("dve", lambda: V.tensor_tensor(out=modrow[0:1, j * 512:(j + 1) * 512], in0=pb[0:1, :], in1=adab[0:1, j * 512:(j + 1) * 512], op=ALU.add),
                             reads=["bank%d" % (it % 2), "adab"], writes=["modrow"])
                        it += 1
                    S.dma("sp", "m_o", lambda: nc.sync.dma_start(out=MODS[l:l + 1, :], in_=modrow[:]), reads=["modrow"], writes=["MODS"])
                S.barrier()

        def rope_tables(ph, R, inv_ap):
            Ct = sb(ph, [R, S_LEN], F32, "ropeC")
            St = sb(ph, [R, S_LEN], F32, "ropeS")
            inv = sb(ph, [R, 1], F32, "inv")
            negpi = sb(ph, [R, 1], F32, "negpi")
            S.dma("sp", "r_inv", lambda: nc.sync.dma_start(out=inv[:], in_=inv_ap), writes=["inv"])
            with ExitStack() as tmp:
                pi_t = sb(tmp, [R, 1024], I32, "posi")
                ang = sb(tmp, [R, 1024], F32, "ang")
                nf = sb(tmp, [R, 1024], F32, "nf")
                ni = sb(tmp, [R, 1024], I32, "ni")
                msk = sb(tmp, [R, 1024], F32, "msk")
                for cch in range(4):
                    cs = slice(cch * 1024, (cch + 1) * 1024)
                    S.dma("sp", "r_pos", lambda: nc.sync.dma_start(out=pi_t[:], in_=pos[:, cs].partition_broadcast(R)), writes=["posi"])
                    S.op("dve", lambda: V.tensor_copy(out=ang[:], in_=pi_t[:]), reads=["posi"], writes=["ang"])
                    S.op("dve", lambda: V.tensor_scalar(out=ang[:], in0=ang[:], scalar1=inv[:, 0:1], scalar2=None, op0=ALU.mult), reads=["ang", "inv"], writes=["ang"])
                    S.op("dve", lambda: V.tensor_scalar(out=nf[:], in0=ang[:], scalar1=1.0 / (2 * PI), scalar2=None, op0=ALU.mult), reads=["ang"], writes=["nf"])
                    S.op("dve", lambda: V.tensor_copy(out=ni[:], in_=nf[:]), reads=["nf"], writes=["ni"])
                    S.op("dve", lambda: V.tensor_copy(out=nf[:], in_=ni[:]), reads=["ni"], writes=["nf"])
                    S.op("dve", lambda: V.scalar_tensor_tensor(out=ang[:], in0=nf[:], scalar=-6.28125, in1=ang[:], op0=ALU.mult, op1=ALU.add), reads=["nf", "ang"], writes=["ang"])
                    S.op("dve", lambda: V.scalar_tensor_tensor(out=ang[:], in0=nf[:], scalar=-0.0019353071795864769, in1=ang[:], op0=ALU.mult, op1=ALU.add), reads=["nf", "ang"], writes=["ang"])

                    def wrap():
                        S.op("dve", lambda: V.tensor_scalar(out=msk[:], in0=ang[:], scalar1=PI, scalar2=-2 * PI, op0=ALU.is_gt, op1=ALU.mult), reads=["ang"], writes=["msk"])
                        S.op("dve", lambda: V.tensor_tensor(out=ang[:], in0=ang[:], in1=msk[:], op=ALU.add), reads=["ang", "msk"], writes=["ang"])
                        S.op("dve", lambda: V.tensor_scalar(out=msk[:], in0=ang[:], scalar1=-PI, scalar2=2 * PI, op0=ALU.is_lt, op1=ALU.mult), reads=["ang"], writes=["msk"])
                        S.op("dve", lambda: V.tensor_tensor(out=ang[:], in0=ang[:], in1=msk[:], op=ALU.add), reads=["ang", "msk"], writes=["ang"])
                        S.op("dve", lambda: V.tensor_scalar(out=ang[:], in0=ang[:], scalar1=PI, scalar2=-PI, op0=ALU.min, op1=ALU.max), reads=["ang"], writes=["ang"])
                    wrap()
                    S.op("act", lambda: A.activation(out=St[:, cs], in_=ang[:], func=AF.Sin), reads=["ang"], writes=["ropeS"])
                    S.op("dve", lambda: V.tensor_scalar(out=ang[:], in0=ang[:], scalar1=PI / 2, scalar2=None, op0=ALU.add), reads=["ang"], writes=["ang"])
                    wrap()
                    S.op("act", lambda: A.activation(out=Ct[:, cs], in_=ang[:], func=AF.Sin), reads=["ang"], writes=["ropeC"])
                S.barrier()
            return Ct, St

        def front_block(tb, xsrc, xin, h16, hT, sc1p, sh1p, kx):
            sl = tb % 2
            xv = xsrc.rearrange("(t s p) d -> t p s d", s=4, p=128)
            S.dma("sp", "xin%d" % sl, lambda: nc.sync.dma_start(out=xin[sl][:], in_=xv[tb]), writes=["xin%d" % sl])
            S.op("dve", lambda: V.tensor_tensor(out=xin[sl][:], in0=xin[sl][:], in1=sc1p[:].unsqueeze(1).broadcast_to([128, 4, D]), op=ALU.mult),
                 reads=["xin%d" % sl, kx[0]], writes=["xin%d" % sl])
            S.op("pool", lambda: G.tensor_tensor(out=h16[:], in0=xin[sl][:], in1=sh1p[:].unsqueeze(1).broadcast_to([128, 4, D]), op=ALU.add),
                 reads=["xin%d" % sl, kx[1]], writes=["h16"])
            for kp in range(4):
                bk = banks[6 + kp % 2]
                bkey = "bank%d" % (6 + kp % 2)
                tv = bk[:].bitcast(BF16)
                fns = []
                for kk in range(2):
                    k = kp * 2 + kk
                    for s in range(4):
                        fns.append(lambda k=k, kk=kk, s=s: PE.transpose(tv[:, kk * 512 + s * 128: kk * 512 + (s + 1) * 128], h16[:, s, k * 128:(k + 1) * 128], ident_b[:]))
                S.ops("pe", fns, reads=["h16", "identb"], writes=[bkey])
                S.op("act", lambda: A.copy(out=hT[:, kp * 2:kp * 2 + 2, :].rearrange("p a b -> p (a b)"), in_=tv), reads=[bkey], writes=["hT"])

        pring = [0]

        def next_bank(lo=0, hi=6):
            b = lo + pring[0] % (hi - lo)
            pring[0] += 1
            return banks[b], "bank%d" % b

        def rsqrt_into(dst, dkey, src_ap, skey, mul, add):
            S.op("dve", lambda: V.tensor_scalar(out=dst, in0=src_ap, scalar1=mul, scalar2=add, op0=ALU.mult, op1=ALU.add), reads=[skey], writes=[dkey])
            S.op("act", lambda: A.activation(out=dst, in_=dst, func=AF.Sqrt), reads=[dkey], writes=[dkey])
            S.op("dve", lambda: V.reciprocal(out=dst, in_=dst), reads=[dkey], writes=[dkey])

        def phase_a_mla(xsrc, l):
            with ExitStack() as ph:
                Ct, St = rope_tables(ph, 64, c_inv_m)
                sc1p, k_sc = bcast_load(ph, MODS[l:l + 1, D:2 * D], "sc1p", plus_one=True)
                sh1p, k_sh = bcast_load(ph, MODS[l:l + 1, 0:D], "sh1p")
                w_in = sb(ph, [128, 8, 768], BF16, "w_in")
                w_uq = sb(ph, [128, 3, 8, 256], BF16, "w_uq")
                w_k = sb(ph, [128, 2, 8, 128], BF16, "w_k")
                w_v = sb(ph, [128, 2, 8, 128], BF16, "w_v")
                qn = sb(ph, [128, 3], F32, "qn")
                kvn = sb(ph, [128, 2], F32, "kvn")
                S.dma("pool", "a_w0", lambda: G.dma_start(out=w_in[:, :, 0:704], in_=mla_w_in.rearrange("(k p) n -> p k n", p=128)), writes=["w_in"])
                S.op("act", lambda: A.mul(out=w_in[:, :, 704:736], in_=w_in[:, :, 672:704], mul=-1.0), reads=["w_in"], writes=["w_in"])
                S.op("act", lambda: A.copy(out=w_in[:, :, 736:768], in_=w_in[:, :, 640:672]), reads=["w_in"], writes=["w_in"])
                uqv = mla_w_uq.rearrange("(k p) (h e) -> p k h e", p=128, e=192)
                for k in range(3):
                    S.dma("pool", "a_w1", lambda: G.dma_start(out=w_uq[:, k, :, 0:192], in_=uqv[:, k, :, :]), writes=["w_uq"])
                S.op("act", lambda: A.mul(out=w_uq[:, :, :, 192:224], in_=w_uq[:, :, :, 160:192], mul=-1.0), reads=["w_uq"], writes=["w_uq"])
                S.op("act", lambda: A.copy(out=w_uq[:, :, :, 224:256], in_=w_uq[:, :, :, 128:160]), reads=["w_uq"], writes=["w_uq"])
                wkv = mla_w_ukv.rearrange("(k p) (h two e) -> p k h two e", p=128, two=2, e=128)
                for k in range(2):
                    S.dma("pool", "a_w2", lambda: G.dma_start(out=w_k[:, k, :, :], in_=wkv[:, k, :, 0, :]), writes=["w_k"])
                    S.dma("pool", "a_w3", lambda: G.dma_start(out=w_v[:, k, :, :], in_=wkv[:, k, :, 1, :]), writes=["w_v"])
                with nc.allow_non_contiguous_dma(reason="tiny norm-gain vectors"):
                    S.dma("sp", "a_n0", lambda: nc.sync.dma_start(out=qn[:], in_=mla_q_norm.rearrange("(k p) -> p k", p=128)), writes=["qn"])
                    S.dma("sp", "a_n1", lambda: nc.sync.dma_start(out=kvn[:], in_=mla_kv_norm.rearrange("(k p) -> p k", p=128)), writes=["kvn"])
                xin = [sb(ph, [128, 4, D], F32, "xin") for _ in range(2)]
                h16 = sb(ph, [128, 4, D], BF16, "h16")
                hT = sb(ph, [128, 8, 512], BF16, "hT")
                lat = sb(ph, [128, 7, 512], F32, "lat")
                sq = sb(ph, [128, 5, 512], BF16, "sq")
                rstd = sb(ph, [128, 2, 512], F32, "rstd")
                cqn = sb(ph, [128, 3, 512], BF16, "cqn")
                ckvn = sb(ph, [128, 2, 512], BF16, "ckvn")
                kpe = sb(ph, [64, 512], BF16, "kpe")
                tmp1 = sb(ph, [64, 512], F32, "tmp1")
                tmp2 = sb(ph, [64, 512], F32, "tmp2")
                qst = sb(ph, [128, 8, 512], BF16, "qst")
                qrst = sb(ph, [64, 8, 512], BF16, "qrst")
                kst = sb(ph, [128, 8, 512], BF16, "kst")
                vst = sb(ph, [128, 4, D], BF16, "vst")
                mspec = [(0, 128, 128), (1, 256, 128), (2, 384, 128), (3, 512, 128), (4, 640, 128), (5, 704, 64), (6, 768, 64)]
                for tb in range(8):
                    ts = slice(tb * 512, (tb + 1) * 512)
                    front_block(tb, xsrc, xin, h16, hT, sc1p, sh1p, (k_sc, k_sh))
                    for (mi, hi_, mm) in mspec:
                        pb, pk = next_bank()
                        S.ops("pe", [(lambda k=k: PE.matmul(pb[0:mm, :], lhsT=w_in[:, k, hi_ - mm:hi_], rhs=hT[:, k, :], start=(k == 0), stop=(k == 7))) for k in range(8)],
                              reads=["w_in", "hT"], writes=[pk])
                        S.op("act", lambda: A.copy(out=lat[0:mm, mi, :], in_=pb[0:mm, :]), reads=[pk], writes=["lat%d" % mi])
                        if mi < 5:
                            S.op("dve", lambda: V.tensor_tensor(out=sq[:, mi, :], in0=lat[:, mi, :], in1=lat[:, mi, :], op=ALU.mult), reads=["lat%d" % mi], writes=["sq%d" % mi])
                    for gi, (c0, c1, n) in enumerate([(0, 3, 384), (3, 5, 256)]):
                        pb, pk = next_bank()
                        S.ops("pe", [(lambda c=c: PE.matmul(pb[:, :], lhsT=ones_b[:], rhs=sq[:, c, :], start=(c == c0), stop=(c == c1 - 1))) for c in range(c0, c1)],
                              reads=["onesb"] + ["sq%d" % c for c in range(c0, c1)], writes=[pk])
                        rsqrt_into(rstd[:, gi, :], "rstd%d" % gi, pb[:, :], pk, 1.0 / n, 1e-6)
                    for c in range(3):
                        S.op("dve", lambda: V.scalar_tensor_tensor(out=cqn[:, c, :], in0=lat[:, c, :], scalar=qn[:, c:c + 1], in1=rstd[:, 0, :], op0=ALU.mult, op1=ALU.mult),
                             reads=["lat%d" % c, "qn", "rstd0"], writes=["cqn"])
                    for c in range(2):
                        S.op("dve", lambda: V.scalar_tensor_tensor(out=ckvn[:, c, :], in0=lat[:, 3 + c, :], scalar=kvn[:, c:c + 1], in1=rstd[:, 1, :], op0=ALU.mult, op1=ALU.mult),
                             reads=["lat%d" % (3 + c), "kvn", "rstd1"], writes=["ckvn"])
                    S.op("dve", lambda: V.tensor_tensor(out=tmp1[:], in0=lat[0:64, 5, :], in1=Ct[:, ts], op=ALU.mult), reads=["lat5"], writes=["tmp1"])
                    S.op("pool", lambda: G.tensor_tensor(out=tmp2[:], in0=lat[0:64, 6, :], in1=St[:, ts], op=ALU.mult), reads=["lat6"], writes=["tmp2"])
                    S.op("dve", lambda: V.tensor_tensor(out=kpe[:], in0=tmp1[:], in1=tmp2[:], op=ALU.add), reads=["tmp1", "tmp2"], writes=["kpe"])
                    S.dma("sp", "a_kpe", lambda: nc.sync.dma_start(out=KPE[:, ts], in_=kpe[:]), reads=["kpe"], writes=["KPE"])
                    for h in range(8):
                        pb, pk = next_bank()
                        S.ops("pe", [(lambda c=c: PE.matmul(pb[:, :], lhsT=w_uq[:, c, h, 0:128], rhs=cqn[:, c, :], start=(c == 0), stop=(c == 2))) for c in range(3)],
                              reads=["w_uq", "cqn"], writes=[pk])
                        S.op("act", lambda: A.copy(out=qst[:, h, :], in_=pb[:, :]), reads=[pk], writes=["qst"])
                        pa, pka = next_bank()
                        S.ops("pe", [(lambda c=c: PE.matmul(pa[0:64, :], lhsT=w_uq[:, c, h, 128:192], rhs=cqn[:, c, :], start=(c == 0), stop=(c == 2))) for c in range(3)],
                              reads=["w_uq", "cqn"], writes=[pka])
                        pr, pkr = next_bank()
                        S.ops("pe", [(lambda c=c: PE.matmul(pr[0:64, :], lhsT=w_uq[:, c, h, 192:256], rhs=cqn[:, c, :], start=(c == 0), stop=(c == 2))) for c in range(3)],
                              reads=["w_uq", "cqn"], writes=[pkr])
                        S.op("dve", lambda: V.tensor_tensor(out=tmp1[:], in0=pa[0:64, :], in1=Ct[:, ts], op=ALU.mult), reads=[pka], writes=["tmp1"])
                        S.op("dve", lambda: V.tensor_tensor(out=tmp2[:], in0=pr[0:64, :], in1=St[:, ts], op=ALU.mult), reads=[pkr], writes=["tmp2"])
                        S.op("pool", lambda: G.tensor_tensor(out=qrst[:, h, :], in0=tmp1[:], in1=tmp2[:], op=ALU.add), reads=["tmp1", "tmp2"], writes=["qrst"])
                        pk_, pkk = next_bank()
                        S.ops("pe", [(lambda c=c: PE.matmul(pk_[:, :], lhsT=w_k[:, c, h, :], rhs=ckvn[:, c, :], start=(c == 0), stop=(c == 1))) for c in range(2)],
                              reads=["w_k", "ckvn"], writes=[pkk])
                        S.op("act", lambda: A.copy(out=kst[:, h, :], in_=pk_[:, :]), reads=[pkk], writes=["kst"])
                    S.dma("sp", "a_q", lambda: nc.sync.dma_start(out=QT[:, 0:128, ts].rearrange("h p t -> p h t"), in_=qst[:]), reads=["qst"], writes=["QT"])
                    S.dma("sp", "a_qr", lambda: nc.sync.dma_start(out=QT[:, 128:192, ts].rearrange("h p t -> p h t"), in_=qrst[:]), reads=["qrst"], writes=["QT"])
                    S.dma("sp", "a_k", lambda: nc.sync.dma_start(out=KT[:, :, ts].rearrange("h p t -> p h t"), in_=kst[:]), reads=["kst"], writes=["KT"])
                    for s in range(4):
                        for hf in range(2):
                            pb, pk = next_bank()
                            S.ops("pe", [(lambda c=c: PE.matmul(pb[:, :], lhsT=ckvn[:, c, s * 128:(s + 1) * 128], rhs=w_v[:, c, hf * 4:(hf + 1) * 4, :].rearrange("p h e -> p (h e)"), start=(c == 0), stop=(c == 1))) for c in range(2)],
                                  reads=["w_v", "ckvn"], writes=[pk])
                            S.op("act" if hf else "dve", (lambda pb=pb, hf=hf: (A.copy if hf else V.tensor_copy)(out=vst[:, s, hf * 512:(hf + 1) * 512], in_=pb[:, :])), reads=[pk], writes=["vst"])
                    S.dma("sp", "a_v", lambda: nc.sync.dma_start(out=VV.rearrange("(t s p) d -> t p s d", s=4, p=128)[tb], in_=vst[:]), reads=["vst"], writes=["VV"])
                S.barrier()

        def phase_a_diff(xsrc, l):
            with ExitStack() as ph:
                Ct, St = rope_tables(ph, 128, c_inv_d)
                sc1p, k_sc = bcast_load(ph, MODS[l:l + 1, D:2 * D], "sc1p", plus_one=True)
                sh1p, k_sh = bcast_load(ph, MODS[l:l + 1, 0:D], "sh1p")
                w = sb(ph, [128, 8, 3072], BF16, "dw")
                wr = sb(ph, [128, 8, 2048], BF16, "dwr")
                wsrc = diff_w_in.rearrange("(k p) n -> p k n", p=128)
                for j in range(3):
                    S.dma("pool", "d_w%d" % j, lambda: G.dma_start(out=w[:, :, j * 1024:(j + 1) * 1024], in_=wsrc[:, :, j * 1024:(j + 1) * 1024]), writes=["dw"])
                S.op("pool", lambda: G.memset(wr[:], 0.0), writes=["dwr"])
                w4 = w[:, :, 0:2048].rearrange("p k (g e) -> p k g e", e=64)
                wr4 = wr[:].rearrange("p k (g e) -> p k g e", e=64)
                for k in range(8):
                    S.op("act", lambda: A.mul(out=wr4[:, k, :, 0:8], in_=w4[:, k, :, 8:16], mul=-1.0), reads=["dw", "dwr"], writes=["dwr"])
                    S.op("act", lambda: A.copy(out=wr4[:, k, :, 8:16], in_=w4[:, k, :, 0:8]), reads=["dw", "dwr"], writes=["dwr"])
                xin = [sb(ph, [128, 4, D], F32, "xin") for _ in range(2)]
                h16 = sb(ph, [128, 4, D], BF16, "h16")
                hT = sb(ph, [128, 8, 512], BF16, "hT")
                tmp1 = sb(ph, [128, 512], F32, "tmp1")
                tmp2 = sb(ph, [128, 512], F32, "tmp2")
                qst = sb(ph, [128, 8, 512], BF16, "qst")
                kst = sb(ph, [128, 8, 512], BF16, "kst")
                vst = sb(ph, [128, 4, D], BF16, "vst")
                for tb in range(8):
                    ts = slice(tb * 512, (tb + 1) * 512)
                    front_block(tb, xsrc, xin, h16, hT, sc1p, sh1p, (k_sc, k_sh))
                    for qk in range(2):
                        st = qst if qk == 0 else kst
                        skey = "qst" if qk == 0 else "kst"
                        for h in range(8):
                            c0 = qk * 1024 + h * 128
                            pa, pka = next_bank()
                            S.ops("pe", [(lambda k=k: PE.matmul(pa[:, :], lhsT=w[:, k, c0:c0 + 128], rhs=hT[:, k, :], start=(k == 0), stop=(k == 7))) for k in range(8)],
                                  reads=["dw", "hT"], writes=[pka])
                            pr, pkr = next_bank()
                            S.ops("pe", [(lambda k=k: PE.matmul(pr[:, :], lhsT=wr[:, k, c0:c0 + 128], rhs=hT[:, k, :], start=(k == 0), stop=(k == 7))) for k in range(8)],
                                  reads=["dwr", "hT"], writes=[pkr])
                            S.op("dve", lambda: V.tensor_tensor(out=tmp1[:], in0=pa[:, :], in1=Ct[:, ts], op=ALU.mult), reads=[pka], writes=["tmp1"])
                            S.op("dve", lambda: V.tensor_tensor(out=tmp2[:], in0=pr[:, :], in1=St[:, ts], op=ALU.mult), reads=[pkr], writes=["tmp2"])
                            S.op("pool", lambda: G.tensor_tensor(out=st[:, h, :], in0=tmp1[:], in1=tmp2[:], op=ALU.add), reads=["tmp1", "tmp2"], writes=[skey])
                    S.dma("sp", "a_q", lambda: nc.sync.dma_start(out=QT[:, 0:128, ts].rearrange("h p t -> p h t"), in_=qst[:]), reads=["qst"], writes=["QT"])
                    S.dma("sp", "a_k", lambda: nc.sync.dma_start(out=KT[:, :, ts].rearrange("h p t -> p h t"), in_=kst[:]), reads=["kst"], writes=["KT"])
                    for s in range(4):
                        for hf in range(2):
                            pb, pk = next_bank()
                            S.ops("pe", [(lambda k=k: PE.matmul(pb[:, :], lhsT=hT[:, k, s * 128:(s + 1) * 128], rhs=w[:, k, 2048 + hf * 512:2048 + (hf + 1) * 512], start=(k == 0), stop=(k == 7))) for k in range(8)],
                                  reads=["dw", "hT"], writes=[pk])
                            S.op("act", lambda: A.copy(out=vst[:, s, hf * 512:(hf + 1) * 512], in_=pb[:, :]), reads=[pk], writes=["vst"])
                    S.dma("sp", "a_v", lambda: nc.sync.dma_start(out=VV.rearrange("(t s p) d -> t p s d", s=4, p=128)[tb], in_=vst[:]), reads=["vst"], writes=["VV"])
                S.barrier()

        def phase_b(kind):
            mla = kind == "mla"
            nmap = 1 if mla else 2
            scale = (192 ** -0.5) if mla else (64 ** -0.5)
            with ExitStack() as ph:
                kbuf = [sb(ph, [128, S_LEN], BF16, "kbuf") for _ in range(2)]
                qbuf = [sb(ph, [128, S_LEN], BF16, "qbuf") for _ in range(2)]
                vbuf = [sb(ph, [128, NBLK, 128], BF16, "vbuf") for _ in range(2)]
                nring = 3 if mla else 2
                PT = [[sb(ph, [128, 512], BF16, "PT") for _ in range(nring)] for _ in range(nmap)]
                rden = [sb(ph, [128, 512], F32, "rden") for _ in range(2)]
                osb = [sb(ph, [128, 512], BF16, "osb") for _ in range(2)]
                if mla:
                    qrbuf = [sb(ph, [64, S_LEN], BF16, "qrbuf") for _ in range(2)]
                    kpe = sb(ph, [64, S_LEN], BF16, "kpeall")
                    S.dma("sp", "b_kpe", lambda: nc.sync.dma_start(out=kpe[:], in_=KPE), writes=["kpeall"])
                else:
                    lamt = sb(ph, [128, 256], F32, "lamt")
                    lp = sb(ph, [128, 128], F32, "lp")
                    ls = sb(ph, [128, 2], F32, "ls")
                    neglam = sb(ph, [128, 1], F32, "neglam")
                    subs = sb(ph, [128, 1], F32, "subs")
                    o1n = sb(ph, [128, 512], F32, "o1n")
                    o2n = sb(ph, [128, 512], F32, "o2n")
                    sq = sb(ph, [128, 512], BF16, "osq")
                    rstd = sb(ph, [128, 512], F32, "orstd")
                    S.dma("sp", "b_lam", lambda: nc.sync.dma_start(out=lamt[:], in_=diff_lambda.partition_broadcast(128)), writes=["lamt"])
                    S.dma("sp", "b_sub", lambda: nc.sync.dma_start(out=subs[:], in_=diff_subln), writes=["subs"])
                    S.op("dve", lambda: V.tensor_tensor(out=lp[:, 0:64], in0=lamt[:, 0:64], in1=lamt[:, 64:128], op=ALU.mult), reads=["lamt"], writes=["lp"])
                    S.op("dve", lambda: V.tensor_tensor(out=lp[:, 64:128], in0=lamt[:, 128:192], in1=lamt[:, 192:256], op=ALU.mult), reads=["lamt", "lp"], writes=["lp"])
                    S.op("dve", lambda: V.reduce_sum(out=ls[:, 0:1], in_=lp[:, 0:64], axis=mybir.AxisListType.X), reads=["lp"], writes=["ls"])
                    S.op("dve", lambda: V.reduce_sum(out=ls[:, 1:2], in_=lp[:, 64:128], axis=mybir.AxisListType.X), reads=["lp", "ls"], writes=["ls"])
                    S.op("act", lambda: A.activation(out=ls[:], in_=ls[:], func=AF.Exp), reads=["ls"], writes=["ls"])
                    S.op("dve", lambda: V.tensor_tensor(out=neglam[:], in0=ls[:, 1:2], in1=ls[:, 0:1], op=ALU.subtract), reads=["ls"], writes=["neglam"])
                    S.op("dve", lambda: V.tensor_scalar(out=neglam[:], in0=neglam[:], scalar1=-LAMBDA_INIT1, scalar2=None, op0=ALU.add), reads=["neglam"], writes=["neglam"])
                    S.op("dve", lambda: V.tensor_scalar(out=subs[:], in0=subs[:], scalar1=1.0 - LAMBDA_INIT1, scalar2=None, op0=ALU.mult), reads=["subs"], writes=["subs"])

                vview = VV.rearrange("(c p) (h e) -> p c h e", p=128, e=128)

                def load_head(h):
                    sl = h % 2
                    S.dma("sp", "b_k%d" % sl, lambda: nc.sync.dma_start(out=kbuf[sl][:], in_=KT[h]), writes=["kbuf%d" % sl])
                    S.dma("sp", "b_q%d" % sl, lambda: nc.sync.dma_start(out=qbuf[sl][:], in_=QT[h, 0:128, :]), writes=["qbuf%d" % sl])
                    S.dma("sp", "b_v%d" % sl, lambda: nc.sync.dma_start(out=vbuf[sl][:], in_=vview[:, :, h, :]), writes=["vbuf%d" % sl])
                    if mla:
                        S.dma("sp", "b_qr%d" % sl, lambda: nc.sync.dma_start(out=qrbuf[sl][:], in_=QT[h, 128:192, :]), writes=["qrbuf%d" % sl])

                load_head(0)
                blk = 0
                for h in range(8):
                    if h + 1 < 8:
                        load_head(h + 1)
                    sl = h % 2
                    for qb in range(8):
                        qs = slice(qb * 512, (qb + 1) * 512)
                        par = blk % 2
                        blk += 1
                        if mla:
                            acc_o, ko = banks[3 + 2 * par], "bank%d" % (3 + 2 * par)
                            acc_d, kd = banks[4 + 2 * par], "bank%d" % (4 + 2 * par)

                            def qk(kc):
                                ks = slice(kc * 128, (kc + 1) * 128)
                                sbk = banks[kc % 3]
                                S.ops("pe", [lambda: PE.matmul(sbk[:, :], lhsT=kbuf[sl][:, ks], rhs=qbuf[sl][:, qs], start=True, stop=False),
                                             lambda: PE.matmul(sbk[:, :], lhsT=kpe[:, ks], rhs=qrbuf[sl][:, qs], start=False, stop=True)],
                                      reads=["kbuf%d" % sl, "qbuf%d" % sl, "qrbuf%d" % sl, "kpeall"], writes=["bank%d" % (kc % 3)])

                            def ex(kc):
                                S.op("act", lambda: A.activation(out=PT[0][kc % 3][:], in_=banks[kc % 3][:, :], func=AF.Exp, scale=scale),
                                     reads=["bank%d" % (kc % 3)], writes=["PT0_%d" % (kc % 3)])

                            def pv(kc):
                                S.ops("pe", [lambda: PE.matmul(acc_o[:, :], lhsT=vbuf[sl][:, kc, :], rhs=PT[0][kc % 3][:], start=(kc == 0), stop=(kc == NBLK - 1)),
                                             lambda: PE.matmul(acc_d[:, :], lhsT=ones_b[:], rhs=PT[0][kc % 3][:], start=(kc == 0), stop=(kc == NBLK - 1))],
                                      reads=["PT0_%d" % (kc % 3), "vbuf%d" % sl, "onesb"],
                                      writes=[ko, kd] if kc == 0 else [], cwrites=[ko, kd] if kc == NBLK - 1 else [])
                            qk(0)
                            qk(1)
                            for kc in range(NBLK):
                                ex(kc)
                                if kc + 2 < NBLK:
                                    qk(kc + 2)
                                pv(kc)
                            S.op("dve", lambda: V.reciprocal(out=rden[par][:], in_=acc_d[:, :]), reads=[kd], writes=["rden%d" % par])
                            S.op("dve", lambda: V.tensor_tensor(out=osb[par][:], in0=acc_o[:, :], in1=rden[par][:], op=ALU.mult), reads=[ko, "rden%d" % par], writes=["osb%d" % par])
                        else:
                            accs = [(banks[4 + i], "bank%d" % (4 + i)) for i in range(4)]

                            def qk(kc):
                                ks = slice(kc * 128, (kc + 1) * 128)
                                for m in range(2):
                                    bi = m * 2 + kc % 2
                                    rows = slice(m * 64, (m + 1) * 64)
                                    S.op("pe", lambda: PE.matmul(banks[bi][:, :], lhsT=kbuf[sl][rows, ks], rhs=qbuf[sl][rows, qs], start=True, stop=True),
                                         reads=["kbuf%d" % sl, "qbuf%d" % sl], writes=["bank%d" % bi])

                            def ex(kc):
                                for m in range(2):
                                    bi = m * 2 + kc % 2
                                    S.op("act", lambda: A.activation(out=PT[m][kc % 2][:], in_=banks[bi][:, :], func=AF.Exp, scale=scale),
                                         reads=["bank%d" % bi], writes=["PT%d_%d" % (m, kc % 2)])

                            def pv(kc):
                                first, last = kc == 0, kc == NBLK - 1
                                fns = []
                                for m in range(2):
                                    fns.append(lambda m=m: PE.matmul(accs[m][0][:, :], lhsT=vbuf[sl][:, kc, :], rhs=PT[m][kc % 2][:], start=first, stop=last))
                                    fns.append(lambda m=m: PE.matmul(accs[2 + m][0][:, :], lhsT=ones_b[:], rhs=PT[m][kc % 2][:], start=first, stop=last))
                                allk = [a[1] for a in accs]
                                S.ops("pe", fns, reads=["PT0_%d" % (kc % 2), "PT1_%d" % (kc % 2), "vbuf%d" % sl, "onesb"],
                                      writes=allk if first else [], cwrites=allk if last else [])
                            qk(0)
                            for kc in range(NBLK):
                                ex(kc)
                                if kc + 1 < NBLK:
                                    qk(kc + 1)
                                pv(kc)
                            S.op("dve", lambda: V.reciprocal(out=rden[0][:], in_=accs[2][0][:, :]), reads=[accs[2][1]], writes=["rden0"])
                            S.op("dve", lambda: V.reciprocal(out=rden[1][:], in_=accs[3][0][:, :]), reads=[accs[3][1]], writes=["rden1"])
                            S.op("dve", lambda: V.tensor_tensor(out=o1n[:], in0=accs[0][0][:, :], in1=rden[0][:], op=ALU.mult), reads=[accs[0][1], "rden0"], writes=["o1n"])
                            S.op("dve", lambda: V.tensor_tensor(out=o2n[:], in0=accs[1][0][:, :], in1=rden[1][:], op=ALU.mult), reads=[accs[1][1], "rden1"], writes=["o2n"])
                            S.op("dve", lambda: V.scalar_tensor_tensor(out=o1n[:], in0=o2n[:], scalar=neglam[:, 0:1], in1=o1n[:], op0=ALU.mult, op1=ALU.add),
                                 reads=["o1n", "o2n", "neglam"], writes=["o1n"])
                            S.op("pool", lambda: G.tensor_tensor(out=sq[:], in0=o1n[:], in1=o1n[:], op=ALU.mult), reads=["o1n"], writes=["osq"])
                            S.op("pe", lambda: PE.matmul(banks[0][:, :], lhsT=ones_b[:], rhs=sq[:], start=True, stop=True), reads=["osq", "onesb"], writes=["bank0"])
                            rsqrt_into(rstd[:], "orstd", banks[0][:, :], "bank0", 1.0 / 128, 1e-5)
                            S.op("dve", lambda: V.scalar_tensor_tensor(out=osb[par][:], in0=o1n[:], scalar=subs[:, 0:1], in1=rstd[:], op0=ALU.mult, op1=ALU.mult),
                                 reads=["o1n", "subs", "orstd"], writes=["osb%d" % par])
                        S.dma("sp", "b_o%d" % par, lambda: nc.sync.dma_start(out=OT[h * 128:(h + 1) * 128, qs], in_=osb[par][:]), reads=["osb%d" % par], writes=["OT"])
                S.barrier()

        def layernorm_block(ph_t, z, zkey, st, mv, rs, nmr, tag):
            for i in range(2):
                S.op("dve", lambda: V.bn_stats(out=st[:, i, :], in_=z[:, i * 512:(i + 1) * 512]), reads=[zkey], writes=["st" + tag])
            S.op("dve", lambda: V.bn_aggr(out=mv[:], in_=st[:].rearrange("p a b -> p (a b)")), reads=["st" + tag], writes=["mv" + tag])
            S.op("act", lambda: A.activation(out=rs[:], in_=mv[:, 1:2], func=AF.Sqrt, bias=eps5[:, 0:1], scale=1.0), reads=["mv" + tag, "eps5"], writes=["rs" + tag])
            S.op("dve", lambda: V.reciprocal(out=rs[:], in_=rs[:]), reads=["rs" + tag], writes=["rs" + tag])
            S.op("dve", lambda: V.scalar_tensor_tensor(out=nmr[:], in0=mv[:, 0:1], scalar=-1.0, in1=rs[:], op0=ALU.mult, op1=ALU.mult), reads=["mv" + tag, "rs" + tag], writes=["nmr" + tag])
            S.op("act", lambda: A.activation(out=z[:], in_=z[:], func=AF.Identity, bias=nmr[:, 0:1], scale=rs[:, 0:1]), reads=[zkey, "rs" + tag, "nmr" + tag], writes=[zkey])

        def phase_c(xsrc, l, w_o_ap):
            with ExitStack() as ph:
                g1p, k_g1 = bcast_load(ph, MODS[l:l + 1, 2 * D:3 * D], "g1p", plus_one=True)
                sc2p, k_sc2 = bcast_load(ph, MODS[l:l + 1, 4 * D:5 * D], "sc2p", plus_one=True)
                sh2p, k_sh2 = bcast_load(ph, MODS[l:l + 1, 3 * D:4 * D], "sh2p")
                lng, k_lng = bcast_load(ph, ln1_g[l:l + 1, :], "lng")
                lnb, k_lnb = bcast_load(ph, ln1_b[l:l + 1, :], "lnb")
                w_o = sb(ph, [128, 8, D], BF16, "w_o")
                rw = sb(ph, [128, 8, NE], F32, "rw")
                oT = sb(ph, [128, 8, S_LEN], BF16, "oTall")
                S.dma("pool", "c_wo", lambda: G.dma_start(out=w_o[:], in_=w_o_ap.rearrange("(k p) n -> p k n", p=128)), writes=["w_o"])
                S.dma("sp", "c_rw", lambda: nc.sync.dma_start(out=rw[:], in_=router_w[l].rearrange("(k p) n -> p k n", p=128)), writes=["rw"])
                for h in range(8):
                    S.dma("sp", "c_ot", lambda: nc.sync.dma_start(out=oT[:, h, :], in_=OT[h * 128:(h + 1) * 128, :]), writes=["oTall"])
                xin = [sb(ph, [128, D], F32, "xin") for _ in range(2)]
                z = [sb(ph, [128, D], F32, "z") for _ in range(2)]
                h2 = [sb(ph, [128, D], F32, "h2") for _ in range(2)]
                h2b = [sb(ph, [128, D], BF16, "h2b") for _ in range(2)]
                h2T = sb(ph, [128, 8, 128], F32, "h2T")
                st = sb(ph, [128, 2, 6], F32, "st")
                mv = sb(ph, [128, 2], F32, "mv")
                rs = sb(ph, [128, 1], F32, "rs")
                nmr = sb(ph, [128, 1], F32, "nmr")
                mx = sb(ph, [128, 1], F32, "mx")
                ssum = sb(ph, [128, 1], F32, "ssum")
                ex = sb(ph, [128, NE], F32, "ex")
                for b in range(NBLK):
                    sl = b % 2
                    rows = slice(b * 128, (b + 1) * 128)
                    S.dma("sp", "c_x%d" % sl, lambda: nc.sync.dma_start(out=xin[sl][:], in_=xsrc[rows, :]), writes=["xin%d" % sl])
                    pt0, k0 = banks[0 + 2 * sl], "bank%d" % (0 + 2 * sl)
                    pt1, k1 = banks[1 + 2 * sl], "bank%d" % (1 + 2 * sl)
                    for (pt, kk, hf) in ((pt0, k0, 0), (pt1, k1, 1)):
                        S.ops("pe", [(lambda h=h: PE.matmul(pt[:, :], lhsT=oT[:, h, rows], rhs=w_o[:, h, hf * 512:(hf + 1) * 512], start=(h == 0), stop=(h == 7))) for h in range(8)],
                              reads=["oTall", "w_o"], writes=[kk])
                        S.op("dve", lambda: V.tensor_tensor(out=z[sl][:, hf * 512:(hf + 1) * 512], in0=pt[:, :], in1=g1p[:, hf * 512:(hf + 1) * 512], op=ALU.mult),
                             reads=[kk, k_g1], writes=["z%d" % sl])
                    S.op("dve", lambda: V.scalar_tensor_tensor(out=z[sl][:], in0=xin[sl][:], scalar=ALPHA, in1=z[sl][:], op0=ALU.mult, op1=ALU.add),
                         reads=["xin%d" % sl, "z%d" % sl], writes=["z%d" % sl])
                    layernorm_block(ph, z[sl], "z%d" % sl, st, mv, rs, nmr, "c")
                    S.op("pool", lambda: G.tensor_tensor(out=z[sl][:], in0=z[sl][:], in1=lng[:], op=ALU.mult), reads=["z%d" % sl, k_lng], writes=["z%d" % sl])
                    S.op("dve", lambda: V.tensor_tensor(out=z[sl][:], in0=z[sl][:], in1=lnb[:], op=ALU.add), reads=["z%d" % sl, k_lnb], writes=["z%d" % sl])
                    S.dma("sp", "c_xm%d" % sl, lambda: nc.sync.dma_start(out=XMID[rows, :], in_=z[sl][:]), reads=["z%d" % sl], writes=["XMID"])
                    S.op("pool", lambda: G.tensor_tensor(out=h2[sl][:], in0=z[sl][:], in1=sc2p[:], op=ALU.mult), reads=["z%d" % sl, k_sc2], writes=["h2_%d" % sl])
                    S.op("dve", lambda: V.tensor_tensor(out=h2[sl][:], in0=h2[sl][:], in1=sh2p[:], op=ALU.add), reads=["h2_%d" % sl, k_sh2], writes=["h2_%d" % sl])
                    S.op("act", lambda: A.copy(out=h2b[sl][:], in_=h2[sl][:]), reads=["h2_%d" % sl], writes=["h2b%d" % sl])
                    S.dma("sp", "c_h2%d" % sl, lambda: nc.sync.dma_start(out=H2[rows, :], in_=h2b[sl][:]), reads=["h2b%d" % sl], writes=["H2"])
                    for hf in range(2):
                        bk, bkey = banks[4 + hf], "bank%d" % (4 + hf)
                        S.ops("pe", [(lambda j=j: PE.transpose(bk[:, j * 128:(j + 1) * 128], h2[sl][:, (hf * 4 + j) * 128:(hf * 4 + j + 1) * 128], ident_f[:])) for j in range(4)],
                              reads=["h2_%d" % sl, "identf"], writes=[bkey])
                        S.op("act", lambda: A.copy(out=h2T[:, hf * 4:(hf + 1) * 4, :].rearrange("p a b -> p (a b)"), in_=bk[:, :]), reads=[bkey], writes=["h2T"])
                    lg, lgk = banks[6], "bank6"
                    S.ops("pe", [(lambda k=k: PE.matmul(lg[:, 0:NE], lhsT=h2T[:, k, :], rhs=rw[:, k, :], start=(k == 0), stop=(k == 7))) for k in range(8)],
                          reads=["h2T", "rw"], writes=[lgk])
                    S.op("dve", lambda: V.reduce_max(out=mx[:], in_=lg[:, 0:NE], axis=mybir.AxisListType.X), reads=[lgk], writes=["mx"])
                    S.op("dve", lambda: V.tensor_scalar(out=mx[:], in0=mx[:], scalar1=-1.0, scalar2=None, op0=ALU.mult), reads=["mx"], writes=["mx"])
                    S.op("act", lambda: A.activation(out=ex[:], in_=lg[:, 0:NE], func=AF.Exp, bias=mx[:, 0:1], scale=1.0, accum_out=ssum[:]), reads=[lgk, "mx"], writes=["ex", "ssum"])
                    S.op("dve", lambda: V.reciprocal(out=ssum[:], in_=ssum[:]), reads=["ssum"], writes=["ssum"])
                    S.op("dve", lambda: V.tensor_scalar(out=aff[:, b, :], in0=ex[:], scalar1=ssum[:, 0:1], scalar2=None, op0=ALU.mult), reads=["ex", "ssum"], writes=["aff"])
                S.barrier()

        def phase_m(l):
            with ExitStack() as ph:
                ltri = sb(ph, [128, 128], F32, "ltri")
                ltri_b = sb(ph, [128, 128], BF16, "ltrib")
                iota = sb(ph, [128, CAP], F32, "iota")
                rcp = sb(ph, [128, NBLK, 2], F32, "rcp")
                S.dma("sp", "m_c0", lambda: nc.sync.dma_start(out=ltri[:], in_=c_ltri), writes=["ltri"])
                S.dma("sp", "m_c1", lambda: nc.sync.dma_start(out=iota[:], in_=c_iota), writes=["iota"])
                S.dma("sp", "m_c2", lambda: nc.sync.dma_start(out=rcp[:], in_=c_rcp), writes=["rcp"])
                S.op("dve", lambda: V.tensor_copy(out=ltri_b[:], in_=ltri[:]), reads=["ltri"], writes=["ltrib"])
                zt = sb(ph, [128, 4, D], F32, "zt")
                S.op("pool", lambda: G.memset(zt[:], 0.0), writes=["zt"])
                fv = FACC.rearrange("(t s p) d -> t p s d", s=4, p=128)
                for tb in range(8):
                    S.dma("sp", "m_z", lambda: nc.sync.dma_start(out=fv[tb], in_=zt[:]), reads=["zt"], writes=["FACC"])
                lo = sb(ph, [128, NE], F32, "lo")
                hi = sb(ph, [128, NE], F32, "hi")
                mid = sb(ph, [128, NE], F32, "mid")
                cmpt = sb(ph, [128, NBLK, NE], F32, "cmpt")
                cnt = sb(ph, [128, NE], F32, "cnt")
                cntb = sb(ph, [128, NE], F32, "cntb")
                ge = sb(ph, [128, NE], F32, "ge")
                dlt = sb(ph, [128, NE], F32, "dlt")
                ones_f = sb(ph, [128, 128], F32, "onesf")
                S.op("dve", lambda: V.memset(ones_f[:], 1.0), writes=["onesf"])
                S.op("dve", lambda: V.memset(lo[:], 0.0), writes=["lo"])
                S.op("dve", lambda: V.memset(hi[:], 1.0), writes=["hi"])
                for it in range(30):
                    S.op("dve", lambda: V.tensor_tensor(out=mid[:], in0=lo[:], in1=hi[:], op=ALU.add), reads=["lo", "hi"], writes=["mid"])
                    S.op("dve", lambda: V.tensor_scalar(out=mid[:], in0=mid[:], scalar1=0.5, scalar2=None, op0=ALU.mult), reads=["mid"], writes=["mid"])
                    S.op("dve", lambda: V.tensor_tensor(out=cmpt[:], in0=aff[:], in1=mid[:].unsqueeze(1).broadcast_to([128, NBLK, NE]), op=ALU.is_ge), reads=["aff", "mid"], writes=["cmpt"])
                    S.op("dve", lambda: V.reduce_sum(out=cnt[:], in_=cmpt[:].rearrange("p c e -> p e c"), axis=mybir.AxisListType.X), reads=["cmpt"], writes=["cnt"])
                    S.op("pe", lambda: PE.matmul(banks[0][:, 0:NE], lhsT=ones_f[:], rhs=cnt[:], start=True, stop=True), reads=["onesf", "cnt"], writes=["bank0"])
                    S.op("dve", lambda: V.tensor_scalar(out=ge[:], in0=banks[0][:, 0:NE], scalar1=float(CAP) - 0.5, scalar2=None, op0=ALU.is_ge), reads=["bank0"], writes=["ge"])
                    S.op("dve", lambda: V.tensor_tensor(out=dlt[:], in0=mid[:], in1=lo[:], op=ALU.subtract), reads=["mid", "lo"], writes=["dlt"])
                    S.op("dve", lambda: V.tensor_tensor(out=dlt[:], in0=dlt[:], in1=ge[:], op=ALU.mult), reads=["dlt", "ge"], writes=["dlt"])
                    S.op("dve", lambda: V.tensor_tensor(out=lo[:], in0=lo[:], in1=dlt[:], op=ALU.add), reads=["lo", "dlt"], writes=["lo"])
                    S.op("dve", lambda: V.tensor_tensor(out=dlt[:], in0=mid[:], in1=hi[:], op=ALU.subtract), reads=["mid", "hi"], writes=["dlt"])
                    S.op("dve", lambda: V.tensor_scalar(out=ge[:], in0=ge[:], scalar1=-1.0, scalar2=1.0, op0=ALU.mult, op1=ALU.add), reads=["ge"], writes=["ge"])
                    S.op("dve", lambda: V.tensor_tensor(out=dlt[:], in0=dlt[:], in1=ge[:], op=ALU.mult), reads=["dlt", "ge"], writes=["dlt"])
                    S.op("dve", lambda: V.tensor_tensor(out=hi[:], in0=hi[:], in1=dlt[:], op=ALU.add), reads=["hi", "dlt"], writes=["hi"])
                maskb = sb(ph, [128, NBLK, NE], BF16, "maskb")
                pre = sb(ph, [128, NBLK, NE], BF16, "pre")
                pref = sb(ph, [128, NBLK, NE], F32, "pref")
                posp = sb(ph, [128, NBLK, NE], F32, "posp")
                S.op("dve", lambda: V.tensor_tensor(out=cmpt[:], in0=aff[:], in1=lo[:].unsqueeze(1).broadcast_to([128, NBLK, NE]), op=ALU.is_ge), reads=["aff", "lo"], writes=["cmpt"])
                S.op("dve", lambda: V.tensor_copy(out=maskb[:], in_=cmpt[:]), reads=["cmpt"], writes=["maskb"])
                S.op("dve", lambda: V.memset(pref[:, 0, :], 0.0), writes=["pref"])
                for c in range(1, NBLK):
                    S.op("dve", lambda: V.tensor_tensor(out=pref[:, c, :], in0=pref[:, c - 1, :], in1=cmpt[:, c - 1, :], op=ALU.add), reads=["pref", "cmpt"], writes=["pref"])
                S.op("dve", lambda: V.tensor_copy(out=pre[:], in_=pref[:]), reads=["pref"], writes=["pre"])
                pb = banks[1]
                S.ops("pe", [lambda: PE.matmul(pb[:, :], lhsT=ltri_b[:], rhs=maskb[:].rearrange("p c e -> p (c e)"), start=True, stop=False),
                             lambda: PE.matmul(pb[:, :], lhsT=ones_b[:], rhs=pre[:].rearrange("p c e -> p (c e)"), start=False, stop=True)],
                      reads=["ltrib", "onesb", "maskb", "pre"], writes=["bank1"])
                S.op("dve", lambda: V.tensor_tensor(out=posp[:].rearrange("p c e -> p (c e)"), in0=pb[:, :], in1=cmpt[:].rearrange("p c e -> p (c e)"), op=ALU.mult), reads=["bank1", "cmpt"], writes=["posp"])
                R = sb(ph, [128, NBLK, NE, 5], BF16, "R")
                r1 = sb(ph, [128, NBLK, NE], F32, "r1")
                r2 = sb(ph, [128, NBLK, NE], F32, "r2")
                S.op("dve", lambda: V.tensor_copy(out=R[:, :, :, 0], in_=rcp[:, :, 0:1].broadcast_to([128, NBLK, NE])), reads=["rcp"], writes=["R"])
                S.op("dve", lambda: V.tensor_copy(out=R[:, :, :, 1], in_=rcp[:, :, 1:2].broadcast_to([128, NBLK, NE])), reads=["rcp", "R"], writes=["R"])
                S.op("dve", lambda: V.tensor_copy(out=R[:, :, :, 2], in_=aff[:]), reads=["aff", "R"], writes=["R"])
                S.op("dve", lambda: V.tensor_tensor(out=r1[:], in0=aff[:], in1=R[:, :, :, 2], op=ALU.subtract), reads=["aff", "R"], writes=["r1"])
                S.op("dve", lambda: V.tensor_copy(out=R[:, :, :, 3], in_=r1[:]), reads=["r1", "R"], writes=["R"])
                S.op("dve", lambda: V.tensor_tensor(out=r2[:], in0=r1[:], in1=R[:, :, :, 3], op=ALU.subtract), reads=["r1", "R"], writes=["r2"])
                S.op("dve", lambda: V.tensor_copy(out=R[:, :, :, 4], in_=r2[:]), reads=["r2", "R"], writes=["R"])
                idx = sb(ph, [128, NE, 4], I32, "idx")
                idxf = sb(ph, [128, NE, 4], F32, "idxf")
                gate = sb(ph, [128, NE, 4], F32, "gate")
                Pm = [sb(ph, [128, NBLK, 128], BF16, "Pm") for _ in range(2)]
                ig = sb(ph, [128, 8], F32, "ig")
                pi_ = 0
                for e in range(NE):
                    for g in range(4):
                        psl = pi_ % 2
                        eng, engn = (V, "dve")
                        S.op(engn, lambda: eng.tensor_tensor(out=Pm[psl][:], in0=posp[:, :, e:e + 1].broadcast_to([128, NBLK, 128]),
                                                             in1=iota[:, g * 128:(g + 1) * 128].unsqueeze(1).broadcast_to([128, NBLK, 128]), op=ALU.is_equal),
                             reads=["posp", "iota"], writes=["Pm%d" % psl])
                        ob, obk = banks[2 + psl], "bank%d" % (2 + psl)
                        S.ops("pe", [(lambda c=c: PE.matmul(ob[:, 0:5], lhsT=Pm[psl][:, c, :], rhs=R[:, c, e, :], start=(c == 0), stop=(c == NBLK - 1))) for c in range(NBLK)],
                              reads=["Pm%d" % psl, "R"], writes=[obk])
                        S.op("act", lambda: A.copy(out=ig[:, psl * 4:psl * 4 + 4], in_=ob[:, 1:5]), reads=[obk], writes=["ig%d" % psl])
                        S.op("dve", lambda: V.scalar_tensor_tensor(out=idxf[:, e, g:g + 1], in0=ob[:, 0:1], scalar=128.0, in1=ig[:, psl * 4:psl * 4 + 1], op0=ALU.mult, op1=ALU.add),
                             reads=[obk, "ig%d" % psl], writes=["idxf"])
                        S.op("dve", lambda: V.tensor_tensor(out=gate[:, e, g:g + 1], in0=ig[:, psl * 4 + 1:psl * 4 + 2], in1=ig[:, psl * 4 + 2:psl * 4 + 3], op=ALU.add), reads=["ig%d" % psl], writes=["gate"])
                        S.op("dve", lambda: V.tensor_tensor(out=gate[:, e, g:g + 1], in0=gate[:, e, g:g + 1], in1=ig[:, psl * 4 + 3:psl * 4 + 4], op=ALU.add), reads=["ig%d" % psl, "gate"], writes=["gate"])
                        pi_ += 1
                S.op("dve", lambda: V.tensor_copy(out=idx[:], in_=idxf[:]), reads=["idxf"], writes=["idx"])
                wi = [sb(ph, [128, 8, 1024], BF16, "wi") for _ in range(3)]
                wo = [sb(ph, [128, 4, D], BF16, "wo") for _ in range(3)]
                xg = [sb(ph, [128, 4, D], BF16, "xg") for _ in range(2)]
                xeT = sb(ph, [128, 8, CAP], BF16, "xeT")
                sg = [sb(ph, [128, CAP], F32, "sg") for _ in range(2)]
                actT = [sb(ph, [128, 4, CAP], BF16, "actT") for _ in range(2)]
                ysb = [sb(ph, [128, 4, D], F32, "ysb") for _ in range(2)]
                wiv = moe_w_in[l].rearrange("e (k p) n -> e p k n", p=128)
                wov = moe_w_out[l].rearrange("e (k p) n -> e p k n", p=128)
                nblk = [0]

                def load_w(e, i):
                    sl = nblk[0] % 3
                    nblk[0] += 1
                    S.dma("pool", "m_wg%d" % sl, lambda: G.dma_start(out=wi[sl][:, :, 0:512], in_=wiv[e][:, :, i * 512:(i + 1) * 512]), writes=["wi%d" % sl])
                    S.dma("pool", "m_wu%d" % sl, lambda: G.dma_start(out=wi[sl][:, :, 512:1024], in_=wiv[e][:, :, FF + i * 512:FF + (i + 1) * 512]), writes=["wi%d" % sl])
                    S.dma("pool", "m_wo%d" % sl, lambda: G.dma_start(out=wo[sl][:], in_=wov[e][:, i * 4:(i + 1) * 4, :]), writes=["wo%d" % sl])
                    return sl

                def gather(e):
                    gs = e % 2
                    for g in range(4):
                        S.dma("pool", "m_g%d" % gs, lambda: G.indirect_dma_start(out=xg[gs][:, g, :], out_offset=None, in_=H2,
                                                                               in_offset=bass.IndirectOffsetOnAxis(ap=idx[:, e, g:g + 1], axis=0)),
                              reads=["idx", "H2"], writes=["xg%d" % gs])

                sched_w = [(e, i) for e in range(NE) for i in range(4)]
                slots = {}
                slots[sched_w[0]] = load_w(*sched_w[0])
                slots[sched_w[1]] = load_w(*sched_w[1])
                gather(0)
                wn = 2
                par2 = 0
                for e in range(NE):
                    gs = e % 2
                    ys = e % 2
                    if e + 1 < NE:
                        gather(e + 1)
                    for k in range(8):
                        bk, bkey = banks[6 + k % 2], "bank%d" % (6 + k % 2)
                        tv = bk[:].bitcast(BF16)
                        S.ops("pe", [(lambda g=g: PE.transpose(tv[:, g * 128:(g + 1) * 128], xg[gs][:, g, k * 128:(k + 1) * 128], ident_b[:])) for g in range(4)],
                              reads=["xg%d" % gs, "identb"], writes=[bkey])
                        S.op("act" if k % 2 else "dve", (lambda k=k, tv=tv: (A.copy if k % 2 else V.tensor_copy)(out=xeT[:, k, :], in_=tv[:, 0:CAP])), reads=[bkey], writes=["xeT"])
                    for i in range(4):
                        if wn < len(sched_w):
                            slots[sched_w[wn]] = load_w(*sched_w[wn])
                            wn += 1
                        ws = slots[(e, i)]
                        asl = (e * 4 + i) % 2
                        for fc in range(4):
                            pg, pgk = banks[0 + 2 * (fc % 2)], "bank%d" % (0 + 2 * (fc % 2))
                            pu, puk = banks[1 + 2 * (fc % 2)], "bank%d" % (1 + 2 * (fc % 2))
                            S.ops("pe", [(lambda k=k: PE.matmul(pg[:, :], lhsT=wi[ws][:, k, fc * 128:(fc + 1) * 128], rhs=xeT[:, k, :], start=(k == 0), stop=(k == 7))) for k in range(8)],
                                  reads=["wi%d" % ws, "xeT"], writes=[pgk])
                            S.ops("pe", [(lambda k=k: PE.matmul(pu[:, :], lhsT=wi[ws][:, k, 512 + fc * 128:512 + (fc + 1) * 128], rhs=xeT[:, k, :], start=(k == 0), stop=(k == 7))) for k in range(8)],
                                  reads=["wi%d" % ws, "xeT"], writes=[puk])
                            S.op("act", lambda: A.activation(out=sg[fc % 2][:], in_=pg[:, :], func=AF.Silu), reads=[pgk], writes=["sg%d" % (fc % 2)])
                            S.op("dve", lambda: V.tensor_tensor(out=actT[asl][:, fc, :], in0=sg[fc % 2][:], in1=pu[:, :], op=ALU.mult), reads=["sg%d" % (fc % 2), puk], writes=["actT%d" % asl])
                        for g in range(4):
                            for hf in range(2):
                                py, pyk = banks[4 + par2 % 2], "bank%d" % (4 + par2 % 2)
                                par2 += 1
                                S.ops("pe", [(lambda fc=fc: PE.matmul(py[:, :], lhsT=actT[asl][:, fc, g * 128:(g + 1) * 128], rhs=wo[ws][:, fc, hf * 512:(hf + 1) * 512], start=(fc == 0), stop=(fc == 3))) for fc in range(4)],
                                      reads=["actT%d" % asl, "wo%d" % ws], writes=[pyk])
                                dst = ysb[ys][:, g, hf * 512:(hf + 1) * 512]
                                if i == 0:
                                    S.op("act", lambda: A.activation(out=dst, in_=py[:, :], func=AF.Copy, scale=gate[:, e, g:g + 1]), reads=[pyk, "gate"], writes=["ysb%d" % ys])
                                else:
                                    S.op("dve", lambda: V.scalar_tensor_tensor(out=dst, in0=py[:, :], scalar=gate[:, e, g:g + 1], in1=dst, op0=ALU.mult, op1=ALU.add),
                                         reads=[pyk, "gate", "ysb%d" % ys], writes=["ysb%d" % ys])
                    for g in range(4):
                        S.dma("pool", "m_sc", lambda: G.indirect_dma_start(out=FACC, out_offset=bass.IndirectOffsetOnAxis(ap=idx[:, e, g:g + 1], axis=0),
                                                                          in_=ysb[ys][:, g, :], in_offset=None, compute_op=ALU.add),
                              reads=["ysb%d" % ys, "idx"], writes=["FACC"] if g == 0 else [])
                    S.lastw["FACC"] = (S.dsem["m_sc"][0], S.dsem["m_sc"][1])
                    S.readers["FACC"] = {}
                S.barrier()

        def phase_d(l, dst):
            with ExitStack() as ph:
                g2p, k_g2 = bcast_load(ph, MODS[l:l + 1, 5 * D:6 * D], "g2p", plus_one=True)
                lng, k_lng = bcast_load(ph, ln2_g[l:l + 1, :], "lng2")
                lnb, k_lnb = bcast_load(ph, ln2_b[l:l + 1, :], "lnb2")
                xin = [sb(ph, [128, D], F32, "xin") for _ in range(2)]
                fin = [sb(ph, [128, D], F32, "fin") for _ in range(2)]
                st = sb(ph, [128, 2, 6], F32, "st")
                mv = sb(ph, [128, 2], F32, "mv")
                rs = sb(ph, [128, 1], F32, "rs")
                nmr = sb(ph, [128, 1], F32, "nmr")
                for b in range(NBLK):
                    sl = b % 2
                    rows = slice(b * 128, (b + 1) * 128)
                    S.dma("sp", "d_x%d" % sl, lambda: nc.sync.dma_start(out=xin[sl][:], in_=XMID[rows, :]), writes=["xin%d" % sl])
                    S.dma("sp", "d_f%d" % sl, lambda: nc.sync.dma_start(out=fin[sl][:], in_=FACC[rows, :]), writes=["fin%d" % sl])
                    S.op("pool", lambda: G.tensor_tensor(out=fin[sl][:], in0=fin[sl][:], in1=g2p[:], op=ALU.mult), reads=["fin%d" % sl, k_g2], writes=["fin%d" % sl])
                    S.op("dve", lambda: V.scalar_tensor_tensor(out=fin[sl][:], in0=xin[sl][:], scalar=ALPHA, in1=fin[sl][:], op0=ALU.mult, op1=ALU.add),
                         reads=["xin%d" % sl, "fin%d" % sl], writes=["fin%d" % sl])
                    layernorm_block(ph, fin[sl], "fin%d" % sl, st, mv, rs, nmr, "d")
                    S.op("pool", lambda: G.tensor_tensor(out=fin[sl][:], in0=fin[sl][:], in1=lng[:], op=ALU.mult), reads=["fin%d" % sl, k_lng], writes=["fin%d" % sl])
                    S.op("dve", lambda: V.tensor_tensor(out=fin[sl][:], in0=fin[sl][:], in1=lnb[:], op=ALU.add), reads=["fin%d" % sl, k_lnb], writes=["fin%d" % sl])
                    S.dma("sp", "d_o%d" % sl, lambda: nc.sync.dma_start(out=dst[rows, :], in_=fin[sl][:]), reads=["fin%d" % sl], writes=["dst"])
                S.barrier()

        phases = [
            lambda: phase_mods(),
            lambda: phase_a_mla(x_in, 0),
            lambda: phase_b("mla"),
            lambda: phase_c(x_in, 0, mla_w_o),
            lambda: phase_m(0),
            lambda: phase_d(0, X1),
            lambda: phase_a_diff(X1, 1),
            lambda: phase_b("diff"),
            lambda: phase_c(X1, 1, diff_w_o),
            lambda: phase_m(1),
            lambda: phase_d(1, out),
        ]
        for i, p in enumerate(phases):
            if i <= upto:
                p()
        if dbg is not None:
            src = {"XMID": XMID, "X1": X1, "FACC": FACC}[dbg]
            with ExitStack() as ph:
                t = sb(ph, [128, 4, D], F32, "dbg")
                for tb in range(8):
                    S.dma("sp", "dbg_i", lambda: nc.sync.dma_start(out=t[:], in_=src.rearrange("(t s p) d -> t p s d", s=4, p=128)[tb]), writes=["dbg"])
                    S.dma("sp", "dbg_o", lambda: nc.sync.dma_start(out=out.rearrange("(t s p) d -> t p s d", s=4, p=128)[tb], in_=t[:]), reads=["dbg"], writes=["outd"])
        S.finish("sp")
    return nc


def _consts():
    inv32 = (10000.0 ** (-np.arange(0, 64, 2, dtype=np.float32) / 64)).astype(np.float32)
    inv_m = np.concatenate([inv32, inv32]).reshape(64, 1).astype(np.float32)
    inv8 = (500000.0 ** (-np.arange(0, 16, 2, dtype=np.float32) / 16)).astype(np.float32)
    blk = np.zeros(64, np.float32)
    blk[0:8] = inv8
    blk[8:16] = inv8
    inv_d = np.concatenate([blk, blk]).reshape(128, 1).astype(np.float32)
    ident = np.eye(128, dtype=np.float32)
    ltri = np.triu(np.ones((128, 128), np.float32))
    iota = np.tile(np.arange(1, CAP + 1, dtype=np.float32)[None, :], (128, 1))
    rcp = np.zeros((128, NBLK, 2), np.float32)
    rcp[:, :, 0] = np.arange(NBLK, dtype=np.float32)[None, :]
    rcp[:, :, 1] = np.arange(128, dtype=np.float32)[:, None]
    return dict(c_inv_m=inv_m, c_inv_d=inv_d, c_ident=ident, c_ltri=ltri, c_iota=iota, c_rcp=rcp)


def make_in_maps(inputs, ncores=8):
    f = lambda a: np.ascontiguousarray(np.asarray(a))
    shared = dict(
        ada_w=f(inputs["ada_w"]), ada_b=f(inputs["ada_b"]),
        ln1_g=f(inputs["ln1_g"]), ln1_b=f(inputs["ln1_b"]), ln2_g=f(inputs["ln2_g"]), ln2_b=f(inputs["ln2_b"]),
        mla_w_in=f(inputs["mla_w_in"][0]), mla_q_norm=f(inputs["mla_q_norm"][0]), mla_kv_norm=f(inputs["mla_kv_norm"][0]),
        mla_w_uq=f(inputs["mla_w_uq"][0]), mla_w_ukv=f(inputs["mla_w_ukv"][0]), mla_w_o=f(inputs["mla_w_o"][0]),
        diff_w_in=f(inputs["diff_w_in"][0]), diff_lambda=f(inputs["diff_lambda"][0]).reshape(1, 256),
        diff_subln=f(inputs["diff_subln"][0]).reshape(128, 1), diff_w_o=f(inputs["diff_w_o"][0]),
        router_w=f(inputs["router_w"]), moe_w_in=f(inputs["moe_w_in"]), moe_w_out=f(inputs["moe_w_out"]),
    )
    shared.update(_consts())
    maps = []
    for c in range(ncores):
        b = c % 4
        m = dict(shared)
        m["x"] = f(inputs["x"][b])
        m["cT"] = f(np.asarray(inputs["c"][b]).reshape(8, 128).T)
        m["pos"] = f(np.asarray(inputs["positions"][b]).reshape(1, S_LEN).astype(np.int32))
        maps.append(m)
    return maps


def kernel(**inputs):
    nc = build()
    maps = make_in_maps(inputs, 8)
    res = run_bass_kernel_spmd(nc, maps, core_ids=list(range(8)))
    return np.stack([np.asarray(res.results[b]["out"], dtype=np.float32) for b in range(4)], axis=0)
```

```python
import math
from contextlib import ExitStack

import numpy as np
import ml_dtypes
import concourse.bass as bass
import concourse.mybir as mybir
from concourse.bass_utils import run_bass_kernel_spmd

F32 = mybir.dt.float32
BF16 = mybir.dt.bfloat16
I32 = mybir.dt.int32
ALU = mybir.AluOpType
AF = mybir.ActivationFunctionType

S_LEN = 4096
D = 1024
NBLK = S_LEN // 128
NE = 16
CAP = 512
FF = 2048
ALPHA = 4 ** 0.25
LAMBDA_INIT1 = 0.8 - 0.6 * math.exp(-0.3)
PI = math.pi


class Sched:
    def __init__(self, nc, es):
        self.nc = nc
        self.es = es
        self.engs = {"pe": nc.tensor, "act": nc.scalar, "dve": nc.vector, "pool": nc.gpsimd, "sp": nc.sync}
        self.prog = {}
        for e in self.engs:
            self.prog[e] = [es.enter_context(nc.semaphore("prog_" + e)), 0]
        self.known = {e: {} for e in self.engs}
        self.lastw = {}
        self.readers = {}
        self.dsem = {}
        self.nsem = 0

    def _wait(self, e, tok):
        if tok is None:
            return
        sem, val = tok
        k = self.known[e]
        if k.get(sem.num, 0) >= val:
            return
        self.engs[e].wait_ge(sem, val)
        k[sem.num] = val

    def deps(self, e, reads, writes):
        for r in reads:
            self._wait(e, self.lastw.get(r))
        for w in writes:
            self._wait(e, self.lastw.get(w))
            for tok in list(self.readers.get(w, {}).values()):
                self._wait(e, tok)

    def commit(self, tok, reads, writes):
        for r in reads:
            self.readers.setdefault(r, {})[tok[0].num] = tok
        for w in writes:
            self.lastw[w] = tok
            self.readers[w] = {}

    def op(self, e, fn, reads=(), writes=(), cwrites=None):
        return self.ops(e, [fn], reads, writes, cwrites)

    def ops(self, e, fns, reads=(), writes=(), cwrites=None):
        self.deps(e, reads, writes)
        ins = None
        for fn in fns:
            ins = fn()
        p = self.prog[e]
        p[1] += 1
        ins.then_inc(p[0], 1)
        tok = (p[0], p[1])
        self.commit(tok, reads, writes if cwrites is None else cwrites)
        return tok

    def dma(self, q, key, fn, reads=(), writes=()):
        self.deps(q, reads, writes)
        ins = fn()
        if key not in self.dsem:
            self.dsem[key] = [self.es.enter_context(self.nc.semaphore("d%d" % self.nsem)), 0]
            self.nsem += 1
        s = self.dsem[key]
        s[1] += 16
        ins.then_inc(s[0], 16)
        tok = (s[0], s[1])
        self.commit(tok, reads, writes)
        return tok

    def _all(self):
        toks = [(p[0], p[1]) for p in self.prog.values() if p[1] > 0]
        toks += [(s[0], s[1]) for s in self.dsem.values() if s[1] > 0]
        return toks

    def barrier(self):
        toks = self._all()
        for e in self.engs:
            for t in toks:
                self._wait(e, t)
        self.lastw = {}
        self.readers = {}

    def finish(self, e="sp"):
        for t in self._all():
            self._wait(e, t)


def build(upto=99, dbg=None):
    nc = bass.Bass("TRN2", target_bir_lowering=False)

    def din(name, shape, dt=F32):
        return nc.dram_tensor(name, list(shape), dt, kind="ExternalInput").ap()

    def dscr(name, shape, dt):
        return nc.dram_tensor(name, list(shape), dt, kind="Internal").ap()

    x_in = din("x", [S_LEN, D])
    cT = din("cT", [128, 8])
    pos = din("pos", [1, S_LEN], I32)
    ada_w = din("ada_w", [2, D, 6 * D])
    ada_b = din("ada_b", [2, 6 * D])
    ln1_g = din("ln1_g", [2, D]); ln1_b = din("ln1_b", [2, D])
    ln2_g = din("ln2_g", [2, D]); ln2_b = din("ln2_b", [2, D])
    mla_w_in = din("mla_w_in", [D, 704])
    mla_q_norm = din("mla_q_norm", [384]); mla_kv_norm = din("mla_kv_norm", [256])
    mla_w_uq = din("mla_w_uq", [384, 1536])
    mla_w_ukv = din("mla_w_ukv", [256, 2048])
    mla_w_o = din("mla_w_o", [D, D])
    diff_w_in = din("diff_w_in", [D, 3072])
    diff_lambda = din("diff_lambda", [1, 256])
    diff_subln = din("diff_subln", [128, 1])
    diff_w_o = din("diff_w_o", [D, D])
    router_w = din("router_w", [2, D, NE])
    moe_w_in = din("moe_w_in", [2, NE, D, 2 * FF])
    moe_w_out = din("moe_w_out", [2, NE, FF, D])
    c_inv_m = din("c_inv_m", [64, 1])
    c_inv_d = din("c_inv_d", [128, 1])
    c_ident = din("c_ident", [128, 128])
    c_ltri = din("c_ltri", [128, 128])
    c_iota = din("c_iota", [128, CAP])
    c_rcp = din("c_rcp", [128, NBLK, 2])

    out = nc.dram_tensor("out", [S_LEN, D], F32, kind="ExternalOutput").ap()

    MODS = dscr("MODS", [2, 6 * D], F32)
    QT = dscr("QT", [8, 192, S_LEN], BF16)
    KT = dscr("KT", [8, 128, S_LEN], BF16)
    KPE = dscr("KPE", [64, S_LEN], BF16)
    VV = dscr("VV", [S_LEN, D], BF16)
    OT = dscr("OT", [D, S_LEN], BF16)
    XMID = dscr("XMID", [S_LEN, D], F32)
    H2 = dscr("H2", [S_LEN, D], BF16)
    FACC = dscr("FACC", [S_LEN, D], F32)
    X1 = dscr("X1", [S_LEN, D], F32)

    es = ExitStack()
    with es:
        S = Sched(nc, es)
        uid = [0]

        def sb(stack, shape, dt, name=None):
            uid[0] += 1
            return stack.enter_context(nc.sbuf_tensor("%s_%d" % (name or "t", uid[0]), list(shape), dt))

        dbanks = [es.enter_context(nc.psum_tensor("dbank%d" % i, [128, 1024], F32)) for i in range(4)]
        banks = [dbanks[i // 2][:, (i % 2) * 512:(i % 2 + 1) * 512] for i in range(8)]
        ident_f = sb(es, [128, 128], F32, "identf")
        ident_b = sb(es, [128, 128], BF16, "identb")
        ones_b = sb(es, [128, 128], BF16, "onesb")
        aff = sb(es, [128, NBLK, NE], F32, "aff")
        eps5 = sb(es, [128, 1], F32, "eps5")
        S.dma("sp", "c0", lambda: nc.sync.dma_start(out=ident_f[:], in_=c_ident), writes=["identf"])
        S.op("dve", lambda: nc.vector.tensor_copy(out=ident_b[:], in_=ident_f[:]), reads=["identf"], writes=["identb"])
        S.op("dve", lambda: nc.vector.memset(ones_b[:], 1.0), writes=["onesb"])
        S.op("dve", lambda: nc.vector.memset(eps5[:], 1e-5), writes=["eps5"])

        V = nc.vector
        A = nc.scalar
        PE = nc.tensor
        G = nc.gpsimd

        def bcast_load(stack, src_row_ap, name, plus_one=False, q="sp"):
            n = src_row_ap.shape[-1]
            t = sb(stack, [128, n], F32, name)
            key = "%s_%d" % (name, uid[0])
            S.dma(q, "bc_" + key, lambda: S.engs[q].dma_start(out=t[:], in_=src_row_ap.partition_broadcast(128)), writes=[key])
            if plus_one:
                S.op("pool", lambda: G.tensor_scalar(out=t[:], in0=t[:], scalar1=1.0, scalar2=None, op0=ALU.add), reads=[key], writes=[key])
            return t, key

        def phase_mods():
            with ExitStack() as ph:
                c_sb = sb(ph, [128, 8], F32, "c")
                c16 = sb(ph, [128, 8], BF16, "c16")
                adab = sb(ph, [1, 6 * D], F32, "adab")
                modrow = sb(ph, [1, 6 * D], F32, "modrow")
                wblk = [sb(ph, [128, 8, 512], BF16, "adaw") for _ in range(3)]
                S.dma("sp", "m_c", lambda: nc.sync.dma_start(out=c_sb[:], in_=cT), writes=["c"])
                S.op("act", lambda: A.activation(out=c16[:], in_=c_sb[:], func=AF.Silu), reads=["c"], writes=["c16"])
                it = 0
                for l in range(2):
                    S.dma("sp", "m_b", lambda: nc.sync.dma_start(out=adab[:], in_=ada_b[l:l + 1, :]), writes=["adab"])
                    wv = ada_w[l].rearrange("(k p) n -> p k n", p=128)
                    for j in range(12):
                        sl = it % 3
                        S.dma("pool", "m_w%d" % sl, lambda: G.dma_start(out=wblk[sl][:], in_=wv[:, :, j * 512:(j + 1) * 512]), writes=["adaw%d" % sl])
                        pb = banks[it % 2]
                        S.ops("pe", [(lambda k=k: PE.matmul(pb[0:1, :], lhsT=c16[:, k:k + 1], rhs=wblk[sl][:, k, :], start=(k == 0), stop=(k == 7))) for k in range(8)],
                              reads=["c16", "adaw%d" % sl], writes=["bank%d" % (it % 2)])
                        S.op("dve", lambda: V.tensor_tensor(out=modrow[0:1, j * 512:(j + 1) * 512], in0=pb[0:1, :], in1=adab[0:1, j * 512:(j + 1) * 512], op=ALU.add),
                             reads=["bank%d" % (it % 2), "adab"], writes=["modrow"])
                        it += 1
                    S.dma("sp", "m_o", lambda: nc.sync.dma_start(out=MODS[l:l + 1, :], in_=modrow[:]), reads=["modrow"], writes=["MODS"])
                S.barrier()

        def rope_tables(ph, R, inv_ap):
            Ct = sb(ph, [R, S_LEN], F32, "ropeC")
            St = sb(ph, [R, S_LEN], F32, "ropeS")
            inv = sb(ph, [R, 1], F32, "inv")
            negpi = sb(ph, [R, 1], F32, "negpi")
            S.dma("sp", "r_inv", lambda: nc.sync.dma_start(out=inv[:], in_=inv_ap), writes=["inv"])
            with ExitStack() as tmp:
                pi_t = sb(tmp, [R, 1024], I32, "posi")
                ang = sb(tmp, [R, 1024], F32, "ang")
                nf = sb(tmp, [R, 1024], F32, "nf")
                ni = sb(tmp, [R, 1024], I32, "ni")
                msk = sb(tmp, [R, 1024], F32, "msk")
                for cch in range(4):
                    cs = slice(cch * 1024, (cch + 1) * 1024)
                    S.dma("sp", "r_pos", lambda: nc.sync.dma_start(out=pi_t[:], in_=pos[:, cs].partition_broadcast(R)), writes=["posi"])
                    S.op("dve", lambda: V.tensor_copy(out=ang[:], in_=pi_t[:]), reads=["posi"], writes=["ang"])
                    S.op("dve", lambda: V.tensor_scalar(out=ang[:], in0=ang[:], scalar1=inv[:, 0:1], scalar2=None, op0=ALU.mult), reads=["ang", "inv"], writes=["ang"])
                    S.op("dve", lambda: V.tensor_scalar(out=nf[:], in0=ang[:], scalar1=1.0 / (2 * PI), scalar2=None, op0=ALU.mult), reads=["ang"], writes=["nf"])
                    S.op("dve", lambda: V.tensor_copy(out=ni[:], in_=nf[:]), reads=["nf"], writes=["ni"])
                    S.op("dve", lambda: V.tensor_copy(out=nf[:], in_=ni[:]), reads=["ni"], writes=["nf"])
                    S.op("dve", lambda: V.scalar_tensor_tensor(out=ang[:], in0=nf[:], scalar=-6.28125, in1=ang[:], op0=ALU.mult, op1=ALU.add), reads=["nf", "ang"], writes=["ang"])
                    S.op("dve", lambda: V.scalar_tensor_tensor(out=ang[:], in0=nf[:], scalar=-0.0019353071795864769, in1=ang[:], op0=ALU.mult, op1=ALU.add), reads=["nf", "ang"], writes=["ang"])

                    def wrap():
                        S.op("dve", lambda: V.tensor_scalar(out=msk[:], in0=ang[:], scalar1=PI, scalar2=-2 * PI, op0=ALU.is_gt, op1=ALU.mult), reads=["ang"], writes=["msk"])
                        S.op("dve", lambda: V.tensor_tensor(out=ang[:], in0=ang[:], in1=msk[:], op=ALU.add), reads=["ang", "msk"], writes=["ang"])
                        S.op("dve", lambda: V.tensor_scalar(out=msk[:], in0=ang[:], scalar1=-PI, scalar2=2 * PI, op0=ALU.is_lt, op1=ALU.mult), reads=["ang"], writes=["msk"])
                        S.op("dve", lambda: V.tensor_tensor(out=ang[:], in0=ang[:], in1=msk[:], op=ALU.add), reads=["ang", "msk"], writes=["ang"])
                        S.op("dve", lambda: V.tensor_scalar(out=ang[:], in0=ang[:], scalar1=PI, scalar2=-PI, op0=ALU.min, op1=ALU.max), reads=["ang"], writes=["ang"])
                    wrap()
                    S.op("act", lambda: A.activation(out=St[:, cs], in_=ang[:], func=AF.Sin), reads=["ang"], writes=["ropeS"])
                    S.op("dve", lambda: V.tensor_scalar(out=ang[:], in0=ang[:], scalar1=PI / 2, scalar2=None, op0=ALU.add), reads=["ang"], writes=["ang"])
                    wrap()
                    S.op("act", lambda: A.activation(out=Ct[:, cs], in_=ang[:], func=AF.Sin), reads=["ang"], writes=["ropeC"])
                S.barrier()
            return Ct, St

        def front_block(tb, xsrc, xin, h16, hT, sc1p, sh1p, kx):
            sl = tb % 2
            xv = xsrc.rearrange("(t s p) d -> t p s d", s=4, p=128)
            S.dma("sp", "xin%d" % sl, lambda: nc.sync.dma_start(out=xin[sl][:], in_=xv[tb]), writes=["xin%d" % sl])
            S.op("dve", lambda: V.tensor_tensor(out=xin[sl][:], in0=xin[sl][:], in1=sc1p[:].unsqueeze(1).broadcast_to([128, 4, D]), op=ALU.mult),
                 reads=["xin%d" % sl, kx[0]], writes=["xin%d" % sl])
            S.op("pool", lambda: G.tensor_tensor(out=h16[:], in0=xin[sl][:], in1=sh1p[:].unsqueeze(1).broadcast_to([128, 4, D]), op=ALU.add),
                 reads=["xin%d" % sl, kx[1]], writes=["h16"])
            for kp in range(4):
                bk = banks[6 + kp % 2]
                bkey = "bank%d" % (6 + kp % 2)
                tv = bk[:].bitcast(BF16)
                fns = []
                for kk in range(2):
                    k = kp * 2 + kk
                    for s in range(4):
                        fns.append(lambda k=k, kk=kk, s=s: PE.transpose(tv[:, kk * 512 + s * 128: kk * 512 + (s + 1) * 128], h16[:, s, k * 128:(k + 1) * 128], ident_b[:]))
                S.ops("pe", fns, reads=["h16", "identb"], writes=[bkey])
                S.op("act", lambda: A.copy(out=hT[:, kp * 2:kp * 2 + 2, :].rearrange("p a b -> p (a b)"), in_=tv), reads=[bkey], writes=["hT"])

        pring = [0]

        def next_bank(lo=0, hi=6):
            b = lo + pring[0] % (hi - lo)
            pring[0] += 1
            return banks[b], "bank%d" % b

        def rsqrt_into(dst, dkey, src_ap, skey, mul, add):
            S.op("dve", lambda: V.tensor_scalar(out=dst, in0=src_ap, scalar1=mul, scalar2=add, op0=ALU.mult, op1=ALU.add), reads=[skey], writes=[dkey])
            S.op("act", lambda: A.activation(out=dst, in_=dst, func=AF.Sqrt), reads=[dkey], writes=[dkey])
            S.op("dve", lambda: V.reciprocal(out=dst, in_=dst), reads=[dkey], writes=[dkey])

        def phase_a_mla(xsrc, l):
            with ExitStack() as ph:
                Ct, St = rope_tables(ph, 64, c_inv_m)
                sc1p, k_sc = bcast_load(ph, MODS[l:l + 1, D:2 * D], "sc1p", plus_one=True)
                sh1p, k_sh = bcast_load(ph, MODS[l:l + 1, 0:D], "sh1p")
                w_in = sb(ph, [128, 8, 768], BF16, "w_in")
                w_uq = sb(ph, [128, 3, 8, 256], BF16, "w_uq")
                w_k = sb(ph, [128, 2, 8, 128], BF16, "w_k")
                w_v = sb(ph, [128, 2, 8, 128], BF16, "w_v")
                qn = sb(ph, [128, 3], F32, "qn")
                kvn = sb(ph, [128, 2], F32, "kvn")
                S.dma("pool", "a_w0", lambda: G.dma_start(out=w_in[:, :, 0:704], in_=mla_w_in.rearrange("(k p) n -> p k n", p=128)), writes=["w_in"])
                S.op("act", lambda: A.mul(out=w_in[:, :, 704:736], in_=w_in[:, :, 672:704], mul=-1.0), reads=["w_in"], writes=["w_in"])
                S.op("act", lambda: A.copy(out=w_in[:, :, 736:768], in_=w_in[:, :, 640:672]), reads=["w_in"], writes=["w_in"])
                uqv = mla_w_uq.rearrange("(k p) (h e) -> p k h e", p=128, e=192)
                for k in range(3):
                    S.dma("pool", "a_w1", lambda: G.dma_start(out=w_uq[:, k, :, 0:192], in_=uqv[:, k, :, :]), writes=["w_uq"])
                S.op("act", lambda: A.mul(out=w_uq[:, :, :, 192:224], in_=w_uq[:, :, :, 160:192], mul=-1.0), reads=["w_uq"], writes=["w_uq"])
                S.op("act", lambda: A.copy(out=w_uq[:, :, :, 224:256], in_=w_uq[:, :, :, 128:160]), reads=["w_uq"], writes=["w_uq"])
                wkv = mla_w_ukv.rearrange("(k p) (h two e) -> p k h two e", p=128, two=2, e=128)
                for k in range(2):
                    S.dma("pool", "a_w2", lambda: G.dma_start(out=w_k[:, k, :, :], in_=wkv[:, k, :, 0, :]), writes=["w_k"])
                    S.dma("pool", "a_w3", lambda: G.dma_start(out=w_v[:, k, :, :], in_=wkv[:, k, :, 1, :]), writes=["w_v"])
                with nc.allow_non_contiguous_dma(reason="tiny norm-gain vectors"):
                    S.dma("sp", "a_n0", lambda: nc.sync.dma_start(out=qn[:], in_=mla_q_norm.rearrange("(k p) -> p k", p=128)), writes=["qn"])
                    S.dma("sp", "a_n1", lambda: nc.sync.dma_start(out=kvn[:], in_=mla_kv_norm.rearrange("(k p) -> p k", p=128)), writes=["kvn"])
                xin = [sb(ph, [128, 4, D], F32, "xin") for _ in range(2)]
                h16 = sb(ph, [128, 4, D], BF16, "h16")
                hT = sb(ph, [128, 8, 512], BF16, "hT")
                lat = sb(ph, [128, 7, 512], F32, "lat")
                sq = sb(ph, [128, 5, 512], BF16, "sq")
                rstd = sb(ph, [128, 2, 512], F32, "rstd")
                cqn = sb(ph, [128, 3, 512], BF16, "cqn")
                ckvn = sb(ph, [128, 2, 512], BF16, "ckvn")
                kpe = sb(ph, [64, 512], BF16, "kpe")
                tmp1 = sb(ph, [64, 512], F32, "tmp1")
                tmp2 = sb(ph, [64, 512], F32, "tmp2")
                qst = sb(ph, [128, 8, 512], BF16, "qst")
                qrst = sb(ph, [64, 8, 512], BF16, "qrst")
                kst = sb(ph, [128, 8, 512], BF16, "kst")
                vst = sb(ph, [128, 4, D], BF16, "vst")
                mspec = [(0, 128, 128), (1, 256, 128), (2, 384, 128), (3, 512, 128), (4, 640, 128), (5, 704, 64), (6, 768, 64)]
                for tb in range(8):
                    ts = slice(tb * 512, (tb + 1) * 512)
                    front_block(tb, xsrc, xin, h16, hT, sc1p, sh1p, (k_sc, k_sh))
                    for (mi, hi_, mm) in mspec:
                        pb, pk = next_bank()
                        S.ops("pe", [(lambda k=k: PE.matmul(pb[0:mm, :], lhsT=w_in[:, k, hi_ - mm:hi_], rhs=hT[:, k, :], start=(k == 0), stop=(k == 7))) for k in range(8)],
                              reads=["w_in", "hT"], writes=[pk])
                        S.op("act", lambda: A.copy(out=lat[0:mm, mi, :], in_=pb[0:mm, :]), reads=[pk], writes=["lat%d" % mi])
                        if mi < 5:
                            S.op("dve", lambda: V.tensor_tensor(out=sq[:, mi, :], in0=lat[:, mi, :], in1=lat[:, mi, :], op=ALU.mult), reads=["lat%d" % mi], writes=["sq%d" % mi])
                    for gi, (c0, c1, n) in enumerate([(0, 3, 384), (3, 5, 256)]):
                        pb, pk = next_bank()
                        S.ops("pe", [(lambda c=c: PE.matmul(pb[:, :], lhsT=ones_b[:], rhs=sq[:, c, :], start=(c == c0), stop=(c == c1 - 1))) for c in range(c0, c1)],
                              reads=["onesb"] + ["sq%d" % c for c in range(c0, c1)], writes=[pk])
                        rsqrt_into(rstd[:, gi, :], "rstd%d" % gi, pb[:, :], pk, 1.0 / n, 1e-6)
                    for c in range(3):
                        S.op("dve", lambda: V.scalar_tensor_tensor(out=cqn[:, c, :], in0=lat[:, c, :], scalar=qn[:, c:c + 1], in1=rstd[:, 0, :], op0=ALU.mult, op1=ALU.mult),
                             reads=["lat%d" % c, "qn", "rstd0"], writes=["cqn"])
                    for c in range(2):
                        S.op("dve", lambda: V.scalar_tensor_tensor(out=ckvn[:, c, :], in0=lat[:, 3 + c, :], scalar=kvn[:, c:c + 1], in1=rstd[:, 1, :], op0=ALU.mult, op1=ALU.mult),
                             reads=["lat%d" % (3 + c), "kvn", "rstd1"], writes=["ckvn"])
                    S.op("dve", lambda: V.tensor_tensor(out=tmp1[:], in0=lat[0:64, 5, :], in1=Ct[:, ts], op=ALU.mult), reads=["lat5"], writes=["tmp1"])
                    S.op("pool", lambda: G.tensor_tensor(out=tmp2[:], in0=lat[0:64, 6, :], in1=St[:, ts], op=ALU.mult), reads=["lat6"], writes=["tmp2"])
                    S.op("dve", lambda: V.tensor_tensor(out=kpe[:], in0=tmp1[:], in1=tmp2[:], op=ALU.add), reads=["tmp1", "tmp2"], writes=["kpe"])
                    S.dma("sp", "a_kpe", lambda: nc.sync.dma_start(out=KPE[:, ts], in_=kpe[:]), reads=["kpe"], writes=["KPE"])
                    for h in range(8):
                        pb, pk = next_bank()
                        S.ops("pe", [(lambda c=c: PE.matmul(pb[:, :], lhsT=w_uq[:, c, h, 0:128], rhs=cqn[:, c, :], start=(c == 0), stop=(c == 2))) for c in range(3)],
                              reads=["w_uq", "cqn"], writes=[pk])
                        S.op("act", lambda: A.copy(out=qst[:, h, :], in_=pb[:, :]), reads=[pk], writes=["qst"])
                        pa, pka = next_bank()
                        S.ops("pe", [(lambda c=c: PE.matmul(pa[0:64, :], lhsT=w_uq[:, c, h, 128:192], rhs=cqn[:, c, :], start=(c == 0), stop=(c == 2))) for c in range(3)],
                              reads=["w_uq", "cqn"], writes=[pka])
                        pr, pkr = next_bank()
                        S.ops("pe", [(lambda c=c: PE.matmul(pr[0:64, :], lhsT=w_uq[:, c, h, 192:256], rhs=cqn[:, c, :], start=(c == 0), stop=(c == 2))) for c in range(3)],
                              reads=["w_uq", "cqn"], writes=[pkr])
                        S.op("dve", lambda: V.tensor_tensor(out=tmp1[:], in0=pa[0:64, :], in1=Ct[:, ts], op=ALU.mult), reads=[pka], writes=["tmp1"])
                        S.op("dve", lambda: V.tensor_tensor(out=tmp2[:], in0=pr[0:64, :], in1=St[:, ts], op=ALU.mult), reads=[pkr], writes=["tmp2"])
                        S.op("pool", lambda: G.tensor_tensor(out=qrst[:, h, :], in0=tmp1[:], in1=tmp2[:], op=ALU.add), reads=["tmp1", "tmp2"], writes=["qrst"])
                        pk_, pkk = next_bank()
                        S.ops("pe", [(lambda c=c: PE.matmul(pk_[:, :], lhsT=w_k[:, c, h, :], rhs=ckvn[:, c, :], start=(c == 0), stop=(c == 1))) for c in range(2)],
                              reads=["w_k", "ckvn"], writes=[pkk])
                        S.op("act", lambda: A.copy(out=kst[:, h, :], in_=pk_[:, :]), reads=[pkk], writes=["kst"])
                    S.dma("sp", "a_q", lambda: nc.sync.dma_start(out=QT[:, 0:128, ts].rearrange("h p t -> p h t"), in_=qst[:]), reads=["qst"], writes=["QT"])
                    S.dma("sp", "a_qr", lambda: nc.sync.dma_start(out=QT[:, 128:192, ts].rearrange("h p t -> p h t"), in_=qrst[:]), reads=["qrst"], writes=["QT"])
                    S.dma("sp", "a_k", lambda: nc.sync.dma_start(out=KT[:, :, ts].rearrange("h p t -> p h t"), in_=kst[:]), reads=["kst"], writes=["KT"])
                    for s in range(4):
                        for hf in range(2):
                            pb, pk = next_bank()
                            S.ops("pe", [(lambda c=c: PE.matmul(pb[:, :], lhsT=ckvn[:, c, s * 128:(s + 1) * 128], rhs=w_v[:, c, hf * 4:(hf + 1) * 4, :].rearrange("p h e -> p (h e)"), start=(c == 0), stop=(c == 1))) for c in range(2)],
                                  reads=["w_v", "ckvn"], writes=[pk])
                            S.op("act" if hf else "dve", (lambda pb=pb, hf=hf: (A.copy if hf else V.tensor_copy)(out=vst[:, s, hf * 512:(hf + 1) * 512], in_=pb[:, :])), reads=[pk], writes=["vst"])
                    S.dma("sp", "a_v", lambda: nc.sync.dma_start(out=VV.rearrange("(t s p) d -> t p s d", s=4, p=128)[tb], in_=vst[:]), reads=["vst"], writes=["VV"])
                S.barrier()

        def phase_a_diff(xsrc, l):
            with ExitStack() as ph:
                Ct, St = rope_tables(ph, 128, c_inv_d)
                sc1p, k_sc = bcast_load(ph, MODS[l:l + 1, D:2 * D], "sc1p", plus_one=True)
                sh1p, k_sh = bcast_load(ph, MODS[l:l + 1, 0:D], "sh1p")
                w = sb(ph, [128, 8, 3072], BF16, "dw")
                wr = sb(ph, [128, 8, 2048], BF16, "dwr")
                wsrc = diff_w_in.rearrange("(k p) n -> p k n", p=128)
                for j in range(3):
                    S.dma("pool", "d_w%d" % j, lambda: G.dma_start(out=w[:, :, j * 1024:(j + 1) * 1024], in_=wsrc[:, :, j * 1024:(j + 1) * 1024]), writes=["dw"])
                S.op("pool", lambda: G.memset(wr[:], 0.0), writes=["dwr"])
                w4 = w[:, :, 0:2048].rearrange("p k (g e) -> p k g e", e=64)
                wr4 = wr[:].rearrange("p k (g e) -> p k g e", e=64)
                for k in range(8):
                    S.op("act", lambda: A.mul(out=wr4[:, k, :, 0:8], in_=w4[:, k, :, 8:16], mul=-1.0), reads=["dw", "dwr"], writes=["dwr"])
                    S.op("act", lambda: A.copy(out=wr4[:, k, :, 8:16], in_=w4[:, k, :, 0:8]), reads=["dw", "dwr"], writes=["dwr"])
                xin = [sb(ph, [128, 4, D], F32, "xin") for _ in range(2)]
                h16 = sb(ph, [128, 4, D], BF16, "h16")
                hT = sb(ph, [128, 8, 512], BF16, "hT")
                tmp1 = sb(ph, [128, 512], F32, "tmp1")
                tmp2 = sb(ph, [128, 512], F32, "tmp2")
                qst = sb(ph, [128, 8, 512], BF16, "qst")
                kst = sb(ph, [128, 8, 512], BF16, "kst")
                vst = sb(ph, [128, 4, D], BF16, "vst")
                for tb in range(8):
                    ts = slice(tb * 512, (tb + 1) * 512)
                    front_block(tb, xsrc, xin, h16, hT, sc1p, sh1p, (k_sc, k_sh))
                    for qk in range(2):
                        st = qst if qk == 0 else kst
                        skey = "qst" if qk == 0 else "kst"
                        for h in range(8):
                            c0 = qk * 1024 + h * 128
                            pa, pka = next_bank()
                            S.ops("pe", [(lambda k=k: PE.matmul(pa[:, :], lhsT=w[:, k, c0:c0 + 128], rhs=hT[:, k, :], start=(k == 0), stop=(k == 7))) for k in range(8)],
                                  reads=["dw", "hT"], writes=[pka])
                            pr, pkr = next_bank()
                            S.ops("pe", [(lambda k=k: PE.matmul(pr[:, :], lhsT=wr[:, k, c0:c0 + 128], rhs=hT[:, k, :], start=(k == 0), stop=(k == 7))) for k in range(8)],
                                  reads=["dwr", "hT"], writes=[pkr])
                            S.op("dve", lambda: V.tensor_tensor(out=tmp1[:], in0=pa[:, :], in1=Ct[:, ts], op=ALU.mult), reads=[pka], writes=["tmp1"])
                            S.op("dve", lambda: V.tensor_tensor(out=tmp2[:], in0=pr[:, :], in1=St[:, ts], op=ALU.mult), reads=[pkr], writes=["tmp2"])
                            S.op("pool", lambda: G.tensor_tensor(out=st[:, h, :], in0=tmp1[:], in1=tmp2[:], op=ALU.add), reads=["tmp1", "tmp2"], writes=[skey])
                    S.dma("sp", "a_q", lambda: nc.sync.dma_start(out=QT[:, 0:128, ts].rearrange("h p t -> p h t"), in_=qst[:]), reads=["qst"], writes=["QT"])
                    S.dma("sp", "a_k", lambda: nc.sync.dma_start(out=KT[:, :, ts].rearrange("h p t -> p h t"), in_=kst[:]), reads=["kst"], writes=["KT"])
                    for s in range(4):
                        for hf in range(2):
                            pb, pk = next_bank()
                            S.ops("pe", [(lambda k=k: PE.matmul(pb[:, :], lhsT=hT[:, k, s * 128:(s + 1) * 128], rhs=w[:, k, 2048 + hf * 512:2048 + (hf + 1) * 512], start=(k == 0), stop=(k == 7))) for k in range(8)],
                                  reads=["dw", "hT"], writes=[pk])
                            S.op("act", lambda: A.copy(out=vst[:, s, hf * 512:(hf + 1) * 512], in_=pb[:, :]), reads=[pk], writes=["vst"])
                    S.dma("sp", "a_v", lambda: nc.sync.dma_start(out=VV.rearrange("(t s p) d -> t p s d", s=4, p=128)[tb], in_=vst[:]), reads=["vst"], writes=["VV"])
                S.barrier()

        def phase_b(kind):
            mla = kind == "mla"
            nmap = 1 if mla else 2
            scale = (192 ** -0.5) if mla else (64 ** -0.5)
            with ExitStack() as ph:
                kbuf = [sb(ph, [128, S_LEN], BF16, "kbuf") for _ in range(2)]
                qbuf = [sb(ph, [128, S_LEN], BF16, "qbuf") for _ in range(2)]
                vbuf = [sb(ph, [128, NBLK, 128], BF16, "vbuf") for _ in range(2)]
                PT = [sb(ph, [128, 1024], BF16, "PT") for _ in range(2)]
                dacc = [sb(ph, [128, 1024], F32, "dacc") for _ in range(2)]
                ones_f = sb(ph, [128, 128], F32, "onesf")
                S.op("dve", lambda: V.memset(ones_f[:], 1.0), writes=["onesf"])
                rden = [sb(ph, [128, 512], F32, "rden") for _ in range(2)]
                osb = [sb(ph, [128, 512], BF16, "osb") for _ in range(2)]
                if mla:
                    qrbuf = [sb(ph, [64, S_LEN], BF16, "qrbuf") for _ in range(2)]
                    kpe = sb(ph, [64, S_LEN], BF16, "kpeall")
                    S.dma("sp", "b_kpe", lambda: nc.sync.dma_start(out=kpe[:], in_=KPE), writes=["kpeall"])
                else:
                    lamt = sb(ph, [128, 256], F32, "lamt")
                    lp = sb(ph, [128, 128], F32, "lp")
                    ls = sb(ph, [128, 2], F32, "ls")
                    neglam = sb(ph, [128, 1], F32, "neglam")
                    subs = sb(ph, [128, 1], F32, "subs")
                    o1n = sb(ph, [128, 512], F32, "o1n")
                    o2n = sb(ph, [128, 512], F32, "o2n")
                    sq = sb(ph, [128, 512], BF16, "osq")
                    rstd = sb(ph, [128, 512], F32, "orstd")
                    S.dma("sp", "b_lam", lambda: nc.sync.dma_start(out=lamt[:], in_=diff_lambda.partition_broadcast(128)), writes=["lamt"])
                    S.dma("sp", "b_sub", lambda: nc.sync.dma_start(out=subs[:], in_=diff_subln), writes=["subs"])
                    S.op("dve", lambda: V.tensor_tensor(out=lp[:, 0:64], in0=lamt[:, 0:64], in1=lamt[:, 64:128], op=ALU.mult), reads=["lamt"], writes=["lp"])
                    S.op("dve", lambda: V.tensor_tensor(out=lp[:, 64:128], in0=lamt[:, 128:192], in1=lamt[:, 192:256], op=ALU.mult), reads=["lamt", "lp"], writes=["lp"])
                    S.op("dve", lambda: V.reduce_sum(out=ls[:, 0:1], in_=lp[:, 0:64], axis=mybir.AxisListType.X), reads=["lp"], writes=["ls"])
                    S.op("dve", lambda: V.reduce_sum(out=ls[:, 1:2], in_=lp[:, 64:128], axis=mybir.AxisListType.X), reads=["lp", "ls"], writes=["ls"])
                    S.op("act", lambda: A.activation(out=ls[:], in_=ls[:], func=AF.Exp), reads=["ls"], writes=["ls"])
                    S.op("dve", lambda: V.tensor_tensor(out=neglam[:], in0=ls[:, 1:2], in1=ls[:, 0:1], op=ALU.subtract), reads=["ls"], writes=["neglam"])
                    S.op("dve", lambda: V.tensor_scalar(out=neglam[:], in0=neglam[:], scalar1=-LAMBDA_INIT1, scalar2=None, op0=ALU.add), reads=["neglam"], writes=["neglam"])
                    S.op("dve", lambda: V.tensor_scalar(out=subs[:], in0=subs[:], scalar1=1.0 - LAMBDA_INIT1, scalar2=None, op0=ALU.mult), reads=["subs"], writes=["subs"])

                vview = VV.rearrange("(c p) (h e) -> p c h e", p=128, e=128)

                def load_head(h):
                    sl = h % 2
                    S.dma("sp", "b_k%d" % sl, lambda: nc.sync.dma_start(out=kbuf[sl][:], in_=KT[h]), writes=["kbuf%d" % sl])
                    S.dma("sp", "b_q%d" % sl, lambda: nc.sync.dma_start(out=qbuf[sl][:], in_=QT[h, 0:128, :]), writes=["qbuf%d" % sl])
                    S.dma("sp", "b_v%d" % sl, lambda: nc.sync.dma_start(out=vbuf[sl][:], in_=vview[:, :, h, :]), writes=["vbuf%d" % sl])
                    if mla:
                        S.dma("sp", "b_qr%d" % sl, lambda: nc.sync.dma_start(out=qrbuf[sl][:], in_=QT[h, 128:192, :]), writes=["qrbuf%d" % sl])

                load_head(0)
                blk = 0
                for h in range(8):
                    if h + 1 < 8:
                        load_head(h + 1)
                    sl = h % 2
                    for qb in range(8):
                        qs = slice(qb * 512, (qb + 1) * 512)
                        par = blk % 2
                        blk += 1
                        if mla:
                            acc_o, ko = banks[4 + par], "bank%d" % (4 + par)
                            den, kd = banks[6 + par], "bank%d" % (6 + par)
                            NU = NBLK // 2

                            def qk(u):
                                db = dbanks[u % 2]
                                fns = []
                                for j in range(2):
                                    kc = 2 * u + j
                                    ks = slice(kc * 128, (kc + 1) * 128)
                                    fns.append(lambda j=j, ks=ks: PE.matmul(db[:, j * 512:(j + 1) * 512], lhsT=kbuf[sl][:, ks], rhs=qbuf[sl][:, qs], start=True, stop=False))
                                    fns.append(lambda j=j, ks=ks: PE.matmul(db[:, j * 512:(j + 1) * 512], lhsT=kpe[:, ks], rhs=qrbuf[sl][:, qs], start=False, stop=True))
                                S.ops("pe", fns, reads=["kbuf%d" % sl, "qbuf%d" % sl, "qrbuf%d" % sl, "kpeall"], writes=["bank%d" % (2 * (u % 2)), "bank%d" % (2 * (u % 2) + 1)])

                            def ex(u):
                                S.op("act", lambda: A.activation(out=PT[u % 2][:], in_=dbanks[u % 2][:, :], func=AF.Exp, scale=scale),
                                     reads=["bank%d" % (2 * (u % 2)), "bank%d" % (2 * (u % 2) + 1)], writes=["PT%d" % (u % 2)])

                            def pv(u):
                                fns = []
                                for j in range(2):
                                    kc = 2 * u + j
                                    fns.append(lambda j=j, kc=kc: PE.matmul(acc_o, lhsT=vbuf[sl][:, kc, :], rhs=PT[u % 2][:, j * 512:(j + 1) * 512], start=(kc == 0), stop=(kc == NBLK - 1)))
                                S.ops("pe", fns, reads=["PT%d" % (u % 2), "vbuf%d" % sl], writes=[ko] if u == 0 else [], cwrites=[ko] if u == NU - 1 else [])
                                if u == 0:
                                    S.op("dve", lambda: V.tensor_copy(out=dacc[par][:], in_=PT[u % 2][:]), reads=["PT%d" % (u % 2)], writes=["dacc%d" % par])
                                else:
                                    S.op("dve", lambda: V.tensor_tensor(out=dacc[par][:], in0=dacc[par][:], in1=PT[u % 2][:], op=ALU.add), reads=["PT%d" % (u % 2), "dacc%d" % par], writes=["dacc%d" % par])
                            qk(0)
                            for u in range(NU):
                                ex(u)
                                if u + 1 < NU:
                                    qk(u + 1)
                                pv(u)
                            S.ops("pe", [lambda: PE.matmul(den, lhsT=ones_f[:], rhs=dacc[par][:, 0:512], start=True, stop=False),
                                         lambda: PE.matmul(den, lhsT=ones_f[:], rhs=dacc[par][:, 512:1024], start=False, stop=True)],
                                  reads=["onesf", "dacc%d" % par], writes=[kd])
                            S.op("dve", lambda: V.reciprocal(out=rden[par][:], in_=den), reads=[kd], writes=["rden%d" % par])
                            S.op("dve", lambda: V.tensor_tensor(out=osb[par][:], in0=acc_o, in1=rden[par][:], op=ALU.mult), reads=[ko, "rden%d" % par], writes=["osb%d" % par])
                        else:
                            accs = [(banks[4 + i], "bank%d" % (4 + i)) for i in range(4)]

                            def qk(kc):
                                ks = slice(kc * 128, (kc + 1) * 128)
                                db = dbanks[kc % 2]
                                fns = []
                                for m in range(2):
                                    rows = slice(m * 64, (m + 1) * 64)
                                    fns.append(lambda m=m, rows=rows: PE.matmul(db[:, m * 512:(m + 1) * 512], lhsT=kbuf[sl][rows, ks], rhs=qbuf[sl][rows, qs], start=True, stop=True))
                                S.ops("pe", fns, reads=["kbuf%d" % sl, "qbuf%d" % sl], writes=["bank%d" % (2 * (kc % 2)), "bank%d" % (2 * (kc % 2) + 1)])

                            def ex(kc):
                                S.op("act", lambda: A.activation(out=PT[kc % 2][:], in_=dbanks[kc % 2][:, :], func=AF.Exp, scale=scale),
                                     reads=["bank%d" % (2 * (kc % 2)), "bank%d" % (2 * (kc % 2) + 1)], writes=["PT%d" % (kc % 2)])

                            def pv(kc):
                                first, last = kc == 0, kc == NBLK - 1
                                fns = [(lambda m=m: PE.matmul(accs[m][0], lhsT=vbuf[sl][:, kc, :], rhs=PT[kc % 2][:, m * 512:(m + 1) * 512], start=first, stop=last)) for m in range(2)]
                                ok_ = [accs[0][1], accs[1][1]]
                                S.ops("pe", fns, reads=["PT%d" % (kc % 2), "vbuf%d" % sl], writes=ok_ if first else [], cwrites=ok_ if last else [])
                                if first:
                                    S.op("dve", lambda: V.tensor_copy(out=dacc[0][:], in_=PT[kc % 2][:]), reads=["PT%d" % (kc % 2)], writes=["dacc0"])
                                else:
                                    S.op("dve", lambda: V.tensor_tensor(out=dacc[0][:], in0=dacc[0][:], in1=PT[kc % 2][:], op=ALU.add), reads=["PT%d" % (kc % 2), "dacc0"], writes=["dacc0"])
                            qk(0)
                            for kc in range(NBLK):
                                ex(kc)
                                if kc + 1 < NBLK:
                                    qk(kc + 1)
                                pv(kc)
                            for m in range(2):
                                S.op("pe", lambda: PE.matmul(accs[2 + m][0], lhsT=ones_f[:], rhs=dacc[0][:, m * 512:(m + 1) * 512], start=True, stop=True), reads=["onesf", "dacc0"], writes=[accs[2 + m][1]])
                            S.op("dve", lambda: V.reciprocal(out=rden[0][:], in_=accs[2][0]), reads=[accs[2][1]], writes=["rden0"])
                            S.op("dve", lambda: V.reciprocal(out=rden[1][:], in_=accs[3][0]), reads=[accs[3][1]], writes=["rden1"])
                            S.op("dve", lambda: V.tensor_tensor(out=o1n[:], in0=accs[0][0], in1=rden[0][:], op=ALU.mult), reads=[accs[0][1], "rden0"], writes=["o1n"])
                            S.op("dve", lambda: V.tensor_tensor(out=o2n[:], in0=accs[1][0], in1=rden[1][:], op=ALU.mult), reads=[accs[1][1], "rden1"], writes=["o2n"])
                            S.op("dve", lambda: V.scalar_tensor_tensor(out=o1n[:], in0=o2n[:], scalar=neglam[:, 0:1], in1=o1n[:], op0=ALU.mult, op1=ALU.add),
                                 reads=["o1n", "o2n", "neglam"], writes=["o1n"])
                            S.op("pool", lambda: G.tensor_tensor(out=sq[:], in0=o1n[:], in1=o1n[:], op=ALU.mult), reads=["o1n"], writes=["osq"])
                            S.op("pe", lambda: PE.matmul(accs[2][0], lhsT=ones_b[:], rhs=sq[:], start=True, stop=True), reads=["osq", "onesb"], writes=[accs[2][1]])
                            rsqrt_into(rstd[:], "orstd", accs[2][0], accs[2][1], 1.0 / 128, 1e-5)
                            S.op("dve", lambda: V.scalar_tensor_tensor(out=osb[par][:], in0=o1n[:], scalar=subs[:, 0:1], in1=rstd[:], op0=ALU.mult, op1=ALU.mult),
                                 reads=["o1n", "subs", "orstd"], writes=["osb%d" % par])
                        S.dma("sp", "b_o%d" % par, lambda: nc.sync.dma_start(out=OT[h * 128:(h + 1) * 128, qs], in_=osb[par][:]), reads=["osb%d" % par], writes=["OT"])
                S.barrier()

        def ln_stats(z, zkey, st, mv, rs, nmr, tag):
            for i in range(2):
                S.op("dve", lambda: V.bn_stats(out=st[:, i, :], in_=z[:, i * 512:(i + 1) * 512]), reads=[zkey], writes=["st" + tag])
            S.op("dve", lambda: V.bn_aggr(out=mv[:], in_=st[:].rearrange("p a b -> p (a b)")), reads=["st" + tag], writes=["mv" + tag])
            S.op("act", lambda: A.activation(out=rs[:], in_=mv[:, 1:2], func=AF.Sqrt, bias=eps5[:, 0:1], scale=1.0), reads=["mv" + tag, "eps5"], writes=["rs" + tag])
            S.op("dve", lambda: V.reciprocal(out=rs[:], in_=rs[:]), reads=["rs" + tag], writes=["rs" + tag])
            S.op("dve", lambda: V.scalar_tensor_tensor(out=nmr[:], in0=mv[:, 0:1], scalar=-1.0, in1=rs[:], op0=ALU.mult, op1=ALU.mult), reads=["mv" + tag, "rs" + tag], writes=["nmr" + tag])

        def ln_apply(z, zkey, rs, nmr, tag):
            S.op("act", lambda: A.activation(out=z[:], in_=z[:], func=AF.Identity, bias=nmr[:, 0:1], scale=rs[:, 0:1]), reads=[zkey, "rs" + tag, "nmr" + tag], writes=[zkey])

        def phase_c(xsrc, l, w_o_ap):
            with ExitStack() as ph:
                g1p, k_g1 = bcast_load(ph, MODS[l:l + 1, 2 * D:3 * D], "g1p", plus_one=True)
                sc2p, k_sc2 = bcast_load(ph, MODS[l:l + 1, 4 * D:5 * D], "sc2p", plus_one=True)
                sh2p, k_sh2 = bcast_load(ph, MODS[l:l + 1, 3 * D:4 * D], "sh2p")
                lng, k_lng = bcast_load(ph, ln1_g[l:l + 1, :], "lng")
                lnb, k_lnb = bcast_load(ph, ln1_b[l:l + 1, :], "lnb")
                w_o = sb(ph, [128, 8, D], BF16, "w_o")
                rw = sb(ph, [128, 8, NE], F32, "rw")
                oT = sb(ph, [128, 8, S_LEN], BF16, "oTall")
                S.dma("pool", "c_wo", lambda: G.dma_start(out=w_o[:], in_=w_o_ap.rearrange("(k p) n -> p k n", p=128)), writes=["w_o"])
                S.dma("sp", "c_rw", lambda: nc.sync.dma_start(out=rw[:], in_=router_w[l].rearrange("(k p) n -> p k n", p=128)), writes=["rw"])
                for h in range(8):
                    S.dma("sp", "c_ot", lambda: nc.sync.dma_start(out=oT[:, h, :], in_=OT[h * 128:(h + 1) * 128, :]), writes=["oTall"])
                NB3 = 3
                xin = [sb(ph, [128, D], F32, "xin") for _ in range(NB3)]
                z = [sb(ph, [128, D], F32, "z") for _ in range(NB3)]
                h2 = [sb(ph, [128, D], F32, "h2") for _ in range(NB3)]
                h2b = [sb(ph, [128, D], BF16, "h2b") for _ in range(2)]
                h2T = [sb(ph, [128, 8, 128], F32, "h2T") for _ in range(2)]
                st = [sb(ph, [128, 2, 6], F32, "st") for _ in range(2)]
                mv = [sb(ph, [128, 2], F32, "mv") for _ in range(2)]
                rs = [sb(ph, [128, 1], F32, "rs") for _ in range(2)]
                nmr = [sb(ph, [128, 1], F32, "nmr") for _ in range(2)]
                mx = [sb(ph, [128, 1], F32, "mx") for _ in range(2)]
                ssum = [sb(ph, [128, 1], F32, "ssum") for _ in range(2)]
                ex = [sb(ph, [128, NE], F32, "ex") for _ in range(2)]

                def st0(b):
                    s3, s2 = b % NB3, b % 2
                    rows = slice(b * 128, (b + 1) * 128)
                    for hf in range(2):
                        pt, kk = banks[2 * s2 + hf], "bank%d" % (2 * s2 + hf)
                        S.ops("pe", [(lambda h=h: PE.matmul(pt[:, :], lhsT=oT[:, h, rows], rhs=w_o[:, h, hf * 512:(hf + 1) * 512], start=(h == 0), stop=(h == 7))) for h in range(8)],
                              reads=["oTall", "w_o"], writes=[kk])
                        S.op("dve", lambda: V.tensor_tensor(out=z[s3][:, hf * 512:(hf + 1) * 512], in0=pt[:, :], in1=g1p[:, hf * 512:(hf + 1) * 512], op=ALU.mult),
                             reads=[kk, k_g1], writes=["z%d" % s3])
                    S.op("dve", lambda: V.scalar_tensor_tensor(out=z[s3][:], in0=xin[s3][:], scalar=ALPHA, in1=z[s3][:], op0=ALU.mult, op1=ALU.add),
                         reads=["xin%d" % s3, "z%d" % s3], writes=["z%d" % s3])
                    ln_stats(z[s3], "z%d" % s3, st[s2], mv[s2], rs[s2], nmr[s2], "c%d" % s2)

                def st1(b):
                    s3, s2 = b % NB3, b % 2
                    rows = slice(b * 128, (b + 1) * 128)
                    ln_apply(z[s3], "z%d" % s3, rs[s2], nmr[s2], "c%d" % s2)
                    S.op("pool", lambda: G.tensor_tensor(out=z[s3][:], in0=z[s3][:], in1=lng[:], op=ALU.mult), reads=["z%d" % s3, k_lng], writes=["z%d" % s3])
                    S.op("dve", lambda: V.tensor_tensor(out=z[s3][:], in0=z[s3][:], in1=lnb[:], op=ALU.add), reads=["z%d" % s3, k_lnb], writes=["z%d" % s3])
                    S.dma("sp", "c_xm%d" % s3, lambda: nc.sync.dma_start(out=XMID[rows, :], in_=z[s3][:]), reads=["z%d" % s3], writes=["XMID"])
                    S.op("pool", lambda: G.tensor_tensor(out=h2[s3][:], in0=z[s3][:], in1=sc2p[:], op=ALU.mult), reads=["z%d" % s3, k_sc2], writes=["h2_%d" % s3])
                    S.op("dve", lambda: V.tensor_tensor(out=h2[s3][:], in0=h2[s3][:], in1=sh2p[:], op=ALU.add), reads=["h2_%d" % s3, k_sh2], writes=["h2_%d" % s3])
                    S.op("act", lambda: A.copy(out=h2b[s2][:], in_=h2[s3][:]), reads=["h2_%d" % s3], writes=["h2b%d" % s2])
                    S.dma("sp", "c_h2%d" % s2, lambda: nc.sync.dma_start(out=H2[rows, :], in_=h2b[s2][:]), reads=["h2b%d" % s2], writes=["H2"])

                def st2(b):
                    s3, s2 = b % NB3, b % 2
                    for hf in range(2):
                        bk, bkey = banks[4 + hf], "bank%d" % (4 + hf)
                        S.ops("pe", [(lambda j=j: PE.transpose(bk[:, j * 128:(j + 1) * 128], h2[s3][:, (hf * 4 + j) * 128:(hf * 4 + j + 1) * 128], ident_f[:])) for j in range(4)],
                              reads=["h2_%d" % s3, "identf"], writes=[bkey])
                        S.op("act", lambda: A.copy(out=h2T[s2][:, hf * 4:(hf + 1) * 4, :].rearrange("p a b -> p (a b)"), in_=bk[:, :]), reads=[bkey], writes=["h2T%d" % s2])
                    lg, lgk = banks[6 + s2], "bank%d" % (6 + s2)
                    S.ops("pe", [(lambda k=k: PE.matmul(lg[:, 0:NE], lhsT=h2T[s2][:, k, :], rhs=rw[:, k, :], start=(k == 0), stop=(k == 7))) for k in range(8)],
                          reads=["h2T%d" % s2, "rw"], writes=[lgk])
                    S.op("dve", lambda: V.reduce_max(out=mx[s2][:], in_=lg[:, 0:NE], axis=mybir.AxisListType.X), reads=[lgk], writes=["mx%d" % s2])
                    S.op("dve", lambda: V.tensor_scalar(out=mx[s2][:], in0=mx[s2][:], scalar1=-1.0, scalar2=None, op0=ALU.mult), reads=["mx%d" % s2], writes=["mx%d" % s2])
                    S.op("act", lambda: A.activation(out=ex[s2][:], in_=lg[:, 0:NE], func=AF.Exp, bias=mx[s2][:, 0:1], scale=1.0, accum_out=ssum[s2][:]), reads=[lgk, "mx%d" % s2], writes=["ex%d" % s2, "ssum%d" % s2])
                    S.op("dve", lambda: V.reciprocal(out=ssum[s2][:], in_=ssum[s2][:]), reads=["ssum%d" % s2], writes=["ssum%d" % s2])
                    S.op("dve", lambda: V.tensor_scalar(out=aff[:, b, :], in0=ex[s2][:], scalar1=ssum[s2][:, 0:1], scalar2=None, op0=ALU.mult), reads=["ex%d" % s2, "ssum%d" % s2], writes=["aff"])

                def ld(b):
                    s3 = b % NB3
                    S.dma("sp", "c_x%d" % s3, lambda: nc.sync.dma_start(out=xin[s3][:], in_=xsrc[b * 128:(b + 1) * 128, :]), writes=["xin%d" % s3])

                stages = [st0, st1, st2]
                ld(0)
                ld(1)
                for step in range(NBLK + len(stages) - 1):
                    for si in reversed(range(len(stages))):
                        b = step - si
                        if si == 0 and step + 2 < NBLK:
                            ld(step + 2)
                        if 0 <= b < NBLK:
                            stages[si](b)
                S.barrier()

        def phase_m(l):
            with ExitStack() as ph:
                ltri = sb(ph, [128, 128], F32, "ltri")
                ltri_b = sb(ph, [128, 128], BF16, "ltrib")
                iota = sb(ph, [128, CAP], F32, "iota")
                rcp = sb(ph, [128, NBLK, 2], F32, "rcp")
                S.dma("sp", "m_c0", lambda: nc.sync.dma_start(out=ltri[:], in_=c_ltri), writes=["ltri"])
                S.dma("sp", "m_c1", lambda: nc.sync.dma_start(out=iota[:], in_=c_iota), writes=["iota"])
                S.dma("sp", "m_c2", lambda: nc.sync.dma_start(out=rcp[:], in_=c_rcp), writes=["rcp"])
                S.op("dve", lambda: V.tensor_copy(out=ltri_b[:], in_=ltri[:]), reads=["ltri"], writes=["ltrib"])
                zt = sb(ph, [128, 4, D], F32, "zt")
                S.op("pool", lambda: G.memset(zt[:], 0.0), writes=["zt"])
                fv = FACC.rearrange("(t s p) d -> t p s d", s=4, p=128)
                for tb in range(8):
                    S.dma("sp", "m_z", lambda: nc.sync.dma_start(out=fv[tb], in_=zt[:]), reads=["zt"], writes=["FACC"])
                lo = sb(ph, [128, NE], F32, "lo")
                mid = sb(ph, [128, NE], F32, "mid")
                cmpt = sb(ph, [128, NBLK, NE], F32, "cmpt")
                cnt = sb(ph, [128, NE], F32, "cnt")
                ge = sb(ph, [128, NE], F32, "ge")
                ones_f = sb(ph, [128, 128], F32, "onesf")
                S.op("dve", lambda: V.memset(ones_f[:], 1.0), writes=["onesf"])
                S.op("dve", lambda: V.memset(lo[:], 0.0), writes=["lo"])
                for it in range(30):
                    w = 2.0 ** -(it + 1)
                    bk, bkk = banks[it % 2], "bank%d" % (it % 2)
                    S.op("dve", lambda: V.tensor_scalar(out=mid[:], in0=lo[:], scalar1=w, scalar2=None, op0=ALU.add), reads=["lo"], writes=["mid"])
                    S.op("dve", lambda: V.tensor_tensor(out=cmpt[:], in0=aff[:], in1=mid[:].unsqueeze(1).broadcast_to([128, NBLK, NE]), op=ALU.is_ge), reads=["aff", "mid"], writes=["cmpt"])
                    S.op("dve", lambda: V.reduce_sum(out=cnt[:], in_=cmpt[:].rearrange("p c e -> p e c"), axis=mybir.AxisListType.X), reads=["cmpt"], writes=["cnt"])
                    S.op("pe", lambda: PE.matmul(bk[:, 0:NE], lhsT=ones_f[:], rhs=cnt[:], start=True, stop=True), reads=["onesf", "cnt"], writes=[bkk])
                    S.op("dve", lambda: V.tensor_scalar(out=ge[:], in0=bk[:, 0:NE], scalar1=float(CAP) - 0.5, scalar2=None, op0=ALU.is_ge), reads=[bkk], writes=["ge"])
                    S.op("dve", lambda: V.scalar_tensor_tensor(out=lo[:], in0=ge[:], scalar=w, in1=lo[:], op0=ALU.mult, op1=ALU.add), reads=["ge", "lo"], writes=["lo"])
                maskb = sb(ph, [128, NBLK, NE], BF16, "maskb")
                pre = sb(ph, [128, NBLK, NE], BF16, "pre")
                pref = sb(ph, [128, NBLK, NE], F32, "pref")
                posp = sb(ph, [128, NBLK, NE], F32, "posp")
                S.op("dve", lambda: V.tensor_tensor(out=cmpt[:], in0=aff[:], in1=lo[:].unsqueeze(1).broadcast_to([128, NBLK, NE]), op=ALU.is_ge), reads=["aff", "lo"], writes=["cmpt"])
                S.op("dve", lambda: V.tensor_copy(out=maskb[:], in_=cmpt[:]), reads=["cmpt"], writes=["maskb"])
                S.op("dve", lambda: V.memset(pref[:, 0, :], 0.0), writes=["pref"])
                for c in range(1, NBLK):
                    S.op("dve", lambda: V.tensor_tensor(out=pref[:, c, :], in0=pref[:, c - 1, :], in1=cmpt[:, c - 1, :], op=ALU.add), reads=["pref", "cmpt"], writes=["pref"])
                S.op("dve", lambda: V.tensor_copy(out=pre[:], in_=pref[:]), reads=["pref"], writes=["pre"])
                pb = banks[1]
                S.ops("pe", [lambda: PE.matmul(pb[:, :], lhsT=ltri_b[:], rhs=maskb[:].rearrange("p c e -> p (c e)"), start=True, stop=False),
                             lambda: PE.matmul(pb[:, :], lhsT=ones_b[:], rhs=pre[:].rearrange("p c e -> p (c e)"), start=False, stop=True)],
                      reads=["ltrib", "onesb", "maskb", "pre"], writes=["bank1"])
                S.op("dve", lambda: V.tensor_tensor(out=posp[:].rearrange("p c e -> p (c e)"), in0=pb[:, :], in1=cmpt[:].rearrange("p c e -> p (c e)"), op=ALU.mult), reads=["bank1", "cmpt"], writes=["posp"])
                R = sb(ph, [128, NBLK, NE, 5], BF16, "R")
                r1 = sb(ph, [128, NBLK, NE], F32, "r1")
                r2 = sb(ph, [128, NBLK, NE], F32, "r2")
                S.op("dve", lambda: V.tensor_copy(out=R[:, :, :, 0], in_=rcp[:, :, 0:1].broadcast_to([128, NBLK, NE])), reads=["rcp"], writes=["R"])
                S.op("dve", lambda: V.tensor_copy(out=R[:, :, :, 1], in_=rcp[:, :, 1:2].broadcast_to([128, NBLK, NE])), reads=["rcp", "R"], writes=["R"])
                S.op("dve", lambda: V.tensor_copy(out=R[:, :, :, 2], in_=aff[:]), reads=["aff", "R"], writes=["R"])
                S.op("dve", lambda: V.tensor_tensor(out=r1[:], in0=aff[:], in1=R[:, :, :, 2], op=ALU.subtract), reads=["aff", "R"], writes=["r1"])
                S.op("dve", lambda: V.tensor_copy(out=R[:, :, :, 3], in_=r1[:]), reads=["r1", "R"], writes=["R"])
                S.op("dve", lambda: V.tensor_tensor(out=r2[:], in0=r1[:], in1=R[:, :, :, 3], op=ALU.subtract), reads=["r1", "R"], writes=["r2"])
                S.op("dve", lambda: V.tensor_copy(out=R[:, :, :, 4], in_=r2[:]), reads=["r2", "R"], writes=["R"])
                idx = sb(ph, [128, NE, 4], I32, "idx")
                idxf = sb(ph, [128, NE, 4], F32, "idxf")
                gate = sb(ph, [128, NE, 4], F32, "gate")
                Pm = [sb(ph, [128, NBLK, 128], BF16, "Pm") for _ in range(2)]
                ig = sb(ph, [128, 8], F32, "ig")
                pcount = [0]

                def compute_idx(e, g):
                    psl = pcount[0] % 2
                    pcount[0] += 1
                    S.op("dve", lambda: V.tensor_tensor(out=Pm[psl][:], in0=posp[:, :, e:e + 1].broadcast_to([128, NBLK, 128]),
                                                        in1=iota[:, g * 128:(g + 1) * 128].unsqueeze(1).broadcast_to([128, NBLK, 128]), op=ALU.is_equal),
                         reads=["posp", "iota"], writes=["Pm%d" % psl])
                    ob, obk = banks[7], "bank7"
                    S.ops("pe", [(lambda c=c: PE.matmul(ob[:, 0:5], lhsT=Pm[psl][:, c, :], rhs=R[:, c, e, :], start=(c == 0), stop=(c == NBLK - 1))) for c in range(NBLK)],
                          reads=["Pm%d" % psl, "R"], writes=[obk])
                    S.op("act", lambda: A.copy(out=ig[:, psl * 4:psl * 4 + 4], in_=ob[:, 1:5]), reads=[obk], writes=["ig%d" % psl])
                    S.op("dve", lambda: V.scalar_tensor_tensor(out=idxf[:, e, g:g + 1], in0=ob[:, 0:1], scalar=128.0, in1=ig[:, psl * 4:psl * 4 + 1], op0=ALU.mult, op1=ALU.add),
                         reads=[obk, "ig%d" % psl], writes=["idxf%d" % e])
                    S.op("dve", lambda: V.tensor_tensor(out=gate[:, e, g:g + 1], in0=ig[:, psl * 4 + 1:psl * 4 + 2], in1=ig[:, psl * 4 + 2:psl * 4 + 3], op=ALU.add), reads=["ig%d" % psl], writes=["gate%d" % e])
                    S.op("dve", lambda: V.tensor_tensor(out=gate[:, e, g:g + 1], in0=gate[:, e, g:g + 1], in1=ig[:, psl * 4 + 3:psl * 4 + 4], op=ALU.add), reads=["ig%d" % psl, "gate%d" % e], writes=["gate%d" % e])
                    if g == 3:
                        S.op("dve", lambda: V.tensor_copy(out=idx[:, e, :], in_=idxf[:, e, :]), reads=["idxf%d" % e], writes=["idx%d" % e])

                for e0 in range(2):
                    for g in range(4):
                        compute_idx(e0, g)
                wi = [sb(ph, [128, 8, 1024], BF16, "wi") for _ in range(3)]
                wo = [sb(ph, [128, 4, D], BF16, "wo") for _ in range(3)]
                xg = [sb(ph, [128, 4, D], BF16, "xg") for _ in range(2)]
                xeT = sb(ph, [128, 8, CAP], BF16, "xeT")
                sg = [sb(ph, [128, CAP], F32, "sg") for _ in range(2)]
                actT = [sb(ph, [128, 4, CAP], BF16, "actT") for _ in range(2)]
                ysb = [sb(ph, [128, 4, D], F32, "ysb") for _ in range(2)]
                wiv = moe_w_in[l].rearrange("e (k p) n -> e p k n", p=128)
                wov = moe_w_out[l].rearrange("e (k p) n -> e p k n", p=128)
                nblk = [0]

                def load_w(e, i):
                    sl = nblk[0] % 3
                    nblk[0] += 1
                    S.dma("pool", "m_wg%d" % sl, lambda: G.dma_start(out=wi[sl][:, :, 0:512], in_=wiv[e][:, :, i * 512:(i + 1) * 512]), writes=["wi%d" % sl])
                    S.dma("pool", "m_wu%d" % sl, lambda: G.dma_start(out=wi[sl][:, :, 512:1024], in_=wiv[e][:, :, FF + i * 512:FF + (i + 1) * 512]), writes=["wi%d" % sl])
                    S.dma("pool", "m_wo%d" % sl, lambda: G.dma_start(out=wo[sl][:], in_=wov[e][:, i * 4:(i + 1) * 4, :]), writes=["wo%d" % sl])
                    return sl

                def gather(e):
                    gs = e % 2
                    for g in range(4):
                        S.dma("pool", "m_g%d" % gs, lambda: G.indirect_dma_start(out=xg[gs][:, g, :], out_offset=None, in_=H2,
                                                                               in_offset=bass.IndirectOffsetOnAxis(ap=idx[:, e, g:g + 1], axis=0)),
                              reads=["idx%d" % e, "H2"], writes=["xg%d" % gs])

                sched_w = [(e, i) for e in range(NE) for i in range(4)]
                slots = {}
                slots[sched_w[0]] = load_w(*sched_w[0])
                slots[sched_w[1]] = load_w(*sched_w[1])
                gather(0)
                wn = 2
                par2 = 0
                for e in range(NE):
                    gs = e % 2
                    ys = e % 2
                    if e + 1 < NE:
                        gather(e + 1)
                    for k in range(8):
                        bk, bkey = banks[6], "bank6"
                        tv = bk[:].bitcast(BF16)
                        S.ops("pe", [(lambda g=g: PE.transpose(tv[:, g * 128:(g + 1) * 128], xg[gs][:, g, k * 128:(k + 1) * 128], ident_b[:])) for g in range(4)],
                              reads=["xg%d" % gs, "identb"], writes=[bkey])
                        S.op("act" if k % 2 else "dve", (lambda k=k, tv=tv: (A.copy if k % 2 else V.tensor_copy)(out=xeT[:, k, :], in_=tv[:, 0:CAP])), reads=[bkey], writes=["xeT"])
                    for i in range(4):
                        if wn < len(sched_w):
                            slots[sched_w[wn]] = load_w(*sched_w[wn])
                            wn += 1
                        ws = slots[(e, i)]
                        asl = (e * 4 + i) % 2
                        for fc in range(4):
                            pg, pgk = banks[0 + 2 * (fc % 2)], "bank%d" % (0 + 2 * (fc % 2))
                            pu, puk = banks[1 + 2 * (fc % 2)], "bank%d" % (1 + 2 * (fc % 2))
                            S.ops("pe", [(lambda k=k: PE.matmul(pg[:, :], lhsT=wi[ws][:, k, fc * 128:(fc + 1) * 128], rhs=xeT[:, k, :], start=(k == 0), stop=(k == 7))) for k in range(8)],
                                  reads=["wi%d" % ws, "xeT"], writes=[pgk])
                            S.ops("pe", [(lambda k=k: PE.matmul(pu[:, :], lhsT=wi[ws][:, k, 512 + fc * 128:512 + (fc + 1) * 128], rhs=xeT[:, k, :], start=(k == 0), stop=(k == 7))) for k in range(8)],
                                  reads=["wi%d" % ws, "xeT"], writes=[puk])
                            S.op("act", lambda: A.activation(out=sg[fc % 2][:], in_=pg[:, :], func=AF.Silu), reads=[pgk], writes=["sg%d" % (fc % 2)])
                            S.op("dve", lambda: V.tensor_tensor(out=actT[asl][:, fc, :], in0=sg[fc % 2][:], in1=pu[:, :], op=ALU.mult), reads=["sg%d" % (fc % 2), puk], writes=["actT%d" % asl])
                        if e + 2 < NE:
                            compute_idx(e + 2, i)
                        for g in range(4):
                            for hf in range(2):
                                py, pyk = banks[4 + par2 % 2], "bank%d" % (4 + par2 % 2)
                                par2 += 1
                                S.ops("pe", [(lambda fc=fc: PE.matmul(py[:, :], lhsT=actT[asl][:, fc, g * 128:(g + 1) * 128], rhs=wo[ws][:, fc, hf * 512:(hf + 1) * 512], start=(fc == 0), stop=(fc == 3))) for fc in range(4)],
                                      reads=["actT%d" % asl, "wo%d" % ws], writes=[pyk])
                                dst = ysb[ys][:, g, hf * 512:(hf + 1) * 512]
                                if i == 0:
                                    S.op("act", lambda: A.activation(out=dst, in_=py[:, :], func=AF.Copy, scale=gate[:, e, g:g + 1]), reads=[pyk, "gate%d" % e], writes=["ysb%d" % ys])
                                else:
                                    S.op("dve", lambda: V.scalar_tensor_tensor(out=dst, in0=py[:, :], scalar=gate[:, e, g:g + 1], in1=dst, op0=ALU.mult, op1=ALU.add),
                                         reads=[pyk, "gate%d" % e, "ysb%d" % ys], writes=["ysb%d" % ys])
                    for g in range(4):
                        S.dma("pool", "m_sc", lambda: G.indirect_dma_start(out=FACC, out_offset=bass.IndirectOffsetOnAxis(ap=idx[:, e, g:g + 1], axis=0),
                                                                          in_=ysb[ys][:, g, :], in_offset=None, compute_op=ALU.add),
                              reads=["ysb%d" % ys, "idx%d" % e], writes=["FACC"] if g == 0 else [])
                    S.lastw["FACC"] = (S.dsem["m_sc"][0], S.dsem["m_sc"][1])
                    S.readers["FACC"] = {}
                S.barrier()

        def phase_d(l, dst):
            with ExitStack() as ph:
                g2p, k_g2 = bcast_load(ph, MODS[l:l + 1, 5 * D:6 * D], "g2p", plus_one=True)
                lng, k_lng = bcast_load(ph, ln2_g[l:l + 1, :], "lng2")
                lnb, k_lnb = bcast_load(ph, ln2_b[l:l + 1, :], "lnb2")
                NB3 = 3
                xin = [sb(ph, [128, D], F32, "xin") for _ in range(NB3)]
                fin = [sb(ph, [128, D], F32, "fin") for _ in range(NB3)]
                st = [sb(ph, [128, 2, 6], F32, "st") for _ in range(2)]
                mv = [sb(ph, [128, 2], F32, "mv") for _ in range(2)]
                rs = [sb(ph, [128, 1], F32, "rs") for _ in range(2)]
                nmr = [sb(ph, [128, 1], F32, "nmr") for _ in range(2)]

                def st0(b):
                    s3, s2 = b % NB3, b % 2
                    rows = slice(b * 128, (b + 1) * 128)
                    S.op("pool", lambda: G.tensor_tensor(out=fin[s3][:], in0=fin[s3][:], in1=g2p[:], op=ALU.mult), reads=["fin%d" % s3, k_g2], writes=["fin%d" % s3])
                    S.op("dve", lambda: V.scalar_tensor_tensor(out=fin[s3][:], in0=xin[s3][:], scalar=ALPHA, in1=fin[s3][:], op0=ALU.mult, op1=ALU.add),
                         reads=["xin%d" % s3, "fin%d" % s3], writes=["fin%d" % s3])
                    ln_stats(fin[s3], "fin%d" % s3, st[s2], mv[s2], rs[s2], nmr[s2], "d%d" % s2)

                def st1(b):
                    s3, s2 = b % NB3, b % 2
                    rows = slice(b * 128, (b + 1) * 128)
                    ln_apply(fin[s3], "fin%d" % s3, rs[s2], nmr[s2], "d%d" % s2)
                    S.op("pool", lambda: G.tensor_tensor(out=fin[s3][:], in0=fin[s3][:], in1=lng[:], op=ALU.mult), reads=["fin%d" % s3, k_lng], writes=["fin%d" % s3])
                    S.op("dve", lambda: V.tensor_tensor(out=fin[s3][:], in0=fin[s3][:], in1=lnb[:], op=ALU.add), reads=["fin%d" % s3, k_lnb], writes=["fin%d" % s3])
                    S.dma("sp", "d_o%d" % s3, lambda: nc.sync.dma_start(out=dst[rows, :], in_=fin[s3][:]), reads=["fin%d" % s3], writes=["dst"])

                def ld(b):
                    s3 = b % NB3
                    rows = slice(b * 128, (b + 1) * 128)
                    S.dma("sp", "d_x%d" % s3, lambda: nc.sync.dma_start(out=xin[s3][:], in_=XMID[rows, :]), writes=["xin%d" % s3])
                    S.dma("sp", "d_f%d" % s3, lambda: nc.sync.dma_start(out=fin[s3][:], in_=FACC[rows, :]), writes=["fin%d" % s3])

                ld(0)
                ld(1)
                for step in range(NBLK + 1):
                    if 0 <= step - 1 < NBLK:
                        st1(step - 1)
                    if step + 2 < NBLK:
                        ld(step + 2)
                    if step < NBLK:
                        st0(step)
                S.barrier()

        phases = [
            lambda: phase_mods(),
            lambda: phase_a_mla(x_in, 0),
            lambda: phase_b("mla"),
            lambda: phase_c(x_in, 0, mla_w_o),
            lambda: phase_m(0),
            lambda: phase_d(0, X1),
            lambda: phase_a_diff(X1, 1),
            lambda: phase_b("diff"),
            lambda: phase_c(X1, 1, diff_w_o),
            lambda: phase_m(1),
            lambda: phase_d(1, out),
        ]
        for i, p in enumerate(phases):
            if i <= upto:
                p()
        if dbg is not None:
            src = {"XMID": XMID, "X1": X1, "FACC": FACC}[dbg]
            with ExitStack() as ph:
                t = sb(ph, [128, 4, D], F32, "dbg")
                for tb in range(8):
                    S.dma("sp", "dbg_i", lambda: nc.sync.dma_start(out=t[:], in_=src.rearrange("(t s p) d -> t p s d", s=4, p=128)[tb]), writes=["dbg"])
                    S.dma("sp", "dbg_o", lambda: nc.sync.dma_start(out=out.rearrange("(t s p) d -> t p s d", s=4, p=128)[tb], in_=t[:]), reads=["dbg"], writes=["outd"])
        S.finish("sp")
    return nc


def _consts():
    inv32 = (10000.0 ** (-np.arange(0, 64, 2, dtype=np.float32) / 64)).astype(np.float32)
    inv_m = np.concatenate([inv32, inv32]).reshape(64, 1).astype(np.float32)
    inv8 = (500000.0 ** (-np.arange(0, 16, 2, dtype=np.float32) / 16)).astype(np.float32)
    blk = np.zeros(64, np.float32)
    blk[0:8] = inv8
    blk[8:16] = inv8
    inv_d = np.concatenate([blk, blk]).reshape(128, 1).astype(np.float32)
    ident = np.eye(128, dtype=np.float32)
    ltri = np.triu(np.ones((128, 128), np.float32))
    iota = np.tile(np.arange(1, CAP + 1, dtype=np.float32)[None, :], (128, 1))
    rcp = np.zeros((128, NBLK, 2), np.float32)
    rcp[:, :, 0] = np.arange(NBLK, dtype=np.float32)[None, :]
    rcp[:, :, 1] = np.arange(128, dtype=np.float32)[:, None]
    return dict(c_inv_m=inv_m, c_inv_d=inv_d, c_ident=ident, c_ltri=ltri, c_iota=iota, c_rcp=rcp)


def make_in_maps(inputs, ncores=8):
    f = lambda a: np.ascontiguousarray(np.asarray(a))
    shared = dict(
        ada_w=f(inputs["ada_w"]), ada_b=f(inputs["ada_b"]),
        ln1_g=f(inputs["ln1_g"]), ln1_b=f(inputs["ln1_b"]), ln2_g=f(inputs["ln2_g"]), ln2_b=f(inputs["ln2_b"]),
        mla_w_in=f(inputs["mla_w_in"][0]), mla_q_norm=f(inputs["mla_q_norm"][0]), mla_kv_norm=f(inputs["mla_kv_norm"][0]),
        mla_w_uq=f(inputs["mla_w_uq"][0]), mla_w_ukv=f(inputs["mla_w_ukv"][0]), mla_w_o=f(inputs["mla_w_o"][0]),
        diff_w_in=f(inputs["diff_w_in"][0]), diff_lambda=f(inputs["diff_lambda"][0]).reshape(1, 256),
        diff_subln=f(inputs["diff_subln"][0]).reshape(128, 1), diff_w_o=f(inputs["diff_w_o"][0]),
        router_w=f(inputs["router_w"]), moe_w_in=f(inputs["moe_w_in"]), moe_w_out=f(inputs["moe_w_out"]),
    )
    shared.update(_consts())
    maps = []
    for c in range(ncores):
        b = c % 4
        m = dict(shared)
        m["x"] = f(inputs["x"][b])
        m["cT"] = f(np.asarray(inputs["c"][b]).reshape(8, 128).T)
        m["pos"] = f(np.asarray(inputs["positions"][b]).reshape(1, S_LEN).astype(np.int32))
        maps.append(m)
    return maps


def kernel(**inputs):
    nc = build()
    maps = make_in_maps(inputs, 8)
    res = run_bass_kernel_spmd(nc, maps, core_ids=list(range(8)))
    return np.stack([np.asarray(res.results[b]["out"], dtype=np.float32) for b in range(4)], axis=0)
```

```python
import math
from contextlib import ExitStack

import numpy as np
import ml_dtypes
import concourse.bass as bass
import concourse.mybir as mybir
from concourse.bass_utils import run_bass_kernel_spmd

F32 = mybir.dt.float32
BF16 = mybir.dt.bfloat16
I32 = mybir.dt.int32
ALU = mybir.AluOpType
AF = mybir.ActivationFunctionType

S_LEN = 4096
D = 1024
NBLK = S_LEN // 128
NE = 16
CAP = 512
FF = 2048
ALPHA = 4 ** 0.25
LAMBDA_INIT1 = 0.8 - 0.6 * math.exp(-0.3)
PI = math.pi
DSPLIT = 768


class Sched:
    def __init__(self, nc, es):
        self.nc = nc
        self.es = es
        self.engs = {"pe": nc.tensor, "act": nc.scalar, "dve": nc.vector, "pool": nc.gpsimd, "sp": nc.sync}
        self.prog = {}
        for e in self.engs:
            self.prog[e] = [es.enter_context(nc.semaphore("prog_" + e)), 0]
        self.known = {e: {} for e in self.engs}
        self.lastw = {}
        self.readers = {}
        self.dsem = {}
        self.nsem = 0

    def _wait(self, e, tok):
        if tok is None:
            return
        sem, val = tok
        k = self.known[e]
        if k.get(sem.num, 0) >= val:
            return
        self.engs[e].wait_ge(sem, val)
        k[sem.num] = val

    def deps(self, e, reads, writes):
        for r in reads:
            self._wait(e, self.lastw.get(r))
        for w in writes:
            self._wait(e, self.lastw.get(w))
            for tok in list(self.readers.get(w, {}).values()):
                self._wait(e, tok)

    def commit(self, tok, reads, writes):
        for r in reads:
            self.readers.setdefault(r, {})[tok[0].num] = tok
        for w in writes:
            self.lastw[w] = tok
            self.readers[w] = {}

    def op(self, e, fn, reads=(), writes=(), cwrites=None):
        return self.ops(e, [fn], reads, writes, cwrites)

    def ops(self, e, fns, reads=(), writes=(), cwrites=None):
        self.deps(e, reads, writes)
        ins = None
        for fn in fns:
            ins = fn()
        p = self.prog[e]
        p[1] += 1
        ins.then_inc(p[0], 1)
        tok = (p[0], p[1])
        self.commit(tok, reads, writes if cwrites is None else cwrites)
        return tok

    def dma(self, q, key, fn, reads=(), writes=()):
        self.deps(q, reads, writes)
        ins = fn()
        if key not in self.dsem:
            self.dsem[key] = [self.es.enter_context(self.nc.semaphore("d%d" % self.nsem)), 0]
            self.nsem += 1
        s = self.dsem[key]
        s[1] += 16
        ins.then_inc(s[0], 16)
        tok = (s[0], s[1])
        self.commit(tok, reads, writes)
        return tok

    def _all(self):
        toks = [(p[0], p[1]) for p in self.prog.values() if p[1] > 0]
        toks += [(s[0], s[1]) for s in self.dsem.values() if s[1] > 0]
        return toks

    def barrier(self):
        toks = self._all()
        for e in self.engs:
            for t in toks:
                self._wait(e, t)
        self.lastw = {}
        self.readers = {}

    def finish(self, e="sp"):
        for t in self._all():
            self._wait(e, t)


def build(upto=99, dbg=None):
    nc = bass.Bass("TRN2", target_bir_lowering=False)

    def din(name, shape, dt=F32):
        return nc.dram_tensor(name, list(shape), dt, kind="ExternalInput").ap()

    def dscr(name, shape, dt):
        return nc.dram_tensor(name, list(shape), dt, kind="Internal").ap()

    x_in = din("x", [S_LEN, D])
    cT = din("cT", [128, 8])
    pos = din("pos", [1, S_LEN], I32)
    ada_w = din("ada_w", [2, D, 6 * D])
    ada_b = din("ada_b", [2, 6 * D])
    ln1_g = din("ln1_g", [2, D]); ln1_b = din("ln1_b", [2, D])
    ln2_g = din("ln2_g", [2, D]); ln2_b = din("ln2_b", [2, D])
    mla_w_in = din("mla_w_in", [D, 704])
    mla_q_norm = din("mla_q_norm", [384]); mla_kv_norm = din("mla_kv_norm", [256])
    mla_w_uq = din("mla_w_uq", [384, 1536])
    mla_w_ukv = din("mla_w_ukv", [256, 2048])
    mla_w_o = din("mla_w_o", [D, D])
    diff_w_in = din("diff_w_in", [D, 3072])
    diff_lambda = din("diff_lambda", [1, 256])
    diff_subln = din("diff_subln", [128, 1])
    diff_w_o = din("diff_w_o", [D, D])
    router_w = din("router_w", [2, D, NE])
    moe_w_in = din("moe_w_in", [2, NE, D, 2 * FF])
    moe_w_out = din("moe_w_out", [2, NE, FF, D])
    c_inv_m = din("c_inv_m", [64, 1])
    c_inv_d = din("c_inv_d", [128, 1])
    c_ident = din("c_ident", [128, 128])
    c_ltri = din("c_ltri", [128, 128])
    c_iota = din("c_iota", [128, CAP])
    c_rcp = din("c_rcp", [128, NBLK, 2])

    out = nc.dram_tensor("out", [S_LEN, D], F32, kind="ExternalOutput").ap()

    MODS = dscr("MODS", [2, 6 * D], F32)
    QT = dscr("QT", [8, 192, S_LEN], BF16)
    KT = dscr("KT", [8, 128, S_LEN], BF16)
    KPE = dscr("KPE", [64, S_LEN], BF16)
    VV = dscr("VV", [S_LEN, D], BF16)
    OT = dscr("OT", [D, S_LEN], BF16)
    XMID = dscr("XMID", [S_LEN, D], F32)
    H2 = dscr("H2", [S_LEN, D], BF16)
    FACC = dscr("FACC", [S_LEN, D], F32)
    X1 = dscr("X1", [S_LEN, D], F32)

    es = ExitStack()
    with es:
        S = Sched(nc, es)
        uid = [0]

        def sb(stack, shape, dt, name=None):
            uid[0] += 1
            return stack.enter_context(nc.sbuf_tensor("%s_%d" % (name or "t", uid[0]), list(shape), dt))

        dbanks = [es.enter_context(nc.psum_tensor("dbank%d" % i, [128, 1024], F32)) for i in range(4)]
        banks = [dbanks[i // 2][:, (i % 2) * 512:(i % 2 + 1) * 512] for i in range(8)]
        ident_f = sb(es, [128, 128], F32, "identf")
        ident_b = sb(es, [128, 128], BF16, "identb")
        ones_b = sb(es, [128, 128], BF16, "onesb")
        aff = sb(es, [128, NBLK, NE], F32, "aff")
        eps5 = sb(es, [128, 1], F32, "eps5")
        S.dma("sp", "c0", lambda: nc.sync.dma_start(out=ident_f[:], in_=c_ident), writes=["identf"])
        S.op("dve", lambda: nc.vector.tensor_copy(out=ident_b[:], in_=ident_f[:]), reads=["identf"], writes=["identb"])
        S.op("dve", lambda: nc.vector.memset(ones_b[:], 1.0), writes=["onesb"])
        S.op("dve", lambda: nc.vector.memset(eps5[:], 1e-5), writes=["eps5"])

        V = nc.vector
        A = nc.scalar
        PE = nc.tensor
        G = nc.gpsimd

        def bcast_load(stack, src_row_ap, name, plus_one=False, q="sp"):
            n = src_row_ap.shape[-1]
            t = sb(stack, [128, n], F32, name)
            key = "%s_%d" % (name, uid[0])
            S.dma(q, "bc_" + key, lambda: S.engs[q].dma_start(out=t[:], in_=src_row_ap.partition_broadcast(128)), writes=[key])
            if plus_one:
                S.op("pool", lambda: G.tensor_scalar(out=t[:], in0=t[:], scalar1=1.0, scalar2=None, op0=ALU.add), reads=[key], writes=[key])
            return t, key

        def phase_mods():
            with ExitStack() as ph:
                c_sb = sb(ph, [128, 8], F32, "c")
                c16 = sb(ph, [128, 8], BF16, "c16")
                adab = sb(ph, [1, 6 * D], F32, "adab")
                modrow = sb(ph, [1, 6 * D], F32, "modrow")
                wblk = [sb(ph, [128, 8, 512], BF16, "adaw") for _ in range(3)]
                S.dma("sp", "m_c", lambda: nc.sync.dma_start(out=c_sb[:], in_=cT), writes=["c"])
                S.op("act", lambda: A.activation(out=c16[:], in_=c_sb[:], func=AF.Silu), reads=["c"], writes=["c16"])
                it = 0
                for l in range(2):
                    S.dma("sp", "m_b", lambda: nc.sync.dma_start(out=adab[:], in_=ada_b[l:l + 1, :]), writes=["adab"])
                    wv = ada_w[l].rearrange("(k p) n -> p k n", p=128)
                    for j in range(12):
                        sl = it % 3
                        S.dma("pool", "m_w%d" % sl, lambda: G.dma_start(out=wblk[sl][:], in_=wv[:, :, j * 512:(j + 1) * 512]), writes=["adaw%d" % sl])
                        pb = banks[it % 2]
                        S.ops("pe", [(lambda k=k: PE.matmul(pb[0:1, :], lhsT=c16[:, k:k + 1], rhs=wblk[sl][:, k, :], start=(k == 0), stop=(k == 7))) for k in range(8)],
                              reads=["c16", "adaw%d" % sl], writes=["bank%d" % (it % 2)])
                        S.op("dve", lambda: V.tensor_tensor(out=modrow[0:1, j * 512:(j + 1) * 512], in0=pb[0:1, :], in1=adab[0:1, j * 512:(j + 1) * 512], op=ALU.add),
                             reads=["bank%d" % (it % 2), "adab"], writes=["modrow"])
                        it += 1
                    S.dma("sp", "m_o", lambda: nc.sync.dma_start(out=MODS[l:l + 1, :], in_=modrow[:]), reads=["modrow"], writes=["MODS"])
                S.barrier()

        def rope_tables(ph, R, inv_ap):
            Ct = sb(ph, [R, S_LEN], F32, "ropeC")
            St = sb(ph, [R, S_LEN], F32, "ropeS")
            inv = sb(ph, [R, 1], F32, "inv")
            negpi = sb(ph, [R, 1], F32, "negpi")
            S.dma("sp", "r_inv", lambda: nc.sync.dma_start(out=inv[:], in_=inv_ap), writes=["inv"])
            with ExitStack() as tmp:
                pi_t = sb(tmp, [R, 1024], I32, "posi")
                ang = sb(tmp, [R, 1024], F32, "ang")
                nf = sb(tmp, [R, 1024], F32, "nf")
                ni = sb(tmp, [R, 1024], I32, "ni")
                msk = sb(tmp, [R, 1024], F32, "msk")
                for cch in range(4):
                    cs = slice(cch * 1024, (cch + 1) * 1024)
                    S.dma("sp", "r_pos", lambda: nc.sync.dma_start(out=pi_t[:], in_=pos[:, cs].partition_broadcast(R)), writes=["posi"])
                    S.op("dve", lambda: V.tensor_copy(out=ang[:], in_=pi_t[:]), reads=["posi"], writes=["ang"])
                    S.op("dve", lambda: V.tensor_scalar(out=ang[:], in0=ang[:], scalar1=inv[:, 0:1], scalar2=None, op0=ALU.mult), reads=["ang", "inv"], writes=["ang"])
                    S.op("dve", lambda: V.tensor_scalar(out=nf[:], in0=ang[:], scalar1=1.0 / (2 * PI), scalar2=None, op0=ALU.mult), reads=["ang"], writes=["nf"])
                    S.op("dve", lambda: V.tensor_copy(out=ni[:], in_=nf[:]), reads=["nf"], writes=["ni"])
                    S.op("dve", lambda: V.tensor_copy(out=nf[:], in_=ni[:]), reads=["ni"], writes=["nf"])
                    S.op("dve", lambda: V.scalar_tensor_tensor(out=ang[:], in0=nf[:], scalar=-6.28125, in1=ang[:], op0=ALU.mult, op1=ALU.add), reads=["nf", "ang"], writes=["ang"])
                    S.op("dve", lambda: V.scalar_tensor_tensor(out=ang[:], in0=nf[:], scalar=-0.0019353071795864769, in1=ang[:], op0=ALU.mult, op1=ALU.add), reads=["nf", "ang"], writes=["ang"])

                    def wrap():
                        S.op("dve", lambda: V.tensor_scalar(out=msk[:], in0=ang[:], scalar1=PI, scalar2=-2 * PI, op0=ALU.is_gt, op1=ALU.mult), reads=["ang"], writes=["msk"])
                        S.op("dve", lambda: V.tensor_tensor(out=ang[:], in0=ang[:], in1=msk[:], op=ALU.add), reads=["ang", "msk"], writes=["ang"])
                        S.op("dve", lambda: V.tensor_scalar(out=msk[:], in0=ang[:], scalar1=-PI, scalar2=2 * PI, op0=ALU.is_lt, op1=ALU.mult), reads=["ang"], writes=["msk"])
                        S.op("dve", lambda: V.tensor_tensor(out=ang[:], in0=ang[:], in1=msk[:], op=ALU.add), reads=["ang", "msk"], writes=["ang"])
                        S.op("dve", lambda: V.tensor_scalar(out=ang[:], in0=ang[:], scalar1=PI, scalar2=-PI, op0=ALU.min, op1=ALU.max), reads=["ang"], writes=["ang"])
                    wrap()
                    S.op("act", lambda: A.activation(out=St[:, cs], in_=ang[:], func=AF.Sin), reads=["ang"], writes=["ropeS"])
                    S.op("dve", lambda: V.tensor_scalar(out=ang[:], in0=ang[:], scalar1=PI / 2, scalar2=None, op0=ALU.add), reads=["ang"], writes=["ang"])
                    wrap()
                    S.op("act", lambda: A.activation(out=Ct[:, cs], in_=ang[:], func=AF.Sin), reads=["ang"], writes=["ropeC"])
                S.barrier()
            return Ct, St

        def front_block(tb, xsrc, xin, h16, hT, sc1p, sh1p, kx):
            sl = tb % 2
            xv = xsrc.rearrange("(t s p) d -> t p s d", s=4, p=128)
            S.dma("sp", "xin%d" % sl, lambda: nc.sync.dma_start(out=xin[sl][:], in_=xv[tb]), writes=["xin%d" % sl])
            S.op("dve", lambda: V.tensor_tensor(out=xin[sl][:], in0=xin[sl][:], in1=sc1p[:].unsqueeze(1).broadcast_to([128, 4, D]), op=ALU.mult),
                 reads=["xin%d" % sl, kx[0]], writes=["xin%d" % sl])
            S.op("pool", lambda: G.tensor_tensor(out=h16[:], in0=xin[sl][:], in1=sh1p[:].unsqueeze(1).broadcast_to([128, 4, D]), op=ALU.add),
                 reads=["xin%d" % sl, kx[1]], writes=["h16"])
            for kp in range(4):
                bk = banks[6 + kp % 2]
                bkey = "bank%d" % (6 + kp % 2)
                tv = bk[:].bitcast(BF16)
                fns = []
                for kk in range(2):
                    k = kp * 2 + kk
                    for s in range(4):
                        fns.append(lambda k=k, kk=kk, s=s: PE.transpose(tv[:, kk * 512 + s * 128: kk * 512 + (s + 1) * 128], h16[:, s, k * 128:(k + 1) * 128], ident_b[:]))
                S.ops("pe", fns, reads=["h16", "identb"], writes=[bkey])
                S.op("act", lambda: A.copy(out=hT[:, kp * 2:kp * 2 + 2, :].rearrange("p a b -> p (a b)"), in_=tv), reads=[bkey], writes=["hT"])

        pring = [0]

        def next_bank(lo=0, hi=6):
            b = lo + pring[0] % (hi - lo)
            pring[0] += 1
            return banks[b], "bank%d" % b

        def rsqrt_into(dst, dkey, src_ap, skey, mul, add):
            S.op("dve", lambda: V.tensor_scalar(out=dst, in0=src_ap, scalar1=mul, scalar2=add, op0=ALU.mult, op1=ALU.add), reads=[skey], writes=[dkey])
            S.op("act", lambda: A.activation(out=dst, in_=dst, func=AF.Sqrt), reads=[dkey], writes=[dkey])
            S.op("dve", lambda: V.reciprocal(out=dst, in_=dst), reads=[dkey], writes=[dkey])

        def phase_a_mla(xsrc, l):
            with ExitStack() as ph:
                Ct, St = rope_tables(ph, 64, c_inv_m)
                sc1p, k_sc = bcast_load(ph, MODS[l:l + 1, D:2 * D], "sc1p", plus_one=True)
                sh1p, k_sh = bcast_load(ph, MODS[l:l + 1, 0:D], "sh1p")
                w_in = sb(ph, [128, 8, 768], BF16, "w_in")
                w_uq = sb(ph, [128, 3, 8, 256], BF16, "w_uq")
                w_k = sb(ph, [128, 2, 8, 128], BF16, "w_k")
                w_v = sb(ph, [128, 2, 8, 128], BF16, "w_v")
                qn = sb(ph, [128, 3], F32, "qn")
                kvn = sb(ph, [128, 2], F32, "kvn")
                S.dma("pool", "a_w0", lambda: G.dma_start(out=w_in[:, :, 0:704], in_=mla_w_in.rearrange("(k p) n -> p k n", p=128)), writes=["w_in"])
                S.op("act", lambda: A.mul(out=w_in[:, :, 704:736], in_=w_in[:, :, 672:704], mul=-1.0), reads=["w_in"], writes=["w_in"])
                S.op("act", lambda: A.copy(out=w_in[:, :, 736:768], in_=w_in[:, :, 640:672]), reads=["w_in"], writes=["w_in"])
                uqv = mla_w_uq.rearrange("(k p) (h e) -> p k h e", p=128, e=192)
                for k in range(3):
                    S.dma("pool", "a_w1", lambda: G.dma_start(out=w_uq[:, k, :, 0:192], in_=uqv[:, k, :, :]), writes=["w_uq"])
                S.op("act", lambda: A.mul(out=w_uq[:, :, :, 192:224], in_=w_uq[:, :, :, 160:192], mul=-1.0), reads=["w_uq"], writes=["w_uq"])
                S.op("act", lambda: A.copy(out=w_uq[:, :, :, 224:256], in_=w_uq[:, :, :, 128:160]), reads=["w_uq"], writes=["w_uq"])
                wkv = mla_w_ukv.rearrange("(k p) (h two e) -> p k h two e", p=128, two=2, e=128)
                for k in range(2):
                    S.dma("pool", "a_w2", lambda: G.dma_start(out=w_k[:, k, :, :], in_=wkv[:, k, :, 0, :]), writes=["w_k"])
                    S.dma("pool", "a_w3", lambda: G.dma_start(out=w_v[:, k, :, :], in_=wkv[:, k, :, 1, :]), writes=["w_v"])
                with nc.allow_non_contiguous_dma(reason="tiny norm-gain vectors"):
                    S.dma("sp", "a_n0", lambda: nc.sync.dma_start(out=qn[:], in_=mla_q_norm.rearrange("(k p) -> p k", p=128)), writes=["qn"])
                    S.dma("sp", "a_n1", lambda: nc.sync.dma_start(out=kvn[:], in_=mla_kv_norm.rearrange("(k p) -> p k", p=128)), writes=["kvn"])
                xin = [sb(ph, [128, 4, D], F32, "xin") for _ in range(2)]
                h16 = sb(ph, [128, 4, D], BF16, "h16")
                hT = sb(ph, [128, 8, 512], BF16, "hT")
                lat = sb(ph, [128, 7, 512], F32, "lat")
                sq = sb(ph, [128, 5, 512], BF16, "sq")
                rstd = sb(ph, [128, 2, 512], F32, "rstd")
                cqn = sb(ph, [128, 3, 512], BF16, "cqn")
                ckvn = sb(ph, [128, 2, 512], BF16, "ckvn")
                kpe = sb(ph, [64, 512], BF16, "kpe")
                tmp1 = sb(ph, [64, 512], F32, "tmp1")
                tmp2 = sb(ph, [64, 512], F32, "tmp2")
                qst = sb(ph, [128, 8, 512], BF16, "qst")
                qrst = sb(ph, [64, 8, 512], BF16, "qrst")
                kst = sb(ph, [128, 8, 512], BF16, "kst")
                vst = sb(ph, [128, 4, D], BF16, "vst")
                mspec = [(0, 128, 128), (1, 256, 128), (2, 384, 128), (3, 512, 128), (4, 640, 128), (5, 704, 64), (6, 768, 64)]
                for tb in range(8):
                    ts = slice(tb * 512, (tb + 1) * 512)
                    front_block(tb, xsrc, xin, h16, hT, sc1p, sh1p, (k_sc, k_sh))
                    for (mi, hi_, mm) in mspec:
                        pb, pk = next_bank()
                        S.ops("pe", [(lambda k=k: PE.matmul(pb[0:mm, :], lhsT=w_in[:, k, hi_ - mm:hi_], rhs=hT[:, k, :], start=(k == 0), stop=(k == 7))) for k in range(8)],
                              reads=["w_in", "hT"], writes=[pk])
                        S.op("act", lambda: A.copy(out=lat[0:mm, mi, :], in_=pb[0:mm, :]), reads=[pk], writes=["lat%d" % mi])
                        if mi < 5:
                            S.op("dve", lambda: V.tensor_tensor(out=sq[:, mi, :], in0=lat[:, mi, :], in1=lat[:, mi, :], op=ALU.mult), reads=["lat%d" % mi], writes=["sq%d" % mi])
                    for gi, (c0, c1, n) in enumerate([(0, 3, 384), (3, 5, 256)]):
                        pb, pk = next_bank()
                        S.ops("pe", [(lambda c=c: PE.matmul(pb[:, :], lhsT=ones_b[:], rhs=sq[:, c, :], start=(c == c0), stop=(c == c1 - 1))) for c in range(c0, c1)],
                              reads=["onesb"] + ["sq%d" % c for c in range(c0, c1)], writes=[pk])
                        rsqrt_into(rstd[:, gi, :], "rstd%d" % gi, pb[:, :], pk, 1.0 / n, 1e-6)
                    for c in range(3):
                        S.op("dve", lambda: V.scalar_tensor_tensor(out=cqn[:, c, :], in0=lat[:, c, :], scalar=qn[:, c:c + 1], in1=rstd[:, 0, :], op0=ALU.mult, op1=ALU.mult),
                             reads=["lat%d" % c, "qn", "rstd0"], writes=["cqn"])
                    for c in range(2):
                        S.op("dve", lambda: V.scalar_tensor_tensor(out=ckvn[:, c, :], in0=lat[:, 3 + c, :], scalar=kvn[:, c:c + 1], in1=rstd[:, 1, :], op0=ALU.mult, op1=ALU.mult),
                             reads=["lat%d" % (3 + c), "kvn", "rstd1"], writes=["ckvn"])
                    S.op("dve", lambda: V.tensor_tensor(out=tmp1[:], in0=lat[0:64, 5, :], in1=Ct[:, ts], op=ALU.mult), reads=["lat5"], writes=["tmp1"])
                    S.op("pool", lambda: G.tensor_tensor(out=tmp2[:], in0=lat[0:64, 6, :], in1=St[:, ts], op=ALU.mult), reads=["lat6"], writes=["tmp2"])
                    S.op("dve", lambda: V.tensor_tensor(out=kpe[:], in0=tmp1[:], in1=tmp2[:], op=ALU.add), reads=["tmp1", "tmp2"], writes=["kpe"])
                    S.dma("sp", "a_kpe", lambda: nc.sync.dma_start(out=KPE[:, ts], in_=kpe[:]), reads=["kpe"], writes=["KPE"])
                    for h in range(8):
                        pb, pk = next_bank()
                        S.ops("pe", [(lambda c=c: PE.matmul(pb[:, :], lhsT=w_uq[:, c, h, 0:128], rhs=cqn[:, c, :], start=(c == 0), stop=(c == 2))) for c in range(3)],
                              reads=["w_uq", "cqn"], writes=[pk])
                        S.op("act", lambda: A.copy(out=qst[:, h, :], in_=pb[:, :]), reads=[pk], writes=["qst"])
                        pa, pka = next_bank()
                        S.ops("pe", [(lambda c=c: PE.matmul(pa[0:64, :], lhsT=w_uq[:, c, h, 128:192], rhs=cqn[:, c, :], start=(c == 0), stop=(c == 2))) for c in range(3)],
                              reads=["w_uq", "cqn"], writes=[pka])
                        pr, pkr = next_bank()
                        S.ops("pe", [(lambda c=c: PE.matmul(pr[0:64, :], lhsT=w_uq[:, c, h, 192:256], rhs=cqn[:, c, :], start=(c == 0), stop=(c == 2))) for c in range(3)],
                              reads=["w_uq", "cqn"], writes=[pkr])
                        S.op("dve", lambda: V.tensor_tensor(out=tmp1[:], in0=pa[0:64, :], in1=Ct[:, ts], op=ALU.mult), reads=[pka], writes=["tmp1"])
                        S.op("dve", lambda: V.tensor_tensor(out=tmp2[:], in0=pr[0:64, :], in1=St[:, ts], op=ALU.mult), reads=[pkr], writes=["tmp2"])
                        S.op("pool", lambda: G.tensor_tensor(out=qrst[:, h, :], in0=tmp1[:], in1=tmp2[:], op=ALU.add), reads=["tmp1", "tmp2"], writes=["qrst"])
                        pk_, pkk = next_bank()
                        S.ops("pe", [(lambda c=c: PE.matmul(pk_[:, :], lhsT=w_k[:, c, h, :], rhs=ckvn[:, c, :], start=(c == 0), stop=(c == 1))) for c in range(2)],
                              reads=["w_k", "ckvn"], writes=[pkk])
                        S.op("act", lambda: A.copy(out=kst[:, h, :], in_=pk_[:, :]), reads=[pkk], writes=["kst"])
                    S.dma("sp", "a_q", lambda: nc.sync.dma_start(out=QT[:, 0:128, ts].rearrange("h p t -> p h t"), in_=qst[:]), reads=["qst"], writes=["QT"])
                    S.dma("sp", "a_qr", lambda: nc.sync.dma_start(out=QT[:, 128:192, ts].rearrange("h p t -> p h t"), in_=qrst[:]), reads=["qrst"], writes=["QT"])
                    S.dma("sp", "a_k", lambda: nc.sync.dma_start(out=KT[:, :, ts].rearrange("h p t -> p h t"), in_=kst[:]), reads=["kst"], writes=["KT"])
                    for s in range(4):
                        for hf in range(2):
                            pb, pk = next_bank()
                            S.ops("pe", [(lambda c=c: PE.matmul(pb[:, :], lhsT=ckvn[:, c, s * 128:(s + 1) * 128], rhs=w_v[:, c, hf * 4:(hf + 1) * 4, :].rearrange("p h e -> p (h e)"), start=(c == 0), stop=(c == 1))) for c in range(2)],
                                  reads=["w_v", "ckvn"], writes=[pk])
                            S.op("act" if hf else "dve", (lambda pb=pb, hf=hf: (A.copy if hf else V.tensor_copy)(out=vst[:, s, hf * 512:(hf + 1) * 512], in_=pb[:, :])), reads=[pk], writes=["vst"])
                    S.dma("sp", "a_v", lambda: nc.sync.dma_start(out=VV.rearrange("(t s p) d -> t p s d", s=4, p=128)[tb], in_=vst[:]), reads=["vst"], writes=["VV"])
                S.barrier()

        def phase_a_diff(xsrc, l):
            with ExitStack() as ph:
                Ct, St = rope_tables(ph, 128, c_inv_d)
                sc1p, k_sc = bcast_load(ph, MODS[l:l + 1, D:2 * D], "sc1p", plus_one=True)
                sh1p, k_sh = bcast_load(ph, MODS[l:l + 1, 0:D], "sh1p")
                w = sb(ph, [128, 8, 3072], BF16, "dw")
                wr = sb(ph, [128, 8, 2048], BF16, "dwr")
                wsrc = diff_w_in.rearrange("(k p) n -> p k n", p=128)
                for j in range(3):
                    S.dma("pool", "d_w%d" % j, lambda: G.dma_start(out=w[:, :, j * 1024:(j + 1) * 1024], in_=wsrc[:, :, j * 1024:(j + 1) * 1024]), writes=["dw"])
                S.op("pool", lambda: G.memset(wr[:], 0.0), writes=["dwr"])
                w4 = w[:, :, 0:2048].rearrange("p k (g e) -> p k g e", e=64)
                wr4 = wr[:].rearrange("p k (g e) -> p k g e", e=64)
                for k in range(8):
                    S.op("act", lambda: A.mul(out=wr4[:, k, :, 0:8], in_=w4[:, k, :, 8:16], mul=-1.0), reads=["dw", "dwr"], writes=["dwr"])
                    S.op("act", lambda: A.copy(out=wr4[:, k, :, 8:16], in_=w4[:, k, :, 0:8]), reads=["dw", "dwr"], writes=["dwr"])
                xin = [sb(ph, [128, 4, D], F32, "xin") for _ in range(2)]
                h16 = sb(ph, [128, 4, D], BF16, "h16")
                hT = sb(ph, [128, 8, 512], BF16, "hT")
                tmp1 = sb(ph, [128, 512], F32, "tmp1")
                tmp2 = sb(ph, [128, 512], F32, "tmp2")
                qst = sb(ph, [128, 8, 512], BF16, "qst")
                kst = sb(ph, [128, 8, 512], BF16, "kst")
                vst = sb(ph, [128, 4, D], BF16, "vst")
                for tb in range(8):
                    ts = slice(tb * 512, (tb + 1) * 512)
                    front_block(tb, xsrc, xin, h16, hT, sc1p, sh1p, (k_sc, k_sh))
                    for qk in range(2):
                        st = qst if qk == 0 else kst
                        skey = "qst" if qk == 0 else "kst"
                        for h in range(8):
                            c0 = qk * 1024 + h * 128
                            pa, pka = next_bank()
                            S.ops("pe", [(lambda k=k: PE.matmul(pa[:, :], lhsT=w[:, k, c0:c0 + 128], rhs=hT[:, k, :], start=(k == 0), stop=(k == 7))) for k in range(8)],
                                  reads=["dw", "hT"], writes=[pka])
                            pr, pkr = next_bank()
                            S.ops("pe", [(lambda k=k: PE.matmul(pr[:, :], lhsT=wr[:, k, c0:c0 + 128], rhs=hT[:, k, :], start=(k == 0), stop=(k == 7))) for k in range(8)],
                                  reads=["dwr", "hT"], writes=[pkr])
                            S.op("dve", lambda: V.tensor_tensor(out=tmp1[:], in0=pa[:, :], in1=Ct[:, ts], op=ALU.mult), reads=[pka], writes=["tmp1"])
                            S.op("dve", lambda: V.tensor_tensor(out=tmp2[:], in0=pr[:, :], in1=St[:, ts], op=ALU.mult), reads=[pkr], writes=["tmp2"])
                            S.op("pool", lambda: G.tensor_tensor(out=st[:, h, :], in0=tmp1[:], in1=tmp2[:], op=ALU.add), reads=["tmp1", "tmp2"], writes=[skey])
                    S.dma("sp", "a_q", lambda: nc.sync.dma_start(out=QT[:, 0:128, ts].rearrange("h p t -> p h t"), in_=qst[:]), reads=["qst"], writes=["QT"])
                    S.dma("sp", "a_k", lambda: nc.sync.dma_start(out=KT[:, :, ts].rearrange("h p t -> p h t"), in_=kst[:]), reads=["kst"], writes=["KT"])
                    for s in range(4):
                        for hf in range(2):
                            pb, pk = next_bank()
                            S.ops("pe", [(lambda k=k: PE.matmul(pb[:, :], lhsT=hT[:, k, s * 128:(s + 1) * 128], rhs=w[:, k, 2048 + hf * 512:2048 + (hf + 1) * 512], start=(k == 0), stop=(k == 7))) for k in range(8)],
                                  reads=["dw", "hT"], writes=[pk])
                            S.op("act", lambda: A.copy(out=vst[:, s, hf * 512:(hf + 1) * 512], in_=pb[:, :]), reads=[pk], writes=["vst"])
                    S.dma("sp", "a_v", lambda: nc.sync.dma_start(out=VV.rearrange("(t s p) d -> t p s d", s=4, p=128)[tb], in_=vst[:]), reads=["vst"], writes=["VV"])
                S.barrier()

        def phase_b(kind):
            mla = kind == "mla"
            nmap = 1 if mla else 2
            scale = (192 ** -0.5) if mla else (64 ** -0.5)
            with ExitStack() as ph:
                kbuf = [sb(ph, [128, S_LEN], BF16, "kbuf") for _ in range(2)]
                qbuf = [sb(ph, [128, S_LEN], BF16, "qbuf") for _ in range(2)]
                vbuf = [sb(ph, [128, NBLK, 128], BF16, "vbuf") for _ in range(2)]
                NPT = 3
                PT = [sb(ph, [128, 1024], BF16, "PT") for _ in range(NPT)]
                dacc = [sb(ph, [128, 1024], F32, "dacc") for _ in range(2)]
                ones_f = sb(ph, [128, 128], F32, "onesf")
                S.op("dve", lambda: V.memset(ones_f[:], 1.0), writes=["onesf"])
                rden = [sb(ph, [128, 512], F32, "rden") for _ in range(2)]
                osb = [sb(ph, [128, 512], BF16, "osb") for _ in range(2)]
                if mla:
                    qrbuf = [sb(ph, [64, S_LEN], BF16, "qrbuf") for _ in range(2)]
                    kpe = sb(ph, [64, S_LEN], BF16, "kpeall")
                    S.dma("sp", "b_kpe", lambda: nc.sync.dma_start(out=kpe[:], in_=KPE), writes=["kpeall"])
                else:
                    lamt = sb(ph, [128, 256], F32, "lamt")
                    lp = sb(ph, [128, 128], F32, "lp")
                    ls = sb(ph, [128, 2], F32, "ls")
                    neglam = sb(ph, [128, 1], F32, "neglam")
                    subs = sb(ph, [128, 1], F32, "subs")
                    o1n = sb(ph, [128, 512], F32, "o1n")
                    o2n = sb(ph, [128, 512], F32, "o2n")
                    sq = sb(ph, [128, 512], BF16, "osq")
                    rstd = sb(ph, [128, 512], F32, "orstd")
                    S.dma("sp", "b_lam", lambda: nc.sync.dma_start(out=lamt[:], in_=diff_lambda.partition_broadcast(128)), writes=["lamt"])
                    S.dma("sp", "b_sub", lambda: nc.sync.dma_start(out=subs[:], in_=diff_subln), writes=["subs"])
                    S.op("dve", lambda: V.tensor_tensor(out=lp[:, 0:64], in0=lamt[:, 0:64], in1=lamt[:, 64:128], op=ALU.mult), reads=["lamt"], writes=["lp"])
                    S.op("dve", lambda: V.tensor_tensor(out=lp[:, 64:128], in0=lamt[:, 128:192], in1=lamt[:, 192:256], op=ALU.mult), reads=["lamt", "lp"], writes=["lp"])
                    S.op("dve", lambda: V.reduce_sum(out=ls[:, 0:1], in_=lp[:, 0:64], axis=mybir.AxisListType.X), reads=["lp"], writes=["ls"])
                    S.op("dve", lambda: V.reduce_sum(out=ls[:, 1:2], in_=lp[:, 64:128], axis=mybir.AxisListType.X), reads=["lp", "ls"], writes=["ls"])
                    S.op("act", lambda: A.activation(out=ls[:], in_=ls[:], func=AF.Exp), reads=["ls"], writes=["ls"])
                    S.op("dve", lambda: V.tensor_tensor(out=neglam[:], in0=ls[:, 1:2], in1=ls[:, 0:1], op=ALU.subtract), reads=["ls"], writes=["neglam"])
                    S.op("dve", lambda: V.tensor_scalar(out=neglam[:], in0=neglam[:], scalar1=-LAMBDA_INIT1, scalar2=None, op0=ALU.add), reads=["neglam"], writes=["neglam"])
                    S.op("dve", lambda: V.tensor_scalar(out=subs[:], in0=subs[:], scalar1=1.0 - LAMBDA_INIT1, scalar2=None, op0=ALU.mult), reads=["subs"], writes=["subs"])

                vview = VV.rearrange("(c p) (h e) -> p c h e", p=128, e=128)

                def load_head(h):
                    sl = h % 2
                    S.dma("sp", "b_k%d" % sl, lambda: nc.sync.dma_start(out=kbuf[sl][:], in_=KT[h]), writes=["kbuf%d" % sl])
                    S.dma("sp", "b_q%d" % sl, lambda: nc.sync.dma_start(out=qbuf[sl][:], in_=QT[h, 0:128, :]), writes=["qbuf%d" % sl])
                    S.dma("sp", "b_v%d" % sl, lambda: nc.sync.dma_start(out=vbuf[sl][:], in_=vview[:, :, h, :]), writes=["vbuf%d" % sl])
                    if mla:
                        S.dma("sp", "b_qr%d" % sl, lambda: nc.sync.dma_start(out=qrbuf[sl][:], in_=QT[h, 128:192, :]), writes=["qrbuf%d" % sl])

                load_head(0)
                blk = 0
                for h in range(8):
                    if h + 1 < 8:
                        load_head(h + 1)
                    sl = h % 2
                    for qb in range(8):
                        qs = slice(qb * 512, (qb + 1) * 512)
                        par = blk % 2
                        blk += 1
                        if mla:
                            acc_o, ko = banks[4 + par], "bank%d" % (4 + par)
                            den, kd = banks[6 + par], "bank%d" % (6 + par)
                            NU = NBLK // 2

                            def qk(u):
                                db = dbanks[u % 2]
                                fns = []
                                for j in range(2):
                                    kc = 2 * u + j
                                    ks = slice(kc * 128, (kc + 1) * 128)
                                    fns.append(lambda j=j, ks=ks: PE.matmul(db[:, j * 512:(j + 1) * 512], lhsT=kbuf[sl][:, ks], rhs=qbuf[sl][:, qs], start=True, stop=False))
                                    fns.append(lambda j=j, ks=ks: PE.matmul(db[:, j * 512:(j + 1) * 512], lhsT=kpe[:, ks], rhs=qrbuf[sl][:, qs], start=False, stop=True))
                                S.ops("pe", fns, reads=["kbuf%d" % sl, "qbuf%d" % sl, "qrbuf%d" % sl, "kpeall"], writes=["bank%d" % (2 * (u % 2)), "bank%d" % (2 * (u % 2) + 1)])

                            def ex(u):
                                S.op("act", lambda: A.activation(out=PT[u % NPT][:], in_=dbanks[u % 2][:, :], func=AF.Exp, scale=scale),
                                     reads=["bank%d" % (2 * (u % 2)), "bank%d" % (2 * (u % 2) + 1)], writes=["PT%d" % (u % NPT)])

                            def pv(u):
                                fns = []
                                for j in range(2):
                                    kc = 2 * u + j
                                    fns.append(lambda j=j, kc=kc: PE.matmul(acc_o, lhsT=vbuf[sl][:, kc, :], rhs=PT[u % NPT][:, j * 512:(j + 1) * 512], start=(kc == 0), stop=(kc == NBLK - 1)))
                                S.ops("pe", fns, reads=["PT%d" % (u % NPT), "vbuf%d" % sl], writes=[ko] if u == 0 else [], cwrites=[ko] if u == NU - 1 else [])
                                for (en, EN, c0, c1) in (("dve", V, 0, DSPLIT), ("pool", G, DSPLIT, 1024)):
                                    if u == 0:
                                        S.op(en, lambda: EN.tensor_copy(out=dacc[par][:, c0:c1], in_=PT[u % NPT][:, c0:c1]), reads=["PT%d" % (u % NPT)], writes=["dacc%d%s" % (par, en)])
                                    else:
                                        S.op(en, lambda: EN.tensor_tensor(out=dacc[par][:, c0:c1], in0=dacc[par][:, c0:c1], in1=PT[u % NPT][:, c0:c1], op=ALU.add), reads=["PT%d" % (u % NPT), "dacc%d%s" % (par, en)], writes=["dacc%d%s" % (par, en)])
                            qk(0)
                            for u in range(NU):
                                ex(u)
                                if u + 1 < NU:
                                    qk(u + 1)
                                pv(u)
                            S.ops("pe", [lambda: PE.matmul(den, lhsT=ones_f[:], rhs=dacc[par][:, 0:512], start=True, stop=False),
                                         lambda: PE.matmul(den, lhsT=ones_f[:], rhs=dacc[par][:, 512:1024], start=False, stop=True)],
                                  reads=["onesf", "dacc%ddve" % par, "dacc%dpool" % par], writes=[kd])
                            S.op("act", lambda: A.activation(out=rden[par][:], in_=den, func=AF.Ln), reads=[kd], writes=["rden%d" % par])
                            S.op("act", lambda: A.activation(out=rden[par][:], in_=rden[par][:], func=AF.Exp, scale=-1.0), reads=["rden%d" % par], writes=["rden%d" % par])
                            S.op("dve", lambda: V.tensor_tensor(out=osb[par][:], in0=acc_o, in1=rden[par][:], op=ALU.mult), reads=[ko, "rden%d" % par], writes=["osb%d" % par])
                        else:
                            accs = [(banks[4 + i], "bank%d" % (4 + i)) for i in range(4)]

                            def qk(kc):
                                ks = slice(kc * 128, (kc + 1) * 128)
                                db = dbanks[kc % 2]
                                fns = []
                                for m in range(2):
                                    rows = slice(m * 64, (m + 1) * 64)
                                    fns.append(lambda m=m, rows=rows: PE.matmul(db[:, m * 512:(m + 1) * 512], lhsT=kbuf[sl][rows, ks], rhs=qbuf[sl][rows, qs], start=True, stop=True))
                                S.ops("pe", fns, reads=["kbuf%d" % sl, "qbuf%d" % sl], writes=["bank%d" % (2 * (kc % 2)), "bank%d" % (2 * (kc % 2) + 1)])

                            def ex(kc):
                                S.op("act", lambda: A.activation(out=PT[kc % NPT][:], in_=dbanks[kc % 2][:, :], func=AF.Exp, scale=scale),
                                     reads=["bank%d" % (2 * (kc % 2)), "bank%d" % (2 * (kc % 2) + 1)], writes=["PT%d" % (kc % NPT)])

                            def pv(kc):
                                first, last = kc == 0, kc == NBLK - 1
                                fns = [(lambda m=m: PE.matmul(accs[m][0], lhsT=vbuf[sl][:, kc, :], rhs=PT[kc % NPT][:, m * 512:(m + 1) * 512], start=first, stop=last)) for m in range(2)]
                                ok_ = [accs[0][1], accs[1][1]]
                                S.ops("pe", fns, reads=["PT%d" % (kc % NPT), "vbuf%d" % sl], writes=ok_ if first else [], cwrites=ok_ if last else [])
                                for (en, EN, c0, c1) in (("dve", V, 0, DSPLIT), ("pool", G, DSPLIT, 1024)):
                                    if first:
                                        S.op(en, lambda: EN.tensor_copy(out=dacc[par][:, c0:c1], in_=PT[kc % NPT][:, c0:c1]), reads=["PT%d" % (kc % NPT)], writes=["dacc%d%s" % (par, en)])
                                    else:
                                        S.op(en, lambda: EN.tensor_tensor(out=dacc[par][:, c0:c1], in0=dacc[par][:, c0:c1], in1=PT[kc % NPT][:, c0:c1], op=ALU.add), reads=["PT%d" % (kc % NPT), "dacc%d%s" % (par, en)], writes=["dacc%d%s" % (par, en)])
                            qk(0)
                            for kc in range(NBLK):
                                ex(kc)
                                if kc + 1 < NBLK:
                                    qk(kc + 1)
                                pv(kc)
                            for m in range(2):
                                S.op("pe", lambda: PE.matmul(accs[2 + m][0], lhsT=ones_f[:], rhs=dacc[par][:, m * 512:(m + 1) * 512], start=True, stop=True), reads=["onesf", "dacc%ddve" % par, "dacc%dpool" % par], writes=[accs[2 + m][1]])
                            for m in range(2):
                                S.op("act", lambda: A.activation(out=rden[m][:], in_=accs[2 + m][0], func=AF.Ln), reads=[accs[2 + m][1]], writes=["rden%d" % m])
                                S.op("act", lambda: A.activation(out=rden[m][:], in_=rden[m][:], func=AF.Exp, scale=-1.0), reads=["rden%d" % m], writes=["rden%d" % m])
                            S.op("dve", lambda: V.tensor_tensor(out=o1n[:], in0=accs[0][0], in1=rden[0][:], op=ALU.mult), reads=[accs[0][1], "rden0"], writes=["o1n"])
                            S.op("dve", lambda: V.tensor_tensor(out=o2n[:], in0=accs[1][0], in1=rden[1][:], op=ALU.mult), reads=[accs[1][1], "rden1"], writes=["o2n"])
                            S.op("dve", lambda: V.scalar_tensor_tensor(out=o1n[:], in0=o2n[:], scalar=neglam[:, 0:1], in1=o1n[:], op0=ALU.mult, op1=ALU.add),
                                 reads=["o1n", "o2n", "neglam"], writes=["o1n"])
                            S.op("pool", lambda: G.tensor_tensor(out=sq[:], in0=o1n[:], in1=o1n[:], op=ALU.mult), reads=["o1n"], writes=["osq"])
                            S.op("pe", lambda: PE.matmul(accs[2][0], lhsT=ones_b[:], rhs=sq[:], start=True, stop=True), reads=["osq", "onesb"], writes=[accs[2][1]])
                            S.op("act", lambda: A.activation(out=rstd[:], in_=accs[2][0], func=AF.Ln, scale=1.0 / 128, bias=eps5[:, 0:1]), reads=[accs[2][1], "eps5"], writes=["orstd"])
                            S.op("act", lambda: A.activation(out=rstd[:], in_=rstd[:], func=AF.Exp, scale=-0.5), reads=["orstd"], writes=["orstd"])
                            S.op("dve", lambda: V.scalar_tensor_tensor(out=osb[par][:], in0=o1n[:], scalar=subs[:, 0:1], in1=rstd[:], op0=ALU.mult, op1=ALU.mult),
                                 reads=["o1n", "subs", "orstd"], writes=["osb%d" % par])
                        S.dma("sp", "b_o%d" % par, lambda: nc.sync.dma_start(out=OT[h * 128:(h + 1) * 128, qs], in_=osb[par][:]), reads=["osb%d" % par], writes=["OT"])
                S.barrier()

        def ln_stats(z, zkey, st, mv, rs, nmr, tag):
            for i in range(2):
                S.op("dve", lambda: V.bn_stats(out=st[:, i, :], in_=z[:, i * 512:(i + 1) * 512]), reads=[zkey], writes=["st" + tag])
            S.op("dve", lambda: V.bn_aggr(out=mv[:], in_=st[:].rearrange("p a b -> p (a b)")), reads=["st" + tag], writes=["mv" + tag])
            S.op("act", lambda: A.activation(out=rs[:], in_=mv[:, 1:2], func=AF.Sqrt, bias=eps5[:, 0:1], scale=1.0), reads=["mv" + tag, "eps5"], writes=["rs" + tag])
            S.op("dve", lambda: V.reciprocal(out=rs[:], in_=rs[:]), reads=["rs" + tag], writes=["rs" + tag])
            S.op("dve", lambda: V.scalar_tensor_tensor(out=nmr[:], in0=mv[:, 0:1], scalar=-1.0, in1=rs[:], op0=ALU.mult, op1=ALU.mult), reads=["mv" + tag, "rs" + tag], writes=["nmr" + tag])

        def ln_apply(z, zkey, rs, nmr, tag):
            S.op("act", lambda: A.activation(out=z[:], in_=z[:], func=AF.Identity, bias=nmr[:, 0:1], scale=rs[:, 0:1]), reads=[zkey, "rs" + tag, "nmr" + tag], writes=[zkey])

        def phase_c(xsrc, l, w_o_ap):
            with ExitStack() as ph:
                g1p, k_g1 = bcast_load(ph, MODS[l:l + 1, 2 * D:3 * D], "g1p", plus_one=True)
                sc2p, k_sc2 = bcast_load(ph, MODS[l:l + 1, 4 * D:5 * D], "sc2p", plus_one=True)
                sh2p, k_sh2 = bcast_load(ph, MODS[l:l + 1, 3 * D:4 * D], "sh2p")
                lng, k_lng = bcast_load(ph, ln1_g[l:l + 1, :], "lng")
                lnb, k_lnb = bcast_load(ph, ln1_b[l:l + 1, :], "lnb")
                w_o = sb(ph, [128, 8, D], BF16, "w_o")
                rw = sb(ph, [128, 8, NE], F32, "rw")
                oT = sb(ph, [128, 8, S_LEN], BF16, "oTall")
                S.dma("pool", "c_wo", lambda: G.dma_start(out=w_o[:], in_=w_o_ap.rearrange("(k p) n -> p k n", p=128)), writes=["w_o"])
                S.dma("sp", "c_rw", lambda: nc.sync.dma_start(out=rw[:], in_=router_w[l].rearrange("(k p) n -> p k n", p=128)), writes=["rw"])
                for h in range(8):
                    S.dma("sp", "c_ot", lambda: nc.sync.dma_start(out=oT[:, h, :], in_=OT[h * 128:(h + 1) * 128, :]), writes=["oTall"])
                NB3 = 3
                xin = [sb(ph, [128, D], F32, "xin") for _ in range(NB3)]
                z = [sb(ph, [128, D], F32, "z") for _ in range(NB3)]
                h2 = [sb(ph, [128, D], F32, "h2") for _ in range(NB3)]
                h2b = [sb(ph, [128, D], BF16, "h2b") for _ in range(2)]
                h2T = [sb(ph, [128, 8, 128], F32, "h2T") for _ in range(2)]
                st = [sb(ph, [128, 2, 6], F32, "st") for _ in range(2)]
                mv = [sb(ph, [128, 2], F32, "mv") for _ in range(2)]
                rs = [sb(ph, [128, 1], F32, "rs") for _ in range(2)]
                nmr = [sb(ph, [128, 1], F32, "nmr") for _ in range(2)]
                mx = [sb(ph, [128, 1], F32, "mx") for _ in range(2)]
                ssum = [sb(ph, [128, 1], F32, "ssum") for _ in range(2)]
                ex = [sb(ph, [128, NE], F32, "ex") for _ in range(2)]

                def st0(b):
                    s3, s2 = b % NB3, b % 2
                    rows = slice(b * 128, (b + 1) * 128)
                    for hf in range(2):
                        pt, kk = banks[2 * s2 + hf], "bank%d" % (2 * s2 + hf)
                        S.ops("pe", [(lambda h=h: PE.matmul(pt[:, :], lhsT=oT[:, h, rows], rhs=w_o[:, h, hf * 512:(hf + 1) * 512], start=(h == 0), stop=(h == 7))) for h in range(8)],
                              reads=["oTall", "w_o"], writes=[kk])
                        S.op("dve", lambda: V.tensor_tensor(out=z[s3][:, hf * 512:(hf + 1) * 512], in0=pt[:, :], in1=g1p[:, hf * 512:(hf + 1) * 512], op=ALU.mult),
                             reads=[kk, k_g1], writes=["z%d" % s3])
                    S.op("dve", lambda: V.scalar_tensor_tensor(out=z[s3][:], in0=xin[s3][:], scalar=ALPHA, in1=z[s3][:], op0=ALU.mult, op1=ALU.add),
                         reads=["xin%d" % s3, "z%d" % s3], writes=["z%d" % s3])
                    ln_stats(z[s3], "z%d" % s3, st[s2], mv[s2], rs[s2], nmr[s2], "c%d" % s2)

                def st1(b):
                    s3, s2 = b % NB3, b % 2
                    rows = slice(b * 128, (b + 1) * 128)
                    ln_apply(z[s3], "z%d" % s3, rs[s2], nmr[s2], "c%d" % s2)
                    S.op("pool", lambda: G.tensor_tensor(out=z[s3][:], in0=z[s3][:], in1=lng[:], op=ALU.mult), reads=["z%d" % s3, k_lng], writes=["z%d" % s3])
                    S.op("dve", lambda: V.tensor_tensor(out=z[s3][:], in0=z[s3][:], in1=lnb[:], op=ALU.add), reads=["z%d" % s3, k_lnb], writes=["z%d" % s3])
                    S.dma("sp", "c_xm%d" % s3, lambda: nc.sync.dma_start(out=XMID[rows, :], in_=z[s3][:]), reads=["z%d" % s3], writes=["XMID"])
                    S.op("pool", lambda: G.tensor_tensor(out=h2[s3][:], in0=z[s3][:], in1=sc2p[:], op=ALU.mult), reads=["z%d" % s3, k_sc2], writes=["h2_%d" % s3])
                    S.op("dve", lambda: V.tensor_tensor(out=h2[s3][:], in0=h2[s3][:], in1=sh2p[:], op=ALU.add), reads=["h2_%d" % s3, k_sh2], writes=["h2_%d" % s3])
                    S.op("act", lambda: A.copy(out=h2b[s2][:], in_=h2[s3][:]), reads=["h2_%d" % s3], writes=["h2b%d" % s2])
                    S.dma("sp", "c_h2%d" % s2, lambda: nc.sync.dma_start(out=H2[rows, :], in_=h2b[s2][:]), reads=["h2b%d" % s2], writes=["H2"])

                def st2(b):
                    s3, s2 = b % NB3, b % 2
                    for hf in range(2):
                        bk, bkey = banks[4 + hf], "bank%d" % (4 + hf)
                        S.ops("pe", [(lambda j=j: PE.transpose(bk[:, j * 128:(j + 1) * 128], h2[s3][:, (hf * 4 + j) * 128:(hf * 4 + j + 1) * 128], ident_f[:])) for j in range(4)],
                              reads=["h2_%d" % s3, "identf"], writes=[bkey])
                        S.op("act", lambda: A.copy(out=h2T[s2][:, hf * 4:(hf + 1) * 4, :].rearrange("p a b -> p (a b)"), in_=bk[:, :]), reads=[bkey], writes=["h2T%d" % s2])
                    lg, lgk = banks[6 + s2], "bank%d" % (6 + s2)
                    S.ops("pe", [(lambda k=k: PE.matmul(lg[:, 0:NE], lhsT=h2T[s2][:, k, :], rhs=rw[:, k, :], start=(k == 0), stop=(k == 7))) for k in range(8)],
                          reads=["h2T%d" % s2, "rw"], writes=[lgk])
                    S.op("dve", lambda: V.reduce_max(out=mx[s2][:], in_=lg[:, 0:NE], axis=mybir.AxisListType.X), reads=[lgk], writes=["mx%d" % s2])
                    S.op("dve", lambda: V.tensor_scalar(out=mx[s2][:], in0=mx[s2][:], scalar1=-1.0, scalar2=None, op0=ALU.mult), reads=["mx%d" % s2], writes=["mx%d" % s2])
                    S.op("act", lambda: A.activation(out=ex[s2][:], in_=lg[:, 0:NE], func=AF.Exp, bias=mx[s2][:, 0:1], scale=1.0, accum_out=ssum[s2][:]), reads=[lgk, "mx%d" % s2], writes=["ex%d" % s2, "ssum%d" % s2])
                    S.op("dve", lambda: V.reciprocal(out=ssum[s2][:], in_=ssum[s2][:]), reads=["ssum%d" % s2], writes=["ssum%d" % s2])
                    S.op("dve", lambda: V.tensor_scalar(out=aff[:, b, :], in0=ex[s2][:], scalar1=ssum[s2][:, 0:1], scalar2=None, op0=ALU.mult), reads=["ex%d" % s2, "ssum%d" % s2], writes=["aff"])

                def ld(b):
                    s3 = b % NB3
                    S.dma("sp", "c_x%d" % s3, lambda: nc.sync.dma_start(out=xin[s3][:], in_=xsrc[b * 128:(b + 1) * 128, :]), writes=["xin%d" % s3])

                stages = [st0, st1, st2]
                ld(0)
                ld(1)
                for step in range(NBLK + len(stages) - 1):
                    for si in reversed(range(len(stages))):
                        b = step - si
                        if si == 0 and step + 2 < NBLK:
                            ld(step + 2)
                        if 0 <= b < NBLK:
                            stages[si](b)
                S.barrier()

        def phase_m(l):
            with ExitStack() as ph:
                ltri = sb(ph, [128, 128], F32, "ltri")
                ltri_b = sb(ph, [128, 128], BF16, "ltrib")
                iota = sb(ph, [128, CAP], F32, "iota")
                rcp = sb(ph, [128, NBLK, 2], F32, "rcp")
                S.dma("sp", "m_c0", lambda: nc.sync.dma_start(out=ltri[:], in_=c_ltri), writes=["ltri"])
                S.dma("sp", "m_c1", lambda: nc.sync.dma_start(out=iota[:], in_=c_iota), writes=["iota"])
                S.dma("sp", "m_c2", lambda: nc.sync.dma_start(out=rcp[:], in_=c_rcp), writes=["rcp"])
                S.op("dve", lambda: V.tensor_copy(out=ltri_b[:], in_=ltri[:]), reads=["ltri"], writes=["ltrib"])
                zt = sb(ph, [128, 4, D], F32, "zt")
                S.op("pool", lambda: G.memset(zt[:], 0.0), writes=["zt"])
                fv = FACC.rearrange("(t s p) d -> t p s d", s=4, p=128)
                for tb in range(8):
                    S.dma("sp", "m_z", lambda: nc.sync.dma_start(out=fv[tb], in_=zt[:]), reads=["zt"], writes=["FACC"])
                lo = sb(ph, [128, NE], F32, "lo")
                mid = sb(ph, [128, NE], F32, "mid")
                cmpt = sb(ph, [128, NBLK, NE], F32, "cmpt")
                cnt = sb(ph, [128, NE], F32, "cnt")
                ge = sb(ph, [128, NE], F32, "ge")
                ones_f = sb(ph, [128, 128], F32, "onesf")
                S.op("dve", lambda: V.memset(ones_f[:], 1.0), writes=["onesf"])
                S.op("dve", lambda: V.memset(lo[:], 0.0), writes=["lo"])
                for it in range(30):
                    w = 2.0 ** -(it + 1)
                    bk, bkk = banks[it % 2], "bank%d" % (it % 2)
                    S.op("dve", lambda: V.tensor_scalar(out=mid[:], in0=lo[:], scalar1=w, scalar2=None, op0=ALU.add), reads=["lo"], writes=["mid"])
                    S.op("dve", lambda: V.tensor_tensor(out=cmpt[:], in0=aff[:], in1=mid[:].unsqueeze(1).broadcast_to([128, NBLK, NE]), op=ALU.is_ge), reads=["aff", "mid"], writes=["cmpt"])
                    S.op("dve", lambda: V.reduce_sum(out=cnt[:], in_=cmpt[:].rearrange("p c e -> p e c"), axis=mybir.AxisListType.X), reads=["cmpt"], writes=["cnt"])
                    S.op("pe", lambda: PE.matmul(bk[:, 0:NE], lhsT=ones_f[:], rhs=cnt[:], start=True, stop=True), reads=["onesf", "cnt"], writes=[bkk])
                    S.op("dve", lambda: V.tensor_scalar(out=ge[:], in0=bk[:, 0:NE], scalar1=float(CAP) - 0.5, scalar2=None, op0=ALU.is_ge), reads=[bkk], writes=["ge"])
                    S.op("dve", lambda: V.scalar_tensor_tensor(out=lo[:], in0=ge[:], scalar=w, in1=lo[:], op0=ALU.mult, op1=ALU.add), reads=["ge", "lo"], writes=["lo"])
                maskb = sb(ph, [128, NBLK, NE], BF16, "maskb")
                pre = sb(ph, [128, NBLK, NE], BF16, "pre")
                pref = sb(ph, [128, NBLK, NE], F32, "pref")
                posp = sb(ph, [128, NBLK, NE], F32, "posp")
                S.op("dve", lambda: V.tensor_tensor(out=cmpt[:], in0=aff[:], in1=lo[:].unsqueeze(1).broadcast_to([128, NBLK, NE]), op=ALU.is_ge), reads=["aff", "lo"], writes=["cmpt"])
                S.op("dve", lambda: V.tensor_copy(out=maskb[:], in_=cmpt[:]), reads=["cmpt"], writes=["maskb"])
                S.op("dve", lambda: V.memset(pref[:, 0, :], 0.0), writes=["pref"])
                for c in range(1, NBLK):
                    S.op("dve", lambda: V.tensor_tensor(out=pref[:, c, :], in0=pref[:, c - 1, :], in1=cmpt[:, c - 1, :], op=ALU.add), reads=["pref", "cmpt"], writes=["pref"])
                S.op("dve", lambda: V.tensor_copy(out=pre[:], in_=pref[:]), reads=["pref"], writes=["pre"])
                pb = banks[1]
                S.ops("pe", [lambda: PE.matmul(pb[:, :], lhsT=ltri_b[:], rhs=maskb[:].rearrange("p c e -> p (c e)"), start=True, stop=False),
                             lambda: PE.matmul(pb[:, :], lhsT=ones_b[:], rhs=pre[:].rearrange("p c e -> p (c e)"), start=False, stop=True)],
                      reads=["ltrib", "onesb", "maskb", "pre"], writes=["bank1"])
                S.op("dve", lambda: V.tensor_tensor(out=posp[:].rearrange("p c e -> p (c e)"), in0=pb[:, :], in1=cmpt[:].rearrange("p c e -> p (c e)"), op=ALU.mult), reads=["bank1", "cmpt"], writes=["posp"])
                R = sb(ph, [128, NBLK, NE, 5], BF16, "R")
                r1 = sb(ph, [128, NBLK, NE], F32, "r1")
                r2 = sb(ph, [128, NBLK, NE], F32, "r2")
                S.op("dve", lambda: V.tensor_copy(out=R[:, :, :, 0], in_=rcp[:, :, 0:1].broadcast_to([128, NBLK, NE])), reads=["rcp"], writes=["R"])
                S.op("dve", lambda: V.tensor_copy(out=R[:, :, :, 1], in_=rcp[:, :, 1:2].broadcast_to([128, NBLK, NE])), reads=["rcp", "R"], writes=["R"])
                S.op("dve", lambda: V.tensor_copy(out=R[:, :, :, 2], in_=aff[:]), reads=["aff", "R"], writes=["R"])
                S.op("dve", lambda: V.tensor_tensor(out=r1[:], in0=aff[:], in1=R[:, :, :, 2], op=ALU.subtract), reads=["aff", "R"], writes=["r1"])
                S.op("dve", lambda: V.tensor_copy(out=R[:, :, :, 3], in_=r1[:]), reads=["r1", "R"], writes=["R"])
                S.op("dve", lambda: V.tensor_tensor(out=r2[:], in0=r1[:], in1=R[:, :, :, 3], op=ALU.subtract), reads=["r1", "R"], writes=["r2"])
                S.op("dve", lambda: V.tensor_copy(out=R[:, :, :, 4], in_=r2[:]), reads=["r2", "R"], writes=["R"])
                idx = sb(ph, [128, NE, 4], I32, "idx")
                idxf = sb(ph, [128, NE, 4], F32, "idxf")
                gate = sb(ph, [128, NE, 4], F32, "gate")
                Pm = [sb(ph, [128, NBLK, 128], BF16, "Pm") for _ in range(2)]
                ig = sb(ph, [128, 8], F32, "ig")
                pcount = [0]

                def compute_idx(e, g):
                    psl = pcount[0] % 2
                    pcount[0] += 1
                    S.op("dve", lambda: V.tensor_tensor(out=Pm[psl][:], in0=posp[:, :, e:e + 1].broadcast_to([128, NBLK, 128]),
                                                        in1=iota[:, g * 128:(g + 1) * 128].unsqueeze(1).broadcast_to([128, NBLK, 128]), op=ALU.is_equal),
                         reads=["posp", "iota"], writes=["Pm%d" % psl])
                    ob, obk = banks[7], "bank7"
                    S.ops("pe", [(lambda c=c: PE.matmul(ob[:, 0:5], lhsT=Pm[psl][:, c, :], rhs=R[:, c, e, :], start=(c == 0), stop=(c == NBLK - 1))) for c in range(NBLK)],
                          reads=["Pm%d" % psl, "R"], writes=[obk])
                    S.op("act", lambda: A.copy(out=ig[:, psl * 4:psl * 4 + 4], in_=ob[:, 1:5]), reads=[obk], writes=["ig%d" % psl])
                    S.op("dve", lambda: V.scalar_tensor_tensor(out=idxf[:, e, g:g + 1], in0=ob[:, 0:1], scalar=128.0, in1=ig[:, psl * 4:psl * 4 + 1], op0=ALU.mult, op1=ALU.add),
                         reads=[obk, "ig%d" % psl], writes=["idxf%d" % e])
                    S.op("dve", lambda: V.tensor_tensor(out=gate[:, e, g:g + 1], in0=ig[:, psl * 4 + 1:psl * 4 + 2], in1=ig[:, psl * 4 + 2:psl * 4 + 3], op=ALU.add), reads=["ig%d" % psl], writes=["gate%d" % e])
                    S.op("dve", lambda: V.tensor_tensor(out=gate[:, e, g:g + 1], in0=gate[:, e, g:g + 1], in1=ig[:, psl * 4 + 3:psl * 4 + 4], op=ALU.add), reads=["ig%d" % psl, "gate%d" % e], writes=["gate%d" % e])
                    if g == 3:
                        S.op("dve", lambda: V.tensor_copy(out=idx[:, e, :], in_=idxf[:, e, :]), reads=["idxf%d" % e], writes=["idx%d" % e])

                for e0 in range(2):
                    for g in range(4):
                        compute_idx(e0, g)
                wi = [sb(ph, [128, 8, 1024], BF16, "wi") for _ in range(3)]
                wo = [sb(ph, [128, 4, D], BF16, "wo") for _ in range(3)]
                xg = [sb(ph, [128, 4, D], BF16, "xg") for _ in range(2)]
                xeT = sb(ph, [128, 8, CAP], BF16, "xeT")
                sg = [sb(ph, [128, CAP], F32, "sg") for _ in range(2)]
                actT = [sb(ph, [128, 4, CAP], BF16, "actT") for _ in range(2)]
                ysb = [sb(ph, [128, 4, D], F32, "ysb") for _ in range(2)]
                wiv = moe_w_in[l].rearrange("e (k p) n -> e p k n", p=128)
                wov = moe_w_out[l].rearrange("e (k p) n -> e p k n", p=128)
                nblk = [0]

                def load_w(e, i):
                    sl = nblk[0] % 3
                    nblk[0] += 1
                    S.dma("pool", "m_wg%d" % sl, lambda: G.dma_start(out=wi[sl][:, :, 0:512], in_=wiv[e][:, :, i * 512:(i + 1) * 512]), writes=["wi%d" % sl])
                    S.dma("pool", "m_wu%d" % sl, lambda: G.dma_start(out=wi[sl][:, :, 512:1024], in_=wiv[e][:, :, FF + i * 512:FF + (i + 1) * 512]), writes=["wi%d" % sl])
                    S.dma("pool", "m_wo%d" % sl, lambda: G.dma_start(out=wo[sl][:], in_=wov[e][:, i * 4:(i + 1) * 4, :]), writes=["wo%d" % sl])
                    return sl

                def gather(e):
                    gs = e % 2
                    for g in range(4):
                        S.dma("pool", "m_g%d" % gs, lambda: G.indirect_dma_start(out=xg[gs][:, g, :], out_offset=None, in_=H2,
                                                                               in_offset=bass.IndirectOffsetOnAxis(ap=idx[:, e, g:g + 1], axis=0)),
                              reads=["idx%d" % e, "H2"], writes=["xg%d" % gs])

                sched_w = [(e, i) for e in range(NE) for i in range(4)]
                slots = {}
                slots[sched_w[0]] = load_w(*sched_w[0])
                slots[sched_w[1]] = load_w(*sched_w[1])
                gather(0)
                wn = 2
                par2 = 0
                for e in range(NE):
                    gs = e % 2
                    ys = e % 2
                    if e + 1 < NE:
                        gather(e + 1)
                    for k in range(8):
                        bk, bkey = banks[6], "bank6"
                        tv = bk[:].bitcast(BF16)
                        S.ops("pe", [(lambda g=g: PE.transpose(tv[:, g * 128:(g + 1) * 128], xg[gs][:, g, k * 128:(k + 1) * 128], ident_b[:])) for g in range(4)],
                              reads=["xg%d" % gs, "identb"], writes=[bkey])
                        S.op("act" if k % 2 else "dve", (lambda k=k, tv=tv: (A.copy if k % 2 else V.tensor_copy)(out=xeT[:, k, :], in_=tv[:, 0:CAP])), reads=[bkey], writes=["xeT"])
                    for i in range(4):
                        if wn < len(sched_w):
                            slots[sched_w[wn]] = load_w(*sched_w[wn])
                            wn += 1
                        ws = slots[(e, i)]
                        asl = (e * 4 + i) % 2
                        for fc in range(4):
                            pg, pgk = banks[0 + 2 * (fc % 2)], "bank%d" % (0 + 2 * (fc % 2))
                            pu, puk = banks[1 + 2 * (fc % 2)], "bank%d" % (1 + 2 * (fc % 2))
                            S.ops("pe", [(lambda k=k: PE.matmul(pg[:, :], lhsT=wi[ws][:, k, fc * 128:(fc + 1) * 128], rhs=xeT[:, k, :], start=(k == 0), stop=(k == 7))) for k in range(8)],
                                  reads=["wi%d" % ws, "xeT"], writes=[pgk])
                            S.ops("pe", [(lambda k=k: PE.matmul(pu[:, :], lhsT=wi[ws][:, k, 512 + fc * 128:512 + (fc + 1) * 128], rhs=xeT[:, k, :], start=(k == 0), stop=(k == 7))) for k in range(8)],
                                  reads=["wi%d" % ws, "xeT"], writes=[puk])
                            S.op("act", lambda: A.activation(out=sg[fc % 2][:], in_=pg[:, :], func=AF.Silu), reads=[pgk], writes=["sg%d" % (fc % 2)])
                            S.op("dve", lambda: V.tensor_tensor(out=actT[asl][:, fc, :], in0=sg[fc % 2][:], in1=pu[:, :], op=ALU.mult), reads=["sg%d" % (fc % 2), puk], writes=["actT%d" % asl])
                        if e + 2 < NE:
                            compute_idx(e + 2, i)
                        for g in range(4):
                            for hf in range(2):
                                py, pyk = banks[4 + par2 % 2], "bank%d" % (4 + par2 % 2)
                                par2 += 1
                                S.ops("pe", [(lambda fc=fc: PE.matmul(py[:, :], lhsT=actT[asl][:, fc, g * 128:(g + 1) * 128], rhs=wo[ws][:, fc, hf * 512:(hf + 1) * 512], start=(fc == 0), stop=(fc == 3))) for fc in range(4)],
                                      reads=["actT%d" % asl, "wo%d" % ws], writes=[pyk])
                                dst = ysb[ys][:, g, hf * 512:(hf + 1) * 512]
                                if i == 0:
                                    S.op("act", lambda: A.activation(out=dst, in_=py[:, :], func=AF.Copy, scale=gate[:, e, g:g + 1]), reads=[pyk, "gate%d" % e], writes=["ysb%d" % ys])
                                else:
                                    S.op("dve", lambda: V.scalar_tensor_tensor(out=dst, in0=py[:, :], scalar=gate[:, e, g:g + 1], in1=dst, op0=ALU.mult, op1=ALU.add),
                                         reads=[pyk, "gate%d" % e, "ysb%d" % ys], writes=["ysb%d" % ys])
                    for g in range(4):
                        S.dma("pool", "m_sc", lambda: G.indirect_dma_start(out=FACC, out_offset=bass.IndirectOffsetOnAxis(ap=idx[:, e, g:g + 1], axis=0),
                                                                          in_=ysb[ys][:, g, :], in_offset=None, compute_op=ALU.add),
                              reads=["ysb%d" % ys, "idx%d" % e], writes=["FACC"] if g == 0 else [])
                    S.lastw["FACC"] = (S.dsem["m_sc"][0], S.dsem["m_sc"][1])
                    S.readers["FACC"] = {}
                S.barrier()

        def phase_d(l, dst):
            with ExitStack() as ph:
                g2p, k_g2 = bcast_load(ph, MODS[l:l + 1, 5 * D:6 * D], "g2p", plus_one=True)
                lng, k_lng = bcast_load(ph, ln2_g[l:l + 1, :], "lng2")
                lnb, k_lnb = bcast_load(ph, ln2_b[l:l + 1, :], "lnb2")
                NB3 = 3
                xin = [sb(ph, [128, D], F32, "xin") for _ in range(NB3)]
                fin = [sb(ph, [128, D], F32, "fin") for _ in range(NB3)]
                st = [sb(ph, [128, 2, 6], F32, "st") for _ in range(2)]
                mv = [sb(ph, [128, 2], F32, "mv") for _ in range(2)]
                rs = [sb(ph, [128, 1], F32, "rs") for _ in range(2)]
                nmr = [sb(ph, [128, 1], F32, "nmr") for _ in range(2)]

                def st0(b):
                    s3, s2 = b % NB3, b % 2
                    rows = slice(b * 128, (b + 1) * 128)
                    S.op("pool", lambda: G.tensor_tensor(out=fin[s3][:], in0=fin[s3][:], in1=g2p[:], op=ALU.mult), reads=["fin%d" % s3, k_g2], writes=["fin%d" % s3])
                    S.op("dve", lambda: V.scalar_tensor_tensor(out=fin[s3][:], in0=xin[s3][:], scalar=ALPHA, in1=fin[s3][:], op0=ALU.mult, op1=ALU.add),
                         reads=["xin%d" % s3, "fin%d" % s3], writes=["fin%d" % s3])
                    ln_stats(fin[s3], "fin%d" % s3, st[s2], mv[s2], rs[s2], nmr[s2], "d%d" % s2)

                def st1(b):
                    s3, s2 = b % NB3, b % 2
                    rows = slice(b * 128, (b + 1) * 128)
                    ln_apply(fin[s3], "fin%d" % s3, rs[s2], nmr[s2], "d%d" % s2)
                    S.op("pool", lambda: G.tensor_tensor(out=fin[s3][:], in0=fin[s3][:], in1=lng[:], op=ALU.mult), reads=["fin%d" % s3, k_lng], writes=["fin%d" % s3])
                    S.op("dve", lambda: V.tensor_tensor(out=fin[s3][:], in0=fin[s3][:], in1=lnb[:], op=ALU.add), reads=["fin%d" % s3, k_lnb], writes=["fin%d" % s3])
                    S.dma("sp", "d_o%d" % s3, lambda: nc.sync.dma_start(out=dst[rows, :], in_=fin[s3][:]), reads=["fin%d" % s3], writes=["dst"])

                def ld(b):
                    s3 = b % NB3
                    rows = slice(b * 128, (b + 1) * 128)
                    S.dma("sp", "d_x%d" % s3, lambda: nc.sync.dma_start(out=xin[s3][:], in_=XMID[rows, :]), writes=["xin%d" % s3])
                    S.dma("sp", "d_f%d" % s3, lambda: nc.sync.dma_start(out=fin[s3][:], in_=FACC[rows, :]), writes=["fin%d" % s3])

                ld(0)
                ld(1)
                for step in range(NBLK + 1):
                    if 0 <= step - 1 < NBLK:
                        st1(step - 1)
                    if step + 2 < NBLK:
                        ld(step + 2)
                    if step < NBLK:
                        st0(step)
                S.barrier()

        phases = [
            lambda: phase_mods(),
            lambda: phase_a_mla(x_in, 0),
            lambda: phase_b("mla"),
            lambda: phase_c(x_in, 0, mla_w_o),
            lambda: phase_m(0),
            lambda: phase_d(0, X1),
            lambda: phase_a_diff(X1, 1),
            lambda: phase_b("diff"),
            lambda: phase_c(X1, 1, diff_w_o),
            lambda: phase_m(1),
            lambda: phase_d(1, out),
        ]
        for i, p in enumerate(phases):
            if i <= upto:
                p()
        if dbg is not None:
            src = {"XMID": XMID, "X1": X1, "FACC": FACC}[dbg]
            with ExitStack() as ph:
                t = sb(ph, [128, 4, D], F32, "dbg")
                for tb in range(8):
                    S.dma("sp", "dbg_i", lambda: nc.sync.dma_start(out=t[:], in_=src.rearrange("(t s p) d -> t p s d", s=4, p=128)[tb]), writes=["dbg"])
                    S.dma("sp", "dbg_o", lambda: nc.sync.dma_start(out=out.rearrange("(t s p) d -> t p s d", s=4, p=128)[tb], in_=t[:]), reads=["dbg"], writes=["outd"])
        S.finish("sp")
    return nc


def _consts():
    inv32 = (10000.0 ** (-np.arange(0, 64, 2, dtype=np.float32) / 64)).astype(np.float32)
    inv_m = np.concatenate([inv32, inv32]).reshape(64, 1).astype(np.float32)
    inv8 = (500000.0 ** (-np.arange(0, 16, 2, dtype=np.float32) / 16)).astype(np.float32)
    blk = np.zeros(64, np.float32)
    blk[0:8] = inv8
    blk[8:16] = inv8
    inv_d = np.concatenate([blk, blk]).reshape(128, 1).astype(np.float32)
    ident = np.eye(128, dtype=np.float32)
    ltri = np.triu(np.ones((128, 128), np.float32))
    iota = np.tile(np.arange(1, CAP + 1, dtype=np.float32)[None, :], (128, 1))
    rcp = np.zeros((128, NBLK, 2), np.float32)
    rcp[:, :, 0] = np.arange(NBLK, dtype=np.float32)[None, :]
    rcp[:, :, 1] = np.arange(128, dtype=np.float32)[:, None]
    return dict(c_inv_m=inv_m, c_inv_d=inv_d, c_ident=ident, c_ltri=ltri, c_iota=iota, c_rcp=rcp)


def make_in_maps(inputs, ncores=8):
    f = lambda a: np.ascontiguousarray(np.asarray(a))
    shared = dict(
        ada_w=f(inputs["ada_w"]), ada_b=f(inputs["ada_b"]),
        ln1_g=f(inputs["ln1_g"]), ln1_b=f(inputs["ln1_b"]), ln2_g=f(inputs["ln2_g"]), ln2_b=f(inputs["ln2_b"]),
        mla_w_in=f(inputs["mla_w_in"][0]), mla_q_norm=f(inputs["mla_q_norm"][0]), mla_kv_norm=f(inputs["mla_kv_norm"][0]),
        mla_w_uq=f(inputs["mla_w_uq"][0]), mla_w_ukv=f(inputs["mla_w_ukv"][0]), mla_w_o=f(inputs["mla_w_o"][0]),
        diff_w_in=f(inputs["diff_w_in"][0]), diff_lambda=f(inputs["diff_lambda"][0]).reshape(1, 256),
        diff_subln=f(inputs["diff_subln"][0]).reshape(128, 1), diff_w_o=f(inputs["diff_w_o"][0]),
        router_w=f(inputs["router_w"]), moe_w_in=f(inputs["moe_w_in"]), moe_w_out=f(inputs["moe_w_out"]),
    )
    shared.update(_consts())
    maps = []
    for c in range(ncores):
        b = c % 4
        m = dict(shared)
        m["x"] = f(inputs["x"][b])
        m["cT"] = f(np.asarray(inputs["c"][b]).reshape(8, 128).T)
        m["pos"] = f(np.asarray(inputs["positions"][b]).reshape(1, S_LEN).astype(np.int32))
        maps.append(m)
    return maps


def kernel(**inputs):
    nc = build()
    maps = make_in_maps(inputs, 8)
    res = run_bass_kernel_spmd(nc, maps, core_ids=list(range(8)))
    return np.stack([np.asarray(res.results[b]["out"], dtype=np.float32) for b in range(4)], axis=0)
```

```python
import math
from contextlib import ExitStack

import numpy as np
import ml_dtypes
import concourse.bass as bass
import concourse.mybir as mybir
from concourse.bass_utils import run_bass_kernel_spmd

F32 = mybir.dt.float32
BF16 = mybir.dt.bfloat16
I32 = mybir.dt.int32
ALU = mybir.AluOpType
AF = mybir.ActivationFunctionType

S_LEN = 4096
D = 1024
NBLK = S_LEN // 128
NE = 16
CAP = 512
FF = 2048
ALPHA = 4 ** 0.25
LAMBDA_INIT1 = 0.8 - 0.6 * math.exp(-0.3)
PI = math.pi
DSPLIT = 768


class Sched:
    def __init__(self, nc, es):
        self.nc = nc
        self.es = es
        self.engs = {"pe": nc.tensor, "act": nc.scalar, "dve": nc.vector, "pool": nc.gpsimd, "sp": nc.sync}
        self.prog = {}
        for e in self.engs:
            self.prog[e] = [es.enter_context(nc.semaphore("prog_" + e)), 0]
        self.known = {e: {} for e in self.engs}
        self.lastw = {}
        self.readers = {}
        self.dsem = {}
        self.free_sems = []
        self.nsem = 0

    def _wait(self, e, tok):
        if tok is None:
            return
        sem, val = tok
        k = self.known[e]
        if k.get(sem.num, 0) >= val:
            return
        self.engs[e].wait_ge(sem, val)
        k[sem.num] = val

    def deps(self, e, reads, writes):
        for r in reads:
            self._wait(e, self.lastw.get(r))
        for w in writes:
            self._wait(e, self.lastw.get(w))
            for tok in list(self.readers.get(w, {}).values()):
                self._wait(e, tok)

    def commit(self, tok, reads, writes):
        for r in reads:
            self.readers.setdefault(r, {})[tok[0].num] = tok
        for w in writes:
            self.lastw[w] = tok
            self.readers[w] = {}

    def op(self, e, fn, reads=(), writes=(), cwrites=None):
        return self.ops(e, [fn], reads, writes, cwrites)

    def ops(self, e, fns, reads=(), writes=(), cwrites=None):
        self.deps(e, reads, writes)
        ins = None
        for fn in fns:
            ins = fn()
        p = self.prog[e]
        p[1] += 1
        ins.then_inc(p[0], 1)
        tok = (p[0], p[1])
        self.commit(tok, reads, writes if cwrites is None else cwrites)
        return tok

    def dma(self, q, key, fn, reads=(), writes=()):
        self.deps(q, reads, writes)
        ins = fn()
        if key not in self.dsem:
            if self.free_sems:
                self.dsem[key] = self.free_sems.pop()
            else:
                self.dsem[key] = [self.es.enter_context(self.nc.semaphore("d%d" % self.nsem)), 0]
                self.nsem += 1
        s = self.dsem[key]
        s[1] += 16
        ins.then_inc(s[0], 16)
        tok = (s[0], s[1])
        self.commit(tok, reads, writes)
        return tok

    def _all(self):
        toks = [(p[0], p[1]) for p in self.prog.values() if p[1] > 0]
        toks += [(s[0], s[1]) for s in self.dsem.values() if s[1] > 0]
        return toks

    def barrier(self):
        toks = self._all()
        for e in self.engs:
            for t in toks:
                self._wait(e, t)
        self.lastw = {}
        self.readers = {}
        self.free_sems.extend(self.dsem.values())
        self.dsem = {}

    def finish(self, e="sp"):
        for t in self._all():
            self._wait(e, t)


def build(upto=99, dbg=None):
    nc = bass.Bass("TRN2", target_bir_lowering=False)

    def din(name, shape, dt=F32):
        return nc.dram_tensor(name, list(shape), dt, kind="ExternalInput").ap()

    def dscr(name, shape, dt):
        return nc.dram_tensor(name, list(shape), dt, kind="Internal").ap()

    x_in = din("x", [S_LEN, D])
    cT = din("cT", [128, 8])
    pos = din("pos", [1, S_LEN], I32)
    ada_w = din("ada_w", [2, D, 6 * D])
    ada_b = din("ada_b", [2, 6 * D])
    ln1_g = din("ln1_g", [2, D]); ln1_b = din("ln1_b", [2, D])
    ln2_g = din("ln2_g", [2, D]); ln2_b = din("ln2_b", [2, D])
    mla_w_in = din("mla_w_in", [D, 704])
    mla_q_norm = din("mla_q_norm", [384]); mla_kv_norm = din("mla_kv_norm", [256])
    mla_w_uq = din("mla_w_uq", [384, 1536])
    mla_w_ukv = din("mla_w_ukv", [256, 2048])
    mla_w_o = din("mla_w_o", [D, D])
    diff_w_in = din("diff_w_in", [D, 3072])
    diff_lambda = din("diff_lambda", [1, 256])
    diff_subln = din("diff_subln", [128, 1])
    diff_w_o = din("diff_w_o", [D, D])
    router_w = din("router_w", [2, D, NE])
    moe_w_in = din("moe_w_in", [2, NE, D, 2 * FF])
    moe_w_out = din("moe_w_out", [2, NE, FF, D])
    c_inv_m = din("c_inv_m", [64, 1])
    c_inv_d = din("c_inv_d", [128, 1])
    c_ident = din("c_ident", [128, 128])
    c_ltri = din("c_ltri", [128, 128])
    c_iota = din("c_iota", [128, CAP])
    c_rcp = din("c_rcp", [128, NBLK, 2])

    out = nc.dram_tensor("out", [S_LEN, D], F32, kind="ExternalOutput").ap()

    MODS = dscr("MODS", [2, 6 * D], F32)
    QT = dscr("QT", [8, 192, S_LEN], BF16)
    KT = dscr("KT", [8, 128, S_LEN], BF16)
    KPE = dscr("KPE", [64, S_LEN], BF16)
    VV = dscr("VV", [S_LEN, D], BF16)
    OT = dscr("OT", [D, S_LEN], BF16)
    XMID = dscr("XMID", [S_LEN, D], F32)
    H2 = dscr("H2", [S_LEN, D], BF16)
    FACC = dscr("FACC", [S_LEN, D], F32)
    X1 = dscr("X1", [S_LEN, D], F32)

    es = ExitStack()
    with es:
        S = Sched(nc, es)
        uid = [0]

        def sb(stack, shape, dt, name=None):
            uid[0] += 1
            return stack.enter_context(nc.sbuf_tensor("%s_%d" % (name or "t", uid[0]), list(shape), dt))

        dbanks = [es.enter_context(nc.psum_tensor("dbank%d" % i, [128, 1024], F32)) for i in range(4)]
        banks = [dbanks[i // 2][:, (i % 2) * 512:(i % 2 + 1) * 512] for i in range(8)]
        ident_f = sb(es, [128, 128], F32, "identf")
        ident_b = sb(es, [128, 128], BF16, "identb")
        ones_b = sb(es, [128, 128], BF16, "onesb")
        aff = sb(es, [128, NBLK, NE], F32, "aff")
        eps5 = sb(es, [128, 1], F32, "eps5")
        S.dma("sp", "c0", lambda: nc.sync.dma_start(out=ident_f[:], in_=c_ident), writes=["identf"])
        S.op("dve", lambda: nc.vector.tensor_copy(out=ident_b[:], in_=ident_f[:]), reads=["identf"], writes=["identb"])
        S.op("dve", lambda: nc.vector.memset(ones_b[:], 1.0), writes=["onesb"])
        S.op("dve", lambda: nc.vector.memset(eps5[:], 1e-5), writes=["eps5"])

        V = nc.vector
        A = nc.scalar
        PE = nc.tensor
        G = nc.gpsimd

        def bcast_load(stack, src_row_ap, name, plus_one=False, q="sp"):
            n = src_row_ap.shape[-1]
            t = sb(stack, [128, n], F32, name)
            key = "%s_%d" % (name, uid[0])
            S.dma(q, "bc_" + key, lambda: S.engs[q].dma_start(out=t[:], in_=src_row_ap.partition_broadcast(128)), writes=[key])
            if plus_one:
                S.op("pool", lambda: G.tensor_scalar(out=t[:], in0=t[:], scalar1=1.0, scalar2=None, op0=ALU.add), reads=[key], writes=[key])
            return t, key

        def phase_mods():
            with ExitStack() as ph:
                c_sb = sb(ph, [128, 8], F32, "c")
                c16 = sb(ph, [128, 8], BF16, "c16")
                adab = sb(ph, [1, 6 * D], F32, "adab")
                modrow = sb(ph, [1, 6 * D], F32, "modrow")
                wblk = [sb(ph, [128, 8, 512], BF16, "adaw") for _ in range(3)]
                S.dma("sp", "m_c", lambda: nc.sync.dma_start(out=c_sb[:], in_=cT), writes=["c"])
                S.op("act", lambda: A.activation(out=c16[:], in_=c_sb[:], func=AF.Silu), reads=["c"], writes=["c16"])
                it = 0
                for l in range(2):
                    S.dma("sp", "m_b", lambda: nc.sync.dma_start(out=adab[:], in_=ada_b[l:l + 1, :]), writes=["adab"])
                    wv = ada_w[l].rearrange("(k p) n -> p k n", p=128)
                    for j in range(12):
                        sl = it % 3
                        S.dma("pool", "m_w%d" % sl, lambda: G.dma_start(out=wblk[sl][:], in_=wv[:, :, j * 512:(j + 1) * 512]), writes=["adaw%d" % sl])
                        pb = banks[it % 2]
                        S.ops("pe", [(lambda k=k: PE.matmul(pb[0:1, :], lhsT=c16[:, k:k + 1], rhs=wblk[sl][:, k, :], start=(k == 0), stop=(k == 7))) for k in range(8)],
                              reads=["c16", "adaw%d" % sl], writes=["bank%d" % (it % 2)])
                        S.op("dve", lambda: V.tensor_tensor(out=modrow[0:1, j * 512:(j + 1) * 512], in0=pb[0:1, :], in1=adab[0:1, j * 512:(j + 1) * 512], op=ALU.add),
                             reads=["bank%d" % (it % 2), "adab"], writes=["modrow"])
                        it += 1
                    S.dma("sp", "m_o", lambda: nc.sync.dma_start(out=MODS[l:l + 1, :], in_=modrow[:]), reads=["modrow"], writes=["MODS"])
                S.barrier()

        def rope_tables(ph, R, inv_ap):
            Ct = sb(ph, [R, S_LEN], F32, "ropeC")
            St = sb(ph, [R, S_LEN], F32, "ropeS")
            inv = sb(ph, [R, 1], F32, "inv")
            negpi = sb(ph, [R, 1], F32, "negpi")
            S.dma("sp", "r_inv", lambda: nc.sync.dma_start(out=inv[:], in_=inv_ap), writes=["inv"])
            with ExitStack() as tmp:
                pi_t = sb(tmp, [R, 1024], I32, "posi")
                ang = sb(tmp, [R, 1024], F32, "ang")
                nf = sb(tmp, [R, 1024], F32, "nf")
                ni = sb(tmp, [R, 1024], I32, "ni")
                msk = sb(tmp, [R, 1024], F32, "msk")
                for cch in range(4):
                    cs = slice(cch * 1024, (cch + 1) * 1024)
                    S.dma("sp", "r_pos", lambda: nc.sync.dma_start(out=pi_t[:], in_=pos[:, cs].partition_broadcast(R)), writes=["posi"])
                    S.op("dve", lambda: V.tensor_copy(out=ang[:], in_=pi_t[:]), reads=["posi"], writes=["ang"])
                    S.op("dve", lambda: V.tensor_scalar(out=ang[:], in0=ang[:], scalar1=inv[:, 0:1], scalar2=None, op0=ALU.mult), reads=["ang", "inv"], writes=["ang"])
                    S.op("dve", lambda: V.tensor_scalar(out=nf[:], in0=ang[:], scalar1=1.0 / (2 * PI), scalar2=None, op0=ALU.mult), reads=["ang"], writes=["nf"])
                    S.op("dve", lambda: V.tensor_copy(out=ni[:], in_=nf[:]), reads=["nf"], writes=["ni"])
                    S.op("dve", lambda: V.tensor_copy(out=nf[:], in_=ni[:]), reads=["ni"], writes=["nf"])
                    S.op("dve", lambda: V.scalar_tensor_tensor(out=ang[:], in0=nf[:], scalar=-6.28125, in1=ang[:], op0=ALU.mult, op1=ALU.add), reads=["nf", "ang"], writes=["ang"])
                    S.op("dve", lambda: V.scalar_tensor_tensor(out=ang[:], in0=nf[:], scalar=-0.0019353071795864769, in1=ang[:], op0=ALU.mult, op1=ALU.add), reads=["nf", "ang"], writes=["ang"])

                    def wrap():
                        S.op("dve", lambda: V.tensor_scalar(out=msk[:], in0=ang[:], scalar1=PI, scalar2=-2 * PI, op0=ALU.is_gt, op1=ALU.mult), reads=["ang"], writes=["msk"])
                        S.op("dve", lambda: V.tensor_tensor(out=ang[:], in0=ang[:], in1=msk[:], op=ALU.add), reads=["ang", "msk"], writes=["ang"])
                        S.op("dve", lambda: V.tensor_scalar(out=msk[:], in0=ang[:], scalar1=-PI, scalar2=2 * PI, op0=ALU.is_lt, op1=ALU.mult), reads=["ang"], writes=["msk"])
                        S.op("dve", lambda: V.tensor_tensor(out=ang[:], in0=ang[:], in1=msk[:], op=ALU.add), reads=["ang", "msk"], writes=["ang"])
                        S.op("dve", lambda: V.tensor_scalar(out=ang[:], in0=ang[:], scalar1=PI, scalar2=-PI, op0=ALU.min, op1=ALU.max), reads=["ang"], writes=["ang"])
                    wrap()
                    S.op("act", lambda: A.activation(out=St[:, cs], in_=ang[:], func=AF.Sin), reads=["ang"], writes=["ropeS"])
                    S.op("dve", lambda: V.tensor_scalar(out=ang[:], in0=ang[:], scalar1=PI / 2, scalar2=None, op0=ALU.add), reads=["ang"], writes=["ang"])
                    wrap()
                    S.op("act", lambda: A.activation(out=Ct[:, cs], in_=ang[:], func=AF.Sin), reads=["ang"], writes=["ropeC"])
                S.barrier()
            return Ct, St

        def front_block(tb, xsrc, xin, h16, hT, sc1p, sh1p, kx):
            h16s = h16
            sl = tb % 2
            h16 = h16[sl % len(h16)]
            hT = hT[sl]
            hk, tk = "h16_%d" % (sl % len(h16s)), "hT%d" % sl
            xv = xsrc.rearrange("(t s p) d -> t p s d", s=4, p=128)
            S.dma("sp", "xin%d" % sl, lambda: nc.sync.dma_start(out=xin[sl][:], in_=xv[tb]), writes=["xin%d" % sl])
            S.op("dve", lambda: V.tensor_tensor(out=xin[sl][:], in0=xin[sl][:], in1=sc1p[:].unsqueeze(1).broadcast_to([128, 4, D]), op=ALU.mult),
                 reads=["xin%d" % sl, kx[0]], writes=["xin%d" % sl])
            S.op("pool", lambda: G.tensor_tensor(out=h16[:], in0=xin[sl][:], in1=sh1p[:].unsqueeze(1).broadcast_to([128, 4, D]), op=ALU.add),
                 reads=["xin%d" % sl, kx[1]], writes=[hk])
            for kp in range(4):
                bk = banks[6 + kp % 2]
                bkey = "bank%d" % (6 + kp % 2)
                tv = bk[:].bitcast(BF16)
                fns = []
                for kk in range(2):
                    k = kp * 2 + kk
                    for s in range(4):
                        fns.append(lambda k=k, kk=kk, s=s: PE.transpose(tv[:, kk * 512 + s * 128: kk * 512 + (s + 1) * 128], h16[:, s, k * 128:(k + 1) * 128], ident_b[:]))
                S.ops("pe", fns, reads=[hk, "identb"], writes=[bkey])
                S.op("act", lambda: A.copy(out=hT[:, kp * 2:kp * 2 + 2, :].rearrange("p a b -> p (a b)"), in_=tv), reads=[bkey], writes=[tk])

        pring = [0]

        def next_bank(lo=0, hi=6):
            b = lo + pring[0] % (hi - lo)
            pring[0] += 1
            return banks[b], "bank%d" % b

        def rsqrt_into(dst, dkey, src_ap, skey, mul, add):
            S.op("dve", lambda: V.tensor_scalar(out=dst, in0=src_ap, scalar1=mul, scalar2=add, op0=ALU.mult, op1=ALU.add), reads=[skey], writes=[dkey])
            S.op("act", lambda: A.activation(out=dst, in_=dst, func=AF.Sqrt), reads=[dkey], writes=[dkey])
            S.op("dve", lambda: V.reciprocal(out=dst, in_=dst), reads=[dkey], writes=[dkey])

        def phase_a_mla(xsrc, l):
            with ExitStack() as ph:
                Ct, St = rope_tables(ph, 64, c_inv_m)
                sc1p, k_sc = bcast_load(ph, MODS[l:l + 1, D:2 * D], "sc1p", plus_one=True)
                sh1p, k_sh = bcast_load(ph, MODS[l:l + 1, 0:D], "sh1p")
                w_in = sb(ph, [128, 8, 768], BF16, "w_in")
                w_uq = sb(ph, [128, 3, 8, 256], BF16, "w_uq")
                w_k = sb(ph, [128, 2, 8, 128], BF16, "w_k")
                w_v = sb(ph, [128, 2, 8, 128], BF16, "w_v")
                qn = sb(ph, [128, 3], F32, "qn")
                kvn = sb(ph, [128, 2], F32, "kvn")
                S.dma("pool", "a_w0", lambda: G.dma_start(out=w_in[:, :, 0:704], in_=mla_w_in.rearrange("(k p) n -> p k n", p=128)), writes=["w_in"])
                S.op("act", lambda: A.mul(out=w_in[:, :, 704:736], in_=w_in[:, :, 672:704], mul=-1.0), reads=["w_in"], writes=["w_in"])
                S.op("act", lambda: A.copy(out=w_in[:, :, 736:768], in_=w_in[:, :, 640:672]), reads=["w_in"], writes=["w_in"])
                uqv = mla_w_uq.rearrange("(k p) (h e) -> p k h e", p=128, e=192)
                for k in range(3):
                    S.dma("pool", "a_w1", lambda: G.dma_start(out=w_uq[:, k, :, 0:192], in_=uqv[:, k, :, :]), writes=["w_uq"])
                S.op("act", lambda: A.mul(out=w_uq[:, :, :, 192:224], in_=w_uq[:, :, :, 160:192], mul=-1.0), reads=["w_uq"], writes=["w_uq"])
                S.op("act", lambda: A.copy(out=w_uq[:, :, :, 224:256], in_=w_uq[:, :, :, 128:160]), reads=["w_uq"], writes=["w_uq"])
                wkv = mla_w_ukv.rearrange("(k p) (h two e) -> p k h two e", p=128, two=2, e=128)
                for k in range(2):
                    S.dma("pool", "a_w2", lambda: G.dma_start(out=w_k[:, k, :, :], in_=wkv[:, k, :, 0, :]), writes=["w_k"])
                    S.dma("pool", "a_w3", lambda: G.dma_start(out=w_v[:, k, :, :], in_=wkv[:, k, :, 1, :]), writes=["w_v"])
                with nc.allow_non_contiguous_dma(reason="tiny norm-gain vectors"):
                    S.dma("sp", "a_n0", lambda: nc.sync.dma_start(out=qn[:], in_=mla_q_norm.rearrange("(k p) -> p k", p=128)), writes=["qn"])
                    S.dma("sp", "a_n1", lambda: nc.sync.dma_start(out=kvn[:], in_=mla_kv_norm.rearrange("(k p) -> p k", p=128)), writes=["kvn"])
                xin = [sb(ph, [128, 4, D], F32, "xin") for _ in range(2)]
                h16 = [sb(ph, [128, 4, D], BF16, "h16")]
                hT = [sb(ph, [128, 8, 512], BF16, "hT") for _ in range(2)]
                lat = sb(ph, [128, 7, 512], F32, "lat")
                sq = sb(ph, [128, 5, 512], BF16, "sq")
                rstd = sb(ph, [128, 2, 512], F32, "rstd")
                cqn = sb(ph, [128, 3, 512], BF16, "cqn")
                ckvn = sb(ph, [128, 2, 512], BF16, "ckvn")
                kpe = sb(ph, [64, 512], BF16, "kpe")
                tmp1 = sb(ph, [64, 512], F32, "tmp1")
                tmp2 = sb(ph, [64, 512], F32, "tmp2")
                qst = sb(ph, [128, 8, 512], BF16, "qst")
                qrst = sb(ph, [64, 8, 512], BF16, "qrst")
                kst = sb(ph, [128, 8, 512], BF16, "kst")
                vst = sb(ph, [128, 4, D], BF16, "vst")
                mspec = [(0, 128, 128), (1, 256, 128), (2, 384, 128), (3, 512, 128), (4, 640, 128), (5, 704, 64), (6, 768, 64)]
                front_block(0, xsrc, xin, h16, hT, sc1p, sh1p, (k_sc, k_sh))
                for tb in range(8):
                    ts = slice(tb * 512, (tb + 1) * 512)
                    if tb + 1 < 8:
                        front_block(tb + 1, xsrc, xin, h16, hT, sc1p, sh1p, (k_sc, k_sh))
                    hTc, hTk = hT[tb % 2], "hT%d" % (tb % 2)
                    for (mi, hi_, mm) in mspec:
                        pb, pk = next_bank()
                        S.ops("pe", [(lambda k=k: PE.matmul(pb[0:mm, :], lhsT=w_in[:, k, hi_ - mm:hi_], rhs=hTc[:, k, :], start=(k == 0), stop=(k == 7))) for k in range(8)],
                              reads=["w_in", hTk], writes=[pk])
                        S.op("act", lambda: A.copy(out=lat[0:mm, mi, :], in_=pb[0:mm, :]), reads=[pk], writes=["lat%d" % mi])
                        if mi < 5:
                            S.op("dve", lambda: V.tensor_tensor(out=sq[:, mi, :], in0=lat[:, mi, :], in1=lat[:, mi, :], op=ALU.mult), reads=["lat%d" % mi], writes=["sq%d" % mi])
                    for gi, (c0, c1, n) in enumerate([(0, 3, 384), (3, 5, 256)]):
                        pb, pk = next_bank()
                        S.ops("pe", [(lambda c=c: PE.matmul(pb[:, :], lhsT=ones_b[:], rhs=sq[:, c, :], start=(c == c0), stop=(c == c1 - 1))) for c in range(c0, c1)],
                              reads=["onesb"] + ["sq%d" % c for c in range(c0, c1)], writes=[pk])
                        rsqrt_into(rstd[:, gi, :], "rstd%d" % gi, pb[:, :], pk, 1.0 / n, 1e-6)
                    for c in range(3):
                        S.op("dve", lambda: V.scalar_tensor_tensor(out=cqn[:, c, :], in0=lat[:, c, :], scalar=qn[:, c:c + 1], in1=rstd[:, 0, :], op0=ALU.mult, op1=ALU.mult),
                             reads=["lat%d" % c, "qn", "rstd0"], writes=["cqn"])
                    for c in range(2):
                        S.op("dve", lambda: V.scalar_tensor_tensor(out=ckvn[:, c, :], in0=lat[:, 3 + c, :], scalar=kvn[:, c:c + 1], in1=rstd[:, 1, :], op0=ALU.mult, op1=ALU.mult),
                             reads=["lat%d" % (3 + c), "kvn", "rstd1"], writes=["ckvn"])
                    S.op("dve", lambda: V.tensor_tensor(out=tmp1[:], in0=lat[0:64, 5, :], in1=Ct[:, ts], op=ALU.mult), reads=["lat5"], writes=["tmp1"])
                    S.op("pool", lambda: G.tensor_tensor(out=tmp2[:], in0=lat[0:64, 6, :], in1=St[:, ts], op=ALU.mult), reads=["lat6"], writes=["tmp2"])
                    S.op("dve", lambda: V.tensor_tensor(out=kpe[:], in0=tmp1[:], in1=tmp2[:], op=ALU.add), reads=["tmp1", "tmp2"], writes=["kpe"])
                    S.dma("sp", "a_kpe", lambda: nc.sync.dma_start(out=KPE[:, ts], in_=kpe[:]), reads=["kpe"], writes=["KPE"])
                    for h in range(8):
                        pb, pk = next_bank()
                        S.ops("pe", [(lambda c=c: PE.matmul(pb[:, :], lhsT=w_uq[:, c, h, 0:128], rhs=cqn[:, c, :], start=(c == 0), stop=(c == 2))) for c in range(3)],
                              reads=["w_uq", "cqn"], writes=[pk])
                        S.op("act", lambda: A.copy(out=qst[:, h, :], in_=pb[:, :]), reads=[pk], writes=["qst"])
                        pa, pka = next_bank()
                        S.ops("pe", [(lambda c=c: PE.matmul(pa[0:64, :], lhsT=w_uq[:, c, h, 128:192], rhs=cqn[:, c, :], start=(c == 0), stop=(c == 2))) for c in range(3)],
                              reads=["w_uq", "cqn"], writes=[pka])
                        pr, pkr = next_bank()
                        S.ops("pe", [(lambda c=c: PE.matmul(pr[0:64, :], lhsT=w_uq[:, c, h, 192:256], rhs=cqn[:, c, :], start=(c == 0), stop=(c == 2))) for c in range(3)],
                              reads=["w_uq", "cqn"], writes=[pkr])
                        S.op("dve", lambda: V.tensor_tensor(out=tmp1[:], in0=pa[0:64, :], in1=Ct[:, ts], op=ALU.mult), reads=[pka], writes=["tmp1"])
                        S.op("dve", lambda: V.tensor_tensor(out=tmp2[:], in0=pr[0:64, :], in1=St[:, ts], op=ALU.mult), reads=[pkr], writes=["tmp2"])
                        S.op("pool", lambda: G.tensor_tensor(out=qrst[:, h, :], in0=tmp1[:], in1=tmp2[:], op=ALU.add), reads=["tmp1", "tmp2"], writes=["qrst"])
                        pk_, pkk = next_bank()
                        S.ops("pe", [(lambda c=c: PE.matmul(pk_[:, :], lhsT=w_k[:, c, h, :], rhs=ckvn[:, c, :], start=(c == 0), stop=(c == 1))) for c in range(2)],
                              reads=["w_k", "ckvn"], writes=[pkk])
                        S.op("act", lambda: A.copy(out=kst[:, h, :], in_=pk_[:, :]), reads=[pkk], writes=["kst"])
                    S.dma("sp", "a_q", lambda: nc.sync.dma_start(out=QT[:, 0:128, ts].rearrange("h p t -> p h t"), in_=qst[:]), reads=["qst"], writes=["QT"])
                    S.dma("sp", "a_qr", lambda: nc.sync.dma_start(out=QT[:, 128:192, ts].rearrange("h p t -> p h t"), in_=qrst[:]), reads=["qrst"], writes=["QT"])
                    S.dma("sp", "a_k", lambda: nc.sync.dma_start(out=KT[:, :, ts].rearrange("h p t -> p h t"), in_=kst[:]), reads=["kst"], writes=["KT"])
                    for s in range(4):
                        for hf in range(2):
                            pb, pk = next_bank()
                            S.ops("pe", [(lambda c=c: PE.matmul(pb[:, :], lhsT=ckvn[:, c, s * 128:(s + 1) * 128], rhs=w_v[:, c, hf * 4:(hf + 1) * 4, :].rearrange("p h e -> p (h e)"), start=(c == 0), stop=(c == 1))) for c in range(2)],
                                  reads=["w_v", "ckvn"], writes=[pk])
                            S.op("act" if hf else "dve", (lambda pb=pb, hf=hf: (A.copy if hf else V.tensor_copy)(out=vst[:, s, hf * 512:(hf + 1) * 512], in_=pb[:, :])), reads=[pk], writes=["vst"])
                    S.dma("sp", "a_v", lambda: nc.sync.dma_start(out=VV.rearrange("(t s p) d -> t p s d", s=4, p=128)[tb], in_=vst[:]), reads=["vst"], writes=["VV"])
                S.barrier()

        def phase_a_diff(xsrc, l):
            with ExitStack() as ph:
                Ct, St = rope_tables(ph, 128, c_inv_d)
                sc1p, k_sc = bcast_load(ph, MODS[l:l + 1, D:2 * D], "sc1p", plus_one=True)
                sh1p, k_sh = bcast_load(ph, MODS[l:l + 1, 0:D], "sh1p")
                w = sb(ph, [128, 8, 3072], BF16, "dw")
                wr = sb(ph, [128, 8, 2048], BF16, "dwr")
                wsrc = diff_w_in.rearrange("(k p) n -> p k n", p=128)
                for j in range(3):
                    S.dma("pool", "d_w%d" % j, lambda: G.dma_start(out=w[:, :, j * 1024:(j + 1) * 1024], in_=wsrc[:, :, j * 1024:(j + 1) * 1024]), writes=["dw"])
                S.op("pool", lambda: G.memset(wr[:], 0.0), writes=["dwr"])
                w4 = w[:, :, 0:2048].rearrange("p k (g e) -> p k g e", e=64)
                wr4 = wr[:].rearrange("p k (g e) -> p k g e", e=64)
                for k in range(8):
                    S.op("act", lambda: A.mul(out=wr4[:, k, :, 0:8], in_=w4[:, k, :, 8:16], mul=-1.0), reads=["dw", "dwr"], writes=["dwr"])
                    S.op("act", lambda: A.copy(out=wr4[:, k, :, 8:16], in_=w4[:, k, :, 0:8]), reads=["dw", "dwr"], writes=["dwr"])
                xin = [sb(ph, [128, 4, D], F32, "xin") for _ in range(2)]
                h16 = [sb(ph, [128, 4, D], BF16, "h16")]
                hT = [sb(ph, [128, 8, 512], BF16, "hT") for _ in range(2)]
                tmp1 = sb(ph, [128, 512], F32, "tmp1")
                tmp2 = sb(ph, [128, 512], F32, "tmp2")
                qst = sb(ph, [128, 8, 512], BF16, "qst")
                kst = sb(ph, [128, 8, 512], BF16, "kst")
                vst = sb(ph, [128, 4, D], BF16, "vst")
                front_block(0, xsrc, xin, h16, hT, sc1p, sh1p, (k_sc, k_sh))
                for tb in range(8):
                    ts = slice(tb * 512, (tb + 1) * 512)
                    if tb + 1 < 8:
                        front_block(tb + 1, xsrc, xin, h16, hT, sc1p, sh1p, (k_sc, k_sh))
                    hTc, hTk = hT[tb % 2], "hT%d" % (tb % 2)
                    for qk in range(2):
                        st = qst if qk == 0 else kst
                        skey = "qst" if qk == 0 else "kst"
                        for h in range(8):
                            c0 = qk * 1024 + h * 128
                            pa, pka = next_bank()
                            S.ops("pe", [(lambda k=k: PE.matmul(pa[:, :], lhsT=w[:, k, c0:c0 + 128], rhs=hTc[:, k, :], start=(k == 0), stop=(k == 7))) for k in range(8)],
                                  reads=["dw", hTk], writes=[pka])
                            pr, pkr = next_bank()
                            S.ops("pe", [(lambda k=k: PE.matmul(pr[:, :], lhsT=wr[:, k, c0:c0 + 128], rhs=hTc[:, k, :], start=(k == 0), stop=(k == 7))) for k in range(8)],
                                  reads=["dwr", hTk], writes=[pkr])
                            S.op("dve", lambda: V.tensor_tensor(out=tmp1[:], in0=pa[:, :], in1=Ct[:, ts], op=ALU.mult), reads=[pka], writes=["tmp1"])
                            S.op("dve", lambda: V.tensor_tensor(out=tmp2[:], in0=pr[:, :], in1=St[:, ts], op=ALU.mult), reads=[pkr], writes=["tmp2"])
                            S.op("pool", lambda: G.tensor_tensor(out=st[:, h, :], in0=tmp1[:], in1=tmp2[:], op=ALU.add), reads=["tmp1", "tmp2"], writes=[skey])
                    S.dma("sp", "a_q", lambda: nc.sync.dma_start(out=QT[:, 0:128, ts].rearrange("h p t -> p h t"), in_=qst[:]), reads=["qst"], writes=["QT"])
                    S.dma("sp", "a_k", lambda: nc.sync.dma_start(out=KT[:, :, ts].rearrange("h p t -> p h t"), in_=kst[:]), reads=["kst"], writes=["KT"])
                    for s in range(4):
                        for hf in range(2):
                            pb, pk = next_bank()
                            S.ops("pe", [(lambda k=k: PE.matmul(pb[:, :], lhsT=hTc[:, k, s * 128:(s + 1) * 128], rhs=w[:, k, 2048 + hf * 512:2048 + (hf + 1) * 512], start=(k == 0), stop=(k == 7))) for k in range(8)],
                                  reads=["dw", hTk], writes=[pk])
                            S.op("act", lambda: A.copy(out=vst[:, s, hf * 512:(hf + 1) * 512], in_=pb[:, :]), reads=[pk], writes=["vst"])
                    S.dma("sp", "a_v", lambda: nc.sync.dma_start(out=VV.rearrange("(t s p) d -> t p s d", s=4, p=128)[tb], in_=vst[:]), reads=["vst"], writes=["VV"])
                S.barrier()

        def phase_b(kind):
            mla = kind == "mla"
            nmap = 1 if mla else 2
            scale = (192 ** -0.5) if mla else (64 ** -0.5)
            with ExitStack() as ph:
                kbuf = [sb(ph, [128, S_LEN], BF16, "kbuf") for _ in range(2)]
                qbuf = [sb(ph, [128, S_LEN], BF16, "qbuf") for _ in range(2)]
                vbuf = [sb(ph, [128, NBLK, 128], BF16, "vbuf") for _ in range(2)]
                NPT = 3
                PT = [sb(ph, [128, 1024], BF16, "PT") for _ in range(NPT)]
                dacc = [sb(ph, [128, 1024], F32, "dacc") for _ in range(2)]
                ones_f = sb(ph, [128, 128], F32, "onesf")
                S.op("dve", lambda: V.memset(ones_f[:], 1.0), writes=["onesf"])
                rden = [sb(ph, [128, 512], F32, "rden") for _ in range(2)]
                osb = [sb(ph, [128, 512], BF16, "osb") for _ in range(2)]
                if mla:
                    qrbuf = [sb(ph, [64, S_LEN], BF16, "qrbuf") for _ in range(2)]
                    kpe = sb(ph, [64, S_LEN], BF16, "kpeall")
                    S.dma("sp", "b_kpe", lambda: nc.sync.dma_start(out=kpe[:], in_=KPE), writes=["kpeall"])
                else:
                    lamt = sb(ph, [128, 256], F32, "lamt")
                    lp = sb(ph, [128, 128], F32, "lp")
                    ls = sb(ph, [128, 2], F32, "ls")
                    neglam = sb(ph, [128, 1], F32, "neglam")
                    subs = sb(ph, [128, 1], F32, "subs")
                    o1n = sb(ph, [128, 512], F32, "o1n")
                    o2n = sb(ph, [128, 512], F32, "o2n")
                    sq = sb(ph, [128, 512], BF16, "osq")
                    rstd = sb(ph, [128, 512], F32, "orstd")
                    S.dma("sp", "b_lam", lambda: nc.sync.dma_start(out=lamt[:], in_=diff_lambda.partition_broadcast(128)), writes=["lamt"])
                    S.dma("sp", "b_sub", lambda: nc.sync.dma_start(out=subs[:], in_=diff_subln), writes=["subs"])
                    S.op("dve", lambda: V.tensor_tensor(out=lp[:, 0:64], in0=lamt[:, 0:64], in1=lamt[:, 64:128], op=ALU.mult), reads=["lamt"], writes=["lp"])
                    S.op("dve", lambda: V.tensor_tensor(out=lp[:, 64:128], in0=lamt[:, 128:192], in1=lamt[:, 192:256], op=ALU.mult), reads=["lamt", "lp"], writes=["lp"])
                    S.op("dve", lambda: V.reduce_sum(out=ls[:, 0:1], in_=lp[:, 0:64], axis=mybir.AxisListType.X), reads=["lp"], writes=["ls"])
                    S.op("dve", lambda: V.reduce_sum(out=ls[:, 1:2], in_=lp[:, 64:128], axis=mybir.AxisListType.X), reads=["lp", "ls"], writes=["ls"])
                    S.op("act", lambda: A.activation(out=ls[:], in_=ls[:], func=AF.Exp), reads=["ls"], writes=["ls"])
                    S.op("dve", lambda: V.tensor_tensor(out=neglam[:], in0=ls[:, 1:2], in1=ls[:, 0:1], op=ALU.subtract), reads=["ls"], writes=["neglam"])
                    S.op("dve", lambda: V.tensor_scalar(out=neglam[:], in0=neglam[:], scalar1=-LAMBDA_INIT1, scalar2=None, op0=ALU.add), reads=["neglam"], writes=["neglam"])
                    S.op("dve", lambda: V.tensor_scalar(out=subs[:], in0=subs[:], scalar1=1.0 - LAMBDA_INIT1, scalar2=None, op0=ALU.mult), reads=["subs"], writes=["subs"])

                vview = VV.rearrange("(c p) (h e) -> p c h e", p=128, e=128)

                def load_head(h):
                    sl = h % 2
                    S.dma("sp", "b_k%d" % sl, lambda: nc.sync.dma_start(out=kbuf[sl][:], in_=KT[h]), writes=["kbuf%d" % sl])
                    S.dma("sp", "b_q%d" % sl, lambda: nc.sync.dma_start(out=qbuf[sl][:], in_=QT[h, 0:128, :]), writes=["qbuf%d" % sl])
                    S.dma("sp", "b_v%d" % sl, lambda: nc.sync.dma_start(out=vbuf[sl][:], in_=vview[:, :, h, :]), writes=["vbuf%d" % sl])
                    if mla:
                        S.dma("sp", "b_qr%d" % sl, lambda: nc.sync.dma_start(out=qrbuf[sl][:], in_=QT[h, 128:192, :]), writes=["qrbuf%d" % sl])

                load_head(0)
                blk = 0
                for h in range(8):
                    if h + 1 < 8:
                        load_head(h + 1)
                    sl = h % 2
                    for qb in range(8):
                        qs = slice(qb * 512, (qb + 1) * 512)
                        par = blk % 2
                        blk += 1
                        if mla:
                            acc_o, ko = banks[4 + par], "bank%d" % (4 + par)
                            den, kd = banks[6 + par], "bank%d" % (6 + par)
                            NU = NBLK // 2

                            def qk(u):
                                db = dbanks[u % 2]
                                fns = []
                                for j in range(2):
                                    kc = 2 * u + j
                                    ks = slice(kc * 128, (kc + 1) * 128)
                                    fns.append(lambda j=j, ks=ks: PE.matmul(db[:, j * 512:(j + 1) * 512], lhsT=kbuf[sl][:, ks], rhs=qbuf[sl][:, qs], start=True, stop=False))
                                    fns.append(lambda j=j, ks=ks: PE.matmul(db[:, j * 512:(j + 1) * 512], lhsT=kpe[:, ks], rhs=qrbuf[sl][:, qs], start=False, stop=True))
                                S.ops("pe", fns, reads=["kbuf%d" % sl, "qbuf%d" % sl, "qrbuf%d" % sl, "kpeall"], writes=["bank%d" % (2 * (u % 2)), "bank%d" % (2 * (u % 2) + 1)])

                            def ex(u):
                                S.op("act", lambda: A.activation(out=PT[u % NPT][:], in_=dbanks[u % 2][:, :], func=AF.Exp, scale=scale),
                                     reads=["bank%d" % (2 * (u % 2)), "bank%d" % (2 * (u % 2) + 1)], writes=["PT%d" % (u % NPT)])

                            def pv(u):
                                fns = []
                                for j in range(2):
                                    kc = 2 * u + j
                                    fns.append(lambda j=j, kc=kc: PE.matmul(acc_o, lhsT=vbuf[sl][:, kc, :], rhs=PT[u % NPT][:, j * 512:(j + 1) * 512], start=(kc == 0), stop=(kc == NBLK - 1)))
                                S.ops("pe", fns, reads=["PT%d" % (u % NPT), "vbuf%d" % sl], writes=[ko] if u == 0 else [], cwrites=[ko] if u == NU - 1 else [])
                                for (en, EN, c0, c1) in (("dve", V, 0, DSPLIT), ("pool", G, DSPLIT, 1024)):
                                    if u == 0:
                                        S.op(en, lambda: EN.tensor_copy(out=dacc[par][:, c0:c1], in_=PT[u % NPT][:, c0:c1]), reads=["PT%d" % (u % NPT)], writes=["dacc%d%s" % (par, en)])
                                    else:
                                        S.op(en, lambda: EN.tensor_tensor(out=dacc[par][:, c0:c1], in0=dacc[par][:, c0:c1], in1=PT[u % NPT][:, c0:c1], op=ALU.add), reads=["PT%d" % (u % NPT), "dacc%d%s" % (par, en)], writes=["dacc%d%s" % (par, en)])
                            qk(0)
                            for u in range(NU):
                                ex(u)
                                if u + 1 < NU:
                                    qk(u + 1)
                                pv(u)
                            S.ops("pe", [lambda: PE.matmul(den, lhsT=ones_f[:], rhs=dacc[par][:, 0:512], start=True, stop=False),
                                         lambda: PE.matmul(den, lhsT=ones_f[:], rhs=dacc[par][:, 512:1024], start=False, stop=True)],
                                  reads=["onesf", "dacc%ddve" % par, "dacc%dpool" % par], writes=[kd])
                            S.op("act", lambda: A.activation(out=rden[par][:], in_=den, func=AF.Ln), reads=[kd], writes=["rden%d" % par])
                            S.op("act", lambda: A.activation(out=rden[par][:], in_=rden[par][:], func=AF.Exp, scale=-1.0), reads=["rden%d" % par], writes=["rden%d" % par])
                            S.op("dve", lambda: V.tensor_tensor(out=osb[par][:], in0=acc_o, in1=rden[par][:], op=ALU.mult), reads=[ko, "rden%d" % par], writes=["osb%d" % par])
                        else:
                            accs = [(banks[4 + i], "bank%d" % (4 + i)) for i in range(4)]

                            def qk(kc):
                                ks = slice(kc * 128, (kc + 1) * 128)
                                db = dbanks[kc % 2]
                                fns = []
                                for m in range(2):
                                    rows = slice(m * 64, (m + 1) * 64)
                                    fns.append(lambda m=m, rows=rows: PE.matmul(db[:, m * 512:(m + 1) * 512], lhsT=kbuf[sl][rows, ks], rhs=qbuf[sl][rows, qs], start=True, stop=True))
                                S.ops("pe", fns, reads=["kbuf%d" % sl, "qbuf%d" % sl], writes=["bank%d" % (2 * (kc % 2)), "bank%d" % (2 * (kc % 2) + 1)])

                            def ex(kc):
                                S.op("act", lambda: A.activation(out=PT[kc % NPT][:], in_=dbanks[kc % 2][:, :], func=AF.Exp, scale=scale),
                                     reads=["bank%d" % (2 * (kc % 2)), "bank%d" % (2 * (kc % 2) + 1)], writes=["PT%d" % (kc % NPT)])

                            def pv(kc):
                                first, last = kc == 0, kc == NBLK - 1
                                fns = [(lambda m=m: PE.matmul(accs[m][0], lhsT=vbuf[sl][:, kc, :], rhs=PT[kc % NPT][:, m * 512:(m + 1) * 512], start=first, stop=last)) for m in range(2)]
                                ok_ = [accs[0][1], accs[1][1]]
                                S.ops("pe", fns, reads=["PT%d" % (kc % NPT), "vbuf%d" % sl], writes=ok_ if first else [], cwrites=ok_ if last else [])
                                for (en, EN, c0, c1) in (("dve", V, 0, DSPLIT), ("pool", G, DSPLIT, 1024)):
                                    if first:
                                        S.op(en, lambda: EN.tensor_copy(out=dacc[par][:, c0:c1], in_=PT[kc % NPT][:, c0:c1]), reads=["PT%d" % (kc % NPT)], writes=["dacc%d%s" % (par, en)])
                                    else:
                                        S.op(en, lambda: EN.tensor_tensor(out=dacc[par][:, c0:c1], in0=dacc[par][:, c0:c1], in1=PT[kc % NPT][:, c0:c1], op=ALU.add), reads=["PT%d" % (kc % NPT), "dacc%d%s" % (par, en)], writes=["dacc%d%s" % (par, en)])
                            qk(0)
                            for kc in range(NBLK):
                                ex(kc)
                                if kc + 1 < NBLK:
                                    qk(kc + 1)
                                pv(kc)
                            for m in range(2):
                                S.op("pe", lambda: PE.matmul(accs[2 + m][0], lhsT=ones_f[:], rhs=dacc[par][:, m * 512:(m + 1) * 512], start=True, stop=True), reads=["onesf", "dacc%ddve" % par, "dacc%dpool" % par], writes=[accs[2 + m][1]])
                            for m in range(2):
                                S.op("act", lambda: A.activation(out=rden[m][:], in_=accs[2 + m][0], func=AF.Ln), reads=[accs[2 + m][1]], writes=["rden%d" % m])
                                S.op("act", lambda: A.activation(out=rden[m][:], in_=rden[m][:], func=AF.Exp, scale=-1.0), reads=["rden%d" % m], writes=["rden%d" % m])
                            S.op("dve", lambda: V.tensor_tensor(out=o1n[:], in0=accs[0][0], in1=rden[0][:], op=ALU.mult), reads=[accs[0][1], "rden0"], writes=["o1n"])
                            S.op("dve", lambda: V.tensor_tensor(out=o2n[:], in0=accs[1][0], in1=rden[1][:], op=ALU.mult), reads=[accs[1][1], "rden1"], writes=["o2n"])
                            S.op("dve", lambda: V.scalar_tensor_tensor(out=o1n[:], in0=o2n[:], scalar=neglam[:, 0:1], in1=o1n[:], op0=ALU.mult, op1=ALU.add),
                                 reads=["o1n", "o2n", "neglam"], writes=["o1n"])
                            S.op("pool", lambda: G.tensor_tensor(out=sq[:], in0=o1n[:], in1=o1n[:], op=ALU.mult), reads=["o1n"], writes=["osq"])
                            S.op("pe", lambda: PE.matmul(accs[2][0], lhsT=ones_b[:], rhs=sq[:], start=True, stop=True), reads=["osq", "onesb"], writes=[accs[2][1]])
                            S.op("act", lambda: A.activation(out=rstd[:], in_=accs[2][0], func=AF.Ln, scale=1.0 / 128, bias=eps5[:, 0:1]), reads=[accs[2][1], "eps5"], writes=["orstd"])
                            S.op("act", lambda: A.activation(out=rstd[:], in_=rstd[:], func=AF.Exp, scale=-0.5), reads=["orstd"], writes=["orstd"])
                            S.op("dve", lambda: V.scalar_tensor_tensor(out=osb[par][:], in0=o1n[:], scalar=subs[:, 0:1], in1=rstd[:], op0=ALU.mult, op1=ALU.mult),
                                 reads=["o1n", "subs", "orstd"], writes=["osb%d" % par])
                        S.dma("sp", "b_o%d" % par, lambda: nc.sync.dma_start(out=OT[h * 128:(h + 1) * 128, qs], in_=osb[par][:]), reads=["osb%d" % par], writes=["OT"])
                S.barrier()

        def ln_stats(z, zkey, st, mv, rs, nmr, tag):
            for i in range(2):
                S.op("dve", lambda: V.bn_stats(out=st[:, i, :], in_=z[:, i * 512:(i + 1) * 512]), reads=[zkey], writes=["st" + tag])
            S.op("dve", lambda: V.bn_aggr(out=mv[:], in_=st[:].rearrange("p a b -> p (a b)")), reads=["st" + tag], writes=["mv" + tag])
            S.op("act", lambda: A.activation(out=rs[:], in_=mv[:, 1:2], func=AF.Sqrt, bias=eps5[:, 0:1], scale=1.0), reads=["mv" + tag, "eps5"], writes=["rs" + tag])
            S.op("dve", lambda: V.reciprocal(out=rs[:], in_=rs[:]), reads=["rs" + tag], writes=["rs" + tag])
            S.op("dve", lambda: V.scalar_tensor_tensor(out=nmr[:], in0=mv[:, 0:1], scalar=-1.0, in1=rs[:], op0=ALU.mult, op1=ALU.mult), reads=["mv" + tag, "rs" + tag], writes=["nmr" + tag])

        def ln_apply(z, zkey, rs, nmr, tag):
            S.op("act", lambda: A.activation(out=z[:], in_=z[:], func=AF.Identity, bias=nmr[:, 0:1], scale=rs[:, 0:1]), reads=[zkey, "rs" + tag, "nmr" + tag], writes=[zkey])

        def phase_c(xsrc, l, w_o_ap):
            with ExitStack() as ph:
                g1p, k_g1 = bcast_load(ph, MODS[l:l + 1, 2 * D:3 * D], "g1p", plus_one=True)
                sc2p, k_sc2 = bcast_load(ph, MODS[l:l + 1, 4 * D:5 * D], "sc2p", plus_one=True)
                sh2p, k_sh2 = bcast_load(ph, MODS[l:l + 1, 3 * D:4 * D], "sh2p")
                lng, k_lng = bcast_load(ph, ln1_g[l:l + 1, :], "lng")
                lnb, k_lnb = bcast_load(ph, ln1_b[l:l + 1, :], "lnb")
                w_o = sb(ph, [128, 8, D], BF16, "w_o")
                rw = sb(ph, [128, 8, NE], F32, "rw")
                oT = sb(ph, [128, 8, S_LEN], BF16, "oTall")
                S.dma("pool", "c_wo", lambda: G.dma_start(out=w_o[:], in_=w_o_ap.rearrange("(k p) n -> p k n", p=128)), writes=["w_o"])
                S.dma("sp", "c_rw", lambda: nc.sync.dma_start(out=rw[:], in_=router_w[l].rearrange("(k p) n -> p k n", p=128)), writes=["rw"])
                for h in range(8):
                    S.dma("sp", "c_ot", lambda: nc.sync.dma_start(out=oT[:, h, :], in_=OT[h * 128:(h + 1) * 128, :]), writes=["oTall"])
                NR = 6
                xin = [sb(ph, [128, D], F32, "xin") for _ in range(NR)]
                z = [sb(ph, [128, D], F32, "z") for _ in range(NR)]
                h2 = [sb(ph, [128, D], F32, "h2") for _ in range(NR)]
                h2b = [sb(ph, [128, D], BF16, "h2b") for _ in range(2)]
                h2T = [sb(ph, [128, 8, 128], F32, "h2T") for _ in range(2)]
                st = [sb(ph, [128, 2, 6], F32, "st") for _ in range(NR)]
                mv = [sb(ph, [128, 2], F32, "mv") for _ in range(NR)]
                rs = [sb(ph, [128, 1], F32, "rs") for _ in range(NR)]
                nmr = [sb(ph, [128, 1], F32, "nmr") for _ in range(NR)]
                mx = [sb(ph, [128, 1], F32, "mx") for _ in range(2)]
                ssum = [sb(ph, [128, 1], F32, "ssum") for _ in range(2)]
                ex = [sb(ph, [128, NE], F32, "ex") for _ in range(2)]

                def ld(b):
                    r = b % NR
                    S.dma("sp", "c_x%d" % r, lambda: nc.sync.dma_start(out=xin[r][:], in_=xsrc[b * 128:(b + 1) * 128, :]), writes=["xin%d" % r])

                def st0(b):
                    r, s2 = b % NR, b % 2
                    rows = slice(b * 128, (b + 1) * 128)
                    for hf in range(2):
                        pt, kk = banks[2 * s2 + hf], "bank%d" % (2 * s2 + hf)
                        S.ops("pe", [(lambda h=h: PE.matmul(pt[:, :], lhsT=oT[:, h, rows], rhs=w_o[:, h, hf * 512:(hf + 1) * 512], start=(h == 0), stop=(h == 7))) for h in range(8)],
                              reads=["oTall", "w_o"], writes=[kk])
                        S.op("dve", lambda: V.tensor_tensor(out=z[r][:, hf * 512:(hf + 1) * 512], in0=pt[:, :], in1=g1p[:, hf * 512:(hf + 1) * 512], op=ALU.mult),
                             reads=[kk, k_g1], writes=["z%d" % r])
                    S.op("dve", lambda: V.scalar_tensor_tensor(out=z[r][:], in0=xin[r][:], scalar=ALPHA, in1=z[r][:], op0=ALU.mult, op1=ALU.add),
                         reads=["xin%d" % r, "z%d" % r], writes=["z%d" % r])
                    for i in range(2):
                        S.op("dve", lambda: V.bn_stats(out=st[r][:, i, :], in_=z[r][:, i * 512:(i + 1) * 512]), reads=["z%d" % r], writes=["stc%d" % r])
                    S.op("dve", lambda: V.bn_aggr(out=mv[r][:], in_=st[r][:].rearrange("p a b -> p (a b)")), reads=["stc%d" % r], writes=["mvc%d" % r])

                def st1(b):
                    r = b % NR
                    S.op("act", lambda: A.activation(out=rs[r][:], in_=mv[r][:, 1:2], func=AF.Sqrt, bias=eps5[:, 0:1], scale=1.0), reads=["mvc%d" % r, "eps5"], writes=["rsc%d" % r])
                    S.op("dve", lambda: V.reciprocal(out=rs[r][:], in_=rs[r][:]), reads=["rsc%d" % r], writes=["rsc%d" % r])
                    S.op("dve", lambda: V.scalar_tensor_tensor(out=nmr[r][:], in0=mv[r][:, 0:1], scalar=-1.0, in1=rs[r][:], op0=ALU.mult, op1=ALU.mult), reads=["mvc%d" % r, "rsc%d" % r], writes=["nmrc%d" % r])
                    S.op("act", lambda: A.activation(out=z[r][:], in_=z[r][:], func=AF.Identity, bias=nmr[r][:, 0:1], scale=rs[r][:, 0:1]), reads=["z%d" % r, "rsc%d" % r, "nmrc%d" % r], writes=["z%d" % r])

                def st2(b):
                    r = b % NR
                    rows = slice(b * 128, (b + 1) * 128)
                    S.op("pool", lambda: G.tensor_tensor(out=z[r][:], in0=z[r][:], in1=lng[:], op=ALU.mult), reads=["z%d" % r, k_lng], writes=["z%d" % r])
                    S.op("dve", lambda: V.tensor_tensor(out=z[r][:], in0=z[r][:], in1=lnb[:], op=ALU.add), reads=["z%d" % r, k_lnb], writes=["z%d" % r])
                    S.dma("sp", "c_xm%d" % r, lambda: nc.sync.dma_start(out=XMID[rows, :], in_=z[r][:]), reads=["z%d" % r], writes=["XMID"])
                    S.op("pool", lambda: G.tensor_tensor(out=h2[r][:], in0=z[r][:], in1=sc2p[:], op=ALU.mult), reads=["z%d" % r, k_sc2], writes=["h2_%d" % r])
                    S.op("dve", lambda: V.tensor_tensor(out=h2[r][:], in0=h2[r][:], in1=sh2p[:], op=ALU.add), reads=["h2_%d" % r, k_sh2], writes=["h2_%d" % r])

                def st3(b):
                    r, s2 = b % NR, b % 2
                    rows = slice(b * 128, (b + 1) * 128)
                    S.op("act", lambda: A.copy(out=h2b[s2][:], in_=h2[r][:]), reads=["h2_%d" % r], writes=["h2b%d" % s2])
                    S.dma("sp", "c_h2%d" % s2, lambda: nc.sync.dma_start(out=H2[rows, :], in_=h2b[s2][:]), reads=["h2b%d" % s2], writes=["H2"])
                    for hf in range(2):
                        bk, bkey = banks[4 + hf], "bank%d" % (4 + hf)
                        S.ops("pe", [(lambda j=j: PE.transpose(bk[:, j * 128:(j + 1) * 128], h2[r][:, (hf * 4 + j) * 128:(hf * 4 + j + 1) * 128], ident_f[:])) for j in range(4)],
                              reads=["h2_%d" % r, "identf"], writes=[bkey])
                        S.op("act", lambda: A.copy(out=h2T[s2][:, hf * 4:(hf + 1) * 4, :].rearrange("p a b -> p (a b)"), in_=bk[:, :]), reads=[bkey], writes=["h2T%d" % s2])

                def st4(b):
                    s2 = b % 2
                    lg, lgk = banks[6 + s2], "bank%d" % (6 + s2)
                    S.ops("pe", [(lambda k=k: PE.matmul(lg[:, 0:NE], lhsT=h2T[s2][:, k, :], rhs=rw[:, k, :], start=(k == 0), stop=(k == 7))) for k in range(8)],
                          reads=["h2T%d" % s2, "rw"], writes=[lgk])
                    S.op("dve", lambda: V.reduce_max(out=mx[s2][:], in_=lg[:, 0:NE], axis=mybir.AxisListType.X), reads=[lgk], writes=["mx%d" % s2])
                    S.op("dve", lambda: V.tensor_scalar(out=mx[s2][:], in0=mx[s2][:], scalar1=-1.0, scalar2=None, op0=ALU.mult), reads=["mx%d" % s2], writes=["mx%d" % s2])
                    S.op("act", lambda: A.activation(out=ex[s2][:], in_=lg[:, 0:NE], func=AF.Exp, bias=mx[s2][:, 0:1], scale=1.0, accum_out=ssum[s2][:]), reads=[lgk, "mx%d" % s2], writes=["ex%d" % s2, "ssum%d" % s2])
                    S.op("dve", lambda: V.reciprocal(out=ssum[s2][:], in_=ssum[s2][:]), reads=["ssum%d" % s2], writes=["ssum%d" % s2])
                    S.op("dve", lambda: V.tensor_scalar(out=aff[:, b, :], in0=ex[s2][:], scalar1=ssum[s2][:, 0:1], scalar2=None, op0=ALU.mult), reads=["ex%d" % s2, "ssum%d" % s2], writes=["aff"])

                stages = [st0, st1, st2, st3, st4]
                ld(0)
                ld(1)
                for step in range(NBLK + len(stages) - 1):
                    for si in reversed(range(len(stages))):
                        b = step - si
                        if si == 0 and step + 2 < NBLK:
                            ld(step + 2)
                        if 0 <= b < NBLK:
                            stages[si](b)
                S.barrier()

        def phase_m(l):
            with ExitStack() as ph:
                ltri = sb(ph, [128, 128], F32, "ltri")
                ltri_b = sb(ph, [128, 128], BF16, "ltrib")
                iota = sb(ph, [128, CAP], F32, "iota")
                rcp = sb(ph, [128, NBLK, 2], F32, "rcp")
                S.dma("sp", "m_c0", lambda: nc.sync.dma_start(out=ltri[:], in_=c_ltri), writes=["ltri"])
                S.dma("sp", "m_c1", lambda: nc.sync.dma_start(out=iota[:], in_=c_iota), writes=["iota"])
                S.dma("sp", "m_c2", lambda: nc.sync.dma_start(out=rcp[:], in_=c_rcp), writes=["rcp"])
                S.op("dve", lambda: V.tensor_copy(out=ltri_b[:], in_=ltri[:]), reads=["ltri"], writes=["ltrib"])
                zt = sb(ph, [128, 4, D], F32, "zt")
                S.op("pool", lambda: G.memset(zt[:], 0.0), writes=["zt"])
                fv = FACC.rearrange("(t s p) d -> t p s d", s=4, p=128)
                for tb in range(8):
                    S.dma("sp", "m_z", lambda: nc.sync.dma_start(out=fv[tb], in_=zt[:]), reads=["zt"], writes=["FACC"])
                lo = sb(ph, [128, NE], F32, "lo")
                mid = sb(ph, [128, NE], F32, "mid")
                cmpt = sb(ph, [128, NBLK, NE], F32, "cmpt")
                cnt = sb(ph, [128, NE], F32, "cnt")
                ge = sb(ph, [128, NE], F32, "ge")
                ones_f = sb(ph, [128, 128], F32, "onesf")
                S.op("dve", lambda: V.memset(ones_f[:], 1.0), writes=["onesf"])
                S.op("dve", lambda: V.memset(lo[:], 0.0), writes=["lo"])
                for it in range(30):
                    w = 2.0 ** -(it + 1)
                    bk, bkk = banks[it % 2], "bank%d" % (it % 2)
                    S.op("dve", lambda: V.tensor_scalar(out=mid[:], in0=lo[:], scalar1=w, scalar2=None, op0=ALU.add), reads=["lo"], writes=["mid"])
                    S.op("dve", lambda: V.tensor_tensor(out=cmpt[:], in0=aff[:], in1=mid[:].unsqueeze(1).broadcast_to([128, NBLK, NE]), op=ALU.is_ge), reads=["aff", "mid"], writes=["cmpt"])
                    S.op("dve", lambda: V.reduce_sum(out=cnt[:], in_=cmpt[:].rearrange("p c e -> p e c"), axis=mybir.AxisListType.X), reads=["cmpt"], writes=["cnt"])
                    S.op("pe", lambda: PE.matmul(bk[:, 0:NE], lhsT=ones_f[:], rhs=cnt[:], start=True, stop=True), reads=["onesf", "cnt"], writes=[bkk])
                    S.op("dve", lambda: V.tensor_scalar(out=ge[:], in0=bk[:, 0:NE], scalar1=float(CAP) - 0.5, scalar2=None, op0=ALU.is_ge), reads=[bkk], writes=["ge"])
                    S.op("dve", lambda: V.scalar_tensor_tensor(out=lo[:], in0=ge[:], scalar=w, in1=lo[:], op0=ALU.mult, op1=ALU.add), reads=["ge", "lo"], writes=["lo"])
                maskb = sb(ph, [128, NBLK, NE], BF16, "maskb")
                pre = sb(ph, [128, NBLK, NE], BF16, "pre")
                pref = sb(ph, [128, NBLK, NE], F32, "pref")
                posp = sb(ph, [128, NBLK, NE], F32, "posp")
                S.op("dve", lambda: V.tensor_tensor(out=cmpt[:], in0=aff[:], in1=lo[:].unsqueeze(1).broadcast_to([128, NBLK, NE]), op=ALU.is_ge), reads=["aff", "lo"], writes=["cmpt"])
                S.op("dve", lambda: V.tensor_copy(out=maskb[:], in_=cmpt[:]), reads=["cmpt"], writes=["maskb"])
                S.op("dve", lambda: V.memset(pref[:, 0, :], 0.0), writes=["pref"])
                for c in range(1, NBLK):
                    S.op("dve", lambda: V.tensor_tensor(out=pref[:, c, :], in0=pref[:, c - 1, :], in1=cmpt[:, c - 1, :], op=ALU.add), reads=["pref", "cmpt"], writes=["pref"])
                S.op("dve", lambda: V.tensor_copy(out=pre[:], in_=pref[:]), reads=["pref"], writes=["pre"])
                pb = banks[1]
                S.ops("pe", [lambda: PE.matmul(pb[:, :], lhsT=ltri_b[:], rhs=maskb[:].rearrange("p c e -> p (c e)"), start=True, stop=False),
                             lambda: PE.matmul(pb[:, :], lhsT=ones_b[:], rhs=pre[:].rearrange("p c e -> p (c e)"), start=False, stop=True)],
                      reads=["ltrib", "onesb", "maskb", "pre"], writes=["bank1"])
                S.op("dve", lambda: V.tensor_tensor(out=posp[:].rearrange("p c e -> p (c e)"), in0=pb[:, :], in1=cmpt[:].rearrange("p c e -> p (c e)"), op=ALU.mult), reads=["bank1", "cmpt"], writes=["posp"])
                R = sb(ph, [128, NBLK, NE, 5], BF16, "R")
                r1 = sb(ph, [128, NBLK, NE], F32, "r1")
                r2 = sb(ph, [128, NBLK, NE], F32, "r2")
                S.op("dve", lambda: V.tensor_copy(out=R[:, :, :, 0], in_=rcp[:, :, 0:1].broadcast_to([128, NBLK, NE])), reads=["rcp"], writes=["R"])
                S.op("dve", lambda: V.tensor_copy(out=R[:, :, :, 1], in_=rcp[:, :, 1:2].broadcast_to([128, NBLK, NE])), reads=["rcp", "R"], writes=["R"])
                S.op("dve", lambda: V.tensor_copy(out=R[:, :, :, 2], in_=aff[:]), reads=["aff", "R"], writes=["R"])
                S.op("dve", lambda: V.tensor_tensor(out=r1[:], in0=aff[:], in1=R[:, :, :, 2], op=ALU.subtract), reads=["aff", "R"], writes=["r1"])
                S.op("dve", lambda: V.tensor_copy(out=R[:, :, :, 3], in_=r1[:]), reads=["r1", "R"], writes=["R"])
                S.op("dve", lambda: V.tensor_tensor(out=r2[:], in0=r1[:], in1=R[:, :, :, 3], op=ALU.subtract), reads=["r1", "R"], writes=["r2"])
                S.op("dve", lambda: V.tensor_copy(out=R[:, :, :, 4], in_=r2[:]), reads=["r2", "R"], writes=["R"])
                idx = sb(ph, [128, NE, 4], I32, "idx")
                idxf = sb(ph, [128, NE, 4], F32, "idxf")
                gate = sb(ph, [128, NE, 4], F32, "gate")
                Pm = [sb(ph, [128, NBLK, 128], BF16, "Pm") for _ in range(2)]
                ig = sb(ph, [128, 8], F32, "ig")
                pcount = [0]

                def compute_idx(e, g):
                    psl = pcount[0] % 2
                    pcount[0] += 1
                    S.op("dve", lambda: V.tensor_tensor(out=Pm[psl][:], in0=posp[:, :, e:e + 1].broadcast_to([128, NBLK, 128]),
                                                        in1=iota[:, g * 128:(g + 1) * 128].unsqueeze(1).broadcast_to([128, NBLK, 128]), op=ALU.is_equal),
                         reads=["posp", "iota"], writes=["Pm%d" % psl])
                    ob, obk = banks[7], "bank7"
                    S.ops("pe", [(lambda c=c: PE.matmul(ob[:, 0:5], lhsT=Pm[psl][:, c, :], rhs=R[:, c, e, :], start=(c == 0), stop=(c == NBLK - 1))) for c in range(NBLK)],
                          reads=["Pm%d" % psl, "R"], writes=[obk])
                    S.op("act", lambda: A.copy(out=ig[:, psl * 4:psl * 4 + 4], in_=ob[:, 1:5]), reads=[obk], writes=["ig%d" % psl])
                    S.op("dve", lambda: V.scalar_tensor_tensor(out=idxf[:, e, g:g + 1], in0=ob[:, 0:1], scalar=128.0, in1=ig[:, psl * 4:psl * 4 + 1], op0=ALU.mult, op1=ALU.add),
                         reads=[obk, "ig%d" % psl], writes=["idxf%d" % e])
                    S.op("dve", lambda: V.tensor_tensor(out=gate[:, e, g:g + 1], in0=ig[:, psl * 4 + 1:psl * 4 + 2], in1=ig[:, psl * 4 + 2:psl * 4 + 3], op=ALU.add), reads=["ig%d" % psl], writes=["gate%d" % e])
                    S.op("dve", lambda: V.tensor_tensor(out=gate[:, e, g:g + 1], in0=gate[:, e, g:g + 1], in1=ig[:, psl * 4 + 3:psl * 4 + 4], op=ALU.add), reads=["ig%d" % psl, "gate%d" % e], writes=["gate%d" % e])
                    if g == 3:
                        S.op("dve", lambda: V.tensor_copy(out=idx[:, e, :], in_=idxf[:, e, :]), reads=["idxf%d" % e], writes=["idx%d" % e])

                for e0 in range(2):
                    for g in range(4):
                        compute_idx(e0, g)
                wi = [sb(ph, [128, 8, 1024], BF16, "wi") for _ in range(3)]
                wo = [sb(ph, [128, 4, D], BF16, "wo") for _ in range(3)]
                xg = [sb(ph, [128, 4, D], BF16, "xg") for _ in range(2)]
                xeT = sb(ph, [128, 8, CAP], BF16, "xeT")
                sg = [sb(ph, [128, CAP], F32, "sg") for _ in range(2)]
                actT = [sb(ph, [128, 4, CAP], BF16, "actT") for _ in range(2)]
                ysb = [sb(ph, [128, 4, D], F32, "ysb") for _ in range(2)]
                wiv = moe_w_in[l].rearrange("e (k p) n -> e p k n", p=128)
                wov = moe_w_out[l].rearrange("e (k p) n -> e p k n", p=128)
                nblk = [0]

                def load_w(e, i):
                    sl = nblk[0] % 3
                    nblk[0] += 1
                    S.dma("pool", "m_wg%d" % sl, lambda: G.dma_start(out=wi[sl][:, :, 0:512], in_=wiv[e][:, :, i * 512:(i + 1) * 512]), writes=["wi%d" % sl])
                    S.dma("pool", "m_wu%d" % sl, lambda: G.dma_start(out=wi[sl][:, :, 512:1024], in_=wiv[e][:, :, FF + i * 512:FF + (i + 1) * 512]), writes=["wi%d" % sl])
                    S.dma("pool", "m_wo%d" % sl, lambda: G.dma_start(out=wo[sl][:], in_=wov[e][:, i * 4:(i + 1) * 4, :]), writes=["wo%d" % sl])
                    return sl

                def gather(e):
                    gs = e % 2
                    for g in range(4):
                        S.dma("pool", "m_g%d" % gs, lambda: G.indirect_dma_start(out=xg[gs][:, g, :], out_offset=None, in_=H2,
                                                                               in_offset=bass.IndirectOffsetOnAxis(ap=idx[:, e, g:g + 1], axis=0)),
                              reads=["idx%d" % e, "H2"], writes=["xg%d" % gs])

                sched_w = [(e, i) for e in range(NE) for i in range(4)]
                slots = {}
                slots[sched_w[0]] = load_w(*sched_w[0])
                slots[sched_w[1]] = load_w(*sched_w[1])
                gather(0)
                wn = 2
                par2 = 0
                for e in range(NE):
                    gs = e % 2
                    ys = e % 2
                    if e + 1 < NE:
                        gather(e + 1)
                    for k in range(8):
                        bk, bkey = banks[6], "bank6"
                        tv = bk[:].bitcast(BF16)
                        S.ops("pe", [(lambda g=g: PE.transpose(tv[:, g * 128:(g + 1) * 128], xg[gs][:, g, k * 128:(k + 1) * 128], ident_b[:])) for g in range(4)],
                              reads=["xg%d" % gs, "identb"], writes=[bkey])
                        S.op("act" if k % 2 else "dve", (lambda k=k, tv=tv: (A.copy if k % 2 else V.tensor_copy)(out=xeT[:, k, :], in_=tv[:, 0:CAP])), reads=[bkey], writes=["xeT"])
                    for i in range(4):
                        if wn < len(sched_w):
                            slots[sched_w[wn]] = load_w(*sched_w[wn])
                            wn += 1
                        ws = slots[(e, i)]
                        asl = (e * 4 + i) % 2
                        for fc in range(4):
                            pg, pgk = banks[0 + 2 * (fc % 2)], "bank%d" % (0 + 2 * (fc % 2))
                            pu, puk = banks[1 + 2 * (fc % 2)], "bank%d" % (1 + 2 * (fc % 2))
                            S.ops("pe", [(lambda k=k: PE.matmul(pg[:, :], lhsT=wi[ws][:, k, fc * 128:(fc + 1) * 128], rhs=xeT[:, k, :], start=(k == 0), stop=(k == 7))) for k in range(8)],
                                  reads=["wi%d" % ws, "xeT"], writes=[pgk])
                            S.ops("pe", [(lambda k=k: PE.matmul(pu[:, :], lhsT=wi[ws][:, k, 512 + fc * 128:512 + (fc + 1) * 128], rhs=xeT[:, k, :], start=(k == 0), stop=(k == 7))) for k in range(8)],
                                  reads=["wi%d" % ws, "xeT"], writes=[puk])
                            S.op("act", lambda: A.activation(out=sg[fc % 2][:], in_=pg[:, :], func=AF.Silu), reads=[pgk], writes=["sg%d" % (fc % 2)])
                            S.op("dve", lambda: V.tensor_tensor(out=actT[asl][:, fc, :], in0=sg[fc % 2][:], in1=pu[:, :], op=ALU.mult), reads=["sg%d" % (fc % 2), puk], writes=["actT%d" % asl])
                        if e + 2 < NE:
                            compute_idx(e + 2, i)
                        for g in range(4):
                            for hf in range(2):
                                py, pyk = banks[4 + par2 % 2], "bank%d" % (4 + par2 % 2)
                                par2 += 1
                                S.ops("pe", [(lambda fc=fc: PE.matmul(py[:, :], lhsT=actT[asl][:, fc, g * 128:(g + 1) * 128], rhs=wo[ws][:, fc, hf * 512:(hf + 1) * 512], start=(fc == 0), stop=(fc == 3))) for fc in range(4)],
                                      reads=["actT%d" % asl, "wo%d" % ws], writes=[pyk])
                                dst = ysb[ys][:, g, hf * 512:(hf + 1) * 512]
                                if i == 0:
                                    S.op("act", lambda: A.activation(out=dst, in_=py[:, :], func=AF.Copy, scale=gate[:, e, g:g + 1]), reads=[pyk, "gate%d" % e], writes=["ysb%d" % ys])
                                else:
                                    S.op("dve", lambda: V.scalar_tensor_tensor(out=dst, in0=py[:, :], scalar=gate[:, e, g:g + 1], in1=dst, op0=ALU.mult, op1=ALU.add),
                                         reads=[pyk, "gate%d" % e, "ysb%d" % ys], writes=["ysb%d" % ys])
                    for g in range(4):
                        S.dma("pool", "m_sc", lambda: G.indirect_dma_start(out=FACC, out_offset=bass.IndirectOffsetOnAxis(ap=idx[:, e, g:g + 1], axis=0),
                                                                          in_=ysb[ys][:, g, :], in_offset=None, compute_op=ALU.add),
                              reads=["ysb%d" % ys, "idx%d" % e], writes=["FACC"] if g == 0 else [])
                    S.lastw["FACC"] = (S.dsem["m_sc"][0], S.dsem["m_sc"][1])
                    S.readers["FACC"] = {}
                S.barrier()

        def phase_d(l, dst):
            with ExitStack() as ph:
                g2p, k_g2 = bcast_load(ph, MODS[l:l + 1, 5 * D:6 * D], "g2p", plus_one=True)
                lng, k_lng = bcast_load(ph, ln2_g[l:l + 1, :], "lng2")
                lnb, k_lnb = bcast_load(ph, ln2_b[l:l + 1, :], "lnb2")
                NR = 6
                xin = [sb(ph, [128, D], F32, "xin") for _ in range(NR)]
                fin = [sb(ph, [128, D], F32, "fin") for _ in range(NR)]
                st = [sb(ph, [128, 2, 6], F32, "st") for _ in range(NR)]
                mv = [sb(ph, [128, 2], F32, "mv") for _ in range(NR)]
                rs = [sb(ph, [128, 1], F32, "rs") for _ in range(NR)]
                nmr = [sb(ph, [128, 1], F32, "nmr") for _ in range(NR)]

                def ld(b):
                    r = b % NR
                    rows = slice(b * 128, (b + 1) * 128)
                    S.dma("sp", "d_x%d" % r, lambda: nc.sync.dma_start(out=xin[r][:], in_=XMID[rows, :]), writes=["xin%d" % r])
                    S.dma("sp", "d_f%d" % r, lambda: nc.sync.dma_start(out=fin[r][:], in_=FACC[rows, :]), writes=["fin%d" % r])

                def st0(b):
                    r = b % NR
                    S.op("pool", lambda: G.tensor_tensor(out=fin[r][:], in0=fin[r][:], in1=g2p[:], op=ALU.mult), reads=["fin%d" % r, k_g2], writes=["fin%d" % r])
                    S.op("dve", lambda: V.scalar_tensor_tensor(out=fin[r][:], in0=xin[r][:], scalar=ALPHA, in1=fin[r][:], op0=ALU.mult, op1=ALU.add),
                         reads=["xin%d" % r, "fin%d" % r], writes=["fin%d" % r])
                    for i in range(2):
                        S.op("dve", lambda: V.bn_stats(out=st[r][:, i, :], in_=fin[r][:, i * 512:(i + 1) * 512]), reads=["fin%d" % r], writes=["std%d" % r])
                    S.op("dve", lambda: V.bn_aggr(out=mv[r][:], in_=st[r][:].rearrange("p a b -> p (a b)")), reads=["std%d" % r], writes=["mvd%d" % r])

                def st1(b):
                    r = b % NR
                    S.op("act", lambda: A.activation(out=rs[r][:], in_=mv[r][:, 1:2], func=AF.Sqrt, bias=eps5[:, 0:1], scale=1.0), reads=["mvd%d" % r, "eps5"], writes=["rsd%d" % r])
                    S.op("dve", lambda: V.reciprocal(out=rs[r][:], in_=rs[r][:]), reads=["rsd%d" % r], writes=["rsd%d" % r])
                    S.op("dve", lambda: V.scalar_tensor_tensor(out=nmr[r][:], in0=mv[r][:, 0:1], scalar=-1.0, in1=rs[r][:], op0=ALU.mult, op1=ALU.mult), reads=["mvd%d" % r, "rsd%d" % r], writes=["nmrd%d" % r])
                    S.op("act", lambda: A.activation(out=fin[r][:], in_=fin[r][:], func=AF.Identity, bias=nmr[r][:, 0:1], scale=rs[r][:, 0:1]), reads=["fin%d" % r, "rsd%d" % r, "nmrd%d" % r], writes=["fin%d" % r])

                def st2(b):
                    r = b % NR
                    rows = slice(b * 128, (b + 1) * 128)
                    S.op("pool", lambda: G.tensor_tensor(out=fin[r][:], in0=fin[r][:], in1=lng[:], op=ALU.mult), reads=["fin%d" % r, k_lng], writes=["fin%d" % r])
                    S.op("dve", lambda: V.tensor_tensor(out=fin[r][:], in0=fin[r][:], in1=lnb[:], op=ALU.add), reads=["fin%d" % r, k_lnb], writes=["fin%d" % r])
                    S.dma("sp", "d_o%d" % r, lambda: nc.sync.dma_start(out=dst[rows, :], in_=fin[r][:]), reads=["fin%d" % r], writes=["dst"])

                stages = [st0, st1, st2]
                ld(0)
                ld(1)
                ld(2)
                for step in range(NBLK + len(stages) - 1):
                    for si in reversed(range(len(stages))):
                        b = step - si
                        if 0 <= b < NBLK:
                            stages[si](b)
                        if si == 1 and step + 3 < NBLK:
                            ld(step + 3)
                S.barrier()

        phases = [
            lambda: phase_mods(),
            lambda: phase_a_mla(x_in, 0),
            lambda: phase_b("mla"),
            lambda: phase_c(x_in, 0, mla_w_o),
            lambda: phase_m(0),
            lambda: phase_d(0, X1),
            lambda: phase_a_diff(X1, 1),
            lambda: phase_b("diff"),
            lambda: phase_c(X1, 1, diff_w_o),
            lambda: phase_m(1),
            lambda: phase_d(1, out),
        ]
        for i, p in enumerate(phases):
            if i <= upto:
                p()
        if dbg is not None:
            src = {"XMID": XMID, "X1": X1, "FACC": FACC}[dbg]
            with ExitStack() as ph:
                t = sb(ph, [128, 4, D], F32, "dbg")
                for tb in range(8):
                    S.dma("sp", "dbg_i", lambda: nc.sync.dma_start(out=t[:], in_=src.rearrange("(t s p) d -> t p s d", s=4, p=128)[tb]), writes=["dbg"])
                    S.dma("sp", "dbg_o", lambda: nc.sync.dma_start(out=out.rearrange("(t s p) d -> t p s d", s=4, p=128)[tb], in_=t[:]), reads=["dbg"], writes=["outd"])
        S.finish("sp")
    return nc


def _consts():
    inv32 = (10000.0 ** (-np.arange(0, 64, 2, dtype=np.float32) / 64)).astype(np.float32)
    inv_m = np.concatenate([inv32, inv32]).reshape(64, 1).astype(np.float32)
    inv8 = (500000.0 ** (-np.arange(0, 16, 2, dtype=np.float32) / 16)).astype(np.float32)
    blk = np.zeros(64, np.float32)
    blk[0:8] = inv8
    blk[8:16] = inv8
    inv_d = np.concatenate([blk, blk]).reshape(128, 1).astype(np.float32)
    ident = np.eye(128, dtype=np.float32)
    ltri = np.triu(np.ones((128, 128), np.float32))
    iota = np.tile(np.arange(1, CAP + 1, dtype=np.float32)[None, :], (128, 1))
    rcp = np.zeros((128, NBLK, 2), np.float32)
    rcp[:, :, 0] = np.arange(NBLK, dtype=np.float32)[None, :]
    rcp[:, :, 1] = np.arange(128, dtype=np.float32)[:, None]
    return dict(c_inv_m=inv_m, c_inv_d=inv_d, c_ident=ident, c_ltri=ltri, c_iota=iota, c_rcp=rcp)


def make_in_maps(inputs, ncores=8):
    f = lambda a: np.ascontiguousarray(np.asarray(a))
    shared = dict(
        ada_w=f(inputs["ada_w"]), ada_b=f(inputs["ada_b"]),
        ln1_g=f(inputs["ln1_g"]), ln1_b=f(inputs["ln1_b"]), ln2_g=f(inputs["ln2_g"]), ln2_b=f(inputs["ln2_b"]),
        mla_w_in=f(inputs["mla_w_in"][0]), mla_q_norm=f(inputs["mla_q_norm"][0]), mla_kv_norm=f(inputs["mla_kv_norm"][0]),
        mla_w_uq=f(inputs["mla_w_uq"][0]), mla_w_ukv=f(inputs["mla_w_ukv"][0]), mla_w_o=f(inputs["mla_w_o"][0]),
        diff_w_in=f(inputs["diff_w_in"][0]), diff_lambda=f(inputs["diff_lambda"][0]).reshape(1, 256),
        diff_subln=f(inputs["diff_subln"][0]).reshape(128, 1), diff_w_o=f(inputs["diff_w_o"][0]),
        router_w=f(inputs["router_w"]), moe_w_in=f(inputs["moe_w_in"]), moe_w_out=f(inputs["moe_w_out"]),
    )
    shared.update(_consts())
    maps = []
    for c in range(ncores):
        b = c % 4
        m = dict(shared)
        m["x"] = f(inputs["x"][b])
        m["cT"] = f(np.asarray(inputs["c"][b]).reshape(8, 128).T)
        m["pos"] = f(np.asarray(inputs["positions"][b]).reshape(1, S_LEN).astype(np.int32))
        maps.append(m)
    return maps


def kernel(**inputs):
    nc = build()
    maps = make_in_maps(inputs, 8)
    res = run_bass_kernel_spmd(nc, maps, core_ids=list(range(8)))
    return np.stack([np.asarray(res.results[b]["out"], dtype=np.float32) for b in range(4)], axis=0)
```

```python
import math
from contextlib import ExitStack

import numpy as np
import ml_dtypes
import concourse.bass as bass
import concourse.mybir as mybir
from concourse.bass_utils import run_bass_kernel_spmd

F32 = mybir.dt.float32
BF16 = mybir.dt.bfloat16
I32 = mybir.dt.int32
ALU = mybir.AluOpType
AF = mybir.ActivationFunctionType

S_LEN = 4096
D = 1024
NBLK = S_LEN // 128
NE = 16
CAP = 512
FF = 2048
ALPHA = 4 ** 0.25
LAMBDA_INIT1 = 0.8 - 0.6 * math.exp(-0.3)
PI = math.pi
DSPLIT = 768
DSPLIT2 = 512


class Sched:
    def __init__(self, nc, es):
        self.nc = nc
        self.es = es
        self.engs = {"pe": nc.tensor, "act": nc.scalar, "dve": nc.vector, "pool": nc.gpsimd, "sp": nc.sync}
        self.prog = {}
        for e in self.engs:
            self.prog[e] = [es.enter_context(nc.semaphore("prog_" + e)), 0]
        self.known = {e: {} for e in self.engs}
        self.lastw = {}
        self.readers = {}
        self.dsem = {}
        self.free_sems = []
        self.nsem = 0

    def _wait(self, e, tok):
        if tok is None:
            return
        sem, val = tok
        k = self.known[e]
        if k.get(sem.num, 0) >= val:
            return
        self.engs[e].wait_ge(sem, val)
        k[sem.num] = val

    def deps(self, e, reads, writes):
        for r in reads:
            self._wait(e, self.lastw.get(r))
        for w in writes:
            self._wait(e, self.lastw.get(w))
            for tok in list(self.readers.get(w, {}).values()):
                self._wait(e, tok)

    def commit(self, tok, reads, writes):
        for r in reads:
            self.readers.setdefault(r, {})[tok[0].num] = tok
        for w in writes:
            self.lastw[w] = tok
            self.readers[w] = {}

    def op(self, e, fn, reads=(), writes=(), cwrites=None):
        return self.ops(e, [fn], reads, writes, cwrites)

    def ops(self, e, fns, reads=(), writes=(), cwrites=None):
        self.deps(e, reads, writes)
        ins = None
        for fn in fns:
            ins = fn()
        p = self.prog[e]
        p[1] += 1
        ins.then_inc(p[0], 1)
        tok = (p[0], p[1])
        self.commit(tok, reads, writes if cwrites is None else cwrites)
        return tok

    def dma(self, q, key, fn, reads=(), writes=()):
        self.deps(q, reads, writes)
        ins = fn()
        if key not in self.dsem:
            if self.free_sems:
                self.dsem[key] = self.free_sems.pop()
            else:
                self.dsem[key] = [self.es.enter_context(self.nc.semaphore("d%d" % self.nsem)), 0]
                self.nsem += 1
        s = self.dsem[key]
        s[1] += 16
        ins.then_inc(s[0], 16)
        tok = (s[0], s[1])
        self.commit(tok, reads, writes)
        return tok

    def _all(self):
        toks = [(p[0], p[1]) for p in self.prog.values() if p[1] > 0]
        toks += [(s[0], s[1]) for s in self.dsem.values() if s[1] > 0]
        return toks

    def barrier(self):
        toks = self._all()
        for e in self.engs:
            for t in toks:
                self._wait(e, t)
        self.lastw = {}
        self.readers = {}
        self.free_sems.extend(self.dsem.values())
        self.dsem = {}

    def finish(self, e="sp"):
        for t in self._all():
            self._wait(e, t)


def build(upto=99, dbg=None):
    nc = bass.Bass("TRN2", target_bir_lowering=False)

    def din(name, shape, dt=F32):
        return nc.dram_tensor(name, list(shape), dt, kind="ExternalInput").ap()

    def dscr(name, shape, dt):
        return nc.dram_tensor(name, list(shape), dt, kind="Internal").ap()

    x_in = din("x", [S_LEN, D])
    cT = din("cT", [128, 8])
    pos = din("pos", [1, S_LEN], I32)
    ada_w = din("ada_w", [2, D, 6 * D])
    ada_b = din("ada_b", [2, 6 * D])
    ln1_g = din("ln1_g", [2, D]); ln1_b = din("ln1_b", [2, D])
    ln2_g = din("ln2_g", [2, D]); ln2_b = din("ln2_b", [2, D])
    mla_w_in = din("mla_w_in", [D, 704])
    mla_q_norm = din("mla_q_norm", [384]); mla_kv_norm = din("mla_kv_norm", [256])
    mla_w_uq = din("mla_w_uq", [384, 1536])
    mla_w_ukv = din("mla_w_ukv", [256, 2048])
    mla_w_o = din("mla_w_o", [D, D])
    diff_w_in = din("diff_w_in", [D, 3072])
    diff_lambda = din("diff_lambda", [1, 256])
    diff_subln = din("diff_subln", [128, 1])
    diff_w_o = din("diff_w_o", [D, D])
    router_w = din("router_w", [2, D, NE])
    moe_w_in = din("moe_w_in", [2, NE, D, 2 * FF])
    moe_w_out = din("moe_w_out", [2, NE, FF, D])
    c_inv_m = din("c_inv_m", [64, 1])
    c_inv_d = din("c_inv_d", [128, 1])
    c_ident = din("c_ident", [128, 128])
    c_ltri = din("c_ltri", [128, 128])
    c_iota = din("c_iota", [128, CAP])
    c_rcp = din("c_rcp", [128, NBLK, 2])

    out = nc.dram_tensor("out", [S_LEN, D], F32, kind="ExternalOutput").ap()

    MODS = dscr("MODS", [2, 6 * D], F32)
    QT = dscr("QT", [8, 192, S_LEN], BF16)
    KT = dscr("KT", [8, 128, S_LEN], BF16)
    KPE = dscr("KPE", [64, S_LEN], BF16)
    VV = dscr("VV", [S_LEN, D], BF16)
    OT = dscr("OT", [D, S_LEN], BF16)
    XMID = dscr("XMID", [S_LEN, D], F32)
    H2 = dscr("H2", [S_LEN, D], BF16)
    FACC = dscr("FACC", [S_LEN, D], F32)
    X1 = dscr("X1", [S_LEN, D], F32)

    es = ExitStack()
    with es:
        S = Sched(nc, es)
        uid = [0]

        def sb(stack, shape, dt, name=None):
            uid[0] += 1
            return stack.enter_context(nc.sbuf_tensor("%s_%d" % (name or "t", uid[0]), list(shape), dt))

        dbanks = [es.enter_context(nc.psum_tensor("dbank%d" % i, [128, 1024], F32)) for i in range(4)]
        banks = [dbanks[i // 2][:, (i % 2) * 512:(i % 2 + 1) * 512] for i in range(8)]
        ident_f = sb(es, [128, 128], F32, "identf")
        ident_b = sb(es, [128, 128], BF16, "identb")
        ones_b = sb(es, [128, 128], BF16, "onesb")
        aff = sb(es, [128, NBLK, NE], F32, "aff")
        eps5 = sb(es, [128, 1], F32, "eps5")
        S.dma("sp", "c0", lambda: nc.sync.dma_start(out=ident_f[:], in_=c_ident), writes=["identf"])
        S.op("dve", lambda: nc.vector.tensor_copy(out=ident_b[:], in_=ident_f[:]), reads=["identf"], writes=["identb"])
        S.op("dve", lambda: nc.vector.memset(ones_b[:], 1.0), writes=["onesb"])
        S.op("dve", lambda: nc.vector.memset(eps5[:], 1e-5), writes=["eps5"])

        V = nc.vector
        A = nc.scalar
        PE = nc.tensor
        G = nc.gpsimd

        def bcast_load(stack, src_row_ap, name, plus_one=False, q="sp"):
            n = src_row_ap.shape[-1]
            t = sb(stack, [128, n], F32, name)
            key = "%s_%d" % (name, uid[0])
            S.dma(q, "bc_" + key, lambda: S.engs[q].dma_start(out=t[:], in_=src_row_ap.partition_broadcast(128)), writes=[key])
            if plus_one:
                S.op("pool", lambda: G.tensor_scalar(out=t[:], in0=t[:], scalar1=1.0, scalar2=None, op0=ALU.add), reads=[key], writes=[key])
            return t, key

        def phase_mods():
            with ExitStack() as ph:
                c_sb = sb(ph, [128, 8], F32, "c")
                c16 = sb(ph, [128, 8], BF16, "c16")
                adab = sb(ph, [1, 6 * D], F32, "adab")
                modrow = sb(ph, [1, 6 * D], F32, "modrow")
                wblk = [sb(ph, [128, 8, 512], BF16, "adaw") for _ in range(3)]
                S.dma("sp", "m_c", lambda: nc.sync.dma_start(out=c_sb[:], in_=cT), writes=["c"])
                S.op("act", lambda: A.activation(out=c16[:], in_=c_sb[:], func=AF.Silu), reads=["c"], writes=["c16"])
                it = 0
                for l in range(2):
                    S.dma("sp", "m_b", lambda: nc.sync.dma_start(out=adab[:], in_=ada_b[l:l + 1, :]), writes=["adab"])
                    wv = ada_w[l].rearrange("(k p) n -> p k n", p=128)
                    for j in range(12):
                        sl = it % 3
                        S.dma("pool", "m_w%d" % sl, lambda: G.dma_start(out=wblk[sl][:], in_=wv[:, :, j * 512:(j + 1) * 512]), writes=["adaw%d" % sl])
                        pb = banks[it % 2]
                        S.ops("pe", [(lambda k=k: PE.matmul(pb[0:1, :], lhsT=c16[:, k:k + 1], rhs=wblk[sl][:, k, :], start=(k == 0), stop=(k == 7))) for k in range(8)],
                              reads=["c16", "adaw%d" % sl], writes=["bank%d" % (it % 2)])
                        S.op("dve", lambda: V.tensor_tensor(out=modrow[0:1, j * 512:(j + 1) * 512], in0=pb[0:1, :], in1=adab[0:1, j * 512:(j + 1) * 512], op=ALU.add),
                             reads=["bank%d" % (it % 2), "adab"], writes=["modrow"])
                        it += 1
                    S.dma("sp", "m_o", lambda: nc.sync.dma_start(out=MODS[l:l + 1, :], in_=modrow[:]), reads=["modrow"], writes=["MODS"])
                S.barrier()

        def rope_tables(ph, R, inv_ap):
            Ct = sb(ph, [R, S_LEN], F32, "ropeC")
            St = sb(ph, [R, S_LEN], F32, "ropeS")
            inv = sb(ph, [R, 1], F32, "inv")
            negpi = sb(ph, [R, 1], F32, "negpi")
            S.dma("sp", "r_inv", lambda: nc.sync.dma_start(out=inv[:], in_=inv_ap), writes=["inv"])
            with ExitStack() as tmp:
                pi_t = sb(tmp, [R, 1024], I32, "posi")
                ang = sb(tmp, [R, 1024], F32, "ang")
                nf = sb(tmp, [R, 1024], F32, "nf")
                ni = sb(tmp, [R, 1024], I32, "ni")
                msk = sb(tmp, [R, 1024], F32, "msk")
                for cch in range(4):
                    cs = slice(cch * 1024, (cch + 1) * 1024)
                    S.dma("sp", "r_pos", lambda: nc.sync.dma_start(out=pi_t[:], in_=pos[:, cs].partition_broadcast(R)), writes=["posi"])
                    S.op("dve", lambda: V.tensor_copy(out=ang[:], in_=pi_t[:]), reads=["posi"], writes=["ang"])
                    S.op("dve", lambda: V.tensor_scalar(out=ang[:], in0=ang[:], scalar1=inv[:, 0:1], scalar2=None, op0=ALU.mult), reads=["ang", "inv"], writes=["ang"])
                    S.op("dve", lambda: V.tensor_scalar(out=nf[:], in0=ang[:], scalar1=1.0 / (2 * PI), scalar2=None, op0=ALU.mult), reads=["ang"], writes=["nf"])
                    S.op("dve", lambda: V.tensor_copy(out=ni[:], in_=nf[:]), reads=["nf"], writes=["ni"])
                    S.op("dve", lambda: V.tensor_copy(out=nf[:], in_=ni[:]), reads=["ni"], writes=["nf"])
                    S.op("dve", lambda: V.scalar_tensor_tensor(out=ang[:], in0=nf[:], scalar=-6.28125, in1=ang[:], op0=ALU.mult, op1=ALU.add), reads=["nf", "ang"], writes=["ang"])
                    S.op("dve", lambda: V.scalar_tensor_tensor(out=ang[:], in0=nf[:], scalar=-0.0019353071795864769, in1=ang[:], op0=ALU.mult, op1=ALU.add), reads=["nf", "ang"], writes=["ang"])

                    def wrap():
                        S.op("dve", lambda: V.tensor_scalar(out=msk[:], in0=ang[:], scalar1=PI, scalar2=-2 * PI, op0=ALU.is_gt, op1=ALU.mult), reads=["ang"], writes=["msk"])
                        S.op("dve", lambda: V.tensor_tensor(out=ang[:], in0=ang[:], in1=msk[:], op=ALU.add), reads=["ang", "msk"], writes=["ang"])
                        S.op("dve", lambda: V.tensor_scalar(out=msk[:], in0=ang[:], scalar1=-PI, scalar2=2 * PI, op0=ALU.is_lt, op1=ALU.mult), reads=["ang"], writes=["msk"])
                        S.op("dve", lambda: V.tensor_tensor(out=ang[:], in0=ang[:], in1=msk[:], op=ALU.add), reads=["ang", "msk"], writes=["ang"])
                        S.op("dve", lambda: V.tensor_scalar(out=ang[:], in0=ang[:], scalar1=PI, scalar2=-PI, op0=ALU.min, op1=ALU.max), reads=["ang"], writes=["ang"])
                    wrap()
                    S.op("act", lambda: A.activation(out=St[:, cs], in_=ang[:], func=AF.Sin), reads=["ang"], writes=["ropeS"])
                    S.op("dve", lambda: V.tensor_scalar(out=ang[:], in0=ang[:], scalar1=PI / 2, scalar2=None, op0=ALU.add), reads=["ang"], writes=["ang"])
                    wrap()
                    S.op("act", lambda: A.activation(out=Ct[:, cs], in_=ang[:], func=AF.Sin), reads=["ang"], writes=["ropeC"])
                S.barrier()
            return Ct, St

        def front_block(tb, xsrc, xin, h16, hT, sc1p, sh1p, kx):
            h16s = h16
            sl = tb % 2
            h16 = h16[sl % len(h16)]
            hT = hT[sl]
            hk, tk = "h16_%d" % (sl % len(h16s)), "hT%d" % sl
            xv = xsrc.rearrange("(t s p) d -> t p s d", s=4, p=128)
            S.dma("sp", "xin%d" % sl, lambda: nc.sync.dma_start(out=xin[sl][:], in_=xv[tb]), writes=["xin%d" % sl])
            S.op("dve", lambda: V.tensor_tensor(out=xin[sl][:], in0=xin[sl][:], in1=sc1p[:].unsqueeze(1).broadcast_to([128, 4, D]), op=ALU.mult),
                 reads=["xin%d" % sl, kx[0]], writes=["xin%d" % sl])
            S.op("pool", lambda: G.tensor_tensor(out=h16[:], in0=xin[sl][:], in1=sh1p[:].unsqueeze(1).broadcast_to([128, 4, D]), op=ALU.add),
                 reads=["xin%d" % sl, kx[1]], writes=[hk])
            for kp in range(4):
                bk = banks[6 + kp % 2]
                bkey = "bank%d" % (6 + kp % 2)
                tv = bk[:].bitcast(BF16)
                fns = []
                for kk in range(2):
                    k = kp * 2 + kk
                    for s in range(4):
                        fns.append(lambda k=k, kk=kk, s=s: PE.transpose(tv[:, kk * 512 + s * 128: kk * 512 + (s + 1) * 128], h16[:, s, k * 128:(k + 1) * 128], ident_b[:]))
                S.ops("pe", fns, reads=[hk, "identb"], writes=[bkey])
                S.op("act", lambda: A.copy(out=hT[:, kp * 2:kp * 2 + 2, :].rearrange("p a b -> p (a b)"), in_=tv), reads=[bkey], writes=[tk])

        pring = [0]

        def next_bank(lo=0, hi=6):
            b = lo + pring[0] % (hi - lo)
            pring[0] += 1
            return banks[b], "bank%d" % b

        def rsqrt_into(dst, dkey, src_ap, skey, mul, add):
            S.op("dve", lambda: V.tensor_scalar(out=dst, in0=src_ap, scalar1=mul, scalar2=add, op0=ALU.mult, op1=ALU.add), reads=[skey], writes=[dkey])
            S.op("act", lambda: A.activation(out=dst, in_=dst, func=AF.Sqrt), reads=[dkey], writes=[dkey])
            S.op("dve", lambda: V.reciprocal(out=dst, in_=dst), reads=[dkey], writes=[dkey])

        def phase_a_mla(xsrc, l):
            with ExitStack() as ph:
                Ct, St = rope_tables(ph, 64, c_inv_m)
                sc1p, k_sc = bcast_load(ph, MODS[l:l + 1, D:2 * D], "sc1p", plus_one=True)
                sh1p, k_sh = bcast_load(ph, MODS[l:l + 1, 0:D], "sh1p")
                w_in = sb(ph, [128, 8, 768], BF16, "w_in")
                w_uq = sb(ph, [128, 3, 8, 256], BF16, "w_uq")
                w_k = sb(ph, [128, 2, 8, 128], BF16, "w_k")
                w_v = sb(ph, [128, 2, 8, 128], BF16, "w_v")
                qn = sb(ph, [128, 3], F32, "qn")
                kvn = sb(ph, [128, 2], F32, "kvn")
                S.dma("pool", "a_w0", lambda: G.dma_start(out=w_in[:, :, 0:704], in_=mla_w_in.rearrange("(k p) n -> p k n", p=128)), writes=["w_in"])
                S.op("act", lambda: A.mul(out=w_in[:, :, 704:736], in_=w_in[:, :, 672:704], mul=-1.0), reads=["w_in"], writes=["w_in"])
                S.op("act", lambda: A.copy(out=w_in[:, :, 736:768], in_=w_in[:, :, 640:672]), reads=["w_in"], writes=["w_in"])
                uqv = mla_w_uq.rearrange("(k p) (h e) -> p k h e", p=128, e=192)
                for k in range(3):
                    S.dma("pool", "a_w1", lambda: G.dma_start(out=w_uq[:, k, :, 0:192], in_=uqv[:, k, :, :]), writes=["w_uq"])
                S.op("act", lambda: A.mul(out=w_uq[:, :, :, 192:224], in_=w_uq[:, :, :, 160:192], mul=-1.0), reads=["w_uq"], writes=["w_uq"])
                S.op("act", lambda: A.copy(out=w_uq[:, :, :, 224:256], in_=w_uq[:, :, :, 128:160]), reads=["w_uq"], writes=["w_uq"])
                wkv = mla_w_ukv.rearrange("(k p) (h two e) -> p k h two e", p=128, two=2, e=128)
                for k in range(2):
                    S.dma("pool", "a_w2", lambda: G.dma_start(out=w_k[:, k, :, :], in_=wkv[:, k, :, 0, :]), writes=["w_k"])
                    S.dma("pool", "a_w3", lambda: G.dma_start(out=w_v[:, k, :, :], in_=wkv[:, k, :, 1, :]), writes=["w_v"])
                with nc.allow_non_contiguous_dma(reason="tiny norm-gain vectors"):
                    S.dma("sp", "a_n0", lambda: nc.sync.dma_start(out=qn[:], in_=mla_q_norm.rearrange("(k p) -> p k", p=128)), writes=["qn"])
                    S.dma("sp", "a_n1", lambda: nc.sync.dma_start(out=kvn[:], in_=mla_kv_norm.rearrange("(k p) -> p k", p=128)), writes=["kvn"])
                xin = [sb(ph, [128, 4, D], F32, "xin") for _ in range(2)]
                h16 = [sb(ph, [128, 4, D], BF16, "h16")]
                hT = [sb(ph, [128, 8, 512], BF16, "hT") for _ in range(2)]
                lat = sb(ph, [128, 7, 512], F32, "lat")
                sq = sb(ph, [128, 5, 512], BF16, "sq")
                rstd = sb(ph, [128, 2, 512], F32, "rstd")
                cqn = sb(ph, [128, 3, 512], BF16, "cqn")
                ckvn = sb(ph, [128, 2, 512], BF16, "ckvn")
                kpe = sb(ph, [64, 512], BF16, "kpe")
                tmp1 = sb(ph, [64, 512], F32, "tmp1")
                tmp2 = sb(ph, [64, 512], F32, "tmp2")
                qst = sb(ph, [128, 8, 512], BF16, "qst")
                qrst = sb(ph, [64, 8, 512], BF16, "qrst")
                kst = sb(ph, [128, 8, 512], BF16, "kst")
                vst = sb(ph, [128, 4, D], BF16, "vst")
                mspec = [(0, 128, 128), (1, 256, 128), (2, 384, 128), (3, 512, 128), (4, 640, 128), (5, 704, 64), (6, 768, 64)]
                front_block(0, xsrc, xin, h16, hT, sc1p, sh1p, (k_sc, k_sh))
                for tb in range(8):
                    ts = slice(tb * 512, (tb + 1) * 512)
                    if tb + 1 < 8:
                        front_block(tb + 1, xsrc, xin, h16, hT, sc1p, sh1p, (k_sc, k_sh))
                    hTc, hTk = hT[tb % 2], "hT%d" % (tb % 2)
                    for (mi, hi_, mm) in mspec:
                        pb, pk = next_bank()
                        S.ops("pe", [(lambda k=k: PE.matmul(pb[0:mm, :], lhsT=w_in[:, k, hi_ - mm:hi_], rhs=hTc[:, k, :], start=(k == 0), stop=(k == 7))) for k in range(8)],
                              reads=["w_in", hTk], writes=[pk])
                        S.op("act", lambda: A.copy(out=lat[0:mm, mi, :], in_=pb[0:mm, :]), reads=[pk], writes=["lat%d" % mi])
                        if mi < 5:
                            S.op("dve", lambda: V.tensor_tensor(out=sq[:, mi, :], in0=lat[:, mi, :], in1=lat[:, mi, :], op=ALU.mult), reads=["lat%d" % mi], writes=["sq%d" % mi])
                    for gi, (c0, c1, n) in enumerate([(0, 3, 384), (3, 5, 256)]):
                        pb, pk = next_bank()
                        S.ops("pe", [(lambda c=c: PE.matmul(pb[:, :], lhsT=ones_b[:], rhs=sq[:, c, :], start=(c == c0), stop=(c == c1 - 1))) for c in range(c0, c1)],
                              reads=["onesb"] + ["sq%d" % c for c in range(c0, c1)], writes=[pk])
                        rsqrt_into(rstd[:, gi, :], "rstd%d" % gi, pb[:, :], pk, 1.0 / n, 1e-6)
                    for c in range(3):
                        S.op("dve", lambda: V.scalar_tensor_tensor(out=cqn[:, c, :], in0=lat[:, c, :], scalar=qn[:, c:c + 1], in1=rstd[:, 0, :], op0=ALU.mult, op1=ALU.mult),
                             reads=["lat%d" % c, "qn", "rstd0"], writes=["cqn"])
                    for c in range(2):
                        S.op("dve", lambda: V.scalar_tensor_tensor(out=ckvn[:, c, :], in0=lat[:, 3 + c, :], scalar=kvn[:, c:c + 1], in1=rstd[:, 1, :], op0=ALU.mult, op1=ALU.mult),
                             reads=["lat%d" % (3 + c), "kvn", "rstd1"], writes=["ckvn"])
                    S.op("dve", lambda: V.tensor_tensor(out=tmp1[:], in0=lat[0:64, 5, :], in1=Ct[:, ts], op=ALU.mult), reads=["lat5"], writes=["tmp1"])
                    S.op("pool", lambda: G.tensor_tensor(out=tmp2[:], in0=lat[0:64, 6, :], in1=St[:, ts], op=ALU.mult), reads=["lat6"], writes=["tmp2"])
                    S.op("dve", lambda: V.tensor_tensor(out=kpe[:], in0=tmp1[:], in1=tmp2[:], op=ALU.add), reads=["tmp1", "tmp2"], writes=["kpe"])
                    S.dma("sp", "a_kpe", lambda: nc.sync.dma_start(out=KPE[:, ts], in_=kpe[:]), reads=["kpe"], writes=["KPE"])
                    for h in range(8):
                        pb, pk = next_bank()
                        S.ops("pe", [(lambda c=c: PE.matmul(pb[:, :], lhsT=w_uq[:, c, h, 0:128], rhs=cqn[:, c, :], start=(c == 0), stop=(c == 2))) for c in range(3)],
                              reads=["w_uq", "cqn"], writes=[pk])
                        S.op("act", lambda: A.copy(out=qst[:, h, :], in_=pb[:, :]), reads=[pk], writes=["qst"])
                        pa, pka = next_bank()
                        S.ops("pe", [(lambda c=c: PE.matmul(pa[0:64, :], lhsT=w_uq[:, c, h, 128:192], rhs=cqn[:, c, :], start=(c == 0), stop=(c == 2))) for c in range(3)],
                              reads=["w_uq", "cqn"], writes=[pka])
                        pr, pkr = next_bank()
                        S.ops("pe", [(lambda c=c: PE.matmul(pr[0:64, :], lhsT=w_uq[:, c, h, 192:256], rhs=cqn[:, c, :], start=(c == 0), stop=(c == 2))) for c in range(3)],
                              reads=["w_uq", "cqn"], writes=[pkr])
                        S.op("dve", lambda: V.tensor_tensor(out=tmp1[:], in0=pa[0:64, :], in1=Ct[:, ts], op=ALU.mult), reads=[pka], writes=["tmp1"])
                        S.op("dve", lambda: V.tensor_tensor(out=tmp2[:], in0=pr[0:64, :], in1=St[:, ts], op=ALU.mult), reads=[pkr], writes=["tmp2"])
                        S.op("pool", lambda: G.tensor_tensor(out=qrst[:, h, :], in0=tmp1[:], in1=tmp2[:], op=ALU.add), reads=["tmp1", "tmp2"], writes=["qrst"])
                        pk_, pkk = next_bank()
                        S.ops("pe", [(lambda c=c: PE.matmul(pk_[:, :], lhsT=w_k[:, c, h, :], rhs=ckvn[:, c, :], start=(c == 0), stop=(c == 1))) for c in range(2)],
                              reads=["w_k", "ckvn"], writes=[pkk])
                        S.op("act", lambda: A.copy(out=kst[:, h, :], in_=pk_[:, :]), reads=[pkk], writes=["kst"])
                    S.dma("sp", "a_q", lambda: nc.sync.dma_start(out=QT[:, 0:128, ts].rearrange("h p t -> p h t"), in_=qst[:]), reads=["qst"], writes=["QT"])
                    S.dma("sp", "a_qr", lambda: nc.sync.dma_start(out=QT[:, 128:192, ts].rearrange("h p t -> p h t"), in_=qrst[:]), reads=["qrst"], writes=["QT"])
                    S.dma("sp", "a_k", lambda: nc.sync.dma_start(out=KT[:, :, ts].rearrange("h p t -> p h t"), in_=kst[:]), reads=["kst"], writes=["KT"])
                    for s in range(4):
                        for hf in range(2):
                            pb, pk = next_bank()
                            S.ops("pe", [(lambda c=c: PE.matmul(pb[:, :], lhsT=ckvn[:, c, s * 128:(s + 1) * 128], rhs=w_v[:, c, hf * 4:(hf + 1) * 4, :].rearrange("p h e -> p (h e)"), start=(c == 0), stop=(c == 1))) for c in range(2)],
                                  reads=["w_v", "ckvn"], writes=[pk])
                            S.op("act" if hf else "dve", (lambda pb=pb, hf=hf: (A.copy if hf else V.tensor_copy)(out=vst[:, s, hf * 512:(hf + 1) * 512], in_=pb[:, :])), reads=[pk], writes=["vst"])
                    S.dma("sp", "a_v", lambda: nc.sync.dma_start(out=VV.rearrange("(t s p) d -> t p s d", s=4, p=128)[tb], in_=vst[:]), reads=["vst"], writes=["VV"])
                S.barrier()

        def phase_a_diff(xsrc, l):
            with ExitStack() as ph:
                Ct, St = rope_tables(ph, 128, c_inv_d)
                sc1p, k_sc = bcast_load(ph, MODS[l:l + 1, D:2 * D], "sc1p", plus_one=True)
                sh1p, k_sh = bcast_load(ph, MODS[l:l + 1, 0:D], "sh1p")
                w = sb(ph, [128, 8, 3072], BF16, "dw")
                wr = sb(ph, [128, 8, 2048], BF16, "dwr")
                wsrc = diff_w_in.rearrange("(k p) n -> p k n", p=128)
                for j in range(3):
                    S.dma("pool", "d_w%d" % j, lambda: G.dma_start(out=w[:, :, j * 1024:(j + 1) * 1024], in_=wsrc[:, :, j * 1024:(j + 1) * 1024]), writes=["dw"])
                S.op("pool", lambda: G.memset(wr[:], 0.0), writes=["dwr"])
                w4 = w[:, :, 0:2048].rearrange("p k (g e) -> p k g e", e=64)
                wr4 = wr[:].rearrange("p k (g e) -> p k g e", e=64)
                for k in range(8):
                    S.op("act", lambda: A.mul(out=wr4[:, k, :, 0:8], in_=w4[:, k, :, 8:16], mul=-1.0), reads=["dw", "dwr"], writes=["dwr"])
                    S.op("act", lambda: A.copy(out=wr4[:, k, :, 8:16], in_=w4[:, k, :, 0:8]), reads=["dw", "dwr"], writes=["dwr"])
                xin = [sb(ph, [128, 4, D], F32, "xin") for _ in range(2)]
                h16 = [sb(ph, [128, 4, D], BF16, "h16")]
                hT = [sb(ph, [128, 8, 512], BF16, "hT") for _ in range(2)]
                tmp1 = sb(ph, [128, 512], F32, "tmp1")
                tmp2 = sb(ph, [128, 512], F32, "tmp2")
                qst = sb(ph, [128, 8, 512], BF16, "qst")
                kst = sb(ph, [128, 8, 512], BF16, "kst")
                vst = sb(ph, [128, 4, D], BF16, "vst")
                front_block(0, xsrc, xin, h16, hT, sc1p, sh1p, (k_sc, k_sh))
                for tb in range(8):
                    ts = slice(tb * 512, (tb + 1) * 512)
                    if tb + 1 < 8:
                        front_block(tb + 1, xsrc, xin, h16, hT, sc1p, sh1p, (k_sc, k_sh))
                    hTc, hTk = hT[tb % 2], "hT%d" % (tb % 2)
                    for qk in range(2):
                        st = qst if qk == 0 else kst
                        skey = "qst" if qk == 0 else "kst"
                        for h in range(8):
                            c0 = qk * 1024 + h * 128
                            pa, pka = next_bank()
                            S.ops("pe", [(lambda k=k: PE.matmul(pa[:, :], lhsT=w[:, k, c0:c0 + 128], rhs=hTc[:, k, :], start=(k == 0), stop=(k == 7))) for k in range(8)],
                                  reads=["dw", hTk], writes=[pka])
                            pr, pkr = next_bank()
                            S.ops("pe", [(lambda k=k: PE.matmul(pr[:, :], lhsT=wr[:, k, c0:c0 + 128], rhs=hTc[:, k, :], start=(k == 0), stop=(k == 7))) for k in range(8)],
                                  reads=["dwr", hTk], writes=[pkr])
                            S.op("dve", lambda: V.tensor_tensor(out=tmp1[:], in0=pa[:, :], in1=Ct[:, ts], op=ALU.mult), reads=[pka], writes=["tmp1"])
                            S.op("dve", lambda: V.tensor_tensor(out=tmp2[:], in0=pr[:, :], in1=St[:, ts], op=ALU.mult), reads=[pkr], writes=["tmp2"])
                            S.op("pool", lambda: G.tensor_tensor(out=st[:, h, :], in0=tmp1[:], in1=tmp2[:], op=ALU.add), reads=["tmp1", "tmp2"], writes=[skey])
                    S.dma("sp", "a_q", lambda: nc.sync.dma_start(out=QT[:, 0:128, ts].rearrange("h p t -> p h t"), in_=qst[:]), reads=["qst"], writes=["QT"])
                    S.dma("sp", "a_k", lambda: nc.sync.dma_start(out=KT[:, :, ts].rearrange("h p t -> p h t"), in_=kst[:]), reads=["kst"], writes=["KT"])
                    for s in range(4):
                        for hf in range(2):
                            pb, pk = next_bank()
                            S.ops("pe", [(lambda k=k: PE.matmul(pb[:, :], lhsT=hTc[:, k, s * 128:(s + 1) * 128], rhs=w[:, k, 2048 + hf * 512:2048 + (hf + 1) * 512], start=(k == 0), stop=(k == 7))) for k in range(8)],
                                  reads=["dw", hTk], writes=[pk])
                            S.op("act", lambda: A.copy(out=vst[:, s, hf * 512:(hf + 1) * 512], in_=pb[:, :]), reads=[pk], writes=["vst"])
                    S.dma("sp", "a_v", lambda: nc.sync.dma_start(out=VV.rearrange("(t s p) d -> t p s d", s=4, p=128)[tb], in_=vst[:]), reads=["vst"], writes=["VV"])
                S.barrier()

        def phase_b(kind):
            mla = kind == "mla"
            nmap = 1 if mla else 2
            scale = (192 ** -0.5) if mla else (64 ** -0.5)
            with ExitStack() as ph:
                kbuf = [sb(ph, [128, S_LEN], BF16, "kbuf") for _ in range(2)]
                qbuf = [sb(ph, [128, S_LEN], BF16, "qbuf") for _ in range(2)]
                vbuf = [sb(ph, [128, NBLK, 128], BF16, "vbuf") for _ in range(2)]
                NPT = 3
                PT = [sb(ph, [128, 1024], BF16, "PT") for _ in range(NPT)]
                dacc = [sb(ph, [128, 1024], F32, "dacc") for _ in range(2)]
                ones_f = sb(ph, [128, 128], F32, "onesf")
                S.op("dve", lambda: V.memset(ones_f[:], 1.0), writes=["onesf"])
                rden = [sb(ph, [128, 512], F32, "rden") for _ in range(2)]
                osb = [sb(ph, [128, 512], BF16, "osb") for _ in range(2)]
                if mla:
                    qrbuf = [sb(ph, [64, S_LEN], BF16, "qrbuf") for _ in range(2)]
                    kpe = sb(ph, [64, S_LEN], BF16, "kpeall")
                    S.dma("sp", "b_kpe", lambda: nc.sync.dma_start(out=kpe[:], in_=KPE), writes=["kpeall"])
                else:
                    lamt = sb(ph, [128, 256], F32, "lamt")
                    lp = sb(ph, [128, 128], F32, "lp")
                    ls = sb(ph, [128, 2], F32, "ls")
                    neglam = sb(ph, [128, 1], F32, "neglam")
                    subs = sb(ph, [128, 1], F32, "subs")
                    o1n = sb(ph, [128, 512], F32, "o1n")
                    o2n = sb(ph, [128, 512], F32, "o2n")
                    sq = sb(ph, [128, 512], BF16, "osq")
                    rstd = sb(ph, [128, 512], F32, "orstd")
                    S.dma("sp", "b_lam", lambda: nc.sync.dma_start(out=lamt[:], in_=diff_lambda.partition_broadcast(128)), writes=["lamt"])
                    S.dma("sp", "b_sub", lambda: nc.sync.dma_start(out=subs[:], in_=diff_subln), writes=["subs"])
                    S.op("dve", lambda: V.tensor_tensor(out=lp[:, 0:64], in0=lamt[:, 0:64], in1=lamt[:, 64:128], op=ALU.mult), reads=["lamt"], writes=["lp"])
                    S.op("dve", lambda: V.tensor_tensor(out=lp[:, 64:128], in0=lamt[:, 128:192], in1=lamt[:, 192:256], op=ALU.mult), reads=["lamt", "lp"], writes=["lp"])
                    S.op("dve", lambda: V.reduce_sum(out=ls[:, 0:1], in_=lp[:, 0:64], axis=mybir.AxisListType.X), reads=["lp"], writes=["ls"])
                    S.op("dve", lambda: V.reduce_sum(out=ls[:, 1:2], in_=lp[:, 64:128], axis=mybir.AxisListType.X), reads=["lp", "ls"], writes=["ls"])
                    S.op("act", lambda: A.activation(out=ls[:], in_=ls[:], func=AF.Exp), reads=["ls"], writes=["ls"])
                    S.op("dve", lambda: V.tensor_tensor(out=neglam[:], in0=ls[:, 1:2], in1=ls[:, 0:1], op=ALU.subtract), reads=["ls"], writes=["neglam"])
                    S.op("dve", lambda: V.tensor_scalar(out=neglam[:], in0=neglam[:], scalar1=-LAMBDA_INIT1, scalar2=None, op0=ALU.add), reads=["neglam"], writes=["neglam"])
                    S.op("dve", lambda: V.tensor_scalar(out=subs[:], in0=subs[:], scalar1=1.0 - LAMBDA_INIT1, scalar2=None, op0=ALU.mult), reads=["subs"], writes=["subs"])

                vview = VV.rearrange("(c p) (h e) -> p c h e", p=128, e=128)

                def load_head(h):
                    sl = h % 2
                    S.dma("sp", "b_k%d" % sl, lambda: nc.sync.dma_start(out=kbuf[sl][:], in_=KT[h]), writes=["kbuf%d" % sl])
                    S.dma("sp", "b_q%d" % sl, lambda: nc.sync.dma_start(out=qbuf[sl][:], in_=QT[h, 0:128, :]), writes=["qbuf%d" % sl])
                    S.dma("sp", "b_v%d" % sl, lambda: nc.sync.dma_start(out=vbuf[sl][:], in_=vview[:, :, h, :]), writes=["vbuf%d" % sl])
                    if mla:
                        S.dma("sp", "b_qr%d" % sl, lambda: nc.sync.dma_start(out=qrbuf[sl][:], in_=QT[h, 128:192, :]), writes=["qrbuf%d" % sl])

                load_head(0)
                blk = 0
                for h in range(8):
                    if h + 1 < 8:
                        load_head(h + 1)
                    sl = h % 2
                    for qb in range(8):
                        qs = slice(qb * 512, (qb + 1) * 512)
                        par = blk % 2
                        blk += 1
                        if mla:
                            acc_o, ko = banks[4 + par], "bank%d" % (4 + par)
                            den, kd = banks[6 + par], "bank%d" % (6 + par)
                            NU = NBLK // 2

                            def qk(u):
                                db = dbanks[u % 2]
                                fns = []
                                for j in range(2):
                                    kc = 2 * u + j
                                    ks = slice(kc * 128, (kc + 1) * 128)
                                    fns.append(lambda j=j, ks=ks: PE.matmul(db[:, j * 512:(j + 1) * 512], lhsT=kbuf[sl][:, ks], rhs=qbuf[sl][:, qs], start=True, stop=False))
                                    fns.append(lambda j=j, ks=ks: PE.matmul(db[:, j * 512:(j + 1) * 512], lhsT=kpe[:, ks], rhs=qrbuf[sl][:, qs], start=False, stop=True))
                                S.ops("pe", fns, reads=["kbuf%d" % sl, "qbuf%d" % sl, "qrbuf%d" % sl, "kpeall"], writes=["bank%d" % (2 * (u % 2)), "bank%d" % (2 * (u % 2) + 1)])

                            def ex(u):
                                S.op("act", lambda: A.activation(out=PT[u % NPT][:], in_=dbanks[u % 2][:, :], func=AF.Exp, scale=scale),
                                     reads=["bank%d" % (2 * (u % 2)), "bank%d" % (2 * (u % 2) + 1)], writes=["PT%d" % (u % NPT)])

                            def pv(u):
                                fns = []
                                for j in range(2):
                                    kc = 2 * u + j
                                    fns.append(lambda j=j, kc=kc: PE.matmul(acc_o, lhsT=vbuf[sl][:, kc, :], rhs=PT[u % NPT][:, j * 512:(j + 1) * 512], start=(kc == 0), stop=(kc == NBLK - 1)))
                                S.ops("pe", fns, reads=["PT%d" % (u % NPT), "vbuf%d" % sl], writes=[ko] if u == 0 else [], cwrites=[ko] if u == NU - 1 else [])
                                for (en, EN, c0, c1) in (("dve", V, 0, DSPLIT), ("pool", G, DSPLIT, 1024)):
                                    if u == 0:
                                        S.op(en, lambda: EN.tensor_copy(out=dacc[par][:, c0:c1], in_=PT[u % NPT][:, c0:c1]), reads=["PT%d" % (u % NPT)], writes=["dacc%d%s" % (par, en)])
                                    else:
                                        S.op(en, lambda: EN.tensor_tensor(out=dacc[par][:, c0:c1], in0=dacc[par][:, c0:c1], in1=PT[u % NPT][:, c0:c1], op=ALU.add), reads=["PT%d" % (u % NPT), "dacc%d%s" % (par, en)], writes=["dacc%d%s" % (par, en)])
                            qk(0)
                            for u in range(NU):
                                ex(u)
                                if u + 1 < NU:
                                    qk(u + 1)
                                pv(u)
                            S.ops("pe", [lambda: PE.matmul(den, lhsT=ones_f[:], rhs=dacc[par][:, 0:512], start=True, stop=False),
                                         lambda: PE.matmul(den, lhsT=ones_f[:], rhs=dacc[par][:, 512:1024], start=False, stop=True)],
                                  reads=["onesf", "dacc%ddve" % par, "dacc%dpool" % par], writes=[kd])
                            S.op("act", lambda: A.activation(out=rden[par][:], in_=den, func=AF.Ln), reads=[kd], writes=["rden%d" % par])
                            S.op("act", lambda: A.activation(out=rden[par][:], in_=rden[par][:], func=AF.Exp, scale=-1.0), reads=["rden%d" % par], writes=["rden%d" % par])
                            S.op("dve", lambda: V.tensor_tensor(out=osb[par][:], in0=acc_o, in1=rden[par][:], op=ALU.mult), reads=[ko, "rden%d" % par], writes=["osb%d" % par])
                        else:
                            accs = [(banks[4 + i], "bank%d" % (4 + i)) for i in range(4)]

                            def qk(kc):
                                ks = slice(kc * 128, (kc + 1) * 128)
                                db = dbanks[kc % 2]
                                fns = []
                                for m in range(2):
                                    rows = slice(m * 64, (m + 1) * 64)
                                    fns.append(lambda m=m, rows=rows: PE.matmul(db[:, m * 512:(m + 1) * 512], lhsT=kbuf[sl][rows, ks], rhs=qbuf[sl][rows, qs], start=True, stop=True))
                                S.ops("pe", fns, reads=["kbuf%d" % sl, "qbuf%d" % sl], writes=["bank%d" % (2 * (kc % 2)), "bank%d" % (2 * (kc % 2) + 1)])

                            def ex(kc):
                                S.op("act", lambda: A.activation(out=PT[kc % NPT][:], in_=dbanks[kc % 2][:, :], func=AF.Exp, scale=scale),
                                     reads=["bank%d" % (2 * (kc % 2)), "bank%d" % (2 * (kc % 2) + 1)], writes=["PT%d" % (kc % NPT)])

                            def pv(kc):
                                first, last = kc == 0, kc == NBLK - 1
                                fns = [(lambda m=m: PE.matmul(accs[m][0], lhsT=vbuf[sl][:, kc, :], rhs=PT[kc % NPT][:, m * 512:(m + 1) * 512], start=first, stop=last)) for m in range(2)]
                                ok_ = [accs[0][1], accs[1][1]]
                                fns.append(lambda: PE.matmul(accs[3][0], lhsT=ones_b[:], rhs=PT[kc % NPT][:, 512:1024], start=first, stop=last))
                                ok_ = [accs[0][1], accs[1][1], accs[3][1]]
                                S.ops("pe", fns, reads=["PT%d" % (kc % NPT), "vbuf%d" % sl, "onesb"], writes=ok_ if first else [], cwrites=ok_ if last else [])
                                for (en, EN, c0, c1) in (("dve", V, 0, DSPLIT2), ("pool", G, DSPLIT2, 512)):
                                    if c0 == c1:
                                        continue
                                    if first:
                                        S.op(en, lambda: EN.tensor_copy(out=dacc[par][:, c0:c1], in_=PT[kc % NPT][:, c0:c1]), reads=["PT%d" % (kc % NPT)], writes=["dacc%d%s" % (par, en)])
                                    else:
                                        S.op(en, lambda: EN.tensor_tensor(out=dacc[par][:, c0:c1], in0=dacc[par][:, c0:c1], in1=PT[kc % NPT][:, c0:c1], op=ALU.add), reads=["PT%d" % (kc % NPT), "dacc%d%s" % (par, en)], writes=["dacc%d%s" % (par, en)])
                            qk(0)
                            for kc in range(NBLK):
                                ex(kc)
                                if kc + 1 < NBLK:
                                    qk(kc + 1)
                                pv(kc)
                            S.op("pe", lambda: PE.matmul(accs[2][0], lhsT=ones_f[:], rhs=dacc[par][:, 0:512], start=True, stop=True), reads=["onesf", "dacc%ddve" % par], writes=[accs[2][1]])
                            for m in range(2):
                                S.op("act", lambda: A.activation(out=rden[m][:], in_=accs[2 + m][0], func=AF.Ln), reads=[accs[2 + m][1]], writes=["rden%d" % m])
                                S.op("act", lambda: A.activation(out=rden[m][:], in_=rden[m][:], func=AF.Exp, scale=-1.0), reads=["rden%d" % m], writes=["rden%d" % m])
                            S.op("dve", lambda: V.tensor_tensor(out=o1n[:], in0=accs[0][0], in1=rden[0][:], op=ALU.mult), reads=[accs[0][1], "rden0"], writes=["o1n"])
                            S.op("dve", lambda: V.tensor_tensor(out=o2n[:], in0=accs[1][0], in1=rden[1][:], op=ALU.mult), reads=[accs[1][1], "rden1"], writes=["o2n"])
                            S.op("dve", lambda: V.scalar_tensor_tensor(out=o1n[:], in0=o2n[:], scalar=neglam[:, 0:1], in1=o1n[:], op0=ALU.mult, op1=ALU.add),
                                 reads=["o1n", "o2n", "neglam"], writes=["o1n"])
                            S.op("pool", lambda: G.tensor_tensor(out=sq[:], in0=o1n[:], in1=o1n[:], op=ALU.mult), reads=["o1n"], writes=["osq"])
                            S.op("pe", lambda: PE.matmul(accs[2][0], lhsT=ones_b[:], rhs=sq[:], start=True, stop=True), reads=["osq", "onesb"], writes=[accs[2][1]])
                            S.op("act", lambda: A.activation(out=rstd[:], in_=accs[2][0], func=AF.Ln, scale=1.0 / 128, bias=eps5[:, 0:1]), reads=[accs[2][1], "eps5"], writes=["orstd"])
                            S.op("act", lambda: A.activation(out=rstd[:], in_=rstd[:], func=AF.Exp, scale=-0.5), reads=["orstd"], writes=["orstd"])
                            S.op("dve", lambda: V.scalar_tensor_tensor(out=osb[par][:], in0=o1n[:], scalar=subs[:, 0:1], in1=rstd[:], op0=ALU.mult, op1=ALU.mult),
                                 reads=["o1n", "subs", "orstd"], writes=["osb%d" % par])
                        S.dma("sp", "b_o%d" % par, lambda: nc.sync.dma_start(out=OT[h * 128:(h + 1) * 128, qs], in_=osb[par][:]), reads=["osb%d" % par], writes=["OT"])
                S.barrier()

        def ln_stats(z, zkey, st, mv, rs, nmr, tag):
            for i in range(2):
                S.op("dve", lambda: V.bn_stats(out=st[:, i, :], in_=z[:, i * 512:(i + 1) * 512]), reads=[zkey], writes=["st" + tag])
            S.op("dve", lambda: V.bn_aggr(out=mv[:], in_=st[:].rearrange("p a b -> p (a b)")), reads=["st" + tag], writes=["mv" + tag])
            S.op("act", lambda: A.activation(out=rs[:], in_=mv[:, 1:2], func=AF.Sqrt, bias=eps5[:, 0:1], scale=1.0), reads=["mv" + tag, "eps5"], writes=["rs" + tag])
            S.op("dve", lambda: V.reciprocal(out=rs[:], in_=rs[:]), reads=["rs" + tag], writes=["rs" + tag])
            S.op("dve", lambda: V.scalar_tensor_tensor(out=nmr[:], in0=mv[:, 0:1], scalar=-1.0, in1=rs[:], op0=ALU.mult, op1=ALU.mult), reads=["mv" + tag, "rs" + tag], writes=["nmr" + tag])

        def ln_apply(z, zkey, rs, nmr, tag):
            S.op("act", lambda: A.activation(out=z[:], in_=z[:], func=AF.Identity, bias=nmr[:, 0:1], scale=rs[:, 0:1]), reads=[zkey, "rs" + tag, "nmr" + tag], writes=[zkey])

        def phase_c(xsrc, l, w_o_ap):
            with ExitStack() as ph:
                g1p, k_g1 = bcast_load(ph, MODS[l:l + 1, 2 * D:3 * D], "g1p", plus_one=True)
                sc2p, k_sc2 = bcast_load(ph, MODS[l:l + 1, 4 * D:5 * D], "sc2p", plus_one=True)
                sh2p, k_sh2 = bcast_load(ph, MODS[l:l + 1, 3 * D:4 * D], "sh2p")
                lng, k_lng = bcast_load(ph, ln1_g[l:l + 1, :], "lng")
                lnb, k_lnb = bcast_load(ph, ln1_b[l:l + 1, :], "lnb")
                w_o = sb(ph, [128, 8, D], BF16, "w_o")
                rw = sb(ph, [128, 8, NE], F32, "rw")
                oT = sb(ph, [128, 8, S_LEN], BF16, "oTall")
                S.dma("pool", "c_wo", lambda: G.dma_start(out=w_o[:], in_=w_o_ap.rearrange("(k p) n -> p k n", p=128)), writes=["w_o"])
                S.dma("sp", "c_rw", lambda: nc.sync.dma_start(out=rw[:], in_=router_w[l].rearrange("(k p) n -> p k n", p=128)), writes=["rw"])
                for h in range(8):
                    S.dma("sp", "c_ot", lambda: nc.sync.dma_start(out=oT[:, h, :], in_=OT[h * 128:(h + 1) * 128, :]), writes=["oTall"])
                NR = 6
                xin = [sb(ph, [128, D], F32, "xin") for _ in range(NR)]
                z = [sb(ph, [128, D], F32, "z") for _ in range(NR)]
                h2 = [sb(ph, [128, D], F32, "h2") for _ in range(NR)]
                h2b = [sb(ph, [128, D], BF16, "h2b") for _ in range(2)]
                h2T = [sb(ph, [128, 8, 128], F32, "h2T") for _ in range(2)]
                st = [sb(ph, [128, 2, 6], F32, "st") for _ in range(NR)]
                mv = [sb(ph, [128, 2], F32, "mv") for _ in range(NR)]
                rs = [sb(ph, [128, 1], F32, "rs") for _ in range(NR)]
                nmr = [sb(ph, [128, 1], F32, "nmr") for _ in range(NR)]
                mx = [sb(ph, [128, 1], F32, "mx") for _ in range(2)]
                ssum = [sb(ph, [128, 1], F32, "ssum") for _ in range(2)]
                ex = [sb(ph, [128, NE], F32, "ex") for _ in range(2)]

                def ld(b):
                    r = b % NR
                    S.dma("sp", "c_x%d" % r, lambda: nc.sync.dma_start(out=xin[r][:], in_=xsrc[b * 128:(b + 1) * 128, :]), writes=["xin%d" % r])

                def st0(b):
                    r, s2 = b % NR, b % 2
                    rows = slice(b * 128, (b + 1) * 128)
                    for hf in range(2):
                        pt, kk = banks[2 * s2 + hf], "bank%d" % (2 * s2 + hf)
                        S.ops("pe", [(lambda h=h: PE.matmul(pt[:, :], lhsT=oT[:, h, rows], rhs=w_o[:, h, hf * 512:(hf + 1) * 512], start=(h == 0), stop=(h == 7))) for h in range(8)],
                              reads=["oTall", "w_o"], writes=[kk])
                        S.op("dve", lambda: V.tensor_tensor(out=z[r][:, hf * 512:(hf + 1) * 512], in0=pt[:, :], in1=g1p[:, hf * 512:(hf + 1) * 512], op=ALU.mult),
                             reads=[kk, k_g1], writes=["z%d" % r])
                    S.op("dve", lambda: V.scalar_tensor_tensor(out=z[r][:], in0=xin[r][:], scalar=ALPHA, in1=z[r][:], op0=ALU.mult, op1=ALU.add),
                         reads=["xin%d" % r, "z%d" % r], writes=["z%d" % r])
                    for i in range(2):
                        S.op("dve", lambda: V.bn_stats(out=st[r][:, i, :], in_=z[r][:, i * 512:(i + 1) * 512]), reads=["z%d" % r], writes=["stc%d" % r])
                    S.op("dve", lambda: V.bn_aggr(out=mv[r][:], in_=st[r][:].rearrange("p a b -> p (a b)")), reads=["stc%d" % r], writes=["mvc%d" % r])

                def st1(b):
                    r = b % NR
                    S.op("act", lambda: A.activation(out=rs[r][:], in_=mv[r][:, 1:2], func=AF.Sqrt, bias=eps5[:, 0:1], scale=1.0), reads=["mvc%d" % r, "eps5"], writes=["rsc%d" % r])
                    S.op("dve", lambda: V.reciprocal(out=rs[r][:], in_=rs[r][:]), reads=["rsc%d" % r], writes=["rsc%d" % r])
                    S.op("dve", lambda: V.scalar_tensor_tensor(out=nmr[r][:], in0=mv[r][:, 0:1], scalar=-1.0, in1=rs[r][:], op0=ALU.mult, op1=ALU.mult), reads=["mvc%d" % r, "rsc%d" % r], writes=["nmrc%d" % r])
                    S.op("act", lambda: A.activation(out=z[r][:], in_=z[r][:], func=AF.Identity, bias=nmr[r][:, 0:1], scale=rs[r][:, 0:1]), reads=["z%d" % r, "rsc%d" % r, "nmrc%d" % r], writes=["z%d" % r])

                def st2(b):
                    r = b % NR
                    rows = slice(b * 128, (b + 1) * 128)
                    S.op("pool", lambda: G.tensor_tensor(out=z[r][:], in0=z[r][:], in1=lng[:], op=ALU.mult), reads=["z%d" % r, k_lng], writes=["z%d" % r])
                    S.op("dve", lambda: V.tensor_tensor(out=z[r][:], in0=z[r][:], in1=lnb[:], op=ALU.add), reads=["z%d" % r, k_lnb], writes=["z%d" % r])
                    S.dma("sp", "c_xm%d" % r, lambda: nc.sync.dma_start(out=XMID[rows, :], in_=z[r][:]), reads=["z%d" % r], writes=["XMID"])
                    S.op("pool", lambda: G.tensor_tensor(out=h2[r][:], in0=z[r][:], in1=sc2p[:], op=ALU.mult), reads=["z%d" % r, k_sc2], writes=["h2_%d" % r])
                    S.op("dve", lambda: V.tensor_tensor(out=h2[r][:], in0=h2[r][:], in1=sh2p[:], op=ALU.add), reads=["h2_%d" % r, k_sh2], writes=["h2_%d" % r])

                def st3(b):
                    r, s2 = b % NR, b % 2
                    rows = slice(b * 128, (b + 1) * 128)
                    S.op("act", lambda: A.copy(out=h2b[s2][:], in_=h2[r][:]), reads=["h2_%d" % r], writes=["h2b%d" % s2])
                    S.dma("sp", "c_h2%d" % s2, lambda: nc.sync.dma_start(out=H2[rows, :], in_=h2b[s2][:]), reads=["h2b%d" % s2], writes=["H2"])
                    for hf in range(2):
                        bk, bkey = banks[4 + hf], "bank%d" % (4 + hf)
                        S.ops("pe", [(lambda j=j: PE.transpose(bk[:, j * 128:(j + 1) * 128], h2[r][:, (hf * 4 + j) * 128:(hf * 4 + j + 1) * 128], ident_f[:])) for j in range(4)],
                              reads=["h2_%d" % r, "identf"], writes=[bkey])
                        S.op("act", lambda: A.copy(out=h2T[s2][:, hf * 4:(hf + 1) * 4, :].rearrange("p a b -> p (a b)"), in_=bk[:, :]), reads=[bkey], writes=["h2T%d" % s2])

                def st4(b):
                    s2 = b % 2
                    lg, lgk = banks[6 + s2], "bank%d" % (6 + s2)
                    S.ops("pe", [(lambda k=k: PE.matmul(lg[:, 0:NE], lhsT=h2T[s2][:, k, :], rhs=rw[:, k, :], start=(k == 0), stop=(k == 7))) for k in range(8)],
                          reads=["h2T%d" % s2, "rw"], writes=[lgk])
                    S.op("dve", lambda: V.reduce_max(out=mx[s2][:], in_=lg[:, 0:NE], axis=mybir.AxisListType.X), reads=[lgk], writes=["mx%d" % s2])
                    S.op("dve", lambda: V.tensor_scalar(out=mx[s2][:], in0=mx[s2][:], scalar1=-1.0, scalar2=None, op0=ALU.mult), reads=["mx%d" % s2], writes=["mx%d" % s2])
                    S.op("act", lambda: A.activation(out=ex[s2][:], in_=lg[:, 0:NE], func=AF.Exp, bias=mx[s2][:, 0:1], scale=1.0, accum_out=ssum[s2][:]), reads=[lgk, "mx%d" % s2], writes=["ex%d" % s2, "ssum%d" % s2])
                    S.op("dve", lambda: V.reciprocal(out=ssum[s2][:], in_=ssum[s2][:]), reads=["ssum%d" % s2], writes=["ssum%d" % s2])
                    S.op("dve", lambda: V.tensor_scalar(out=aff[:, b, :], in0=ex[s2][:], scalar1=ssum[s2][:, 0:1], scalar2=None, op0=ALU.mult), reads=["ex%d" % s2, "ssum%d" % s2], writes=["aff"])

                stages = [st0, st1, st2, st3, st4]
                ld(0)
                ld(1)
                for step in range(NBLK + len(stages) - 1):
                    for si in reversed(range(len(stages))):
                        b = step - si
                        if si == 0 and step + 2 < NBLK:
                            ld(step + 2)
                        if 0 <= b < NBLK:
                            stages[si](b)
                S.barrier()

        def phase_m(l):
            with ExitStack() as ph:
                ltri = sb(ph, [128, 128], F32, "ltri")
                ltri_b = sb(ph, [128, 128], BF16, "ltrib")
                iota = sb(ph, [128, CAP], F32, "iota")
                rcp = sb(ph, [128, NBLK, 2], F32, "rcp")
                S.dma("sp", "m_c0", lambda: nc.sync.dma_start(out=ltri[:], in_=c_ltri), writes=["ltri"])
                S.dma("sp", "m_c1", lambda: nc.sync.dma_start(out=iota[:], in_=c_iota), writes=["iota"])
                S.dma("sp", "m_c2", lambda: nc.sync.dma_start(out=rcp[:], in_=c_rcp), writes=["rcp"])
                S.op("dve", lambda: V.tensor_copy(out=ltri_b[:], in_=ltri[:]), reads=["ltri"], writes=["ltrib"])
                zt = sb(ph, [128, 4, D], F32, "zt")
                S.op("pool", lambda: G.memset(zt[:], 0.0), writes=["zt"])
                fv = FACC.rearrange("(t s p) d -> t p s d", s=4, p=128)
                for tb in range(8):
                    S.dma("sp", "m_z", lambda: nc.sync.dma_start(out=fv[tb], in_=zt[:]), reads=["zt"], writes=["FACC"])
                lo = sb(ph, [128, NE], F32, "lo")
                mid = sb(ph, [128, NE], F32, "mid")
                cmpt = sb(ph, [128, NBLK, NE], F32, "cmpt")
                cnt = sb(ph, [128, NE], F32, "cnt")
                ge = sb(ph, [128, NE], F32, "ge")
                ones_f = sb(ph, [128, 128], F32, "onesf")
                S.op("dve", lambda: V.memset(ones_f[:], 1.0), writes=["onesf"])
                S.op("dve", lambda: V.memset(lo[:], 0.0), writes=["lo"])
                for it in range(30):
                    w = 2.0 ** -(it + 1)
                    bk, bkk = banks[it % 2], "bank%d" % (it % 2)
                    S.op("dve", lambda: V.tensor_scalar(out=mid[:], in0=lo[:], scalar1=w, scalar2=None, op0=ALU.add), reads=["lo"], writes=["mid"])
                    S.op("dve", lambda: V.tensor_tensor(out=cmpt[:], in0=aff[:], in1=mid[:].unsqueeze(1).broadcast_to([128, NBLK, NE]), op=ALU.is_ge), reads=["aff", "mid"], writes=["cmpt"])
                    S.op("dve", lambda: V.reduce_sum(out=cnt[:], in_=cmpt[:].rearrange("p c e -> p e c"), axis=mybir.AxisListType.X), reads=["cmpt"], writes=["cnt"])
                    S.op("pe", lambda: PE.matmul(bk[:, 0:NE], lhsT=ones_f[:], rhs=cnt[:], start=True, stop=True), reads=["onesf", "cnt"], writes=[bkk])
                    S.op("dve", lambda: V.tensor_scalar(out=ge[:], in0=bk[:, 0:NE], scalar1=float(CAP) - 0.5, scalar2=None, op0=ALU.is_ge), reads=[bkk], writes=["ge"])
                    S.op("dve", lambda: V.scalar_tensor_tensor(out=lo[:], in0=ge[:], scalar=w, in1=lo[:], op0=ALU.mult, op1=ALU.add), reads=["ge", "lo"], writes=["lo"])
                maskb = sb(ph, [128, NBLK, NE], BF16, "maskb")
                pre = sb(ph, [128, NBLK, NE], BF16, "pre")
                pref = sb(ph, [128, NBLK, NE], F32, "pref")
                posp = sb(ph, [128, NBLK, NE], F32, "posp")
                S.op("dve", lambda: V.tensor_tensor(out=cmpt[:], in0=aff[:], in1=lo[:].unsqueeze(1).broadcast_to([128, NBLK, NE]), op=ALU.is_ge), reads=["aff", "lo"], writes=["cmpt"])
                S.op("dve", lambda: V.tensor_copy(out=maskb[:], in_=cmpt[:]), reads=["cmpt"], writes=["maskb"])
                S.op("dve", lambda: V.memset(pref[:, 0, :], 0.0), writes=["pref"])
                for c in range(1, NBLK):
                    S.op("dve", lambda: V.tensor_tensor(out=pref[:, c, :], in0=pref[:, c - 1, :], in1=cmpt[:, c - 1, :], op=ALU.add), reads=["pref", "cmpt"], writes=["pref"])
                S.op("dve", lambda: V.tensor_copy(out=pre[:], in_=pref[:]), reads=["pref"], writes=["pre"])
                pb = banks[1]
                S.ops("pe", [lambda: PE.matmul(pb[:, :], lhsT=ltri_b[:], rhs=maskb[:].rearrange("p c e -> p (c e)"), start=True, stop=False),
                             lambda: PE.matmul(pb[:, :], lhsT=ones_b[:], rhs=pre[:].rearrange("p c e -> p (c e)"), start=False, stop=True)],
                      reads=["ltrib", "onesb", "maskb", "pre"], writes=["bank1"])
                S.op("dve", lambda: V.tensor_tensor(out=posp[:].rearrange("p c e -> p (c e)"), in0=pb[:, :], in1=cmpt[:].rearrange("p c e -> p (c e)"), op=ALU.mult), reads=["bank1", "cmpt"], writes=["posp"])
                R = sb(ph, [128, NBLK, NE, 5], BF16, "R")
                r1 = sb(ph, [128, NBLK, NE], F32, "r1")
                r2 = sb(ph, [128, NBLK, NE], F32, "r2")
                S.op("dve", lambda: V.tensor_copy(out=R[:, :, :, 0], in_=rcp[:, :, 0:1].broadcast_to([128, NBLK, NE])), reads=["rcp"], writes=["R"])
                S.op("dve", lambda: V.tensor_copy(out=R[:, :, :, 1], in_=rcp[:, :, 1:2].broadcast_to([128, NBLK, NE])), reads=["rcp", "R"], writes=["R"])
                S.op("dve", lambda: V.tensor_copy(out=R[:, :, :, 2], in_=aff[:]), reads=["aff", "R"], writes=["R"])
                S.op("dve", lambda: V.tensor_tensor(out=r1[:], in0=aff[:], in1=R[:, :, :, 2], op=ALU.subtract), reads=["aff", "R"], writes=["r1"])
                S.op("dve", lambda: V.tensor_copy(out=R[:, :, :, 3], in_=r1[:]), reads=["r1", "R"], writes=["R"])
                S.op("dve", lambda: V.tensor_tensor(out=r2[:], in0=r1[:], in1=R[:, :, :, 3], op=ALU.subtract), reads=["r1", "R"], writes=["r2"])
                S.op("dve", lambda: V.tensor_copy(out=R[:, :, :, 4], in_=r2[:]), reads=["r2", "R"], writes=["R"])
                idx = sb(ph, [128, NE, 4], I32, "idx")
                idxf = sb(ph, [128, NE, 4], F32, "idxf")
                gate = sb(ph, [128, NE, 4], F32, "gate")
                Pm = [sb(ph, [128, NBLK, 128], BF16, "Pm") for _ in range(2)]
                ig = sb(ph, [128, 8], F32, "ig")
                pcount = [0]

                def compute_idx(e, g):
                    psl = pcount[0] % 2
                    pcount[0] += 1
                    S.op("dve", lambda: V.tensor_tensor(out=Pm[psl][:], in0=posp[:, :, e:e + 1].broadcast_to([128, NBLK, 128]),
                                                        in1=iota[:, g * 128:(g + 1) * 128].unsqueeze(1).broadcast_to([128, NBLK, 128]), op=ALU.is_equal),
                         reads=["posp", "iota"], writes=["Pm%d" % psl])
                    ob, obk = banks[7], "bank7"
                    S.ops("pe", [(lambda c=c: PE.matmul(ob[:, 0:5], lhsT=Pm[psl][:, c, :], rhs=R[:, c, e, :], start=(c == 0), stop=(c == NBLK - 1))) for c in range(NBLK)],
                          reads=["Pm%d" % psl, "R"], writes=[obk])
                    S.op("act", lambda: A.copy(out=ig[:, psl * 4:psl * 4 + 4], in_=ob[:, 1:5]), reads=[obk], writes=["ig%d" % psl])
                    S.op("dve", lambda: V.scalar_tensor_tensor(out=idxf[:, e, g:g + 1], in0=ob[:, 0:1], scalar=128.0, in1=ig[:, psl * 4:psl * 4 + 1], op0=ALU.mult, op1=ALU.add),
                         reads=[obk, "ig%d" % psl], writes=["idxf%d" % e])
                    S.op("dve", lambda: V.tensor_tensor(out=gate[:, e, g:g + 1], in0=ig[:, psl * 4 + 1:psl * 4 + 2], in1=ig[:, psl * 4 + 2:psl * 4 + 3], op=ALU.add), reads=["ig%d" % psl], writes=["gate%d" % e])
                    S.op("dve", lambda: V.tensor_tensor(out=gate[:, e, g:g + 1], in0=gate[:, e, g:g + 1], in1=ig[:, psl * 4 + 3:psl * 4 + 4], op=ALU.add), reads=["ig%d" % psl, "gate%d" % e], writes=["gate%d" % e])
                    if g == 3:
                        S.op("dve", lambda: V.tensor_copy(out=idx[:, e, :], in_=idxf[:, e, :]), reads=["idxf%d" % e], writes=["idx%d" % e])

                for e0 in range(2):
                    for g in range(4):
                        compute_idx(e0, g)
                wi = [sb(ph, [128, 8, 1024], BF16, "wi") for _ in range(3)]
                wo = [sb(ph, [128, 4, D], BF16, "wo") for _ in range(3)]
                xg = [sb(ph, [128, 4, D], BF16, "xg") for _ in range(2)]
                xeT = sb(ph, [128, 8, CAP], BF16, "xeT")
                sg = [sb(ph, [128, CAP], F32, "sg") for _ in range(2)]
                actT = [sb(ph, [128, 4, CAP], BF16, "actT") for _ in range(2)]
                ysb = [sb(ph, [128, 4, D], F32, "ysb") for _ in range(2)]
                wiv = moe_w_in[l].rearrange("e (k p) n -> e p k n", p=128)
                wov = moe_w_out[l].rearrange("e (k p) n -> e p k n", p=128)
                nblk = [0]

                def load_w(e, i):
                    sl = nblk[0] % 3
                    nblk[0] += 1
                    S.dma("pool", "m_wg%d" % sl, lambda: G.dma_start(out=wi[sl][:, :, 0:512], in_=wiv[e][:, :, i * 512:(i + 1) * 512]), writes=["wi%d" % sl])
                    S.dma("pool", "m_wu%d" % sl, lambda: G.dma_start(out=wi[sl][:, :, 512:1024], in_=wiv[e][:, :, FF + i * 512:FF + (i + 1) * 512]), writes=["wi%d" % sl])
                    S.dma("pool", "m_wo%d" % sl, lambda: G.dma_start(out=wo[sl][:], in_=wov[e][:, i * 4:(i + 1) * 4, :]), writes=["wo%d" % sl])
                    return sl

                def gather(e):
                    gs = e % 2
                    for g in range(4):
                        S.dma("pool", "m_g%d" % gs, lambda: G.indirect_dma_start(out=xg[gs][:, g, :], out_offset=None, in_=H2,
                                                                               in_offset=bass.IndirectOffsetOnAxis(ap=idx[:, e, g:g + 1], axis=0)),
                              reads=["idx%d" % e, "H2"], writes=["xg%d" % gs])

                sched_w = [(e, i) for e in range(NE) for i in range(4)]
                slots = {}
                slots[sched_w[0]] = load_w(*sched_w[0])
                slots[sched_w[1]] = load_w(*sched_w[1])
                gather(0)
                wn = 2
                par2 = 0
                for e in range(NE):
                    gs = e % 2
                    ys = e % 2
                    if e + 1 < NE:
                        gather(e + 1)
                    for k in range(8):
                        bk, bkey = banks[6], "bank6"
                        tv = bk[:].bitcast(BF16)
                        S.ops("pe", [(lambda g=g: PE.transpose(tv[:, g * 128:(g + 1) * 128], xg[gs][:, g, k * 128:(k + 1) * 128], ident_b[:])) for g in range(4)],
                              reads=["xg%d" % gs, "identb"], writes=[bkey])
                        S.op("act" if k % 2 else "dve", (lambda k=k, tv=tv: (A.copy if k % 2 else V.tensor_copy)(out=xeT[:, k, :], in_=tv[:, 0:CAP])), reads=[bkey], writes=["xeT"])
                    for i in range(4):
                        if wn < len(sched_w):
                            slots[sched_w[wn]] = load_w(*sched_w[wn])
                            wn += 1
                        ws = slots[(e, i)]
                        asl = (e * 4 + i) % 2
                        for fc in range(4):
                            pg, pgk = banks[0 + 2 * (fc % 2)], "bank%d" % (0 + 2 * (fc % 2))
                            pu, puk = banks[1 + 2 * (fc % 2)], "bank%d" % (1 + 2 * (fc % 2))
                            S.ops("pe", [(lambda k=k: PE.matmul(pg[:, :], lhsT=wi[ws][:, k, fc * 128:(fc + 1) * 128], rhs=xeT[:, k, :], start=(k == 0), stop=(k == 7))) for k in range(8)],
                                  reads=["wi%d" % ws, "xeT"], writes=[pgk])
                            S.ops("pe", [(lambda k=k: PE.matmul(pu[:, :], lhsT=wi[ws][:, k, 512 + fc * 128:512 + (fc + 1) * 128], rhs=xeT[:, k, :], start=(k == 0), stop=(k == 7))) for k in range(8)],
                                  reads=["wi%d" % ws, "xeT"], writes=[puk])
                            S.op("act", lambda: A.activation(out=sg[fc % 2][:], in_=pg[:, :], func=AF.Silu), reads=[pgk], writes=["sg%d" % (fc % 2)])
                            S.op("dve", lambda: V.tensor_tensor(out=actT[asl][:, fc, :], in0=sg[fc % 2][:], in1=pu[:, :], op=ALU.mult), reads=["sg%d" % (fc % 2), puk], writes=["actT%d" % asl])
                        if e + 2 < NE:
                            compute_idx(e + 2, i)
                        for g in range(4):
                            for hf in range(2):
                                py, pyk = banks[4 + par2 % 2], "bank%d" % (4 + par2 % 2)
                                par2 += 1
                                S.ops("pe", [(lambda fc=fc: PE.matmul(py[:, :], lhsT=actT[asl][:, fc, g * 128:(g + 1) * 128], rhs=wo[ws][:, fc, hf * 512:(hf + 1) * 512], start=(fc == 0), stop=(fc == 3))) for fc in range(4)],
                                      reads=["actT%d" % asl, "wo%d" % ws], writes=[pyk])
                                dst = ysb[ys][:, g, hf * 512:(hf + 1) * 512]
                                if i == 0:
                                    S.op("act", lambda: A.activation(out=dst, in_=py[:, :], func=AF.Copy, scale=gate[:, e, g:g + 1]), reads=[pyk, "gate%d" % e], writes=["ysb%d" % ys])
                                else:
                                    S.op("dve", lambda: V.scalar_tensor_tensor(out=dst, in0=py[:, :], scalar=gate[:, e, g:g + 1], in1=dst, op0=ALU.mult, op1=ALU.add),
                                         reads=[pyk, "gate%d" % e, "ysb%d" % ys], writes=["ysb%d" % ys])
                    for g in range(4):
                        S.dma("pool", "m_sc", lambda: G.indirect_dma_start(out=FACC, out_offset=bass.IndirectOffsetOnAxis(ap=idx[:, e, g:g + 1], axis=0),
                                                                          in_=ysb[ys][:, g, :], in_offset=None, compute_op=ALU.add),
                              reads=["ysb%d" % ys, "idx%d" % e], writes=["FACC"] if g == 0 else [])
                    S.lastw["FACC"] = (S.dsem["m_sc"][0], S.dsem["m_sc"][1])
                    S.readers["FACC"] = {}
                S.barrier()

        def phase_d(l, dst):
            with ExitStack() as ph:
                g2p, k_g2 = bcast_load(ph, MODS[l:l + 1, 5 * D:6 * D], "g2p", plus_one=True)
                lng, k_lng = bcast_load(ph, ln2_g[l:l + 1, :], "lng2")
                lnb, k_lnb = bcast_load(ph, ln2_b[l:l + 1, :], "lnb2")
                NR = 6
                xin = [sb(ph, [128, D], F32, "xin") for _ in range(NR)]
                fin = [sb(ph, [128, D], F32, "fin") for _ in range(NR)]
                st = [sb(ph, [128, 2, 6], F32, "st") for _ in range(NR)]
                mv = [sb(ph, [128, 2], F32, "mv") for _ in range(NR)]
                rs = [sb(ph, [128, 1], F32, "rs") for _ in range(NR)]
                nmr = [sb(ph, [128, 1], F32, "nmr") for _ in range(NR)]

                def ld(b):
                    r = b % NR
                    rows = slice(b * 128, (b + 1) * 128)
                    S.dma("sp", "d_x%d" % r, lambda: nc.sync.dma_start(out=xin[r][:], in_=XMID[rows, :]), writes=["xin%d" % r])
                    S.dma("sp", "d_f%d" % r, lambda: nc.sync.dma_start(out=fin[r][:], in_=FACC[rows, :]), writes=["fin%d" % r])

                def st0(b):
                    r = b % NR
                    S.op("pool", lambda: G.tensor_tensor(out=fin[r][:], in0=fin[r][:], in1=g2p[:], op=ALU.mult), reads=["fin%d" % r, k_g2], writes=["fin%d" % r])
                    S.op("dve", lambda: V.scalar_tensor_tensor(out=fin[r][:], in0=xin[r][:], scalar=ALPHA, in1=fin[r][:], op0=ALU.mult, op1=ALU.add),
                         reads=["xin%d" % r, "fin%d" % r], writes=["fin%d" % r])
                    for i in range(2):
                        S.op("dve", lambda: V.bn_stats(out=st[r][:, i, :], in_=fin[r][:, i * 512:(i + 1) * 512]), reads=["fin%d" % r], writes=["std%d" % r])
                    S.op("dve", lambda: V.bn_aggr(out=mv[r][:], in_=st[r][:].rearrange("p a b -> p (a b)")), reads=["std%d" % r], writes=["mvd%d" % r])

                def st1(b):
                    r = b % NR
                    S.op("act", lambda: A.activation(out=rs[r][:], in_=mv[r][:, 1:2], func=AF.Sqrt, bias=eps5[:, 0:1], scale=1.0), reads=["mvd%d" % r, "eps5"], writes=["rsd%d" % r])
                    S.op("dve", lambda: V.reciprocal(out=rs[r][:], in_=rs[r][:]), reads=["rsd%d" % r], writes=["rsd%d" % r])
                    S.op("dve", lambda: V.scalar_tensor_tensor(out=nmr[r][:], in0=mv[r][:, 0:1], scalar=-1.0, in1=rs[r][:], op0=ALU.mult, op1=ALU.mult), reads=["mvd%d" % r, "rsd%d" % r], writes=["nmrd%d" % r])
                    S.op("act", lambda: A.activation(out=fin[r][:], in_=fin[r][:], func=AF.Identity, bias=nmr[r][:, 0:1], scale=rs[r][:, 0:1]), reads=["fin%d" % r, "rsd%d" % r, "nmrd%d" % r], writes=["fin%d" % r])

                def st2(b):
                    r = b % NR
                    rows = slice(b * 128, (b + 1) * 128)
                    S.op("pool", lambda: G.tensor_tensor(out=fin[r][:], in0=fin[r][:], in1=lng[:], op=ALU.mult), reads=["fin%d" % r, k_lng], writes=["fin%d" % r])
                    S.op("dve", lambda: V.tensor_tensor(out=fin[r][:], in0=fin[r][:], in1=lnb[:], op=ALU.add), reads=["fin%d" % r, k_lnb], writes=["fin%d" % r])
                    S.dma("sp", "d_o%d" % r, lambda: nc.sync.dma_start(out=dst[rows, :], in_=fin[r][:]), reads=["fin%d" % r], writes=["dst"])

                stages = [st0, st1, st2]
                ld(0)
                ld(1)
                ld(2)
                for step in range(NBLK + len(stages) - 1):
                    for si in reversed(range(len(stages))):
                        b = step - si
                        if 0 <= b < NBLK:
                            stages[si](b)
                        if si == 1 and step + 3 < NBLK:
                            ld(step + 3)
                S.barrier()

        phases = [
            lambda: phase_mods(),
            lambda: phase_a_mla(x_in, 0),
            lambda: phase_b("mla"),
            lambda: phase_c(x_in, 0, mla_w_o),
            lambda: phase_m(0),
            lambda: phase_d(0, X1),
            lambda: phase_a_diff(X1, 1),
            lambda: phase_b("diff"),
            lambda: phase_c(X1, 1, diff_w_o),
            lambda: phase_m(1),
            lambda: phase_d(1, out),
        ]
        for i, p in enumerate(phases):
            if i <= upto:
                p()
        if dbg is not None:
            src = {"XMID": XMID, "X1": X1, "FACC": FACC}[dbg]
            with ExitStack() as ph:
                t = sb(ph, [128, 4, D], F32, "dbg")
                for tb in range(8):
                    S.dma("sp", "dbg_i", lambda: nc.sync.dma_start(out=t[:], in_=src.rearrange("(t s p) d -> t p s d", s=4, p=128)[tb]), writes=["dbg"])
                    S.dma("sp", "dbg_o", lambda: nc.sync.dma_start(out=out.rearrange("(t s p) d -> t p s d", s=4, p=128)[tb], in_=t[:]), reads=["dbg"], writes=["outd"])
        S.finish("sp")
    return nc


def _consts():
    inv32 = (10000.0 ** (-np.arange(0, 64, 2, dtype=np.float32) / 64)).astype(np.float32)
    inv_m = np.concatenate([inv32, inv32]).reshape(64, 1).astype(np.float32)
    inv8 = (500000.0 ** (-np.arange(0, 16, 2, dtype=np.float32) / 16)).astype(np.float32)
    blk = np.zeros(64, np.float32)
    blk[0:8] = inv8
    blk[8:16] = inv8
    inv_d = np.concatenate([blk, blk]).reshape(128, 1).astype(np.float32)
    ident = np.eye(128, dtype=np.float32)
    ltri = np.triu(np.ones((128, 128), np.float32))
    iota = np.tile(np.arange(1, CAP + 1, dtype=np.float32)[None, :], (128, 1))
    rcp = np.zeros((128, NBLK, 2), np.float32)
    rcp[:, :, 0] = np.arange(NBLK, dtype=np.float32)[None, :]
    rcp[:, :, 1] = np.arange(128, dtype=np.float32)[:, None]
    return dict(c_inv_m=inv_m, c_inv_d=inv_d, c_ident=ident, c_ltri=ltri, c_iota=iota, c_rcp=rcp)


def make_in_maps(inputs, ncores=8):
    f = lambda a: np.ascontiguousarray(np.asarray(a))
    shared = dict(
        ada_w=f(inputs["ada_w"]), ada_b=f(inputs["ada_b"]),
        ln1_g=f(inputs["ln1_g"]), ln1_b=f(inputs["ln1_b"]), ln2_g=f(inputs["ln2_g"]), ln2_b=f(inputs["ln2_b"]),
        mla_w_in=f(inputs["mla_w_in"][0]), mla_q_norm=f(inputs["mla_q_norm"][0]), mla_kv_norm=f(inputs["mla_kv_norm"][0]),
        mla_w_uq=f(inputs["mla_w_uq"][0]), mla_w_ukv=f(inputs["mla_w_ukv"][0]), mla_w_o=f(inputs["mla_w_o"][0]),
        diff_w_in=f(inputs["diff_w_in"][0]), diff_lambda=f(inputs["diff_lambda"][0]).reshape(1, 256),
        diff_subln=f(inputs["diff_subln"][0]).reshape(128, 1), diff_w_o=f(inputs["diff_w_o"][0]),
        router_w=f(inputs["router_w"]), moe_w_in=f(inputs["moe_w_in"]), moe_w_out=f(inputs["moe_w_out"]),
    )
    shared.update(_consts())
    maps = []
    for c in range(ncores):
        b = c % 4
        m = dict(shared)
        m["x"] = f(inputs["x"][b])
        m["cT"] = f(np.asarray(inputs["c"][b]).reshape(8, 128).T)
        m["pos"] = f(np.asarray(inputs["positions"][b]).reshape(1, S_LEN).astype(np.int32))
        maps.append(m)
    return maps


def kernel(**inputs):
    nc = build()
    maps = make_in_maps(inputs, 8)
    res = run_bass_kernel_spmd(nc, maps, core_ids=list(range(8)))
    return np.stack([np.asarray(res.results[b]["out"], dtype=np.float32) for b in range(4)], axis=0)
```
